# Optimizing a Trainium2 kernel written in Bass

```python
import math
import jax, jax.numpy as jnp
from jax import lax
import numpy as np

D_MODEL = 1024
BATCH = 8
SEQ = 4096
DEPTH = 1

HEAD_DIM = 64
NSA_HEADS = 8
NSA_GROUPS = 2
NSA_HPG = NSA_HEADS // NSA_GROUPS
CMP_BLOCK = 32
CMP_STRIDE = 16
CMP_HIDDEN = 256
SLC_BLOCK = 64
SLC_TOPN = 16
SLC_FORCE = 1e4
WINDOW = 512
DSA_HEADS = 8
KV_RANK = 128
IDX_HEADS = 4
IDX_DIM = 64
IDX_TOPK_MAX = 256
REL_BUCKETS = 32
REL_EXACT = 16
REL_MAX_DIST = 1024
N_REL_HEADS = NSA_HEADS + DSA_HEADS
N_EXPERT_GROUPS = 4
EXPERTS_PER_GROUP = 8
N_EXPERTS = N_EXPERT_GROUPS * EXPERTS_PER_GROUP
EXPERT_TOPK = 2
D_EXPERT = 256
EXPERT_BLOCK = 256
QBLK = 128
NSA_WIDTH = NSA_HEADS * HEAD_DIM
DSA_WIDTH = DSA_HEADS * HEAD_DIM
ALPHA = (2.0 * DEPTH) ** 0.25
BETA = (8.0 * DEPTH) ** -0.25
NEG = -1e30
SPLIT_SIZES = (NSA_WIDTH,) + (NSA_GROUPS * HEAD_DIM,) * 6 + (NSA_HEADS * 3, DSA_WIDTH, KV_RANK, IDX_HEADS * IDX_DIM, IDX_DIM, IDX_HEADS, D_MODEL, D_MODEL)
D_IN = int(sum(SPLIT_SIZES))
SPLIT_POINTS = tuple(int(v) for v in np.cumsum(SPLIT_SIZES)[:-1])

kernel_name = 'hybrid_nsa_dsa_hmoe_deepnorm'


def layer_norm(x, g, b, eps=1e-5):
    xf = x.astype(jnp.float32)
    xc = xf - jnp.mean(xf, axis=-1, keepdims=True)
    var = jnp.mean(xc * xc, axis=-1, keepdims=True)
    return (xc * lax.rsqrt(var + eps) * g + b).astype(x.dtype)


def rms_norm(x, g, eps=1e-6):
    xf = x.astype(jnp.float32)
    return (xf * lax.rsqrt(jnp.mean(xf * xf, axis=-1, keepdims=True) + eps) * g).astype(x.dtype)


def masked_softmax(logits, mask):
    l = jnp.where(mask, logits.astype(jnp.float32), NEG)
    m = jnp.max(l, axis=-1, keepdims=True)
    e = jnp.where(mask, jnp.exp(l - m), 0.0)
    p = e / jnp.maximum(jnp.sum(e, axis=-1, keepdims=True), 1e-30)
    return p.astype(logits.dtype)


def rel_bucket(dist):
    n = jnp.maximum(dist, 0)
    nf = jnp.maximum(n, 1).astype(jnp.float32)
    large = REL_EXACT + (jnp.log(nf / REL_EXACT) / math.log(REL_MAX_DIST / REL_EXACT) * (REL_BUCKETS - REL_EXACT)).astype(jnp.int32)
    return jnp.where(n < REL_EXACT, n, jnp.minimum(large, REL_BUCKETS - 1))


def compress_blocks(tok, pe, w1, w2):
    b, t, g, d = tok.shape
    nc = (t - CMP_BLOCK) // CMP_STRIDE + 1
    idx = CMP_STRIDE * np.arange(nc)[:, None] + np.arange(CMP_BLOCK)[None, :]
    blk = tok[:, idx] + pe[None, None, :, None, :]
    blk = jnp.moveaxis(blk, 3, 2).reshape(b, nc, g, CMP_BLOCK * d)
    return jax.nn.gelu(blk @ w1) @ w2


def cmp_to_slc_map(t):
    nc = (t - CMP_BLOCK) // CMP_STRIDE + 1
    ns = t // SLC_BLOCK
    cs = CMP_STRIDE * np.arange(nc)[:, None]
    ss = SLC_BLOCK * np.arange(ns)[None, :]
    ov = np.minimum(cs + CMP_BLOCK, ss + SLC_BLOCK) - np.maximum(cs, ss)
    return np.clip(ov, 0, None).astype(np.float32) / CMP_STRIDE


def nsa_block(q, k_cmp, v_cmp, k_slc, v_slc, k_win, v_win, gates, rel_a, cmp_map, tq, q0):
    scale = HEAD_DIM ** -0.5
    nc = k_cmp.shape[0]
    t = k_slc.shape[0]
    ns = t // SLC_BLOCK
    n_top = min(SLC_TOPN, ns)
    g_ar = jnp.arange(NSA_GROUPS)[:, None, None]
    cmp_end = CMP_STRIDE * jnp.arange(nc) + CMP_BLOCK - 1
    dist = tq[:, None] - cmp_end[None, :]
    logits = jnp.einsum('tghd,cgd->ghtc', q, k_cmp) * scale + rel_a[rel_bucket(dist)].transpose(2, 3, 0, 1)
    p_cmp = masked_softmax(logits, dist >= 0)
    o_cmp = jnp.einsum('ghtc,cgd->tghd', p_cmp, v_cmp)
    imp = jnp.einsum('ghtc,cn->gtn', p_cmp, cmp_map)
    blk = jnp.arange(ns)[None, :]
    cur = (tq // SLC_BLOCK)[:, None]
    forced = (blk == 0) | (blk == cur) | (blk == cur - 1)
    avail = blk * SLC_BLOCK <= tq[:, None]
    score = jnp.where(avail, imp.astype(jnp.float32) + SLC_FORCE * forced, NEG)
    sel_score, sel_idx = lax.top_k(score, n_top)
    sel_ok = sel_score > 0.5 * NEG
    k_sb = k_slc.reshape(ns, SLC_BLOCK, NSA_GROUPS, HEAD_DIM).transpose(2, 0, 1, 3)
    v_sb = v_slc.reshape(ns, SLC_BLOCK, NSA_GROUPS, HEAD_DIM).transpose(2, 0, 1, 3)
    k_g = k_sb[g_ar, sel_idx].reshape(NSA_GROUPS, QBLK, n_top * SLC_BLOCK, HEAD_DIM)
    v_g = v_sb[g_ar, sel_idx].reshape(NSA_GROUPS, QBLK, n_top * SLC_BLOCK, HEAD_DIM)
    kpos = (sel_idx[..., None] * SLC_BLOCK + jnp.arange(SLC_BLOCK)).reshape(NSA_GROUPS, QBLK, n_top * SLC_BLOCK)
    dist = tq[None, :, None] - kpos
    mask = jnp.repeat(sel_ok, SLC_BLOCK, axis=-1) & (dist >= 0)
    bias = jnp.moveaxis(rel_a[rel_bucket(dist), g_ar], -1, 1)
    logits = jnp.einsum('tghd,gtkd->ghtk', q, k_g) * scale + bias
    p = masked_softmax(logits, mask[:, None])
    o_slc = jnp.einsum('ghtk,gtkd->tghd', p, v_g)
    kw = lax.dynamic_slice_in_dim(k_win, q0, WINDOW + QBLK, axis=0)
    vw = lax.dynamic_slice_in_dim(v_win, q0, WINDOW + QBLK, axis=0)
    kpos = q0 - WINDOW + jnp.arange(WINDOW + QBLK)
    dist = tq[:, None] - kpos[None, :]
    mask = (kpos[None, :] >= 0) & (dist >= 0) & (dist < WINDOW)
    logits = jnp.einsum('tghd,kgd->ghtk', q, kw) * scale + rel_a[rel_bucket(dist)].transpose(2, 3, 0, 1)
    p = masked_softmax(logits, mask)
    o_win = jnp.einsum('ghtk,kgd->tghd', p, vw)
    o = gates[..., 0:1] * o_cmp + gates[..., 1:2] * o_slc + gates[..., 2:3] * o_win
    return o.reshape(QBLK, NSA_WIDTH)


def dsa_block(q_lat, ckv, q_idx, k_idx, w_idx, rel_b, tq):
    t = ckv.shape[0]
    k_sel = min(IDX_TOPK_MAX, t // 4)
    rs = jax.nn.relu(jnp.einsum('tid,sd->tis', q_idx, k_idx) * IDX_DIM ** -0.5)
    index = jnp.einsum('tis,ti->ts', rs, w_idx).astype(jnp.float32)
    index = jnp.where(jnp.arange(t)[None, :] <= tq[:, None], index, NEG)
    _, sel = lax.top_k(index, k_sel)
    ok = sel <= tq[:, None]
    c_g = ckv[sel]
    bias = rel_b[rel_bucket(tq[:, None] - sel)].transpose(2, 0, 1)
    logits = jnp.einsum('thr,tkr->htk', q_lat, c_g) * HEAD_DIM ** -0.5 + bias
    p = masked_softmax(logits, ok[None])
    return jnp.einsum('htk,tkr->thr', p, c_g)


def moe(h, w_grp, b_grp, w_rtr, b_rtr, w_gate, w_up, w_down):
    b, t, d = h.shape
    n = b * t
    hf = h.reshape(n, d)
    g_logits = (hf @ w_grp + b_grp).astype(jnp.float32)
    g_sel = jnp.argmax(g_logits, axis=-1)
    p_grp = jnp.take_along_axis(jax.nn.softmax(g_logits, axis=-1), g_sel[:, None], axis=1)[:, 0]
    e_logits = (hf @ w_rtr + b_rtr).astype(jnp.float32).reshape(n, N_EXPERT_GROUPS, EXPERTS_PER_GROUP)
    e_logits = jnp.take_along_axis(e_logits, g_sel[:, None, None], axis=1)[:, 0]
    top_l, top_i = lax.top_k(e_logits, EXPERT_TOPK)
    p_exp = jax.nn.softmax(top_l, axis=-1)
    w_a = (p_grp[:, None] * p_exp).reshape(-1)
    e_a = (g_sel[:, None] * EXPERTS_PER_GROUP + top_i).reshape(-1).astype(jnp.int32)
    tok_a = jnp.repeat(jnp.arange(n, dtype=jnp.int32), EXPERT_TOPK)
    n_a = n * EXPERT_TOPK
    order = jnp.argsort(e_a)
    e_s, tok_s, w_s = e_a[order], tok_a[order], w_a[order]
    counts = jax.ops.segment_sum(jnp.ones_like(e_a), e_a, num_segments=N_EXPERTS)
    padded = (counts + EXPERT_BLOCK - 1) // EXPERT_BLOCK * EXPERT_BLOCK
    off = jnp.cumsum(counts) - counts
    pend = jnp.cumsum(padded)
    poff = pend - padded
    dest = poff[e_s] + jnp.arange(n_a, dtype=jnp.int32) - off[e_s]
    n_blk = -(-n_a // EXPERT_BLOCK) + N_EXPERTS
    tok_pad = jnp.zeros((n_blk * EXPERT_BLOCK,), jnp.int32).at[dest].set(tok_s)
    w_pad = jnp.zeros((n_blk * EXPERT_BLOCK,), jnp.float32).at[dest].set(w_s)
    blk_exp = jnp.minimum(jnp.searchsorted(pend, jnp.arange(n_blk, dtype=jnp.int32) * EXPERT_BLOCK, side='right'), N_EXPERTS - 1)
    x_pad = hf[tok_pad].reshape(n_blk, EXPERT_BLOCK, d)

    def expert_block(args):
        xb, e = args
        return (jax.nn.silu(xb @ w_gate[e]) * (xb @ w_up[e])) @ w_down[e]

    y = lax.map(expert_block, (x_pad, blk_exp)).reshape(-1, d)
    out = jnp.zeros_like(hf).at[tok_pad].add(y * w_pad[:, None].astype(y.dtype))
    return out.reshape(b, t, d)


def setup_inputs(seed: int = 0) -> dict:
    key = jax.random.key(seed)
    ks = jax.random.split(key, 26)
    L = DEPTH

    def nrm(k, shape, scale):
        return jax.random.normal(k, shape, jnp.float32) * scale

    return {
        'x': nrm(ks[0], (BATCH, SEQ, D_MODEL), 1.0),
        'w_in': nrm(ks[1], (L, D_MODEL, D_IN), D_MODEL ** -0.5),
        'cmp_pe_k': nrm(ks[2], (L, CMP_BLOCK, HEAD_DIM), 0.1),
        'cmp_pe_v': nrm(ks[3], (L, CMP_BLOCK, HEAD_DIM), 0.1),
        'cmp_w1_k': nrm(ks[4], (L, CMP_BLOCK * HEAD_DIM, CMP_HIDDEN), (CMP_BLOCK * HEAD_DIM) ** -0.5),
        'cmp_w2_k': nrm(ks[5], (L, CMP_HIDDEN, HEAD_DIM), CMP_HIDDEN ** -0.5),
        'cmp_w1_v': nrm(ks[6], (L, CMP_BLOCK * HEAD_DIM, CMP_HIDDEN), (CMP_BLOCK * HEAD_DIM) ** -0.5),
        'cmp_w2_v': nrm(ks[7], (L, CMP_HIDDEN, HEAD_DIM), CMP_HIDDEN ** -0.5),
        'ckv_norm_g': 1.0 + nrm(ks[8], (L, KV_RANK), 0.02),
        'w_uk': nrm(ks[9], (L, DSA_HEADS, HEAD_DIM, KV_RANK), KV_RANK ** -0.5),
        'w_uv': nrm(ks[10], (L, DSA_HEADS, KV_RANK, HEAD_DIM), KV_RANK ** -0.5),
        'rel_bias': nrm(ks[11], (REL_BUCKETS, N_REL_HEADS), 0.1),
        'w_branch_a': nrm(ks[12], (L, NSA_WIDTH, D_MODEL), NSA_WIDTH ** -0.5 * BETA),
        'w_branch_b': nrm(ks[13], (L, DSA_WIDTH, D_MODEL), DSA_WIDTH ** -0.5 * BETA),
        'w_out': nrm(ks[14], (L, D_MODEL, D_MODEL), D_MODEL ** -0.5 * BETA),
        'ln1_g': 1.0 + nrm(ks[15], (L, D_MODEL), 0.02),
        'ln1_b': nrm(ks[16], (L, D_MODEL), 0.02),
        'w_grp': nrm(ks[17], (L, D_MODEL, N_EXPERT_GROUPS), D_MODEL ** -0.5),
        'b_grp': nrm(ks[18], (L, N_EXPERT_GROUPS), 0.01),
        'w_rtr': nrm(ks[19], (L, D_MODEL, N_EXPERTS), D_MODEL ** -0.5),
        'b_rtr': nrm(ks[20], (L, N_EXPERTS), 0.01),
        'w_gate': nrm(ks[21], (L, N_EXPERTS, D_MODEL, D_EXPERT), D_MODEL ** -0.5),
        'w_up': nrm(ks[22], (L, N_EXPERTS, D_MODEL, D_EXPERT), D_MODEL ** -0.5),
        'w_down': nrm(ks[23], (L, N_EXPERTS, D_EXPERT, D_MODEL), D_EXPERT ** -0.5 * BETA),
        'ln2_g': 1.0 + nrm(ks[24], (L, D_MODEL), 0.02),
        'ln2_b': nrm(ks[25], (L, D_MODEL), 0.02),
    }


def reference(x, w_in, cmp_pe_k, cmp_pe_v, cmp_w1_k, cmp_w2_k, cmp_w1_v, cmp_w2_v, ckv_norm_g, w_uk, w_uv, rel_bias, w_branch_a, w_branch_b, w_out, ln1_g, ln1_b, w_grp, b_grp, w_rtr, b_rtr, w_gate, w_up, w_down, ln2_g, ln2_b):
    b, t, _ = x.shape
    n_qblk = t // QBLK
    cmp_map = jnp.asarray(cmp_to_slc_map(t))
    rel_a = rel_bias[:, :NSA_HEADS].reshape(REL_BUCKETS, NSA_GROUPS, NSA_HPG)
    rel_b = rel_bias[:, NSA_HEADS:]
    kv_shape = (b, t, NSA_GROUPS, HEAD_DIM)
    pad = ((0, 0), (WINDOW, 0), (0, 0), (0, 0))
    h = x
    for l in range(DEPTH):
        z = jnp.einsum('btd,de->bte', h, w_in[l])
        (q_a, kc, vc, ks, vs, kw, vw, g_nsa, q_b, ckv, q_idx, k_idx, w_idx, gate_a, gate_b) = jnp.split(z, SPLIT_POINTS, axis=-1)
        q_a = q_a.reshape(b, t, NSA_GROUPS, NSA_HPG, HEAD_DIM)
        k_cmp = compress_blocks(kc.reshape(kv_shape), cmp_pe_k[l], cmp_w1_k[l], cmp_w2_k[l])
        v_cmp = compress_blocks(vc.reshape(kv_shape), cmp_pe_v[l], cmp_w1_v[l], cmp_w2_v[l])
        ks = ks.reshape(kv_shape)
        vs = vs.reshape(kv_shape)
        kw = jnp.pad(kw.reshape(kv_shape), pad)
        vw = jnp.pad(vw.reshape(kv_shape), pad)
        g_nsa = jax.nn.sigmoid(g_nsa.reshape(b, t, NSA_GROUPS, NSA_HPG, 3))
        q_lat = jnp.einsum('bthd,hdr->bthr', q_b.reshape(b, t, DSA_HEADS, HEAD_DIM), w_uk[l])
        ckv = rms_norm(ckv, ckv_norm_g[l])
        q_idx = q_idx.reshape(b, t, IDX_HEADS, IDX_DIM)
        w_idx = w_idx * IDX_HEADS ** -0.5

        def sweep(i):
            bi = i // n_qblk
            q0 = (i % n_qblk) * QBLK
            tq = q0 + jnp.arange(QBLK)

            def sl(a):
                return lax.dynamic_slice_in_dim(a[bi], q0, QBLK, axis=0)

            o_a = nsa_block(sl(q_a), k_cmp[bi], v_cmp[bi], ks[bi], vs[bi], kw[bi], vw[bi], sl(g_nsa), rel_a, cmp_map, tq, q0)
            o_lat = dsa_block(sl(q_lat), ckv[bi], sl(q_idx), k_idx[bi], sl(w_idx), rel_b, tq)
            return o_a, o_lat

        o_a, o_lat = lax.map(sweep, jnp.arange(b * n_qblk))
        o_a = o_a.reshape(b, t, NSA_WIDTH)
        o_b = jnp.einsum('bthr,hrd->bthd', o_lat.reshape(b, t, DSA_HEADS, KV_RANK), w_uv[l]).reshape(b, t, DSA_WIDTH)
        merged = jax.nn.sigmoid(gate_a) * (o_a @ w_branch_a[l]) + jax.nn.sigmoid(gate_b) * (o_b @ w_branch_b[l])
        h = layer_norm(ALPHA * h + merged @ w_out[l], ln1_g[l], ln1_b[l])
        h = layer_norm(ALPHA * h + moe(h, w_grp[l], b_grp[l], w_rtr[l], b_rtr[l], w_gate[l], w_up[l], w_down[l]), ln2_g[l], ln2_b[l])
    return h
```

```python
import math
from contextlib import ExitStack
import numpy as np
import ml_dtypes
import concourse.bass as bass
import concourse.mybir as mybir
from concourse.bass_utils import run_bass_kernel_spmd

F32 = mybir.dt.float32
BF16 = mybir.dt.bfloat16
I32 = mybir.dt.int32
AF = mybir.ActivationFunctionType
ALU = mybir.AluOpType
AX = mybir.AxisListType

T = 4096
D = 1024
NT = T // 128
D_IN = 4316
ALPHA = 2.0 ** 0.25
CAP = 512
NEGM = 32768.0
BIS_ITERS = 20
DEBUG = None

O_QA, O_KC, O_VC, O_KS, O_VS, O_KW, O_VW, O_GN, O_QB, O_CKV, O_QI, O_KI, O_WI, O_GA, O_GB = (
    0, 512, 640, 768, 896, 1024, 1152, 1280, 1304, 1816, 1944, 2200, 2264, 2268, 3292)


class Res:
    __slots__ = ("name", "w", "r", "dsem", "dcount", "excl")

    def __init__(self, name):
        self.name = name
        self.excl = False
        self.w = None
        self.r = {}
        self.dsem = None
        self.dcount = 0


class Prog:
    ENG = ("pe", "act", "dve", "pool", "sp")

    def __init__(self, nc, stack):
        self.nc = nc
        self.stack = stack
        self.eobj = {"pe": nc.tensor, "act": nc.scalar, "dve": nc.vector, "pool": nc.gpsimd, "sp": nc.sync}
        self.sem = {e: stack.enter_context(nc.semaphore("c_" + e)) for e in self.ENG}
        self.cnt = {e: 0 for e in self.ENG}
        self.seen = {e: {} for e in self.ENG}
        self.ops = {e: [] for e in self.ENG}
        self.dres = []
        self.nres = 0

    def res(self, name=None):
        self.nres += 1
        return Res(name or "r%d" % self.nres)

    def _waits(self, eng, reads, writes):
        need = {}

        def add(ev, war=False):
            if ev is None:
                return
            sem, val, src = ev
            if src == eng and eng == "pe":
                return
            k = id(sem)
            if self.seen[eng].get(k, (None, 0))[1] >= val:
                return
            if k not in need or need[k][1] < val:
                need[k] = (sem, val)

        for r in reads:
            add(r.w)
        for w in writes:
            add(w.w)
            for ev in w.r.values():
                add(ev, True)
        out = []
        for k, (sem, val) in need.items():
            self.seen[eng][k] = (sem, val)
            out.append((sem, val))
        return out

    def op(self, eng, fn, R=(), W=()):
        W = list(W) + [r for r in R if r.excl]
        R = [r for r in R if not r.excl]
        waits = self._waits(eng, R, W)
        self.cnt[eng] += 1
        ev = (self.sem[eng], self.cnt[eng], eng)
        self.ops[eng].append((fn, waits, (self.sem[eng], 1)))
        for r in R:
            r.r[eng] = ev
        for w in W:
            w.w = ev
            w.r = {}

    def dma(self, q, out, in_, R=(), W=(), fn=None):
        waits = self._waits(q, R, W)
        tgt = W[0]
        if tgt.dsem is None:
            tgt.dsem = self.stack.enter_context(self.nc.semaphore("d_" + tgt.name))
            self.dres.append(tgt)
        tgt.dcount += 16
        ev = (tgt.dsem, tgt.dcount, None)
        if fn is None:
            fn = lambda e, o=out, i=in_: e.dma_start(out=o, in_=i)
        self.ops[q].append((fn, waits, (tgt.dsem, 16)))
        for r in R:
            r.r["dma%d" % id(tgt)] = ev
        for w in W:
            w.w = ev
            w.r = {}

    def flush(self, final=False):
        tail = {}
        for e in self.ENG:
            ws = []
            for e2 in self.ENG:
                if e2 != e and self.cnt[e2] > 0 and self.seen[e].get(id(self.sem[e2]), (None, 0))[1] < self.cnt[e2]:
                    ws.append((self.sem[e2], self.cnt[e2]))
                    self.seen[e][id(self.sem[e2])] = (self.sem[e2], self.cnt[e2])
            for r in self.dres:
                if self.seen[e].get(id(r.dsem), (None, 0))[1] < r.dcount:
                    ws.append((r.dsem, r.dcount))
                    self.seen[e][id(r.dsem)] = (r.dsem, r.dcount)
            tail[e] = ws
        ops = self.ops
        eobj = self.eobj
        self.regs = {}
        with self.nc.Block() as block:
            def run(e, engine):
                for fn, waits, inc in ops[e]:
                    for s, v in waits:
                        engine.wait_ge(s, v)
                    ins = fn(engine)
                    ins.then_inc(inc[0], inc[1])
                for s, v in tail[e]:
                    engine.wait_ge(s, v)

            @block.tensor
            def _(t):
                run("pe", t)

            @block.scalar
            def _(s):
                run("act", s)

            @block.vector
            def _(v):
                run("dve", v)

            @block.gpsimd
            def _(g):
                run("pool", g)

            @block.sync
            def _(sy):
                run("sp", sy)
        self.ops = {e: [] for e in self.ENG}

    def breg(self, e, val):
        if val not in self.regs:
            self.regs[val] = e.to_reg(val)
        return self.regs[val]

    def mm(self, out, lhsT, rhs, start, stop, R=(), W=()):
        self.op("pe", lambda e: e.matmul(out, lhsT, rhs, start=start, stop=stop, skip_group_check=True), R, W)

    def tr(self, out, in_, ident, R=(), W=()):
        self.op("pe", lambda e: e.transpose(out, in_, ident), R, W)

    def act(self, out, in_, func, R=(), W=(), bias=None, scale=None, accum=None, eng="act"):
        kw = {}
        if bias is not None:
            kw["bias"] = bias
        if scale is not None:
            kw["scale"] = scale
        if accum is not None:
            kw["accum_out"] = accum
        self.op("act", lambda e: e.activation(out, in_, func, **kw), R, W)

    def ts(self, eng, out, in0, s1, s2, op0, op1=None, R=(), W=(), accum=None):
        kw = {}
        if op1 is not None:
            kw["op1"] = op1
        if accum is not None:
            kw["accum_out"] = accum
        self.op(eng, lambda e: e.tensor_scalar(out, in0, s1, s2, op0, **kw), R, W)

    def tt(self, eng, out, in0, in1, op, R=(), W=()):
        self.op(eng, lambda e: e.tensor_tensor(out, in0, in1, op), R, W)

    def stt(self, out, in0, scalar, in1, op0, op1, R=(), W=(), accum=None):
        kw = {}
        if accum is not None:
            kw["accum_out"] = accum
        self.op("dve", lambda e: e.scalar_tensor_tensor(out, in0, scalar, in1, op0, op1, **kw), R, W)

    def cp(self, eng, out, in_, R=(), W=()):
        if eng == "act":
            self.op("act", lambda e: e.copy(out, in_), R, W)
        else:
            self.op(eng, lambda e: e.tensor_copy(out, in_), R, W)

    def memset(self, eng, ap, val, W=()):
        self.op(eng, lambda e: e.memset(ap, val), (), W)


class Buf:
    def __init__(self, t, res):
        self.t = t
        self.r = res

    def __getitem__(self, k):
        return self.t[k]


def _rel_bucket_np(dist):
    n = np.maximum(dist, 0)
    nf = np.maximum(n, 1).astype(np.float32)
    large = 16 + (np.log(nf / 16) / math.log(1024 / 16) * 16).astype(np.int32)
    return np.where(n < 16, n, np.minimum(large, 31))


def _host_consts(rel_bias):
    c = {}
    dd = np.arange(0, 1152)
    bk = _rel_bucket_np(dd)
    relvec = rel_bias[bk]
    p = np.arange(128)[:, None]
    j = np.arange(128)[None, :]
    relT = np.zeros((8, 128, 16, 128), np.float32)
    for di in range(8):
        dist = np.clip(128 * di + j - p, 0, 1151)
        relT[di] = relvec[dist].transpose(0, 2, 1)
    c["relTa"] = np.ascontiguousarray(relT[:, :, :8, :].transpose(1, 0, 2, 3)).reshape(128, 8, 2, 512)
    c["relTb"] = np.ascontiguousarray(relT[:, :, 8:, :].transpose(1, 0, 2, 3)).reshape(128, 8, 1024)
    c31 = relvec[1151]
    c["c31a"] = np.ascontiguousarray(np.repeat(c31[:8].reshape(2, 4, 1), 128, axis=2).reshape(2, 512))
    c["c31b"] = np.ascontiguousarray(np.repeat(c31[8:].reshape(8, 1), 128, axis=1).reshape(1, 1024))
    v = np.arange(503)[None, :]
    dist = p - 16 * v + 3937
    c["cmpv"] = np.ascontiguousarray(relvec[np.clip(dist, 0, 1151)][:, :, :8].transpose(0, 2, 1))
    c["cmpm"] = np.where(dist >= 0, 0.0, -30000.0).astype(np.float32)
    mc = np.where(j < p, -NEGM, 0.0).astype(np.float32)
    mw = np.where(j >= p, -NEGM, 0.0).astype(np.float32)
    c["mc4"] = np.tile(mc, (1, 4))
    c["mw4"] = np.tile(mw, (1, 4))
    c["ident"] = np.eye(128, dtype=np.float32)
    c["d30"] = np.tile(np.eye(128, dtype=np.float32) * NEGM, (1, 4))
    e_all = np.zeros((64, 32, 128), np.float32)
    for kt in range(32):
        e_all[2 * kt, kt, :64] = NEGM
        e_all[2 * kt + 1, kt, 64:] = NEGM
    c["eall"] = e_all.reshape(64, 32 * 128)
    c["mcq"] = np.where(j > p, -1e30, 0.0).astype(np.float32)
    cp_ = (np.arange(128) >= 64).astype(np.int64)[:, None]
    u = np.arange(128)[None, :] - 63
    c["wext"] = np.where((u == cp_) | (u == cp_ - 1), 1e4, np.where(u > cp_, -1e30, 0.0)).astype(np.float32)
    cs = 16 * np.arange(255)[:, None]
    ss = 64 * np.arange(64)[None, :]
    ov = np.minimum(cs + 32, ss + 64) - np.maximum(cs, ss)
    cm = np.zeros((256, 64), np.float32)
    cm[:255] = np.clip(ov, 0, None).astype(np.float32) / 16
    c["cmap"] = cm
    c["ustrict"] = (np.arange(128)[:, None] < np.arange(128)[None, :]).astype(np.float32)
    c["iota32"] = np.tile(np.arange(32, dtype=np.float32)[None, :], (128, 1))
    c["tokid"] = (np.arange(128)[:, None] + 128 * np.arange(32)[None, :]).astype(np.int32)
    return c


class Ctx:
    pass


def build(debug=None):
    nc = bass.Bass("TRN2", target_bir_lowering=False)
    es = ExitStack()
    p = Prog(nc, es)
    g = Ctx()
    g.nc, g.p, g.es, g.debug = nc, p, es, debug
    g.dbg_out = {}

    def din(name, shape, dt=F32):
        return Buf(nc.dram_tensor(name, list(shape), dt, kind="ExternalInput").ap(), p.res(name))

    def dscr(name, shape, dt=F32):
        kind = "ExternalOutput" if (debug and name in debug) else "Internal"
        return Buf(nc.dram_tensor(name, list(shape), dt, kind=kind).ap(), p.res(name))

    g.din, g.dscr = din, dscr

    def sb(name, shape, dt=F32, stack=None):
        t = (stack or es).enter_context(nc.sbuf_tensor("s_" + name, list(shape), dt))
        return Buf(t, p.res(name))

    def ps(name, shape, dt=F32, stack=None):
        t = (stack or es).enter_context(nc.psum_tensor(name, list(shape), dt))
        r = p.res(name)
        r.excl = True
        return Buf(t, r)

    g.sb, g.ps = sb, ps

    I = g.I = {}
    for name, shape in [
        ("xT", (D, T)), ("x", (T, D)), ("w_in", (D, D_IN)),
        ("pe_kT", (64, 32)), ("pe_vT", (64, 32)),
        ("cw1_k", (2048, 256)), ("cw2_k", (256, 64)), ("cw1_v", (2048, 256)), ("cw2_v", (256, 64)),
        ("ckv_g", (1, 128)), ("w_uk", (8, 64, 128)), ("w_uv", (8, 128, 64)), ("relflat", (1, 512)),
        ("w_ba", (512, D)), ("w_bb", (512, D)), ("w_out", (D, D)),
        ("ln1_g", (1, D)), ("ln1_b", (1, D)), ("wr", (D, 36)), ("br", (1, 36)),
        ("w_gate", (32, D, 256)), ("w_up", (32, D, 256)), ("w_down", (32, 256, D)),
        ("ln2_g", (1, D)), ("ln2_b", (1, D)),
        ("relTa", (128, 8, 2, 512)), ("relTb", (128, 8, 1024)), ("c31a", (2, 512)), ("c31b", (1, 1024)),
        ("cmpv", (128, 8, 503)), ("cmpm", (128, 503)), ("mc4", (128, 512)), ("mw4", (128, 512)),
        ("ident", (128, 128)), ("d30", (128, 512)), ("eall", (64, 4096)), ("mcq", (128, 128)),
        ("wext", (128, 128)), ("cmap", (256, 64)), ("ustrict", (128, 128)), ("iota32", (128, 32)),
    ]:
        I[name] = din(name, shape)
    I["tokid"] = din("tokid", (128, 32), I32)
    g.out = Buf(nc.dram_tensor("out", [T, D], F32, kind="ExternalOutput").ap(), p.res("out"))

    S = g.S = {}
    S["qa_s"] = dscr("qa_s", (NT, 2, 64, 4, 128), BF16)
    S["ql_s"] = dscr("ql_s", (NT, 128, 8, 128), BF16)
    S["qi_s"] = dscr("qi_s", (NT, 64, 4, 128), BF16)
    S["g_s"] = dscr("g_s", (NT, 128, 16, 128), F32)
    S["h1_s"] = dscr("h1_s", (T, D), F32)
    S["slot"] = dscr("slot", (32 * CAP, 16), I32)
    S["y_s"] = dscr("y_s", (32 * CAP, D), F32)

    Rz = g.Rz = {}
    Rz["ksT"] = sb("ksT", (128, T), BF16)
    Rz["kwT"] = sb("kwT", (128, T), BF16)
    Rz["ckvT"] = sb("ckvT", (128, T), BF16)
    Rz["kiT"] = sb("kiT", (64, T), BF16)
    Rz["vsA"] = sb("vsA", (128, NT, 2, 65), BF16)
    Rz["vwA"] = sb("vwA", (128, NT, 2, 65), BF16)
    Rz["ckvA"] = sb("ckvA", (128, NT, 129), BF16)
    Rz["gn"] = sb("gn", (128, NT, 24), F32)
    Rz["wabs"] = sb("wabs", (128, NT, 4), F32)
    Rz["wsgn"] = sb("wsgn", (128, NT, 4), F32)
    Rz["kcmpT"] = sb("kcmpT", (128, 256), BF16)
    Rz["vcmpM"] = sb("vcmpM", (128, 2, 2, 128), BF16)
    Rz["kmax"] = sb("kmax", (1, 4), F32)
    Rz["identb"] = sb("identb", (128, 128), BF16)
    Rz["identf"] = sb("identf", (128, 128), F32)
    Rz["ones_bf"] = sb("ones_bf", (128, 128), BF16)

    g.kcT = sb("kcT", (128, T), BF16)
    g.vcT = sb("vcT", (128, T), BF16)
    g.dest = sb("dest_i", (128, NT, 2), I32)
    g.wts = sb("wts", (128, NT, 2), F32)
    g.banks = [ps("bank%d" % i, (128, 512), F32) for i in range(8)]

    stage = (debug or {}).get("stage", 99)
    st = phase1(g)
    if debug:
        dump_resident(g)
    p.flush()
    st.close()
    if stage >= 2:
        st = phase2(g)
        p.flush()
        st.close()
    if stage >= 3:
        st = phase3(g)
        p.flush()
        st.close()
    if stage >= 4:
        st = phase4(g)
        p.flush()
        st.close()
    if stage >= 5:
        st = phase5(g)
        p.flush()
        st.close()
    return nc, g


def dump_resident(g):
    nc, p = g.nc, g.p
    for name, buf in g.Rz.items():
        shape = list(buf.t.shape)
        d = nc.dram_tensor("dbg_" + name, shape, buf.t.dtype, kind="ExternalOutput").ap()
        r = p.res("dbg_" + name)
        idx = tuple(slice(None) for _ in shape)
        p.dma("sp", d[idx], buf.t[idx], R=[buf.r], W=[r])


def phase1(g):
    nc, p, sb, I, S, Rz = g.nc, g.p, g.sb, g.I, g.S, g.Rz
    st = ExitStack()
    banks = g.banks
    bi = [0]

    def nbank():
        b = banks[bi[0] % 8]
        bi[0] += 1
        return b

    ev_rr = [0]

    def ev_eng():
        ev_rr[0] += 1
        return "act" if ev_rr[0] % 2 else "dve"

    stf = sb("p1_cst", (128, 128), F32, st)
    p.dma("sp", stf[:], I["ident"][:, :], R=[I["ident"].r], W=[stf.r])
    p.cp("dve", Rz["identb"][:], stf[:], R=[stf.r], W=[Rz["identb"].r])
    p.cp("dve", Rz["identf"][:], stf[:], R=[stf.r], W=[Rz["identf"].r])
    p.memset("dve", Rz["ones_bf"][:], 1.0, W=[Rz["ones_bf"].r])
    p.memset("pool", Rz["vsA"][:, :, :, 64:65], 1.0, W=[Rz["vsA"].r])
    p.memset("pool", Rz["vwA"][:, :, :, 64:65], 1.0, W=[Rz["vwA"].r])
    p.memset("pool", Rz["ckvA"][:, :, 128:129], 1.0, W=[Rz["ckvA"].r])

    xTb = sb("xTb", (128, 8, T), BF16, st)
    xst = [sb("xst%d" % i, (128, 1024), F32, st) for i in range(2)]
    xq = [p.res("xTq%d" % q) for q in range(4)]
    engs = ["dve", "act", "dve", "act"]
    xk = [0]

    def load_x(q):
        for c in range(8):
            s_ = xst[xk[0] % 2]
            xk[0] += 1
            p.dma("sp", s_[:], I["xT"][c * 128:(c + 1) * 128, q * 1024:(q + 1) * 1024], R=[I["xT"].r], W=[s_.r])
            p.cp(engs[c % 4], xTb[:, c, q * 1024:(q + 1) * 1024], s_[:], R=[s_.r], W=[xq[q]])

    cut = 99
    wst = [sb("wst%d" % i, (128, 8, 128), F32, st) for i in range(2)]
    wbf = [sb("wbf%d" % i, (128, 8, 128), BF16, st) for i in range(2)]
    wk = [0]

    def load_w(col0, M):
        k = wk[0] % 2
        wk[0] += 1
        p.dma("sp", wst[k][:, :, 0:M], I["w_in"][:, col0:col0 + M].rearrange("(c p) m -> p c m", p=128),
              R=[I["w_in"].r], W=[wst[k].r])
        p.cp("act" if k else "dve", wbf[k][:, :, 0:M], wst[k][:, :, 0:M], R=[wst[k].r], W=[wbf[k].r])
        return wbf[k]

    groups = []

    def fm_group(col0, M, evac):
        groups.append((col0, M, evac))

    def run_groups():
        nxt = load_w(groups[0][0], groups[0][1])
        load_x(0)
        for gi, (col0, M, evac) in enumerate(groups):
            w = nxt
            if gi + 1 < len(groups):
                nxt = load_w(groups[gi + 1][0], groups[gi + 1][1])
            for tb in range(8):
                if gi == 0 and tb % 2 == 0 and tb < 6:
                    load_x(tb // 2 + 1)
                b = nbank()
                for c in range(8):
                    p.mm(b[0:M, :], w[:, c, 0:M], xTb[:, c, tb * 512:(tb + 1) * 512], c == 0, c == 7,
                         R=[w.r, xq[tb // 2]], W=[b.r])
                evac(tb, b)

    stg_bf = [sb("stgb%d" % i, (128, 512), BF16, st) for i in range(4)]
    stg_f = [sb("stgf%d" % i, (128, 512), F32, st) for i in range(2)] * 2
    sk = [0]

    def nstg(lst):
        sk[0] += 1
        return lst[sk[0] % 4]

    def evac_copy(dst_buf, scale=None):
        def f(tb, b):
            M = dst_buf.t.shape[0]
            e = ev_eng()
            o = dst_buf[:, tb * 512:(tb + 1) * 512]
            if e == "act":
                p.act(o, b[0:M, :], AF.Copy, R=[b.r], W=[dst_buf.r])
            else:
                p.cp("dve", o, b[0:M, :], R=[b.r], W=[dst_buf.r])
        return f

    for m in range(4):
        def ev(tb, b, m=m):
            s_ = nstg(stg_bf)
            p.act(s_[:], b[:], AF.Copy, scale=0.125, R=[b.r], W=[s_.r])
            gq, hh0 = m // 2, 2 * (m % 2)
            for hl in range(2):
                dst = S["qa_s"][tb * 4:(tb + 1) * 4, gq, :, hh0 + hl, :].rearrange("t d q -> d t q")
                src = s_[hl * 64:(hl + 1) * 64, :].rearrange("d (t q) -> d t q", q=128)
                p.dma("sp", dst, src, R=[s_.r], W=[S["qa_s"].r])
        fm_group(O_QA + 128 * m, 128, ev)

    kcT, vcT = g.kcT, g.vcT
    fm_group(O_KC, 128, evac_copy(kcT))
    fm_group(O_VC, 128, evac_copy(vcT))
    fm_group(O_KS, 128, evac_copy(Rz["ksT"]))
    fm_group(O_KW, 128, evac_copy(Rz["kwT"]))
    fm_group(O_KI, 64, evac_copy(Rz["kiT"]))

    wukf = sb("wukf", (128, 4, 128), F32, st)
    wukb = sb("wukb", (128, 4, 128), BF16, st)
    p.dma("sp", wukf[:], I["w_uk"][:, :, :].rearrange("(m hl) d r -> (hl d) m r", hl=2), R=[I["w_uk"].r], W=[wukf.r])
    p.cp("dve", wukb[:], wukf[:], R=[wukf.r], W=[wukb.r])
    for m in range(4):
        def ev(tb, b, m=m):
            s_ = nstg(stg_bf)
            p.cp("dve", s_[:], b[:], R=[b.r], W=[s_.r])
            for hl in range(2):
                b2 = nbank()
                p.mm(b2[:, :], wukb[hl * 64:(hl + 1) * 64, m, :], s_[hl * 64:(hl + 1) * 64, :], True, True,
                     R=[wukb.r, s_.r], W=[b2.r])
                s2 = nstg(stg_bf)
                p.act(s2[:], b2[:], AF.Copy, scale=0.125, R=[b2.r], W=[s2.r])
                dst = S["ql_s"][tb * 4:(tb + 1) * 4, :, 2 * m + hl, :].rearrange("t r q -> r t q")
                p.dma("sp", dst, s2[:].rearrange("r (t q) -> r t q", q=128), R=[s2.r], W=[S["ql_s"].r])
        fm_group(O_QB + 128 * m, 128, ev)

    for m in range(2):
        def ev(tb, b, m=m):
            s_ = nstg(stg_bf)
            p.act(s_[:], b[:], AF.Copy, scale=0.125, R=[b.r], W=[s_.r])
            for hl in range(2):
                dst = S["qi_s"][tb * 4:(tb + 1) * 4, :, 2 * m + hl, :].rearrange("t d q -> d t q")
                src = s_[hl * 64:(hl + 1) * 64, :].rearrange("d (t q) -> d t q", q=128)
                p.dma("sp", dst, src, R=[s_.r], W=[S["qi_s"].r])
        fm_group(O_QI + 128 * m, 128, ev)

    for m in range(16):
        def ev(tb, b, m=m):
            s_ = nstg(stg_f)
            p.act(s_[:], b[:], AF.Sigmoid, R=[b.r], W=[s_.r])
            dst = S["g_s"][tb * 4:(tb + 1) * 4, :, m, :].rearrange("t c q -> c t q")
            p.dma("sp", dst, s_[:].rearrange("c (t q) -> c t q", q=128), R=[s_.r], W=[S["g_s"].r])
        fm_group(O_GA + 128 * m, 128, ev)

    run_groups()
    wtf = sb("wtf", (128, 8, 412), F32, st)
    wtb = sb("wtb", (128, 8, 412), BF16, st)
    for (c0, n, o) in [(O_VS, 128, 0), (O_VW, 128, 128), (O_CKV, 128, 256), (O_GN, 24, 384), (O_WI, 4, 408)]:
        r_ = p.res("wtf%d" % o)
        p.dma("sp", wtf[:, :, o:o + n], I["w_in"][:, c0:c0 + n].rearrange("(c p) m -> p c m", p=128),
              R=[I["w_in"].r], W=[r_])
        p.cp("dve", wtb[:, :, o:o + n], wtf[:, :, o:o + n], R=[r_], W=[wtb.r])
    gbc = sb("gbc", (128, 128), F32, st)
    p.dma("sp", gbc[:], I["ckv_g"][0:1, :].partition_broadcast(128), R=[I["ckv_g"].r], W=[gbc.r])
    junk = sb("p1junk", (128, 128), F32, st)
    ssq = sb("p1ssq", (128, 4), F32, st)
    sub = (g.debug or {}).get("sub", 99)
    for tt in range(NT if sub >= 1 else 0):
        b = nbank()
        for c in range(8):
            p.mm(b[:, 0:412], xTb[:, c, tt * 128:(tt + 1) * 128], wtb[:, c, :], c == 0, c == 7,
                 R=[wtb.r, xq[tt // 8]], W=[b.r])
        p.cp("dve", Rz["vsA"][:, tt, :, 0:64], b[:, 0:128].rearrange("p (g d) -> p g d", g=2), R=[b.r], W=[Rz["vsA"].r])
        p.cp("dve", Rz["vwA"][:, tt, :, 0:64], b[:, 128:256].rearrange("p (g d) -> p g d", g=2), R=[b.r], W=[Rz["vwA"].r])
        if sub <= 1:
            continue
        p.act(junk[:], b[:, 256:384], AF.Square, R=[b.r], W=[junk.r, ssq.r], accum=ssq[:, 0:1])
        sub2 = (g.debug or {}).get("sub2", 99)
        if sub2 <= 0:
            continue
        p.ts("dve", ssq[:, 1:2], ssq[:, 0:1], 1.0 / 128, 1e-6, ALU.mult, ALU.add, R=[ssq.r], W=[ssq.r])
        if sub2 <= 1:
            continue
        p.act(ssq[:, 2:3], ssq[:, 1:2], AF.Sqrt, R=[ssq.r], W=[ssq.r])
        if sub2 <= 2:
            continue
        p.op("dve", lambda e: e.reciprocal(ssq[:, 3:4], ssq[:, 2:3]), R=[ssq.r], W=[ssq.r])
        if sub2 <= 3:
            continue
        p.stt(Rz["ckvA"][:, tt, 0:128], b[:, 256:384], ssq[:, 3:4], gbc[:], ALU.mult, ALU.mult,
              R=[b.r, ssq.r, gbc.r], W=[Rz["ckvA"].r])
        if sub <= 2:
            continue
        p.act(Rz["gn"][:, tt, :], b[:, 384:408], AF.Sigmoid, R=[b.r], W=[Rz["gn"].r])
        p.act(Rz["wabs"][:, tt, :], b[:, 408:412], AF.Abs, scale=0.5, R=[b.r], W=[Rz["wabs"].r])
        p.act(Rz["wsgn"][:, tt, :], b[:, 408:412], AF.Sign, R=[b.r], W=[Rz["wsgn"].r])
        if sub <= 3:
            continue
        b2 = nbank()
        tv = b2.t[:].bitcast(BF16)
        p.tr(tv[:, 0:128], Rz["ckvA"][:, tt, 0:128], Rz["identb"][:], R=[Rz["ckvA"].r, Rz["identb"].r], W=[b2.r])
        p.cp("act", Rz["ckvT"][:, tt * 128:(tt + 1) * 128], tv[:, 0:128], R=[b2.r], W=[Rz["ckvT"].r])

    if cut <= 6:
        return st
    p.flush()
    st.close()
    st = ExitStack()
    phase1b(g, st, kcT, vcT, nbank)
    return st


def phase1b(g, st, kcT, vcT, nbank):
    nc, p, sb, I, S, Rz = g.nc, g.p, g.sb, g.I, g.S, g.Rz
    w1s = sb("w1s", (128, 8, 256), F32, st)
    w2s = sb("w2s", (128, 2, 64), F32, st)
    pes = sb("pes", (128, 32), F32, st)
    peb = sb("peb", (128, 32), BF16, st)
    cst = sb("cst", (128, 2), F32, st)
    u = sb("cu", (128, 256), F32, st)
    t1 = sb("ct1", (128, 256), F32, st)
    t2 = sb("ct2", (128, 256), F32, st)
    cms = sb("cms", (128, 2, 64), F32, st)
    p.dma("sp", cms[:], I["cmap"][:, :].rearrange("(c p) n -> p c n", p=128), R=[I["cmap"].r], W=[cms.r])
    for gq in range(2):
        p.cp("dve", Rz["vcmpM"][:, :, gq, 64:128], cms[:], R=[cms.r], W=[Rz["vcmpM"].r])
    for kv, (srcT, w1n, w2n, pen) in enumerate([(kcT, "cw1_k", "cw2_k", "pe_kT"), (vcT, "cw1_v", "cw2_v", "pe_vT")]):
        w1b = sb("w1b%d" % kv, (128, 32, 256), BF16, st)
        w2p = sb("w2p%d" % kv, (128, 2, 2, 128), BF16, st)
        w2b = sb("w2b%d" % kv, (128, 2, 64), BF16, st)
        gel = sb("gel%d" % kv, (128, 2, 2, 256), BF16, st)
        p.memset("pool", gel[:], 0.0, W=[gel.r])
        p.memset("pool", w2p[:], 0.0, W=[w2p.r])
        for lq in range(4):
            for half in range(2):
                p.dma("sp", w1s[half * 64:(half + 1) * 64, :, :],
                      I[w1n][lq * 512:(lq + 1) * 512, :].rearrange("(l d) h -> d l h", d=64),
                      R=[I[w1n].r], W=[w1s.r])
            p.cp("act", w1b[:, lq * 8:(lq + 1) * 8, :], w1s[:], R=[w1s.r], W=[w1b.r])
        p.dma("sp", w2s[:], I[w2n][:, :].rearrange("(c p) d -> p c d", p=128), R=[I[w2n].r], W=[w2s.r])
        p.cp("dve", w2b[:], w2s[:], R=[w2s.r], W=[w2b.r])
        for gq in range(2):
            p.cp("dve", w2p[:, :, gq, gq * 64:(gq + 1) * 64], w2s[:], R=[w2s.r], W=[w2p.r])
        for half in range(2):
            p.dma("sp", pes[half * 64:(half + 1) * 64, :], I[pen][:, :], R=[I[pen].r], W=[pes.r])
        p.cp("dve", peb[:], pes[:], R=[pes.r], W=[peb.r])
        for gq in range(2):
            rows = slice(gq * 64, (gq + 1) * 64)
            for hc in range(2):
                bH, bC = nbank(), nbank()
                for l in range(32):
                    p.mm(bH[:, 0:255], w1b[rows, l, hc * 128:(hc + 1) * 128], srcT.t[rows, l:l + 16 * 254 + 1:16],
                         l == 0, l == 31, R=[w1b.r, srcT.r], W=[bH.r])
                for l in range(32):
                    p.mm(bC[:, 0:1], w1b[rows, l, hc * 128:(hc + 1) * 128], peb[rows, l:l + 1],
                         l == 0, l == 31, R=[w1b.r, peb.r], W=[bC.r])
                p.cp("dve", cst[:, 0:1], bC[:, 0:1], R=[bC.r], W=[cst.r])
                p.act(u[:, 0:255], bH[:, 0:255], AF.Identity, bias=cst[:, 0:1], R=[bH.r, cst.r], W=[u.r])
                p.tt("dve", t1[:, 0:255], u[:, 0:255], u[:, 0:255], ALU.mult, R=[u.r], W=[t1.r])
                p.ts("dve", t1[:, 0:255], t1[:, 0:255], 0.044715, 1.0, ALU.mult, ALU.add, R=[t1.r], W=[t1.r])
                p.tt("dve", t1[:, 0:255], t1[:, 0:255], u[:, 0:255], ALU.mult, R=[t1.r, u.r], W=[t1.r])
                p.act(t2[:, 0:255], t1[:, 0:255], AF.Tanh, scale=0.7978845608028654, R=[t1.r], W=[t2.r])
                p.stt(t2[:, 0:255], t2[:, 0:255], 1.0, u[:, 0:255], ALU.add, ALU.mult, R=[t2.r, u.r], W=[t2.r])
                p.ts("dve", gel[:, gq, hc, 0:255], t2[:, 0:255], 0.5, None, ALU.mult, R=[t2.r], W=[gel.r])
        if kv == 0:
            b = nbank()
            n = 0
            for gq in range(2):
                for hc in range(2):
                    p.mm(b[:, 0:256], w2p[:, hc, gq, :], gel[:, gq, hc, :], n == 0, n == 3, R=[w2p.r, gel.r], W=[b.r])
                    n += 1
            p.cp("dve", Rz["kcmpT"][:], b[:, 0:256], R=[b.r], W=[Rz["kcmpT"].r])
        else:
            for gq in range(2):
                for cc in range(2):
                    b = nbank()
                    for hc in range(2):
                        p.mm(b[:, 0:64], gel[:, gq, hc, cc * 128:(cc + 1) * 128], w2b[:, hc, :], hc == 0, hc == 1,
                             R=[gel.r, w2b.r], W=[b.r])
                    p.cp("dve", Rz["vcmpM"][:, cc, gq, 0:64], b[:, 0:64], R=[b.r], W=[Rz["vcmpM"].r])

    sq = sb("sq", (128, T), BF16, st)
    row = sb("kmrow", (1, T), F32, st)
    tmp = sb("kmtmp", (1, 8), F32, st)
    rl = sb("relrow", (1, 512), F32, st)
    p.dma("sp", rl[:], I["relflat"][:, :], R=[I["relflat"].r], W=[rl.r])
    p.act(rl[:], rl[:], AF.Abs, R=[rl.r], W=[rl.r])
    p.op("dve", lambda e: e.reduce_max(tmp[:, 0:1], rl[:], AX.X), R=[rl.r], W=[tmp.r])
    for which, srcs in enumerate([(Rz["ksT"], Rz["kwT"]), (Rz["ckvT"],)]):
        first = True
        for s_ in srcs:
            p.tt("dve", sq[:], s_[:], s_[:], ALU.mult, R=[s_.r], W=[sq.r])
            for kb in range(8):
                b = nbank()
                p.mm(b[0:1, :], Rz["ones_bf"][:, 0:1], sq[:, kb * 512:(kb + 1) * 512], True, True,
                     R=[sq.r, Rz["ones_bf"].r], W=[b.r])
                if first:
                    p.cp("dve", row[:, kb * 512:(kb + 1) * 512], b[0:1, :], R=[b.r], W=[row.r])
                else:
                    p.tt("dve", row[:, kb * 512:(kb + 1) * 512], row[:, kb * 512:(kb + 1) * 512], b[0:1, :], ALU.add,
                         R=[b.r, row.r], W=[row.r])
            first = False
        p.op("dve", lambda e: e.reduce_max(tmp[:, 1:2], row[:], AX.X), R=[row.r], W=[tmp.r])
        p.act(tmp[:, 2:3], tmp[:, 1:2], AF.Sqrt, R=[tmp.r], W=[tmp.r])
        p.ts("dve", Rz["kmax"][:, 2 * which:2 * which + 1], tmp[:, 2:3], -1.03, None, ALU.mult, R=[tmp.r], W=[Rz["kmax"].r])
        p.ts("dve", Rz["kmax"][:, 2 * which + 1:2 * which + 2], tmp[:, 0:1], -1.0, None, ALU.mult, R=[tmp.r], W=[Rz["kmax"].r])


def phase2(g):
    nc, p, sb, I, S, Rz = g.nc, g.p, g.sb, g.I, g.S, g.Rz
    st = ExitStack()
    B = g.banks
    ident, identf, ones = Rz["identb"], Rz["identf"], Rz["ones_bf"]
    S["oT_s"] = g.dscr("oT_s", (NT, 128, 8, 128), BF16)
    ntiles = (g.debug or {}).get("ntiles", NT)

    stg = sb("c_stg", (128, 1024), F32, st)
    relTa = sb("relTa", (128, 8, 2, 512), BF16, st)
    relTb = sb("relTb", (128, 8, 1024), BF16, st)
    relTw = sb("relTw", (128, 2, 512), BF16, st)
    eall = sb("eall", (128, 32, 128), BF16, st)
    onesN = sb("onesN", (128, 128), BF16, st)
    p.memset("pool", eall[:], 0.0, W=[eall.r])
    p.memset("pool", onesN[:], 0.0, W=[onesN.r])
    p.memset("pool", onesN[0:1, :], 1.0, W=[onesN.r])
    d30 = sb("d30", (128, 512), BF16, st)
    mcmp = sb("mcmp", (128, 8, 503), BF16, st)
    wext = sb("wext", (128, 128), F32, st)
    mcq = sb("mcq", (128, 128), F32, st)
    rowsA = [sb("rowsA%d" % i, (128, 512), BF16, st) for i in range(2)]
    rowsB = sb("rowsB", (128, 1024), BF16, st)
    for r_ in rowsA + [rowsB]:
        p.memset("pool", r_[:], 0.0, W=[r_.r])
    wuvP = sb("wuvP", (128, 8, 128), BF16, st)
    st0 = ExitStack()
    mc4 = sb("mc4", (128, 512), F32, st0)
    mw4 = sb("mw4", (128, 512), F32, st0)
    c31A = sb("c31A", (128, 2, 512), F32, st0)
    c31B = sb("c31B", (128, 1024), F32, st0)
    p.dma("sp", mc4[:], I["mc4"][:, :], R=[I["mc4"].r], W=[mc4.r])
    p.dma("sp", mw4[:], I["mw4"][:, :], R=[I["mw4"].r], W=[mw4.r])
    p.dma("sp", wext[:], I["wext"][:, :], R=[I["wext"].r], W=[wext.r])
    p.dma("sp", mcq[:], I["mcq"][:, :], R=[I["mcq"].r], W=[mcq.r])
    for gq in range(2):
        p.dma("sp", c31A[:, gq, :], I["c31a"][gq:gq + 1, :].partition_broadcast(128), R=[I["c31a"].r], W=[c31A.r])
    p.dma("sp", c31B[:], I["c31b"][0:1, :].partition_broadcast(128), R=[I["c31b"].r], W=[c31B.r])
    for d in range(8):
        for gq in range(2):
            p.dma("sp", stg[:, 0:512], I["relTa"][:, d, gq, :], R=[I["relTa"].r], W=[stg.r])
            p.tt("dve", stg[:, 0:512], stg[:, 0:512], c31A[:, gq, :], ALU.subtract, R=[stg.r, c31A.r], W=[stg.r])
            if d == 0:
                p.tt("dve", stg[:, 0:512], stg[:, 0:512], mc4[:], ALU.add, R=[stg.r, mc4.r], W=[stg.r])
            p.cp("dve", relTa[:, d, gq, :], stg[:, 0:512], R=[stg.r], W=[relTa.r])
            if d == 4:
                p.tt("dve", stg[:, 0:512], stg[:, 0:512], mw4[:], ALU.add, R=[stg.r, mw4.r], W=[stg.r])
                p.cp("dve", relTw[:, gq, :], stg[:, 0:512], R=[stg.r], W=[relTw.r])
        p.dma("sp", stg[:], I["relTb"][:, d, :], R=[I["relTb"].r], W=[stg.r])
        p.tt("dve", stg[:], stg[:], c31B[:], ALU.subtract, R=[stg.r, c31B.r], W=[stg.r])
        if d == 0:
            for hf in range(2):
                p.tt("dve", stg[:, hf * 512:(hf + 1) * 512], stg[:, hf * 512:(hf + 1) * 512], mc4[:], ALU.add,
                     R=[stg.r, mc4.r], W=[stg.r])
        p.cp("dve", relTb[:, d, :], stg[:], R=[stg.r], W=[relTb.r])
    for q4 in range(4):
        p.dma("sp", stg[0:64, :], I["eall"][:, q4 * 1024:(q4 + 1) * 1024], R=[I["eall"].r], W=[stg.r])
        p.cp("dve", eall[0:64, q4 * 8:(q4 + 1) * 8, :], stg[0:64, :].rearrange("p (a b) -> p a b", b=128), R=[stg.r], W=[eall.r])
    p.dma("sp", stg[:, 0:512], I["d30"][:, :], R=[I["d30"].r], W=[stg.r])
    p.cp("dve", d30[:], stg[:, 0:512], R=[stg.r], W=[d30.r])
    for h in range(8):
        p.dma("sp", stg[:, 0:503], I["cmpv"][:, h, :], R=[I["cmpv"].r], W=[stg.r])
        p.dma("sp", stg[:, 512:1015], I["cmpm"][:, :], R=[I["cmpm"].r], W=[stg.r])
        p.tt("dve", mcmp[:, h, :], stg[:, 0:503], stg[:, 512:1015], ALU.add, R=[stg.r], W=[mcmp.r])
    p.memset("dve", eall[64:65, :, :], 1.0, W=[eall.r])
    p.memset("pool", wuvP[:], 0.0, W=[wuvP.r])
    for h in range(8):
        p.dma("sp", stg[:, 0:64], I["w_uv"][h, :, :], R=[I["w_uv"].r], W=[stg.r])
        p.cp("dve", wuvP[:, h, (h % 2) * 64:(h % 2) * 64 + 64], stg[:, 0:64], R=[stg.r], W=[wuvP.r])

    p.flush()
    st0.close()
    qTz = [sb("qTz%d" % i, (128, 512), BF16, st) for i in range(2)]
    for q_ in qTz:
        p.memset("pool", q_[:], 0.0, W=[q_.r])
    qlT = sb("qlT", (128, 1024), BF16, st)
    qiT = sb("qiT", (64, 512), BF16, st)
    zidx = sb("zidx", (128, T), F32, st)
    selD = Buf(g.kcT.t, g.kcT.r)
    junk = Buf(g.vcT.t, g.vcT.r)
    rr = [sb("rr%d" % i, (128, 512), F32, st) for i in range(2)]
    sq = sb("sqq", (128, 1024), BF16, st)
    srow = Buf(stg.t[0:1, :], stg.r)
    scmp = sb("scmp", (128, 8, 256), F32, st)
    pn = sb("pn", (128, 8, 256), BF16, st)
    pnT = sb("pnT", (128, 16, 128), BF16, st)
    sm = sb("sm", (128, 16), F32, st)
    sm2 = sb("sm2", (128, 16), F32, st)
    imp = sb("imp", (128, 64), F32, st)
    sc1 = sb("sc1", (128, 64), F32, st)
    sc2 = sb("sc2", (128, 64), F32, st)
    m8 = sb("m8", (128, 16), F32, st)
    selb = sb("selb", (128, 64), BF16, st)
    selT4 = [sb("selT4%d" % i, (128, 512), BF16, st) for i in range(2)]
    for s_ in selT4:
        p.memset("pool", s_[:], 0.0, W=[s_.r])
    PT = [sb("PT%d" % i, (128, 512), BF16, st) for i in range(2)]
    ocmp = sb("ocmp", (128, 512), F32, st)
    oa32 = sb("oa32", (128, 512), F32, st)
    oab = sb("oab", (128, 512), BF16, st)
    coef = sb("coef", (128, 32), F32, st)
    oln = sb("oln", (128, 8, 128), BF16, st)
    olT = sb("olT", (128, 8, 128), BF16, st)
    oT = sb("oT", (128, 8, 128), BF16, st)
    bis = sb("bis", (128, 8), F32, st)
    p.memset("pool", pn[:], 0.0, W=[pn.r])
    p.memset("pool", scmp[:], 0.0, W=[scmp.r])

    def bfv(bank):
        return bank.t[:].bitcast(BF16)

    selDs = [selD, junk]
    pw = sb("pw", (128, BIS_ITERS + 1), F32, st)
    wct = sb("wct", (128, BIS_ITERS + 1), F32, st)
    for k in range(BIS_ITERS + 1):
        p.memset("pool", pw[:, k:k + 1], 2.0 ** -(k + 1), W=[pw.r])

    def stream_D(i):
        nk = 128 * (i + 1)
        sD = selDs[i % 2]
        p.dma("sp", qiT[:], S["qi_s"][i, :, :, :].rearrange("d h q -> d (h q)"), R=[S["qi_s"].r], W=[qiT.r])
        for kb in range((nk + 511) // 512):
            k0 = kb * 512
            kn = min(512, nk - k0)
            for hi in range(4):
                bk = B[hi % 2]
                r_ = rr[hi % 2]
                p.mm(bk[:, 0:kn], qiT[:, hi * 128:(hi + 1) * 128], Rz["kiT"][:, k0:k0 + kn], True, True,
                     R=[qiT.r, Rz["kiT"].r], W=[bk.r])
                p.act(r_[:, 0:kn], bk[:, 0:kn], AF.Relu, scale=Rz["wabs"][:, i, hi:hi + 1], R=[bk.r, Rz["wabs"].r], W=[r_.r])
                if hi == 0:
                    p.ts("dve", zidx[:, k0:k0 + kn], r_[:, 0:kn], Rz["wsgn"][:, i, 0:1], None, ALU.mult,
                         R=[r_.r, Rz["wsgn"].r], W=[zidx.r])
                else:
                    p.stt(zidx[:, k0:k0 + kn], r_[:, 0:kn], Rz["wsgn"][:, i, hi:hi + 1], zidx[:, k0:k0 + kn], ALU.mult, ALU.add,
                          R=[r_.r, Rz["wsgn"].r, zidx.r], W=[zidx.r])
            yield
        p.op("dve", lambda e: e.tensor_reduce(bis[:, 0:1], zidx[:, 0:nk], AX.X, ALU.min), R=[zidx.r], W=[bis.r])
        p.op("dve", lambda e: e.reduce_max(bis[:, 1:2], zidx[:, 0:nk], AX.X), R=[zidx.r], W=[bis.r])
        p.stt(bis[:, 2:3], bis[:, 1:2], 1.0, bis[:, 0:1], ALU.add, ALU.subtract, R=[bis.r], W=[bis.r])
        p.ts("dve", wct[:], pw[:], bis[:, 2:3], None, ALU.mult, R=[pw.r, bis.r], W=[wct.r])
        p.tt("dve", bis[:, 3:4], bis[:, 0:1], wct[:, 0:1], ALU.add, R=[bis.r, wct.r], W=[bis.r])
        p.tt("dve", zidx[:, nk - 128:nk], zidx[:, nk - 128:nk], mcq[:], ALU.add, R=[zidx.r, mcq.r], W=[zidx.r])
        yield
        for k in range(BIS_ITERS):
            p.ts("dve", sD[:, 0:nk], zidx[:, 0:nk], bis[:, 3:4], None, ALU.is_ge, ALU.add, R=[zidx.r, bis.r], W=[sD.r, bis.r],
                 accum=bis[:, 4:5])
            p.stt(bis[:, 5:6], bis[:, 4:5], 256.0, wct[:, k:k + 1], ALU.is_ge, ALU.mult, R=[bis.r, wct.r], W=[bis.r])
            p.ts("dve", bis[:, 3:4], bis[:, 3:4], wct[:, k + 1:k + 2], bis[:, 5:6], ALU.subtract, ALU.add, R=[bis.r, wct.r], W=[bis.r])
            yield
        p.tt("dve", bis[:, 6:7], bis[:, 3:4], wct[:, BIS_ITERS:BIS_ITERS + 1], ALU.subtract, R=[bis.r, wct.r], W=[bis.r])
        p.ts("dve", sD[:, 0:nk], zidx[:, 0:nk], bis[:, 6:7], 1.0, ALU.is_ge, ALU.subtract, R=[zidx.r, bis.r], W=[sD.r])
        yield

    def stream_P(i):
        sD = selDs[i % 2]
        for gq in range(2):
            p.dma("sp", qTz[gq][gq * 64:(gq + 1) * 64, :], S["qa_s"][i, gq, :, :, :].rearrange("d h q -> d (h q)"),
                  R=[S["qa_s"].r], W=[qTz[gq].r])
        p.dma("sp", qlT[:], S["ql_s"][i, :, :, :].rearrange("r h q -> r (h q)"), R=[S["ql_s"].r], W=[qlT.r])
        for gq in range(2):
            p.act(sq[:, 0:512], qTz[gq][:], AF.Square, R=[qTz[gq].r], W=[sq.r])
            p.mm(B[7][0:1, :], ones[:, 0:1], sq[:, 0:512], True, True, R=[ones.r, sq.r], W=[B[7].r])
            p.act(srow[:, 0:512], B[7][0:1, :], AF.Sqrt, R=[B[7].r], W=[srow.r])
            p.ts("dve", rowsA[gq][0:1, :], srow[:, 0:512], Rz["kmax"][0:1, 0:1], Rz["kmax"][0:1, 1:2], ALU.mult, ALU.add,
                 R=[srow.r, Rz["kmax"].r], W=[rowsA[gq].r])
            p.dma("sp", selT4[gq][64:65, :], rowsA[gq][0:1, :], R=[rowsA[gq].r], W=[selT4[gq].r])
        p.act(sq[:], qlT[:], AF.Square, R=[qlT.r], W=[sq.r])
        for hf in range(2):
            p.mm(B[7][0:1, :], ones[:, 0:1], sq[:, hf * 512:(hf + 1) * 512], True, True, R=[ones.r, sq.r], W=[B[7].r])
            p.act(srow[:, hf * 512:(hf + 1) * 512], B[7][0:1, :], AF.Sqrt, R=[B[7].r], W=[srow.r])
        p.ts("dve", rowsB[0:1, :], srow[:], Rz["kmax"][0:1, 2:3], Rz["kmax"][0:1, 3:4], ALU.mult, ALU.add,
             R=[srow.r, Rz["kmax"].r], W=[rowsB.r])
        yield
        off = 248 - 8 * i
        for h in range(8):
            gq, hh = h // 4, h % 4
            rows = slice(gq * 64, (gq + 1) * 64)
            bL = B[2 + h // 2]
            p.mm(bL[:, (h % 2) * 256:(h % 2) * 256 + 255], qTz[gq][rows, hh * 128:(hh + 1) * 128], Rz["kcmpT"][rows, 0:255],
                 h % 2 == 0, h % 2 == 1, R=[qTz[gq].r, Rz["kcmpT"].r], W=[bL.r])
        for b4 in range(4):
            bL = B[2 + b4]
            p.tt("dve", scmp[:, 2 * b4:2 * b4 + 2, 0:255], bL[:, :].rearrange("p (h c) -> p h c", c=256)[:, :, 0:255],
                 mcmp[:, 2 * b4:2 * b4 + 2, off:off + 255], ALU.add, R=[bL.r, mcmp.r], W=[scmp.r])
        yield
        p.op("dve", lambda e: e.reduce_max(sm[:, 0:8], scmp[:, :, 0:255], AX.X), R=[scmp.r], W=[sm.r])
        p.ts("dve", sm[:, 8:16], sm[:, 0:8], -1000.0, -1.0, ALU.max, ALU.mult, R=[sm.r], W=[sm.r])
        p.tt("dve", scmp[:, :, 0:255], scmp[:, :, 0:255], sm[:, 8:16].unsqueeze(2).to_broadcast([128, 8, 255]), ALU.add,
             R=[scmp.r, sm.r], W=[scmp.r])
        p.act(scmp[:, :, 0:255], scmp[:, :, 0:255], AF.Exp, R=[scmp.r], W=[scmp.r])
        yield
        p.op("dve", lambda e: e.reduce_sum(sm2[:, 0:8], scmp[:, :, 0:255], AX.X), R=[scmp.r], W=[sm2.r])
        p.ts("dve", sm2[:, 0:8], sm2[:, 0:8], 1e-30, None, ALU.max, R=[sm2.r], W=[sm2.r])
        p.op("dve", lambda e: e.reciprocal(sm2[:, 8:16], sm2[:, 0:8]), R=[sm2.r], W=[sm2.r])
        p.tt("dve", pn[:, :, 0:255], scmp[:, :, 0:255], sm2[:, 8:16].unsqueeze(2).to_broadcast([128, 8, 255]), ALU.mult,
             R=[scmp.r, sm2.r], W=[pn.r])
        yield
        for hf in range(2):
            bk = B[2 + hf]
            tb_ = bfv(bk)
            for j in range(8):
                h, cc = hf * 4 + j // 2, j % 2
                p.tr(tb_[:, j * 128:(j + 1) * 128], pn[:, h, cc * 128:(cc + 1) * 128], ident[:], R=[pn.r, ident.r], W=[bk.r])
            p.cp("act" if hf else "dve", pnT[:, hf * 8:(hf + 1) * 8, :], tb_[:, :].rearrange("p (c q) -> p c q", q=128), R=[bk.r], W=[pnT.r])
        yield
        for gq in range(2):
            accb = B[6 + gq]
            for hh in range(4):
                h = gq * 4 + hh
                for cc in range(2):
                    p.mm(accb[:, hh * 128:(hh + 1) * 128], pnT[:, 2 * h + cc, :], Rz["vcmpM"][:, cc, gq, :], hh == 0 and cc == 0, hh == 3 and cc == 1,
                         R=[pnT.r, Rz["vcmpM"].r], W=[accb.r])
            p.cp("act", ocmp[:, gq * 256:(gq + 1) * 256].rearrange("p (h d) -> p h d", d=64),
                 accb[:, :].rearrange("p (h j) -> p h j", j=128)[:, :, 0:64], R=[accb.r], W=[ocmp.r])
            p.op("dve", lambda e, accb=accb: e.reduce_sum(imp[:], accb[:, :].rearrange("p (h j) -> p j h", j=128)[:, 64:128, :], AX.X),
                 R=[accb.r], W=[imp.r])
            p.tt("dve", sc1[:], imp[:], wext[:, 63 - 2 * i:127 - 2 * i], ALU.add, R=[imp.r, wext.r], W=[sc1.r])
            p.ts("dve", sc1[:, 0:1], imp[:, 0:1], 1e4, None, ALU.add, R=[imp.r], W=[sc1.r])
            p.op("dve", lambda e: e.max(m8[:, 0:8], sc1[:]), R=[sc1.r], W=[m8.r])
            p.op("dve", lambda e: e.match_replace(sc2[:], m8[:, 0:8], sc1[:], -1e30), R=[sc1.r, m8.r], W=[sc2.r])
            p.op("dve", lambda e: e.max(m8[:, 8:16], sc2[:]), R=[sc2.r], W=[m8.r])
            p.ts("dve", m8[:, 15:16], m8[:, 15:16], -1e29, None, ALU.max, R=[m8.r], W=[m8.r])
            p.ts("dve", selb[:], sc1[:], m8[:, 15:16], 1.0, ALU.is_ge, ALU.subtract, R=[sc1.r, m8.r], W=[selb.r])
            tb2 = bfv(accb)
            p.tr(tb2[0:64, 0:128], selb[:], ident[:], R=[selb.r, ident.r], W=[accb.r])
            p.cp("dve", selT4[gq][0:64, :].rearrange("p (h q) -> p h q", q=128),
                 tb2[0:64, 0:128].unsqueeze(1).to_broadcast([64, 4, 128]), R=[accb.r], W=[selT4[gq].r])
            yield
        nS = 0
        for gq in range(2):
            for br_ in range(2):
                kts = list(range(0, i + 1)) if br_ == 0 else list(range(max(0, i - 4), i + 1))
                accb = B[4 + br_]
                kT, vA = (Rz["ksT"], Rz["vsA"]) if br_ == 0 else (Rz["kwT"], Rz["vwA"])
                units = []
                for n, kt in enumerate(kts):
                    units.append((n, kt, B[2 + nS % 2], PT[nS % 2]))
                    nS += 1

                def emit_S(u, gq=gq, br_=br_, kT=kT):
                    n, kt, bS, pt = u
                    d = i - kt
                    p.mm(bS[:, :], kT[:, kt * 128:(kt + 1) * 128], qTz[gq][:], True, False, R=[kT.r, qTz[gq].r], W=[bS.r])
                    if br_ == 0:
                        p.mm(bS[:, :], eall[:, kt, :], selT4[gq][:], False, d >= 8, R=[eall.r, selT4[gq].r], W=[bS.r])
                    if d < 8:
                        rel = relTw[:, gq, :] if (br_ == 1 and d == 4) else relTa[:, d, gq, :]
                        p.mm(bS[:, :], ident[:], rel, False, br_ == 0, R=[ident.r, relTa.r, relTw.r], W=[bS.r])
                    if br_ == 1:
                        p.mm(bS[:, :], onesN[:], rowsA[gq][:], False, True, R=[onesN.r, rowsA[gq].r], W=[bS.r])

                emit_S(units[0])
                for ui, u in enumerate(units):
                    n, kt, bS, pt = u
                    p.act(pt[:], bS[:, :], AF.Exp, R=[bS.r], W=[pt.r])
                    if ui + 1 < len(units):
                        emit_S(units[ui + 1])
                    for hh in range(4):
                        p.mm(accb[:, hh * 65:(hh + 1) * 65], pt[:, hh * 128:(hh + 1) * 128], vA[:, kt, gq, :],
                             n == 0 and hh == 0, n == len(kts) - 1, R=[pt.r, vA.r], W=[accb.r])
                    yield
                accv = accb[:, 0:260].rearrange("p (h e) -> p h e", e=65)
                p.ts("dve", coef[:, 0:4], accv[:, :, 64], 1e-30, None, ALU.max, R=[accb.r], W=[coef.r])
                p.op("dve", lambda e: e.reciprocal(coef[:, 4:8], coef[:, 0:4]), R=[coef.r], W=[coef.r])
                gv = Rz["gn"][:, i, gq * 12:(gq + 1) * 12].rearrange("p (h t) -> p h t", t=3)
                p.tt("dve", coef[:, 8:12], coef[:, 4:8], gv[:, :, 1 + br_], ALU.mult, R=[coef.r, Rz["gn"].r], W=[coef.r])
                for hh in range(4):
                    h = gq * 4 + hh
                    if br_ == 0:
                        p.ts("dve", oa32[:, h * 64:(h + 1) * 64], ocmp[:, h * 64:(h + 1) * 64], Rz["gn"][:, i, 3 * h:3 * h + 1], None, ALU.mult,
                             R=[ocmp.r, Rz["gn"].r], W=[oa32.r])
                        p.stt(oa32[:, h * 64:(h + 1) * 64], accv[:, hh, 0:64], coef[:, 8 + hh:9 + hh], oa32[:, h * 64:(h + 1) * 64], ALU.mult, ALU.add,
                              R=[accb.r, coef.r, oa32.r], W=[oa32.r])
                    else:
                        p.stt(oab[:, h * 64:(h + 1) * 64], accv[:, hh, 0:64], coef[:, 8 + hh:9 + hh], oa32[:, h * 64:(h + 1) * 64], ALU.mult, ALU.add,
                              R=[accb.r, coef.r, oa32.r], W=[oab.r])
                yield
        tb_ = bfv(B[7])
        for c in range(4):
            p.tr(tb_[:, c * 128:(c + 1) * 128], oab[:, c * 128:(c + 1) * 128], ident[:], R=[oab.r, ident.r], W=[B[7].r])
        p.cp("act", oT[:, 0:4, :], tb_[:, 0:512].rearrange("p (c q) -> p c q", q=128), R=[B[7].r], W=[oT.r])
        yield
        accD = [B[4], B[5], B[6]]
        hb = [(0, 0), (0, 1), (0, 2), (1, 0), (1, 1), (1, 2), (2, 0), (2, 1)]
        dunits = [(kt, hf) for kt in range(i + 1) for hf in range(2)]

        def emit_SD(u):
            kt, hf = u
            d = i - kt
            bS = B[2 + hf]
            cols = slice(hf * 512, (hf + 1) * 512)
            p.mm(bS[:, :], Rz["ckvT"][:, kt * 128:(kt + 1) * 128], qlT[:, cols], True, False, R=[Rz["ckvT"].r, qlT.r], W=[bS.r])
            p.mm(bS[:, :], sD[:, kt * 128:(kt + 1) * 128], d30[:], False, False, R=[sD.r, d30.r], W=[bS.r])
            if d < 8:
                p.mm(bS[:, :], ident[:], relTb[:, d, cols], False, False, R=[ident.r, relTb.r], W=[bS.r])
            p.mm(bS[:, :], onesN[:], rowsB[:, cols], False, True, R=[onesN.r, rowsB.r], W=[bS.r])

        emit_SD(dunits[0])
        for ui, (kt, hf) in enumerate(dunits):
            bS = B[2 + hf]
            pt = PT[hf]
            p.act(pt[:], bS[:, :], AF.Exp, R=[bS.r], W=[pt.r])
            if ui + 1 < len(dunits):
                emit_SD(dunits[ui + 1])
            for hh in range(4):
                h = hf * 4 + hh
                bk, sl = hb[h]
                p.mm(accD[bk][:, sl * 129:(sl + 1) * 129], pt[:, hh * 128:(hh + 1) * 128], Rz["ckvA"][:, kt, :],
                     kt == 0 and sl == 0, kt == i, R=[pt.r, Rz["ckvA"].r], W=[accD[bk].r])
            if hf == 1:
                yield
        for h in range(8):
            bk, sl = hb[h]
            p.ts("dve", coef[:, 16 + h:17 + h], accD[bk][:, sl * 129 + 128:sl * 129 + 129], 1e-30, None, ALU.max, R=[accD[bk].r], W=[coef.r])
        p.op("dve", lambda e: e.reciprocal(coef[:, 24:32], coef[:, 16:24]), R=[coef.r], W=[coef.r])
        for h in range(8):
            bk, sl = hb[h]
            p.ts("dve", oln[:, h, :], accD[bk][:, sl * 129:sl * 129 + 128], coef[:, 24 + h:25 + h], None, ALU.mult,
                 R=[accD[bk].r, coef.r], W=[oln.r])
        yield
        for hf in range(2):
            tb_ = bfv(B[7])
            for hh in range(4):
                p.tr(tb_[:, hh * 128:(hh + 1) * 128], oln[:, hf * 4 + hh, :], ident[:], R=[oln.r, ident.r], W=[B[7].r])
            p.cp("act", olT[:, hf * 4:(hf + 1) * 4, :], tb_[:, 0:512].rearrange("p (c q) -> p c q", q=128), R=[B[7].r], W=[olT.r])
        for c in range(4):
            for hl in range(2):
                p.mm(B[7][:, c * 128:(c + 1) * 128], wuvP[:, 2 * c + hl, :], olT[:, 2 * c + hl, :], c == 0 and hl == 0, c == 3 and hl == 1,
                     R=[wuvP.r, olT.r], W=[B[7].r])
        p.cp("act", oT[:, 4:8, :], B[7][:, :].rearrange("p (c q) -> p c q", q=128), R=[B[7].r], W=[oT.r])
        p.dma("pool", S["oT_s"][i, :, :, :], oT[:], R=[oT.r], W=[S["oT_s"].r])
        yield

    def len_P(i):
        return 1 + 6 + 2 * ((i + 1) + min(5, i + 1)) + 4 + 1 + (i + 1) + 2

    def len_D(i):
        return (128 * (i + 1) + 511) // 512 + 1 + BIS_ITERS + 1

    for _ in stream_D(0):
        pass
    for i in range(ntiles):
        gd = stream_D(i + 1) if i + 1 < ntiles else None
        ratio = (len_D(i + 1) / float(len_P(i))) if gd is not None else 0.0
        credit = 0.0
        for _ in stream_P(i):
            credit += ratio
            while gd is not None and credit >= 1.0:
                credit -= 1.0
                try:
                    next(gd)
                except StopIteration:
                    gd = None
        if gd is not None:
            for _ in gd:
                pass
    return st


def layer_norm_tile(p, r, rs, gbc, bbc, out, tmp, R_extra=()):
    p.tt("dve", rs[:, 2:3], rs[:, 0:1], rs[:, 1:2], ALU.add, R=[rs.r], W=[rs.r])
    p.ts("dve", rs[:, 3:4], rs[:, 2:3], -1.0 / D, None, ALU.mult, R=[rs.r], W=[rs.r])
    p.act(tmp[:], r[:], AF.Square, bias=rs[:, 3:4], accum=rs[:, 4:5], R=[r.r, rs.r], W=[tmp.r, rs.r])
    p.ts("dve", rs[:, 5:6], rs[:, 4:5], 1.0 / D, 1e-5, ALU.mult, ALU.add, R=[rs.r], W=[rs.r])
    p.act(rs[:, 6:7], rs[:, 5:6], AF.Sqrt, R=[rs.r], W=[rs.r])
    p.op("dve", lambda e: e.reciprocal(rs[:, 7:8], rs[:, 6:7]), R=[rs.r], W=[rs.r])
    p.ts("dve", tmp[:], r[:], rs[:, 3:4], rs[:, 7:8], ALU.add, ALU.mult, R=[r.r, rs.r], W=[tmp.r])
    p.tt("dve", tmp[:], tmp[:], gbc[:], ALU.mult, R=[tmp.r, gbc.r], W=[tmp.r])
    p.tt("dve", out[:], tmp[:], bbc[:], ALU.add, R=[tmp.r, bbc.r], W=[out.r])


def phase3(g):
    nc, p, sb, I, S, Rz = g.nc, g.p, g.sb, g.I, g.S, g.Rz
    st = ExitStack()
    B = g.banks
    stg = sb("w_stg", (128, 1024), F32, st)
    wba = sb("wba", (128, 4, 1024), BF16, st)
    wbb = sb("wbb", (128, 4, 1024), BF16, st)
    wout = sb("wout", (128, 8, 1024), BF16, st)
    for (dst, src, n) in [(wba, "w_ba", 4), (wbb, "w_bb", 4), (wout, "w_out", 8)]:
        for c in range(n):
            p.dma("sp", stg[:], I[src][c * 128:(c + 1) * 128, :], R=[I[src].r], W=[stg.r])
            p.cp("dve", dst[:, c, :], stg[:], R=[stg.r], W=[dst.r])
    bc = {}
    for nm in ("ln1_g", "ln1_b", "ln2_g", "ln2_b"):
        bc[nm] = sb("bc_" + nm, (128, 1024), F32, st)
        p.dma("sp", bc[nm][:], I[nm][0:1, :].partition_broadcast(128), R=[I[nm].r], W=[bc[nm].r])
    oT_l = [sb("oT2_%d" % j, (128, 8, 128), BF16, st) for j in range(2)]
    gT_l = [sb("gT_%d" % j, (128, 16, 128), F32, st) for j in range(2)]
    xt_l = [sb("xt_%d" % j, (128, 1024), F32, st) for j in range(2)]
    t1 = sb("t1", (128, 512), F32, st)
    t2 = sb("t2", (128, 512), F32, st)
    mT = sb("mT", (128, 8, 128), BF16, st)
    r = sb("r", (128, 1024), F32, st)
    h1 = sb("h1", (128, 1024), F32, st)
    tmp = sb("lntmp", (128, 1024), F32, st)
    rs = sb("rs", (128, 8), F32, st)
    h1T = sb("h1T", (128, 8, 128), F32, st)
    wr = sb("wr", (128, 8, 36), F32, st)
    brb = sb("brb", (128, 36), F32, st)
    lg = sb("lg", (128, 36), F32, st)
    rt = sb("rt", (128, 16), F32, st)
    rj = sb("rj", (128, 32), F32, st)
    og = sb("og", (128, 4), F32, st)
    esel = sb("esel", (128, 8), F32, st)
    m8r = sb("m8r", (128, 8), F32, st)
    oh = sb("oh", (128, 2, 8), F32, st)
    Ak = sb("Ak", (128, 2, 32), F32, st)
    Abf = sb("Abf", (128, 32), BF16, st)
    ustr = sb("ustr", (128, 128), BF16, st)
    posf = sb("posf", (128, 32), F32, st)
    base = sb("base", (128, 32), F32, st)
    iot = sb("iot", (128, 32), F32, st)
    tokid = sb("tokid", (128, 32), I32, st)
    tokrow = sb("tokrow", (128, 32, 16), I32, st)
    fill = sb("fill", (128, 2048), I32, st)
    p.dma("sp", wr[:], I["wr"][:, :].rearrange("(c p) n -> p c n", p=128), R=[I["wr"].r], W=[wr.r])
    p.dma("sp", brb[:], I["br"][0:1, :].partition_broadcast(128), R=[I["br"].r], W=[brb.r])
    p.dma("sp", stg[:, 0:128], I["ustrict"][:, :], R=[I["ustrict"].r], W=[stg.r])
    p.cp("dve", ustr[:], stg[:, 0:128], R=[stg.r], W=[ustr.r])
    p.dma("sp", iot[:], I["iota32"][:, :], R=[I["iota32"].r], W=[iot.r])
    p.dma("sp", tokid[:], I["tokid"][:, :], R=[I["tokid"].r], W=[tokid.r])
    p.cp("dve", tokrow[:], tokid[:].unsqueeze(2).to_broadcast([128, 32, 16]), R=[tokid.r], W=[tokrow.r])
    p.memset("dve", base[:], 0.0, W=[base.r])
    p.memset("pool", fill[:], 5000, W=[fill.r])
    p.dma("sp", S["slot"][:, :].rearrange("(p r) c -> p (r c)", p=128), fill[:], R=[fill.r], W=[S["slot"].r])
    nt3 = (g.debug or {}).get("ntiles", NT)

    def loads3(i):
        j = i % 2
        p.dma("sp", oT_l[j][:], S["oT_s"][i, :, :, :], R=[S["oT_s"].r], W=[oT_l[j].r])
        p.dma("sp", gT_l[j][:], S["g_s"][i, :, :, :], R=[S["g_s"].r], W=[gT_l[j].r])
        p.dma("sp", xt_l[j][:], I["x"][i * 128:(i + 1) * 128, :], R=[I["x"].r], W=[xt_l[j].r])

    loads3(0)
    for i in range(nt3):
        oT, gT, xt = oT_l[i % 2], gT_l[i % 2], xt_l[i % 2]
        if i + 1 < nt3:
            loads3(i + 1)
        for hf in range(2):
            for m4 in range(4):
                m = hf * 4 + m4
                for kc in range(4):
                    p.mm(B[0][:, m4 * 128:(m4 + 1) * 128], wba[:, kc, m * 128:(m + 1) * 128], oT[:, kc, :], m4 == 0 and kc == 0, kc == 3,
                         R=[wba.r, oT.r], W=[B[0].r])
                for kc in range(4):
                    p.mm(B[1][:, m4 * 128:(m4 + 1) * 128], wbb[:, kc, m * 128:(m + 1) * 128], oT[:, 4 + kc, :], m4 == 0 and kc == 0, kc == 3,
                         R=[wbb.r, oT.r], W=[B[1].r])
            p.tt("dve", t1[:], B[0][:, :], gT[:, hf * 4:(hf + 1) * 4, :].rearrange("p m q -> p (m q)"), ALU.mult, R=[B[0].r, gT.r], W=[t1.r])
            p.tt("dve", t2[:], B[1][:, :], gT[:, 8 + hf * 4:8 + (hf + 1) * 4, :].rearrange("p m q -> p (m q)"), ALU.mult, R=[B[1].r, gT.r], W=[t2.r])
            p.tt("dve", mT[:, hf * 4:(hf + 1) * 4, :].rearrange("p m q -> p (m q)"), t1[:], t2[:], ALU.add, R=[t1.r, t2.r], W=[mT.r])
        for hf in range(2):
            bk = B[2 + hf]
            for c in range(8):
                p.mm(bk[:, :], mT[:, c, :], wout[:, c, hf * 512:(hf + 1) * 512], c == 0, c == 7, R=[mT.r, wout.r], W=[bk.r])
            p.stt(r[:, hf * 512:(hf + 1) * 512], xt[:, hf * 512:(hf + 1) * 512], ALPHA, bk[:, :], ALU.mult, ALU.add,
                  R=[xt.r, bk.r], W=[r.r, rs.r], accum=rs[:, hf:hf + 1])
        layer_norm_tile(p, r, rs, bc["ln1_g"], bc["ln1_b"], h1, tmp)
        p.dma("pool", S["h1_s"][i * 128:(i + 1) * 128, :], h1[:], R=[h1.r], W=[S["h1_s"].r])
        for c in range(8):
            bk = B[4 + c // 4]
            p.tr(bk[:, (c % 4) * 128:(c % 4 + 1) * 128], h1[:, c * 128:(c + 1) * 128], Rz["identf"][:], R=[h1.r, Rz["identf"].r], W=[bk.r])
        for hf in range(2):
            p.cp("act", h1T[:, hf * 4:(hf + 1) * 4, :], B[4 + hf][:, :].rearrange("p (c q) -> p c q", q=128), R=[B[4 + hf].r], W=[h1T.r])
        for c in range(8):
            p.mm(B[6][:, 0:36], h1T[:, c, :], wr[:, c, :], c == 0, c == 7, R=[h1T.r, wr.r], W=[B[6].r])
        p.tt("dve", lg[:], B[6][:, 0:36], brb[:], ALU.add, R=[B[6].r, brb.r], W=[lg.r])
        p.op("dve", lambda e: e.reduce_max(rt[:, 0:1], lg[:, 0:4], AX.X), R=[lg.r], W=[rt.r])
        p.ts("dve", og[:], lg[:, 0:4], rt[:, 0:1], None, ALU.is_equal, R=[lg.r, rt.r], W=[og.r])
        p.ts("dve", rt[:, 1:2], rt[:, 0:1], -1.0, None, ALU.mult, R=[rt.r], W=[rt.r])
        p.act(rj[:, 0:4], lg[:, 0:4], AF.Exp, bias=rt[:, 1:2], accum=rt[:, 2:3], R=[lg.r, rt.r], W=[rj.r, rt.r])
        p.op("dve", lambda e: e.reciprocal(rt[:, 3:4], rt[:, 2:3]), R=[rt.r], W=[rt.r])
        p.ts("dve", esel[:], lg[:, 4:12], og[:, 0:1], None, ALU.mult, R=[lg.r, og.r], W=[esel.r])
        for gi in range(1, 4):
            p.stt(esel[:], lg[:, 4 + 8 * gi:12 + 8 * gi], og[:, gi:gi + 1], esel[:], ALU.mult, ALU.add, R=[lg.r, og.r, esel.r], W=[esel.r])
        p.op("dve", lambda e: e.max(m8r[:], esel[:]), R=[esel.r], W=[m8r.r])
        p.tt("dve", rt[:, 4:5], m8r[:, 1:2], m8r[:, 0:1], ALU.subtract, R=[m8r.r], W=[rt.r])
        p.act(rt[:, 5:6], rt[:, 4:5], AF.Exp, R=[rt.r], W=[rt.r])
        p.ts("dve", rt[:, 6:7], rt[:, 5:6], 1.0, None, ALU.add, R=[rt.r], W=[rt.r])
        p.op("dve", lambda e: e.reciprocal(rt[:, 7:8], rt[:, 6:7]), R=[rt.r], W=[rt.r])
        p.tt("dve", rt[:, 8:9], rt[:, 7:8], rt[:, 5:6], ALU.mult, R=[rt.r], W=[rt.r])
        p.tt("dve", g.wts[:, i, 0:1], rt[:, 7:8], rt[:, 3:4], ALU.mult, R=[rt.r], W=[g.wts.r])
        p.tt("dve", g.wts[:, i, 1:2], rt[:, 8:9], rt[:, 3:4], ALU.mult, R=[rt.r], W=[g.wts.r])
        for k in range(2):
            p.ts("dve", oh[:, k, :], esel[:], m8r[:, k:k + 1], None, ALU.is_equal, R=[esel.r, m8r.r], W=[oh.r])
            p.tt("dve", Ak[:, k, :].rearrange("p (a b) -> p a b", b=8), og[:].unsqueeze(2).to_broadcast([128, 4, 8]),
                 oh[:, k, :].unsqueeze(1).to_broadcast([128, 4, 8]), ALU.mult, R=[og.r, oh.r], W=[Ak.r])
        p.tt("dve", Abf[:], Ak[:, 0, :], Ak[:, 1, :], ALU.add, R=[Ak.r], W=[Abf.r])
        p.mm(B[7][:, 0:32], ustr[:], Abf[:], True, False, R=[ustr.r, Abf.r], W=[B[7].r])
        p.mm(B[7][:, 64:96], Rz["ones_bf"][:], Abf[:], False, True, R=[Rz["ones_bf"].r, Abf.r], W=[B[7].r])
        p.tt("dve", posf[:], B[7][:, 0:32], base[:], ALU.add, R=[B[7].r, base.r], W=[posf.r])
        p.tt("dve", base[:], B[7][:, 64:96], base[:], ALU.add, R=[B[7].r, base.r], W=[base.r])
        for k in range(2):
            p.stt(rj[:, 0:32], posf[:], 1.0, Ak[:, k, :], ALU.mult, ALU.mult, R=[posf.r, Ak.r], W=[rj.r, rt.r], accum=rt[:, 10 + k:11 + k])
            p.stt(rj[:, 0:32], iot[:], 1.0, Ak[:, k, :], ALU.mult, ALU.mult, R=[iot.r, Ak.r], W=[rj.r, rt.r], accum=rt[:, 12 + k:13 + k])
            p.ts("dve", rt[:, 14:15], rt[:, 10 + k:11 + k], float(CAP), 1e6, ALU.is_ge, ALU.mult, R=[rt.r], W=[rt.r])
            p.ts("dve", rt[:, 9:10], rt[:, 10 + k:11 + k], float(CAP), None, ALU.is_lt, R=[rt.r], W=[rt.r])
            p.tt("dve", g.wts[:, i, k:k + 1], g.wts[:, i, k:k + 1], rt[:, 9:10], ALU.mult, R=[rt.r, g.wts.r], W=[g.wts.r])
            p.stt(rt[:, 15:16], rt[:, 12 + k:13 + k], float(CAP), rt[:, 10 + k:11 + k], ALU.mult, ALU.add, R=[rt.r], W=[rt.r])
            p.tt("dve", rt[:, 15:16], rt[:, 15:16], rt[:, 14:15], ALU.add, R=[rt.r], W=[rt.r])
            p.cp("dve", g.dest[:, i, k:k + 1], rt[:, 15:16], R=[rt.r], W=[g.dest.r])
            p.dma("pool", None, None, R=[g.dest.r, tokrow.r], W=[S["slot"].r],
                  fn=lambda e, i=i, k=k: e.indirect_dma_start(
                      out=S["slot"][:, :], out_offset=bass.IndirectOffsetOnAxis(ap=g.dest[:, i, k:k + 1], axis=0),
                      in_=tokrow[:, i, :], in_offset=None, bounds_check=p.breg(e, 32 * CAP - 1), oob_is_err=False))
    return st


def phase4(g):
    nc, p, sb, I, S, Rz = g.nc, g.p, g.sb, g.I, g.S, g.Rz
    st = ExitStack()
    B = g.banks
    ident = Rz["identb"]
    nexp = (g.debug or {}).get("nexp", 32)
    sid = sb("sid", (128, 4, 16), I32, st)
    xg = [sb("xg%d" % i, (128, 1024), F32, st) for i in range(4)]
    xgb = [sb("xgb%d" % i, (128, 1024), BF16, st) for i in range(2)]
    xgT = sb("xgT", (128, 8, 512), BF16, st)
    hT = sb("hT", (128, 2, 512), BF16, st)
    sg = [sb("sg%d" % i, (128, 512), F32, st) for i in range(2)]
    yb = [sb("yb%d" % i, (128, 1024), F32, st) for i in range(2)]
    wgf = sb("wgf", (128, 8, 256), F32, st)
    wuf = sb("wuf", (128, 8, 256), F32, st)
    wdf = sb("wdf", (128, 2, 1024), F32, st)
    wg = [sb("wg%d" % i, (128, 8, 256), BF16, st) for i in range(2)]
    wu = [sb("wu%d" % i, (128, 8, 256), BF16, st) for i in range(2)]
    wd = [sb("wd%d" % i, (128, 2, 1024), BF16, st) for i in range(2)]
    for x_ in xg:
        p.memset("pool", x_[:], 0.0, W=[x_.r])
    ceng = ["dve", "act", "dve", "act"]
    xgT2 = [xgT, sb("xgTb", (128, 8, 512), BF16, st)]
    sid2 = [sid, sb("sidb", (128, 4, 16), I32, st)]

    def load_weights(e_):
        k = e_ % 2
        p.dma("sp", wgf[:], I["w_gate"][e_, :, :].rearrange("(p c) f -> p c f", c=8), R=[I["w_gate"].r], W=[wgf.r])
        p.dma("sp", wuf[:], I["w_up"][e_, :, :].rearrange("(p c) f -> p c f", c=8), R=[I["w_up"].r], W=[wuf.r])
        p.dma("sp", wdf[:], I["w_down"][e_, :, :].rearrange("(c p) n -> p c n", p=128), R=[I["w_down"].r], W=[wdf.r])
        p.cp("act", wg[k][:], wgf[:], R=[wgf.r], W=[wg[k].r])
        p.cp("dve", wu[k][:], wuf[:], R=[wuf.r], W=[wu[k].r])
        p.cp("act", wd[k][:], wdf[:], R=[wdf.r], W=[wd[k].r])

    def gather_tokens(e_):
        k = e_ % 2
        sd = sid2[k]
        p.dma("sp", sd[:], S["slot"][e_ * CAP:(e_ + 1) * CAP, :].rearrange("(s p) c -> p s c", p=128),
              R=[S["slot"].r], W=[sd.r])
        for s_ in range(4):
            p.dma("pool", None, None, R=[sd.r, S["h1_s"].r], W=[xg[s_].r],
                  fn=lambda e, s_=s_, sd=sd: e.indirect_dma_start(
                      out=xg[s_][:, :], out_offset=None, in_=S["h1_s"][:, :],
                      in_offset=bass.IndirectOffsetOnAxis(ap=sd[:, s_, 0:1], axis=0), bounds_check=p.breg(e, T - 1), oob_is_err=False))
            xb = xgb[s_ % 2]
            p.cp(ceng[s_], xb[:], xg[s_][:], R=[xg[s_].r], W=[xb.r])
            bk = B[6 + s_ % 2]
            tv = bk.t[:].bitcast(BF16)
            for c in range(8):
                p.tr(tv[:, c * 128:(c + 1) * 128], xb.t[:, c:1024:8], ident[:], R=[xb.r, ident.r], W=[bk.r])
            p.cp("act" if s_ % 2 else "dve", xgT2[k][:, :, s_ * 128:(s_ + 1) * 128], tv[:, :].rearrange("p (c q) -> p c q", q=128),
                 R=[bk.r], W=[xgT2[k].r])

    def compute(e_):
        k = e_ % 2
        xT_ = xgT2[k]
        for fc in range(2):
            bG, bU = B[2 * fc], B[2 * fc + 1]
            for c in range(8):
                p.mm(bG[:, :], wg[k][:, c, fc * 128:(fc + 1) * 128], xT_[:, c, :], c == 0, c == 7, R=[wg[k].r, xT_.r], W=[bG.r])
            for c in range(8):
                p.mm(bU[:, :], wu[k][:, c, fc * 128:(fc + 1) * 128], xT_[:, c, :], c == 0, c == 7, R=[wu[k].r, xT_.r], W=[bU.r])
            p.act(sg[fc][:], bG[:, :], AF.Silu, R=[bG.r], W=[sg[fc].r])
            p.tt("dve", hT[:, fc, :], sg[fc][:], bU[:, :], ALU.mult, R=[sg[fc].r, bU.r], W=[hT.r])
        for s_ in range(4):
            y_ = yb[s_ % 2]
            for hf in range(2):
                bY = B[4 + (2 * s_ + hf) % 2]
                for fc in range(2):
                    p.mm(bY[:, :], hT[:, fc, s_ * 128:(s_ + 1) * 128], wd[k][:, fc, hf * 512:(hf + 1) * 512], fc == 0, fc == 1,
                         R=[hT.r, wd[k].r], W=[bY.r])
                if hf == 0:
                    p.act(y_[:, 0:512], bY[:, :], AF.Copy, R=[bY.r], W=[y_.r])
                else:
                    p.cp("dve", y_[:, 512:1024], bY[:, :], R=[bY.r], W=[y_.r])
            p.dma("sp", S["y_s"][e_ * CAP + s_ * 128:e_ * CAP + (s_ + 1) * 128, :], y_[:], R=[y_.r], W=[S["y_s"].r])

    load_weights(0)
    gather_tokens(0)
    for e_ in range(nexp):
        if e_ + 1 < nexp:
            load_weights(e_ + 1)
            gather_tokens(e_ + 1)
        compute(e_)
    return st


def phase5(g):
    nc, p, sb, I, S, Rz = g.nc, g.p, g.sb, g.I, g.S, g.Rz
    st = ExitStack()
    bc = {}
    for nm in ("ln2_g", "ln2_b"):
        bc[nm] = sb("bc5_" + nm, (128, 1024), F32, st)
        p.dma("sp", bc[nm][:], I[nm][0:1, :].partition_broadcast(128), R=[I[nm].r], W=[bc[nm].r])
    y = [[sb("y%d_%d" % (k, j), (128, 1024), F32, st) for k in range(2)] for j in range(2)]
    h1 = [sb("h1_5%d" % j, (128, 1024), F32, st) for j in range(2)]
    r = sb("r5", (128, 1024), F32, st)
    tmp = sb("tmp5", (128, 1024), F32, st)
    ot = [sb("ot5%d" % j, (128, 1024), F32, st) for j in range(2)]
    rs = sb("rs5", (128, 8), F32, st)
    for j in range(2):
        for k in range(2):
            p.memset("dve", y[j][k][:], 0.0, W=[y[j][k].r])
    for i in range((g.debug or {}).get("ntiles", NT)):
        j = i % 2
        p.dma("sp", h1[j][:], S["h1_s"][i * 128:(i + 1) * 128, :], R=[S["h1_s"].r], W=[h1[j].r])
        for k in range(2):
            p.dma("pool", None, None, R=[g.dest.r, S["y_s"].r], W=[y[j][k].r],
                  fn=lambda e, i=i, k=k, j=j: e.indirect_dma_start(
                      out=y[j][k][:, :], out_offset=None, in_=S["y_s"][:, :],
                      in_offset=bass.IndirectOffsetOnAxis(ap=g.dest[:, i, k:k + 1], axis=0), bounds_check=p.breg(e, 32 * CAP - 1), oob_is_err=False))
        p.ts("dve", r[:], h1[j][:], ALPHA, None, ALU.mult, R=[h1[j].r], W=[r.r])
        p.stt(r[:], y[j][0][:], g.wts[:, i, 0:1], r[:], ALU.mult, ALU.add, R=[y[j][0].r, g.wts.r, r.r], W=[r.r])
        p.stt(r[:], y[j][1][:], g.wts[:, i, 1:2], r[:], ALU.mult, ALU.add, R=[y[j][1].r, g.wts.r, r.r], W=[r.r])
        p.op("dve", lambda e: e.reduce_sum(rs[:, 0:1], r[:], AX.X), R=[r.r], W=[rs.r])
        p.memset("dve", rs[:, 1:2], 0.0, W=[rs.r])
        layer_norm_tile(p, r, rs, bc["ln2_g"], bc["ln2_b"], ot[j], tmp)
        p.dma("sp", g.out[i * 128:(i + 1) * 128, :], ot[j][:], R=[ot[j].r], W=[g.out.r])
    return st


def host_inputs(inputs):
    f = lambda a: np.ascontiguousarray(np.asarray(a, dtype=np.float32))
    rel_bias = f(inputs["rel_bias"])
    shared = {
        "w_in": f(inputs["w_in"][0]),
        "pe_kT": f(inputs["cmp_pe_k"][0].T), "pe_vT": f(inputs["cmp_pe_v"][0].T),
        "cw1_k": f(inputs["cmp_w1_k"][0]), "cw2_k": f(inputs["cmp_w2_k"][0]),
        "cw1_v": f(inputs["cmp_w1_v"][0]), "cw2_v": f(inputs["cmp_w2_v"][0]),
        "ckv_g": f(inputs["ckv_norm_g"][0]).reshape(1, 128),
        "w_uk": f(inputs["w_uk"][0]), "w_uv": f(inputs["w_uv"][0]),
        "relflat": rel_bias.reshape(1, 512),
        "w_ba": f(inputs["w_branch_a"][0]), "w_bb": f(inputs["w_branch_b"][0]), "w_out": f(inputs["w_out"][0]),
        "ln1_g": f(inputs["ln1_g"][0]).reshape(1, D), "ln1_b": f(inputs["ln1_b"][0]).reshape(1, D),
        "wr": f(np.concatenate([np.asarray(inputs["w_grp"][0]), np.asarray(inputs["w_rtr"][0])], axis=1)),
        "br": f(np.concatenate([np.asarray(inputs["b_grp"][0]), np.asarray(inputs["b_rtr"][0])], axis=0)).reshape(1, 36),
        "w_gate": f(inputs["w_gate"][0]), "w_up": f(inputs["w_up"][0]), "w_down": f(inputs["w_down"][0]),
        "ln2_g": f(inputs["ln2_g"][0]).reshape(1, D), "ln2_b": f(inputs["ln2_b"][0]).reshape(1, D),
    }
    shared.update(_host_consts(rel_bias))
    x = np.asarray(inputs["x"], dtype=np.float32)
    maps = []
    for b in range(x.shape[0]):
        m = dict(shared)
        m["x"] = np.ascontiguousarray(x[b])
        m["xT"] = np.ascontiguousarray(x[b].T)
        maps.append(m)
    return maps


def kernel(**inputs):
    maps = host_inputs(inputs)
    nc, g = build()
    res = run_bass_kernel_spmd(nc, maps, core_ids=list(range(8)))
    return np.stack([np.asarray(r["out"], dtype=np.float32) for r in res.results], axis=0)
```

```python
import math
from contextlib import ExitStack
import numpy as np
import ml_dtypes
import concourse.bass as bass
import concourse.mybir as mybir
from concourse.bass_utils import run_bass_kernel_spmd

F32 = mybir.dt.float32
BF16 = mybir.dt.bfloat16
I32 = mybir.dt.int32
AF = mybir.ActivationFunctionType
ALU = mybir.AluOpType
AX = mybir.AxisListType

T = 4096
D = 1024
NT = T // 128
D_IN = 4316
ALPHA = 2.0 ** 0.25
CAP = 512
NEGM = 32768.0
BIS_ITERS = 20
DEBUG = None

O_QA, O_KC, O_VC, O_KS, O_VS, O_KW, O_VW, O_GN, O_QB, O_CKV, O_QI, O_KI, O_WI, O_GA, O_GB = (
    0, 512, 640, 768, 896, 1024, 1152, 1280, 1304, 1816, 1944, 2200, 2264, 2268, 3292)


class Res:
    __slots__ = ("name", "w", "r", "dsem", "dcount", "excl")

    def __init__(self, name):
        self.name = name
        self.excl = False
        self.w = None
        self.r = {}
        self.dsem = None
        self.dcount = 0


class Prog:
    ENG = ("pe", "act", "dve", "pool", "sp")

    def __init__(self, nc, stack):
        self.nc = nc
        self.stack = stack
        self.eobj = {"pe": nc.tensor, "act": nc.scalar, "dve": nc.vector, "pool": nc.gpsimd, "sp": nc.sync}
        self.sem = {e: stack.enter_context(nc.semaphore("c_" + e)) for e in self.ENG}
        self.cnt = {e: 0 for e in self.ENG}
        self.seen = {e: {} for e in self.ENG}
        self.ops = {e: [] for e in self.ENG}
        self.dres = []
        self.nres = 0

    def res(self, name=None):
        self.nres += 1
        return Res(name or "r%d" % self.nres)

    def _waits(self, eng, reads, writes):
        need = {}

        def add(ev, war=False):
            if ev is None:
                return
            sem, val, src = ev
            if src == eng and eng == "pe":
                return
            k = id(sem)
            if self.seen[eng].get(k, (None, 0))[1] >= val:
                return
            if k not in need or need[k][1] < val:
                need[k] = (sem, val)

        for r in reads:
            add(r.w)
        for w in writes:
            add(w.w)
            for ev in w.r.values():
                add(ev, True)
        out = []
        for k, (sem, val) in need.items():
            self.seen[eng][k] = (sem, val)
            out.append((sem, val))
        return out

    def op(self, eng, fn, R=(), W=()):
        W = list(W) + [r for r in R if r.excl]
        R = [r for r in R if not r.excl]
        waits = self._waits(eng, R, W)
        self.cnt[eng] += 1
        ev = (self.sem[eng], self.cnt[eng], eng)
        self.ops[eng].append((fn, waits, (self.sem[eng], 1)))
        for r in R:
            r.r[eng] = ev
        for w in W:
            w.w = ev
            w.r = {}

    def dma(self, q, out, in_, R=(), W=(), fn=None):
        waits = self._waits(q, R, W)
        tgt = W[0]
        if tgt.dsem is None:
            tgt.dsem = self.stack.enter_context(self.nc.semaphore("d_" + tgt.name))
            self.dres.append(tgt)
        tgt.dcount += 16
        ev = (tgt.dsem, tgt.dcount, None)
        if fn is None:
            fn = lambda e, o=out, i=in_: e.dma_start(out=o, in_=i)
        self.ops[q].append((fn, waits, (tgt.dsem, 16)))
        for r in R:
            r.r["dma%d" % id(tgt)] = ev
        for w in W:
            w.w = ev
            w.r = {}

    def flush(self, final=False):
        tail = {}
        for e in self.ENG:
            ws = []
            for e2 in self.ENG:
                if e2 != e and self.cnt[e2] > 0 and self.seen[e].get(id(self.sem[e2]), (None, 0))[1] < self.cnt[e2]:
                    ws.append((self.sem[e2], self.cnt[e2]))
                    self.seen[e][id(self.sem[e2])] = (self.sem[e2], self.cnt[e2])
            for r in self.dres:
                if self.seen[e].get(id(r.dsem), (None, 0))[1] < r.dcount:
                    ws.append((r.dsem, r.dcount))
                    self.seen[e][id(r.dsem)] = (r.dsem, r.dcount)
            tail[e] = ws
        ops = self.ops
        eobj = self.eobj
        self.regs = {}
        with self.nc.Block() as block:
            def run(e, engine):
                for fn, waits, inc in ops[e]:
                    for s, v in waits:
                        engine.wait_ge(s, v)
                    ins = fn(engine)
                    ins.then_inc(inc[0], inc[1])
                for s, v in tail[e]:
                    engine.wait_ge(s, v)

            @block.tensor
            def _(t):
                run("pe", t)

            @block.scalar
            def _(s):
                run("act", s)

            @block.vector
            def _(v):
                run("dve", v)

            @block.gpsimd
            def _(g):
                run("pool", g)

            @block.sync
            def _(sy):
                run("sp", sy)
        self.ops = {e: [] for e in self.ENG}

    def breg(self, e, val):
        if val not in self.regs:
            self.regs[val] = e.to_reg(val)
        return self.regs[val]

    def mm(self, out, lhsT, rhs, start, stop, R=(), W=()):
        self.op("pe", lambda e: e.matmul(out, lhsT, rhs, start=start, stop=stop, skip_group_check=True), R, W)

    def tr(self, out, in_, ident, R=(), W=()):
        self.op("pe", lambda e: e.transpose(out, in_, ident), R, W)

    def act(self, out, in_, func, R=(), W=(), bias=None, scale=None, accum=None, eng="act"):
        kw = {}
        if bias is not None:
            kw["bias"] = bias
        if scale is not None:
            kw["scale"] = scale
        if accum is not None:
            kw["accum_out"] = accum
        self.op("act", lambda e: e.activation(out, in_, func, **kw), R, W)

    def ts(self, eng, out, in0, s1, s2, op0, op1=None, R=(), W=(), accum=None):
        kw = {}
        if op1 is not None:
            kw["op1"] = op1
        if accum is not None:
            kw["accum_out"] = accum
        self.op(eng, lambda e: e.tensor_scalar(out, in0, s1, s2, op0, **kw), R, W)

    def tt(self, eng, out, in0, in1, op, R=(), W=()):
        self.op(eng, lambda e: e.tensor_tensor(out, in0, in1, op), R, W)

    def stt(self, out, in0, scalar, in1, op0, op1, R=(), W=(), accum=None):
        kw = {}
        if accum is not None:
            kw["accum_out"] = accum
        self.op("dve", lambda e: e.scalar_tensor_tensor(out, in0, scalar, in1, op0, op1, **kw), R, W)

    def cp(self, eng, out, in_, R=(), W=()):
        if eng == "act":
            self.op("act", lambda e: e.copy(out, in_), R, W)
        else:
            self.op(eng, lambda e: e.tensor_copy(out, in_), R, W)

    def memset(self, eng, ap, val, W=()):
        self.op(eng, lambda e: e.memset(ap, val), (), W)


class Buf:
    def __init__(self, t, res):
        self.t = t
        self.r = res

    def __getitem__(self, k):
        return self.t[k]


def _rel_bucket_np(dist):
    n = np.maximum(dist, 0)
    nf = np.maximum(n, 1).astype(np.float32)
    large = 16 + (np.log(nf / 16) / math.log(1024 / 16) * 16).astype(np.int32)
    return np.where(n < 16, n, np.minimum(large, 31))


def _host_consts(rel_bias):
    c = {}
    dd = np.arange(0, 1152)
    bk = _rel_bucket_np(dd)
    relvec = rel_bias[bk]
    p = np.arange(128)[:, None]
    j = np.arange(128)[None, :]
    relT = np.zeros((8, 128, 16, 128), np.float32)
    for di in range(8):
        dist = np.clip(128 * di + j - p, 0, 1151)
        relT[di] = relvec[dist].transpose(0, 2, 1)
    c["relTa"] = np.ascontiguousarray(relT[:, :, :8, :].transpose(1, 0, 2, 3)).reshape(128, 8, 2, 512)
    c["relTb"] = np.ascontiguousarray(relT[:, :, 8:, :].transpose(1, 0, 2, 3)).reshape(128, 8, 1024)
    c31 = relvec[1151]
    c["c31a"] = np.ascontiguousarray(np.repeat(c31[:8].reshape(2, 4, 1), 128, axis=2).reshape(2, 512))
    c["c31b"] = np.ascontiguousarray(np.repeat(c31[8:].reshape(8, 1), 128, axis=1).reshape(1, 1024))
    v = np.arange(503)[None, :]
    dist = p - 16 * v + 3937
    c["cmpv"] = np.ascontiguousarray(relvec[np.clip(dist, 0, 1151)][:, :, :8].transpose(0, 2, 1))
    c["cmpm"] = np.where(dist >= 0, 0.0, -30000.0).astype(np.float32)
    mc = np.where(j < p, -NEGM, 0.0).astype(np.float32)
    mw = np.where(j >= p, -NEGM, 0.0).astype(np.float32)
    c["mc4"] = np.tile(mc, (1, 4))
    c["mw4"] = np.tile(mw, (1, 4))
    c["ident"] = np.eye(128, dtype=np.float32)
    c["d30"] = np.tile(np.eye(128, dtype=np.float32) * NEGM, (1, 4))
    e_all = np.zeros((64, 32, 128), np.float32)
    for kt in range(32):
        e_all[2 * kt, kt, :64] = NEGM
        e_all[2 * kt + 1, kt, 64:] = NEGM
    c["eall"] = e_all.reshape(64, 32 * 128)
    c["mcq"] = np.where(j > p, -1e30, 0.0).astype(np.float32)
    cp_ = (np.arange(128) >= 64).astype(np.int64)[:, None]
    u = np.arange(128)[None, :] - 63
    c["wext"] = np.where((u == cp_) | (u == cp_ - 1), 1e4, np.where(u > cp_, -1e30, 0.0)).astype(np.float32)
    cs = 16 * np.arange(255)[:, None]
    ss = 64 * np.arange(64)[None, :]
    ov = np.minimum(cs + 32, ss + 64) - np.maximum(cs, ss)
    cm = np.zeros((256, 64), np.float32)
    cm[:255] = np.clip(ov, 0, None).astype(np.float32) / 16
    c["cmap"] = cm
    c["ustrict"] = (np.arange(128)[:, None] < np.arange(128)[None, :]).astype(np.float32)
    c["iota32"] = np.tile(np.arange(32, dtype=np.float32)[None, :], (128, 1))
    c["tokid"] = (np.arange(128)[:, None] + 128 * np.arange(32)[None, :]).astype(np.int32)
    return c


class Ctx:
    pass


def build(debug=None):
    nc = bass.Bass("TRN2", target_bir_lowering=False)
    es = ExitStack()
    p = Prog(nc, es)
    g = Ctx()
    g.nc, g.p, g.es, g.debug = nc, p, es, debug
    g.dbg_out = {}

    def din(name, shape, dt=F32):
        return Buf(nc.dram_tensor(name, list(shape), dt, kind="ExternalInput").ap(), p.res(name))

    def dscr(name, shape, dt=F32):
        kind = "ExternalOutput" if (debug and name in debug) else "Internal"
        return Buf(nc.dram_tensor(name, list(shape), dt, kind=kind).ap(), p.res(name))

    g.din, g.dscr = din, dscr

    def sb(name, shape, dt=F32, stack=None):
        t = (stack or es).enter_context(nc.sbuf_tensor("s_" + name, list(shape), dt))
        return Buf(t, p.res(name))

    def ps(name, shape, dt=F32, stack=None):
        t = (stack or es).enter_context(nc.psum_tensor(name, list(shape), dt))
        r = p.res(name)
        r.excl = True
        return Buf(t, r)

    g.sb, g.ps = sb, ps

    I = g.I = {}
    for name, shape in [
        ("xT", (D, T)), ("x", (T, D)), ("w_in", (D, D_IN)),
        ("pe_kT", (64, 32)), ("pe_vT", (64, 32)),
        ("cw1_k", (2048, 256)), ("cw2_k", (256, 64)), ("cw1_v", (2048, 256)), ("cw2_v", (256, 64)),
        ("ckv_g", (1, 128)), ("w_uk", (8, 64, 128)), ("w_uv", (8, 128, 64)), ("relflat", (1, 512)),
        ("w_ba", (512, D)), ("w_bb", (512, D)), ("w_out", (D, D)),
        ("ln1_g", (1, D)), ("ln1_b", (1, D)), ("wr", (D, 36)), ("br", (1, 36)),
        ("w_gate", (32, D, 256)), ("w_up", (32, D, 256)), ("w_down", (32, 256, D)),
        ("ln2_g", (1, D)), ("ln2_b", (1, D)),
        ("relTa", (128, 8, 2, 512)), ("relTb", (128, 8, 1024)), ("c31a", (2, 512)), ("c31b", (1, 1024)),
        ("cmpv", (128, 8, 503)), ("cmpm", (128, 503)), ("mc4", (128, 512)), ("mw4", (128, 512)),
        ("ident", (128, 128)), ("d30", (128, 512)), ("eall", (64, 4096)), ("mcq", (128, 128)),
        ("wext", (128, 128)), ("cmap", (256, 64)), ("ustrict", (128, 128)), ("iota32", (128, 32)),
    ]:
        I[name] = din(name, shape)
    I["tokid"] = din("tokid", (128, 32), I32)
    g.out = Buf(nc.dram_tensor("out", [T, D], F32, kind="ExternalOutput").ap(), p.res("out"))

    S = g.S = {}
    S["qa_s"] = dscr("qa_s", (NT, 2, 64, 4, 128), BF16)
    S["ql_s"] = dscr("ql_s", (NT, 128, 8, 128), BF16)
    S["qi_s"] = dscr("qi_s", (NT, 64, 4, 128), BF16)
    S["g_s"] = dscr("g_s", (NT, 128, 16, 128), F32)
    S["h1_s"] = dscr("h1_s", (T, D), F32)
    S["slot"] = dscr("slot", (32 * CAP, 16), I32)
    S["y_s"] = dscr("y_s", (32 * CAP, D), F32)

    Rz = g.Rz = {}
    Rz["ksT"] = sb("ksT", (128, T), BF16)
    Rz["kwT"] = sb("kwT", (128, T), BF16)
    Rz["ckvT"] = sb("ckvT", (128, T), BF16)
    Rz["kiT"] = sb("kiT", (64, T), BF16)
    Rz["vsA"] = sb("vsA", (128, NT, 2, 65), BF16)
    Rz["vwA"] = sb("vwA", (128, NT, 2, 65), BF16)
    Rz["ckvA"] = sb("ckvA", (128, NT, 129), BF16)
    Rz["gn"] = sb("gn", (128, NT, 24), F32)
    Rz["wabs"] = sb("wabs", (128, NT, 4), F32)
    Rz["wsgn"] = sb("wsgn", (128, NT, 4), F32)
    Rz["kcmpT"] = sb("kcmpT", (128, 256), BF16)
    Rz["vcmpM"] = sb("vcmpM", (128, 2, 2, 128), BF16)
    Rz["kmax"] = sb("kmax", (1, 4), F32)
    Rz["identb"] = sb("identb", (128, 128), BF16)
    Rz["identf"] = sb("identf", (128, 128), F32)
    Rz["ones_bf"] = sb("ones_bf", (128, 128), BF16)

    g.kcT = sb("kcT", (128, T), BF16)
    g.vcT = sb("vcT", (128, T), BF16)
    g.dest = sb("dest_i", (128, NT, 2), I32)
    g.wts = sb("wts", (128, NT, 2), F32)
    g.banks = [ps("bank%d" % i, (128, 512), F32) for i in range(8)]

    stage = (debug or {}).get("stage", 99)
    st = phase1(g)
    if debug:
        dump_resident(g)
    p.flush()
    st.close()
    if stage >= 2:
        st = phase2(g)
        p.flush()
        st.close()
    if stage >= 3:
        st = phase3(g)
        p.flush()
        st.close()
    if stage >= 4:
        st = phase4(g)
        p.flush()
        st.close()
    if stage >= 5:
        st = phase5(g)
        p.flush()
        st.close()
    return nc, g


def dump_resident(g):
    nc, p = g.nc, g.p
    for name, buf in g.Rz.items():
        shape = list(buf.t.shape)
        d = nc.dram_tensor("dbg_" + name, shape, buf.t.dtype, kind="ExternalOutput").ap()
        r = p.res("dbg_" + name)
        idx = tuple(slice(None) for _ in shape)
        p.dma("sp", d[idx], buf.t[idx], R=[buf.r], W=[r])


def phase1(g):
    nc, p, sb, I, S, Rz = g.nc, g.p, g.sb, g.I, g.S, g.Rz
    st = ExitStack()
    banks = g.banks
    bi = [0]

    def nbank():
        b = banks[bi[0] % 8]
        bi[0] += 1
        return b

    ev_rr = [0]

    def ev_eng():
        ev_rr[0] += 1
        return "act" if ev_rr[0] % 2 else "dve"

    stf = sb("p1_cst", (128, 128), F32, st)
    p.dma("sp", stf[:], I["ident"][:, :], R=[I["ident"].r], W=[stf.r])
    p.cp("dve", Rz["identb"][:], stf[:], R=[stf.r], W=[Rz["identb"].r])
    p.cp("dve", Rz["identf"][:], stf[:], R=[stf.r], W=[Rz["identf"].r])
    p.memset("dve", Rz["ones_bf"][:], 1.0, W=[Rz["ones_bf"].r])
    p.memset("pool", Rz["vsA"][:, :, :, 64:65], 1.0, W=[Rz["vsA"].r])
    p.memset("pool", Rz["vwA"][:, :, :, 64:65], 1.0, W=[Rz["vwA"].r])
    p.memset("pool", Rz["ckvA"][:, :, 128:129], 1.0, W=[Rz["ckvA"].r])

    xTb = sb("xTb", (128, 8, T), BF16, st)
    xst = [sb("xst%d" % i, (128, 1024), F32, st) for i in range(2)]
    xq = [p.res("xTq%d" % q) for q in range(4)]
    engs = ["dve", "act", "dve", "act"]
    xk = [0]

    def load_x(q):
        for c in range(8):
            s_ = xst[xk[0] % 2]
            xk[0] += 1
            p.dma("sp", s_[:], I["xT"][c * 128:(c + 1) * 128, q * 1024:(q + 1) * 1024], R=[I["xT"].r], W=[s_.r])
            p.cp(engs[c % 4], xTb[:, c, q * 1024:(q + 1) * 1024], s_[:], R=[s_.r], W=[xq[q]])

    cut = 99
    wst = [sb("wst%d" % i, (128, 8, 128), F32, st) for i in range(2)]
    wbf = [sb("wbf%d" % i, (128, 8, 128), BF16, st) for i in range(2)]
    wk = [0]

    def load_w(col0, M):
        k = wk[0] % 2
        wk[0] += 1
        p.dma("sp", wst[k][:, :, 0:M], I["w_in"][:, col0:col0 + M].rearrange("(c p) m -> p c m", p=128),
              R=[I["w_in"].r], W=[wst[k].r])
        p.cp("act" if k else "dve", wbf[k][:, :, 0:M], wst[k][:, :, 0:M], R=[wst[k].r], W=[wbf[k].r])
        return wbf[k]

    groups = []

    def fm_group(col0, M, evac):
        groups.append((col0, M, evac))

    def run_groups():
        nxt = load_w(groups[0][0], groups[0][1])
        load_x(0)
        for gi, (col0, M, evac) in enumerate(groups):
            w = nxt
            if gi + 1 < len(groups):
                nxt = load_w(groups[gi + 1][0], groups[gi + 1][1])
            for tb in range(8):
                if gi == 0 and tb % 2 == 0 and tb < 6:
                    load_x(tb // 2 + 1)
                b = nbank()
                for c in range(8):
                    p.mm(b[0:M, :], w[:, c, 0:M], xTb[:, c, tb * 512:(tb + 1) * 512], c == 0, c == 7,
                         R=[w.r, xq[tb // 2]], W=[b.r])
                evac(tb, b)

    stg_bf = [sb("stgb%d" % i, (128, 512), BF16, st) for i in range(4)]
    stg_f = [sb("stgf%d" % i, (128, 512), F32, st) for i in range(2)] * 2
    sk = [0]

    def nstg(lst):
        sk[0] += 1
        return lst[sk[0] % 4]

    def evac_copy(dst_buf, scale=None):
        def f(tb, b):
            M = dst_buf.t.shape[0]
            e = ev_eng()
            o = dst_buf[:, tb * 512:(tb + 1) * 512]
            if e == "act":
                p.act(o, b[0:M, :], AF.Copy, R=[b.r], W=[dst_buf.r])
            else:
                p.cp("dve", o, b[0:M, :], R=[b.r], W=[dst_buf.r])
        return f

    for m in range(4):
        def ev(tb, b, m=m):
            s_ = nstg(stg_bf)
            p.act(s_[:], b[:], AF.Copy, scale=0.125, R=[b.r], W=[s_.r])
            gq, hh0 = m // 2, 2 * (m % 2)
            for hl in range(2):
                dst = S["qa_s"][tb * 4:(tb + 1) * 4, gq, :, hh0 + hl, :].rearrange("t d q -> d t q")
                src = s_[hl * 64:(hl + 1) * 64, :].rearrange("d (t q) -> d t q", q=128)
                p.dma("sp", dst, src, R=[s_.r], W=[S["qa_s"].r])
        fm_group(O_QA + 128 * m, 128, ev)

    kcT, vcT = g.kcT, g.vcT
    fm_group(O_KC, 128, evac_copy(kcT))
    fm_group(O_VC, 128, evac_copy(vcT))
    fm_group(O_KS, 128, evac_copy(Rz["ksT"]))
    fm_group(O_KW, 128, evac_copy(Rz["kwT"]))
    fm_group(O_KI, 64, evac_copy(Rz["kiT"]))

    wukf = sb("wukf", (128, 4, 128), F32, st)
    wukb = sb("wukb", (128, 4, 128), BF16, st)
    p.dma("sp", wukf[:], I["w_uk"][:, :, :].rearrange("(m hl) d r -> (hl d) m r", hl=2), R=[I["w_uk"].r], W=[wukf.r])
    p.cp("dve", wukb[:], wukf[:], R=[wukf.r], W=[wukb.r])
    for m in range(4):
        def ev(tb, b, m=m):
            s_ = nstg(stg_bf)
            p.cp("dve", s_[:], b[:], R=[b.r], W=[s_.r])
            for hl in range(2):
                b2 = nbank()
                p.mm(b2[:, :], wukb[hl * 64:(hl + 1) * 64, m, :], s_[hl * 64:(hl + 1) * 64, :], True, True,
                     R=[wukb.r, s_.r], W=[b2.r])
                s2 = nstg(stg_bf)
                p.act(s2[:], b2[:], AF.Copy, scale=0.125, R=[b2.r], W=[s2.r])
                dst = S["ql_s"][tb * 4:(tb + 1) * 4, :, 2 * m + hl, :].rearrange("t r q -> r t q")
                p.dma("sp", dst, s2[:].rearrange("r (t q) -> r t q", q=128), R=[s2.r], W=[S["ql_s"].r])
        fm_group(O_QB + 128 * m, 128, ev)

    for m in range(2):
        def ev(tb, b, m=m):
            s_ = nstg(stg_bf)
            p.act(s_[:], b[:], AF.Copy, scale=0.125, R=[b.r], W=[s_.r])
            for hl in range(2):
                dst = S["qi_s"][tb * 4:(tb + 1) * 4, :, 2 * m + hl, :].rearrange("t d q -> d t q")
                src = s_[hl * 64:(hl + 1) * 64, :].rearrange("d (t q) -> d t q", q=128)
                p.dma("sp", dst, src, R=[s_.r], W=[S["qi_s"].r])
        fm_group(O_QI + 128 * m, 128, ev)

    for m in range(16):
        def ev(tb, b, m=m):
            s_ = nstg(stg_f)
            p.act(s_[:], b[:], AF.Sigmoid, R=[b.r], W=[s_.r])
            dst = S["g_s"][tb * 4:(tb + 1) * 4, :, m, :].rearrange("t c q -> c t q")
            p.dma("sp", dst, s_[:].rearrange("c (t q) -> c t q", q=128), R=[s_.r], W=[S["g_s"].r])
        fm_group(O_GA + 128 * m, 128, ev)

    run_groups()
    wtf = sb("wtf", (128, 8, 412), F32, st)
    wtb = sb("wtb", (128, 8, 412), BF16, st)
    for (c0, n, o) in [(O_VS, 128, 0), (O_VW, 128, 128), (O_CKV, 128, 256), (O_GN, 24, 384), (O_WI, 4, 408)]:
        r_ = p.res("wtf%d" % o)
        p.dma("sp", wtf[:, :, o:o + n], I["w_in"][:, c0:c0 + n].rearrange("(c p) m -> p c m", p=128),
              R=[I["w_in"].r], W=[r_])
        p.cp("dve", wtb[:, :, o:o + n], wtf[:, :, o:o + n], R=[r_], W=[wtb.r])
    gbc = sb("gbc", (128, 128), F32, st)
    p.dma("sp", gbc[:], I["ckv_g"][0:1, :].partition_broadcast(128), R=[I["ckv_g"].r], W=[gbc.r])
    junk = sb("p1junk", (128, 128), F32, st)
    ssq = sb("p1ssq", (128, 4), F32, st)
    sub = (g.debug or {}).get("sub", 99)
    for tt in range(NT if sub >= 1 else 0):
        b = nbank()
        for c in range(8):
            p.mm(b[:, 0:412], xTb[:, c, tt * 128:(tt + 1) * 128], wtb[:, c, :], c == 0, c == 7,
                 R=[wtb.r, xq[tt // 8]], W=[b.r])
        p.cp("dve", Rz["vsA"][:, tt, :, 0:64], b[:, 0:128].rearrange("p (g d) -> p g d", g=2), R=[b.r], W=[Rz["vsA"].r])
        p.cp("dve", Rz["vwA"][:, tt, :, 0:64], b[:, 128:256].rearrange("p (g d) -> p g d", g=2), R=[b.r], W=[Rz["vwA"].r])
        if sub <= 1:
            continue
        p.act(junk[:], b[:, 256:384], AF.Square, R=[b.r], W=[junk.r, ssq.r], accum=ssq[:, 0:1])
        sub2 = (g.debug or {}).get("sub2", 99)
        if sub2 <= 0:
            continue
        p.ts("dve", ssq[:, 1:2], ssq[:, 0:1], 1.0 / 128, 1e-6, ALU.mult, ALU.add, R=[ssq.r], W=[ssq.r])
        if sub2 <= 1:
            continue
        p.act(ssq[:, 2:3], ssq[:, 1:2], AF.Sqrt, R=[ssq.r], W=[ssq.r])
        if sub2 <= 2:
            continue
        p.op("dve", lambda e: e.reciprocal(ssq[:, 3:4], ssq[:, 2:3]), R=[ssq.r], W=[ssq.r])
        if sub2 <= 3:
            continue
        p.stt(Rz["ckvA"][:, tt, 0:128], b[:, 256:384], ssq[:, 3:4], gbc[:], ALU.mult, ALU.mult,
              R=[b.r, ssq.r, gbc.r], W=[Rz["ckvA"].r])
        if sub <= 2:
            continue
        p.act(Rz["gn"][:, tt, :], b[:, 384:408], AF.Sigmoid, R=[b.r], W=[Rz["gn"].r])
        p.act(Rz["wabs"][:, tt, :], b[:, 408:412], AF.Abs, scale=0.5, R=[b.r], W=[Rz["wabs"].r])
        p.act(Rz["wsgn"][:, tt, :], b[:, 408:412], AF.Sign, R=[b.r], W=[Rz["wsgn"].r])
        if sub <= 3:
            continue
        b2 = nbank()
        tv = b2.t[:].bitcast(BF16)
        p.tr(tv[:, 0:128], Rz["ckvA"][:, tt, 0:128], Rz["identb"][:], R=[Rz["ckvA"].r, Rz["identb"].r], W=[b2.r])
        p.cp("act", Rz["ckvT"][:, tt * 128:(tt + 1) * 128], tv[:, 0:128], R=[b2.r], W=[Rz["ckvT"].r])

    if cut <= 6:
        return st
    p.flush()
    st.close()
    st = ExitStack()
    phase1b(g, st, kcT, vcT, nbank)
    return st


def phase1b(g, st, kcT, vcT, nbank):
    nc, p, sb, I, S, Rz = g.nc, g.p, g.sb, g.I, g.S, g.Rz
    w1s = sb("w1s", (128, 8, 256), F32, st)
    w2s = sb("w2s", (128, 2, 64), F32, st)
    pes = sb("pes", (128, 32), F32, st)
    peb = sb("peb", (128, 32), BF16, st)
    cst = sb("cst", (128, 2), F32, st)
    u = sb("cu", (128, 256), F32, st)
    t1 = sb("ct1", (128, 256), F32, st)
    t2 = sb("ct2", (128, 256), F32, st)
    cms = sb("cms", (128, 2, 64), F32, st)
    p.dma("sp", cms[:], I["cmap"][:, :].rearrange("(c p) n -> p c n", p=128), R=[I["cmap"].r], W=[cms.r])
    for gq in range(2):
        p.cp("dve", Rz["vcmpM"][:, :, gq, 64:128], cms[:], R=[cms.r], W=[Rz["vcmpM"].r])
    for kv, (srcT, w1n, w2n, pen) in enumerate([(kcT, "cw1_k", "cw2_k", "pe_kT"), (vcT, "cw1_v", "cw2_v", "pe_vT")]):
        w1b = sb("w1b%d" % kv, (128, 32, 256), BF16, st)
        w2p = sb("w2p%d" % kv, (128, 2, 2, 128), BF16, st)
        w2b = sb("w2b%d" % kv, (128, 2, 64), BF16, st)
        gel = sb("gel%d" % kv, (128, 2, 2, 256), BF16, st)
        p.memset("pool", gel[:], 0.0, W=[gel.r])
        p.memset("pool", w2p[:], 0.0, W=[w2p.r])
        for lq in range(4):
            for half in range(2):
                p.dma("sp", w1s[half * 64:(half + 1) * 64, :, :],
                      I[w1n][lq * 512:(lq + 1) * 512, :].rearrange("(l d) h -> d l h", d=64),
                      R=[I[w1n].r], W=[w1s.r])
            p.cp("act", w1b[:, lq * 8:(lq + 1) * 8, :], w1s[:], R=[w1s.r], W=[w1b.r])
        p.dma("sp", w2s[:], I[w2n][:, :].rearrange("(c p) d -> p c d", p=128), R=[I[w2n].r], W=[w2s.r])
        p.cp("dve", w2b[:], w2s[:], R=[w2s.r], W=[w2b.r])
        for gq in range(2):
            p.cp("dve", w2p[:, :, gq, gq * 64:(gq + 1) * 64], w2s[:], R=[w2s.r], W=[w2p.r])
        for half in range(2):
            p.dma("sp", pes[half * 64:(half + 1) * 64, :], I[pen][:, :], R=[I[pen].r], W=[pes.r])
        p.cp("dve", peb[:], pes[:], R=[pes.r], W=[peb.r])
        for gq in range(2):
            rows = slice(gq * 64, (gq + 1) * 64)
            for hc in range(2):
                bH, bC = nbank(), nbank()
                for l in range(32):
                    p.mm(bH[:, 0:255], w1b[rows, l, hc * 128:(hc + 1) * 128], srcT.t[rows, l:l + 16 * 254 + 1:16],
                         l == 0, l == 31, R=[w1b.r, srcT.r], W=[bH.r])
                for l in range(32):
                    p.mm(bC[:, 0:1], w1b[rows, l, hc * 128:(hc + 1) * 128], peb[rows, l:l + 1],
                         l == 0, l == 31, R=[w1b.r, peb.r], W=[bC.r])
                p.cp("dve", cst[:, 0:1], bC[:, 0:1], R=[bC.r], W=[cst.r])
                p.act(u[:, 0:255], bH[:, 0:255], AF.Identity, bias=cst[:, 0:1], R=[bH.r, cst.r], W=[u.r])
                p.tt("dve", t1[:, 0:255], u[:, 0:255], u[:, 0:255], ALU.mult, R=[u.r], W=[t1.r])
                p.ts("dve", t1[:, 0:255], t1[:, 0:255], 0.044715, 1.0, ALU.mult, ALU.add, R=[t1.r], W=[t1.r])
                p.tt("dve", t1[:, 0:255], t1[:, 0:255], u[:, 0:255], ALU.mult, R=[t1.r, u.r], W=[t1.r])
                p.act(t2[:, 0:255], t1[:, 0:255], AF.Tanh, scale=0.7978845608028654, R=[t1.r], W=[t2.r])
                p.stt(t2[:, 0:255], t2[:, 0:255], 1.0, u[:, 0:255], ALU.add, ALU.mult, R=[t2.r, u.r], W=[t2.r])
                p.ts("dve", gel[:, gq, hc, 0:255], t2[:, 0:255], 0.5, None, ALU.mult, R=[t2.r], W=[gel.r])
        if kv == 0:
            b = nbank()
            n = 0
            for gq in range(2):
                for hc in range(2):
                    p.mm(b[:, 0:256], w2p[:, hc, gq, :], gel[:, gq, hc, :], n == 0, n == 3, R=[w2p.r, gel.r], W=[b.r])
                    n += 1
            p.cp("dve", Rz["kcmpT"][:], b[:, 0:256], R=[b.r], W=[Rz["kcmpT"].r])
        else:
            for gq in range(2):
                for cc in range(2):
                    b = nbank()
                    for hc in range(2):
                        p.mm(b[:, 0:64], gel[:, gq, hc, cc * 128:(cc + 1) * 128], w2b[:, hc, :], hc == 0, hc == 1,
                             R=[gel.r, w2b.r], W=[b.r])
                    p.cp("dve", Rz["vcmpM"][:, cc, gq, 0:64], b[:, 0:64], R=[b.r], W=[Rz["vcmpM"].r])

    sq = sb("sq", (128, T), BF16, st)
    row = sb("kmrow", (1, T), F32, st)
    tmp = sb("kmtmp", (1, 8), F32, st)
    rl = sb("relrow", (1, 512), F32, st)
    p.dma("sp", rl[:], I["relflat"][:, :], R=[I["relflat"].r], W=[rl.r])
    p.act(rl[:], rl[:], AF.Abs, R=[rl.r], W=[rl.r])
    p.op("dve", lambda e: e.reduce_max(tmp[:, 0:1], rl[:], AX.X), R=[rl.r], W=[tmp.r])
    for which, srcs in enumerate([(Rz["ksT"], Rz["kwT"]), (Rz["ckvT"],)]):
        first = True
        for s_ in srcs:
            p.tt("dve", sq[:], s_[:], s_[:], ALU.mult, R=[s_.r], W=[sq.r])
            for kb in range(8):
                b = nbank()
                p.mm(b[0:1, :], Rz["ones_bf"][:, 0:1], sq[:, kb * 512:(kb + 1) * 512], True, True,
                     R=[sq.r, Rz["ones_bf"].r], W=[b.r])
                if first:
                    p.cp("dve", row[:, kb * 512:(kb + 1) * 512], b[0:1, :], R=[b.r], W=[row.r])
                else:
                    p.tt("dve", row[:, kb * 512:(kb + 1) * 512], row[:, kb * 512:(kb + 1) * 512], b[0:1, :], ALU.add,
                         R=[b.r, row.r], W=[row.r])
            first = False
        p.op("dve", lambda e: e.reduce_max(tmp[:, 1:2], row[:], AX.X), R=[row.r], W=[tmp.r])
        p.act(tmp[:, 2:3], tmp[:, 1:2], AF.Sqrt, R=[tmp.r], W=[tmp.r])
        p.ts("dve", Rz["kmax"][:, 2 * which:2 * which + 1], tmp[:, 2:3], -1.03, None, ALU.mult, R=[tmp.r], W=[Rz["kmax"].r])
        p.ts("dve", Rz["kmax"][:, 2 * which + 1:2 * which + 2], tmp[:, 0:1], -1.0, None, ALU.mult, R=[tmp.r], W=[Rz["kmax"].r])


def phase2(g):
    nc, p, sb, I, S, Rz = g.nc, g.p, g.sb, g.I, g.S, g.Rz
    st = ExitStack()
    B = g.banks
    ident, identf, ones = Rz["identb"], Rz["identf"], Rz["ones_bf"]
    S["oT_s"] = g.dscr("oT_s", (NT, 128, 8, 128), BF16)
    ntiles = (g.debug or {}).get("ntiles", NT)

    stg = sb("c_stg", (128, 1024), F32, st)
    relTa = sb("relTa", (128, 8, 2, 512), BF16, st)
    relTb = sb("relTb", (128, 8, 1024), BF16, st)
    relTw = sb("relTw", (128, 2, 512), BF16, st)
    eall = sb("eall", (128, 32, 128), BF16, st)
    onesN = sb("onesN", (128, 128), BF16, st)
    p.memset("pool", eall[:], 0.0, W=[eall.r])
    p.memset("pool", onesN[:], 0.0, W=[onesN.r])
    p.memset("pool", onesN[0:1, :], 1.0, W=[onesN.r])
    d30 = sb("d30", (128, 512), BF16, st)
    mcmp = sb("mcmp", (128, 8, 503), BF16, st)
    wext = sb("wext", (128, 128), F32, st)
    mcq = sb("mcq", (128, 128), F32, st)
    rowsA = [sb("rowsA%d" % i, (128, 512), BF16, st) for i in range(2)]
    rowsB = sb("rowsB", (128, 1024), BF16, st)
    for r_ in rowsA + [rowsB]:
        p.memset("pool", r_[:], 0.0, W=[r_.r])
    wuvP = sb("wuvP", (128, 8, 128), BF16, st)
    st0 = ExitStack()
    mc4 = sb("mc4", (128, 512), F32, st0)
    mw4 = sb("mw4", (128, 512), F32, st0)
    c31A = sb("c31A", (128, 2, 512), F32, st0)
    c31B = sb("c31B", (128, 1024), F32, st0)
    p.dma("sp", mc4[:], I["mc4"][:, :], R=[I["mc4"].r], W=[mc4.r])
    p.dma("sp", mw4[:], I["mw4"][:, :], R=[I["mw4"].r], W=[mw4.r])
    p.dma("sp", wext[:], I["wext"][:, :], R=[I["wext"].r], W=[wext.r])
    p.dma("sp", mcq[:], I["mcq"][:, :], R=[I["mcq"].r], W=[mcq.r])
    for gq in range(2):
        p.dma("sp", c31A[:, gq, :], I["c31a"][gq:gq + 1, :].partition_broadcast(128), R=[I["c31a"].r], W=[c31A.r])
    p.dma("sp", c31B[:], I["c31b"][0:1, :].partition_broadcast(128), R=[I["c31b"].r], W=[c31B.r])
    for d in range(8):
        for gq in range(2):
            p.dma("sp", stg[:, 0:512], I["relTa"][:, d, gq, :], R=[I["relTa"].r], W=[stg.r])
            p.tt("dve", stg[:, 0:512], stg[:, 0:512], c31A[:, gq, :], ALU.subtract, R=[stg.r, c31A.r], W=[stg.r])
            if d == 0:
                p.tt("dve", stg[:, 0:512], stg[:, 0:512], mc4[:], ALU.add, R=[stg.r, mc4.r], W=[stg.r])
            p.cp("dve", relTa[:, d, gq, :], stg[:, 0:512], R=[stg.r], W=[relTa.r])
            if d == 4:
                p.tt("dve", stg[:, 0:512], stg[:, 0:512], mw4[:], ALU.add, R=[stg.r, mw4.r], W=[stg.r])
                p.cp("dve", relTw[:, gq, :], stg[:, 0:512], R=[stg.r], W=[relTw.r])
        p.dma("sp", stg[:], I["relTb"][:, d, :], R=[I["relTb"].r], W=[stg.r])
        p.tt("dve", stg[:], stg[:], c31B[:], ALU.subtract, R=[stg.r, c31B.r], W=[stg.r])
        if d == 0:
            for hf in range(2):
                p.tt("dve", stg[:, hf * 512:(hf + 1) * 512], stg[:, hf * 512:(hf + 1) * 512], mc4[:], ALU.add,
                     R=[stg.r, mc4.r], W=[stg.r])
        p.cp("dve", relTb[:, d, :], stg[:], R=[stg.r], W=[relTb.r])
    for q4 in range(4):
        p.dma("sp", stg[0:64, :], I["eall"][:, q4 * 1024:(q4 + 1) * 1024], R=[I["eall"].r], W=[stg.r])
        p.cp("dve", eall[0:64, q4 * 8:(q4 + 1) * 8, :], stg[0:64, :].rearrange("p (a b) -> p a b", b=128), R=[stg.r], W=[eall.r])
    p.dma("sp", stg[:, 0:512], I["d30"][:, :], R=[I["d30"].r], W=[stg.r])
    p.cp("dve", d30[:], stg[:, 0:512], R=[stg.r], W=[d30.r])
    for h in range(8):
        p.dma("sp", stg[:, 0:503], I["cmpv"][:, h, :], R=[I["cmpv"].r], W=[stg.r])
        p.dma("sp", stg[:, 512:1015], I["cmpm"][:, :], R=[I["cmpm"].r], W=[stg.r])
        p.tt("dve", mcmp[:, h, :], stg[:, 0:503], stg[:, 512:1015], ALU.add, R=[stg.r], W=[mcmp.r])
    p.memset("dve", eall[64:65, :, :], 1.0, W=[eall.r])
    p.memset("pool", wuvP[:], 0.0, W=[wuvP.r])
    for h in range(8):
        p.dma("sp", stg[:, 0:64], I["w_uv"][h, :, :], R=[I["w_uv"].r], W=[stg.r])
        p.cp("dve", wuvP[:, h, (h % 2) * 64:(h % 2) * 64 + 64], stg[:, 0:64], R=[stg.r], W=[wuvP.r])

    p.flush()
    st0.close()
    qTz = [sb("qTz%d" % i, (128, 512), BF16, st) for i in range(2)]
    for q_ in qTz:
        p.memset("pool", q_[:], 0.0, W=[q_.r])
    qlT = sb("qlT", (128, 1024), BF16, st)
    qiT = sb("qiT", (64, 512), BF16, st)
    zidx = sb("zidx", (128, T), F32, st)
    selD = Buf(g.kcT.t, g.kcT.r)
    junk = Buf(g.vcT.t, g.vcT.r)
    rr = [sb("rr%d" % i, (128, 512), F32, st) for i in range(2)]
    sq = sb("sqq", (128, 1024), BF16, st)
    srow = Buf(stg.t[0:1, :], stg.r)
    scmp = sb("scmp", (128, 8, 256), F32, st)
    pn = sb("pn", (128, 8, 256), BF16, st)
    pnT = sb("pnT", (128, 16, 128), BF16, st)
    sm = sb("sm", (128, 16), F32, st)
    sm2 = sb("sm2", (128, 16), F32, st)
    imp = sb("imp", (128, 64), F32, st)
    sc1 = sb("sc1", (128, 64), F32, st)
    sc2 = sb("sc2", (128, 64), F32, st)
    m8 = sb("m8", (128, 16), F32, st)
    selb = sb("selb", (128, 64), BF16, st)
    selT4 = [sb("selT4%d" % i, (128, 512), BF16, st) for i in range(2)]
    for s_ in selT4:
        p.memset("pool", s_[:], 0.0, W=[s_.r])
    PT = [sb("PT%d" % i, (128, 512), BF16, st) for i in range(4)]
    ocmp = sb("ocmp", (128, 512), F32, st)
    oa32 = sb("oa32", (128, 512), F32, st)
    oab = sb("oab", (128, 512), BF16, st)
    coef = sb("coef", (128, 32), F32, st)
    oln = sb("oln", (128, 8, 128), BF16, st)
    olT = sb("olT", (128, 8, 128), BF16, st)
    oT = sb("oT", (128, 8, 128), BF16, st)
    bis = sb("bis", (128, 8), F32, st)
    p.memset("pool", pn[:], 0.0, W=[pn.r])
    p.memset("pool", scmp[:], 0.0, W=[scmp.r])

    def bfv(bank):
        return bank.t[:].bitcast(BF16)

    selDs = [selD, junk]
    pw = sb("pw", (128, BIS_ITERS + 1), F32, st)
    wct = sb("wct", (128, BIS_ITERS + 1), F32, st)
    for k in range(BIS_ITERS + 1):
        p.memset("pool", pw[:, k:k + 1], 2.0 ** -(k + 1), W=[pw.r])

    def stream_D(i):
        nk = 128 * (i + 1)
        sD = selDs[i % 2]
        p.dma("sp", qiT[:], S["qi_s"][i, :, :, :].rearrange("d h q -> d (h q)"), R=[S["qi_s"].r], W=[qiT.r])
        for kb in range((nk + 511) // 512):
            k0 = kb * 512
            kn = min(512, nk - k0)
            for hi in range(4):
                bk = B[hi % 2]
                r_ = rr[hi % 2]
                p.mm(bk[:, 0:kn], qiT[:, hi * 128:(hi + 1) * 128], Rz["kiT"][:, k0:k0 + kn], True, True,
                     R=[qiT.r, Rz["kiT"].r], W=[bk.r])
                p.act(r_[:, 0:kn], bk[:, 0:kn], AF.Relu, scale=Rz["wabs"][:, i, hi:hi + 1], R=[bk.r, Rz["wabs"].r], W=[r_.r])
                if hi == 0:
                    p.ts("dve", zidx[:, k0:k0 + kn], r_[:, 0:kn], Rz["wsgn"][:, i, 0:1], None, ALU.mult,
                         R=[r_.r, Rz["wsgn"].r], W=[zidx.r])
                else:
                    p.stt(zidx[:, k0:k0 + kn], r_[:, 0:kn], Rz["wsgn"][:, i, hi:hi + 1], zidx[:, k0:k0 + kn], ALU.mult, ALU.add,
                          R=[r_.r, Rz["wsgn"].r, zidx.r], W=[zidx.r])
            yield
        p.op("dve", lambda e: e.tensor_reduce(bis[:, 0:1], zidx[:, 0:nk], AX.X, ALU.min), R=[zidx.r], W=[bis.r])
        p.op("dve", lambda e: e.reduce_max(bis[:, 1:2], zidx[:, 0:nk], AX.X), R=[zidx.r], W=[bis.r])
        p.stt(bis[:, 2:3], bis[:, 1:2], 1.0, bis[:, 0:1], ALU.add, ALU.subtract, R=[bis.r], W=[bis.r])
        p.ts("dve", wct[:], pw[:], bis[:, 2:3], None, ALU.mult, R=[pw.r, bis.r], W=[wct.r])
        p.tt("dve", bis[:, 3:4], bis[:, 0:1], wct[:, 0:1], ALU.add, R=[bis.r, wct.r], W=[bis.r])
        p.tt("dve", zidx[:, nk - 128:nk], zidx[:, nk - 128:nk], mcq[:], ALU.add, R=[zidx.r, mcq.r], W=[zidx.r])
        yield
        for k in range(BIS_ITERS):
            p.ts("dve", sD[:, 0:nk], zidx[:, 0:nk], bis[:, 3:4], None, ALU.is_ge, ALU.add, R=[zidx.r, bis.r], W=[sD.r, bis.r],
                 accum=bis[:, 4:5])
            p.stt(bis[:, 5:6], bis[:, 4:5], 256.0, wct[:, k:k + 1], ALU.is_ge, ALU.mult, R=[bis.r, wct.r], W=[bis.r])
            p.ts("dve", bis[:, 3:4], bis[:, 3:4], wct[:, k + 1:k + 2], bis[:, 5:6], ALU.subtract, ALU.add, R=[bis.r, wct.r], W=[bis.r])
            yield
        p.tt("dve", bis[:, 6:7], bis[:, 3:4], wct[:, BIS_ITERS:BIS_ITERS + 1], ALU.subtract, R=[bis.r, wct.r], W=[bis.r])
        p.ts("dve", sD[:, 0:nk], zidx[:, 0:nk], bis[:, 6:7], 1.0, ALU.is_ge, ALU.subtract, R=[zidx.r, bis.r], W=[sD.r])
        yield

    def stream_P(i):
        sD = selDs[i % 2]
        for gq in range(2):
            p.dma("sp", qTz[gq][gq * 64:(gq + 1) * 64, :], S["qa_s"][i, gq, :, :, :].rearrange("d h q -> d (h q)"),
                  R=[S["qa_s"].r], W=[qTz[gq].r])
        p.dma("sp", qlT[:], S["ql_s"][i, :, :, :].rearrange("r h q -> r (h q)"), R=[S["ql_s"].r], W=[qlT.r])
        for gq in range(2):
            p.act(sq[:, 0:512], qTz[gq][:], AF.Square, R=[qTz[gq].r], W=[sq.r])
            p.mm(B[7][0:1, :], ones[:, 0:1], sq[:, 0:512], True, True, R=[ones.r, sq.r], W=[B[7].r])
            p.act(srow[:, 0:512], B[7][0:1, :], AF.Sqrt, R=[B[7].r], W=[srow.r])
            p.ts("dve", rowsA[gq][0:1, :], srow[:, 0:512], Rz["kmax"][0:1, 0:1], Rz["kmax"][0:1, 1:2], ALU.mult, ALU.add,
                 R=[srow.r, Rz["kmax"].r], W=[rowsA[gq].r])
            p.dma("sp", selT4[gq][64:65, :], rowsA[gq][0:1, :], R=[rowsA[gq].r], W=[selT4[gq].r])
        p.act(sq[:], qlT[:], AF.Square, R=[qlT.r], W=[sq.r])
        for hf in range(2):
            p.mm(B[7][0:1, :], ones[:, 0:1], sq[:, hf * 512:(hf + 1) * 512], True, True, R=[ones.r, sq.r], W=[B[7].r])
            p.act(srow[:, hf * 512:(hf + 1) * 512], B[7][0:1, :], AF.Sqrt, R=[B[7].r], W=[srow.r])
        p.ts("dve", rowsB[0:1, :], srow[:], Rz["kmax"][0:1, 2:3], Rz["kmax"][0:1, 3:4], ALU.mult, ALU.add,
             R=[srow.r, Rz["kmax"].r], W=[rowsB.r])
        yield
        off = 248 - 8 * i
        for h in range(8):
            gq, hh = h // 4, h % 4
            rows = slice(gq * 64, (gq + 1) * 64)
            bL = B[2 + h // 2]
            p.mm(bL[:, (h % 2) * 256:(h % 2) * 256 + 255], qTz[gq][rows, hh * 128:(hh + 1) * 128], Rz["kcmpT"][rows, 0:255],
                 h % 2 == 0, h % 2 == 1, R=[qTz[gq].r, Rz["kcmpT"].r], W=[bL.r])
        for b4 in range(4):
            bL = B[2 + b4]
            p.tt("dve", scmp[:, 2 * b4:2 * b4 + 2, 0:255], bL[:, :].rearrange("p (h c) -> p h c", c=256)[:, :, 0:255],
                 mcmp[:, 2 * b4:2 * b4 + 2, off:off + 255], ALU.add, R=[bL.r, mcmp.r], W=[scmp.r])
        yield
        p.op("dve", lambda e: e.reduce_max(sm[:, 0:8], scmp[:, :, 0:255], AX.X), R=[scmp.r], W=[sm.r])
        p.ts("dve", sm[:, 8:16], sm[:, 0:8], -1000.0, -1.0, ALU.max, ALU.mult, R=[sm.r], W=[sm.r])
        p.tt("dve", scmp[:, :, 0:255], scmp[:, :, 0:255], sm[:, 8:16].unsqueeze(2).to_broadcast([128, 8, 255]), ALU.add,
             R=[scmp.r, sm.r], W=[scmp.r])
        p.act(scmp[:, :, 0:255], scmp[:, :, 0:255], AF.Exp, R=[scmp.r], W=[scmp.r])
        yield
        p.op("dve", lambda e: e.reduce_sum(sm2[:, 0:8], scmp[:, :, 0:255], AX.X), R=[scmp.r], W=[sm2.r])
        p.ts("dve", sm2[:, 0:8], sm2[:, 0:8], 1e-30, None, ALU.max, R=[sm2.r], W=[sm2.r])
        p.op("dve", lambda e: e.reciprocal(sm2[:, 8:16], sm2[:, 0:8]), R=[sm2.r], W=[sm2.r])
        p.tt("dve", pn[:, :, 0:255], scmp[:, :, 0:255], sm2[:, 8:16].unsqueeze(2).to_broadcast([128, 8, 255]), ALU.mult,
             R=[scmp.r, sm2.r], W=[pn.r])
        yield
        for hf in range(2):
            bk = B[2 + hf]
            tb_ = bfv(bk)
            for j in range(8):
                h, cc = hf * 4 + j // 2, j % 2
                p.tr(tb_[:, j * 128:(j + 1) * 128], pn[:, h, cc * 128:(cc + 1) * 128], ident[:], R=[pn.r, ident.r], W=[bk.r])
            p.cp("act" if hf else "dve", pnT[:, hf * 8:(hf + 1) * 8, :], tb_[:, :].rearrange("p (c q) -> p c q", q=128), R=[bk.r], W=[pnT.r])
        yield
        for gq in range(2):
            accb = B[6 + gq]
            for hh in range(4):
                h = gq * 4 + hh
                for cc in range(2):
                    p.mm(accb[:, hh * 128:(hh + 1) * 128], pnT[:, 2 * h + cc, :], Rz["vcmpM"][:, cc, gq, :], hh == 0 and cc == 0, hh == 3 and cc == 1,
                         R=[pnT.r, Rz["vcmpM"].r], W=[accb.r])
            p.cp("act", ocmp[:, gq * 256:(gq + 1) * 256].rearrange("p (h d) -> p h d", d=64),
                 accb[:, :].rearrange("p (h j) -> p h j", j=128)[:, :, 0:64], R=[accb.r], W=[ocmp.r])
            p.op("dve", lambda e, accb=accb: e.reduce_sum(imp[:], accb[:, :].rearrange("p (h j) -> p j h", j=128)[:, 64:128, :], AX.X),
                 R=[accb.r], W=[imp.r])
            p.tt("dve", sc1[:], imp[:], wext[:, 63 - 2 * i:127 - 2 * i], ALU.add, R=[imp.r, wext.r], W=[sc1.r])
            p.ts("dve", sc1[:, 0:1], imp[:, 0:1], 1e4, None, ALU.add, R=[imp.r], W=[sc1.r])
            p.op("dve", lambda e: e.max(m8[:, 0:8], sc1[:]), R=[sc1.r], W=[m8.r])
            p.op("dve", lambda e: e.match_replace(sc2[:], m8[:, 0:8], sc1[:], -1e30), R=[sc1.r, m8.r], W=[sc2.r])
            p.op("dve", lambda e: e.max(m8[:, 8:16], sc2[:]), R=[sc2.r], W=[m8.r])
            p.ts("dve", m8[:, 15:16], m8[:, 15:16], -1e29, None, ALU.max, R=[m8.r], W=[m8.r])
            p.ts("dve", selb[:], sc1[:], m8[:, 15:16], 1.0, ALU.is_ge, ALU.subtract, R=[sc1.r, m8.r], W=[selb.r])
            tb2 = bfv(accb)
            p.tr(tb2[0:64, 0:128], selb[:], ident[:], R=[selb.r, ident.r], W=[accb.r])
            p.cp("dve", selT4[gq][0:64, :].rearrange("p (h q) -> p h q", q=128),
                 tb2[0:64, 0:128].unsqueeze(1).to_broadcast([64, 4, 128]), R=[accb.r], W=[selT4[gq].r])
            yield
        DEPTH = 4
        sbanks = [B[2], B[3], B[6], B[7]]
        units = []
        for gq in range(2):
            for br_ in range(2):
                kts = list(range(0, i + 1)) if br_ == 0 else list(range(max(0, i - 4), i + 1))
                for n, kt in enumerate(kts):
                    units.append((gq, br_, n, kt, len(kts)))

        def emit_S(ui):
            gq, br_, n, kt, nk_ = units[ui]
            bS = sbanks[ui % DEPTH]
            kT = Rz["ksT"] if br_ == 0 else Rz["kwT"]
            d = i - kt
            p.mm(bS[:, :], kT[:, kt * 128:(kt + 1) * 128], qTz[gq][:], True, False, R=[kT.r, qTz[gq].r], W=[bS.r])
            if br_ == 0:
                p.mm(bS[:, :], eall[:, kt, :], selT4[gq][:], False, d >= 8, R=[eall.r, selT4[gq].r], W=[bS.r])
            if d < 8:
                rel = relTw[:, gq, :] if (br_ == 1 and d == 4) else relTa[:, d, gq, :]
                p.mm(bS[:, :], ident[:], rel, False, br_ == 0, R=[ident.r, relTa.r, relTw.r], W=[bS.r])
            if br_ == 1:
                p.mm(bS[:, :], onesN[:], rowsA[gq][:], False, True, R=[onesN.r, rowsA[gq].r], W=[bS.r])

        def epilogue(gq, br_):
            accb = B[4 + br_]
            accv = accb[:, 0:260].rearrange("p (h e) -> p h e", e=65)
            p.ts("dve", coef[:, 0:4], accv[:, :, 64], 1e-30, None, ALU.max, R=[accb.r], W=[coef.r])
            p.op("dve", lambda e: e.reciprocal(coef[:, 4:8], coef[:, 0:4]), R=[coef.r], W=[coef.r])
            gv = Rz["gn"][:, i, gq * 12:(gq + 1) * 12].rearrange("p (h t) -> p h t", t=3)
            p.tt("dve", coef[:, 8:12], coef[:, 4:8], gv[:, :, 1 + br_], ALU.mult, R=[coef.r, Rz["gn"].r], W=[coef.r])
            for hh in range(4):
                h = gq * 4 + hh
                if br_ == 0:
                    p.ts("dve", oa32[:, h * 64:(h + 1) * 64], ocmp[:, h * 64:(h + 1) * 64], Rz["gn"][:, i, 3 * h:3 * h + 1], None, ALU.mult,
                         R=[ocmp.r, Rz["gn"].r], W=[oa32.r])
                    p.stt(oa32[:, h * 64:(h + 1) * 64], accv[:, hh, 0:64], coef[:, 8 + hh:9 + hh], oa32[:, h * 64:(h + 1) * 64], ALU.mult, ALU.add,
                          R=[accb.r, coef.r, oa32.r], W=[oa32.r])
                else:
                    p.stt(oab[:, h * 64:(h + 1) * 64], accv[:, hh, 0:64], coef[:, 8 + hh:9 + hh], oa32[:, h * 64:(h + 1) * 64], ALU.mult, ALU.add,
                          R=[accb.r, coef.r, oa32.r], W=[oab.r])

        for ui in range(min(DEPTH - 1, len(units))):
            emit_S(ui)
        for ui, (gq, br_, n, kt, nk_) in enumerate(units):
            bS = sbanks[ui % DEPTH]
            pt = PT[ui % DEPTH]
            accb = B[4 + br_]
            vA = Rz["vsA"] if br_ == 0 else Rz["vwA"]
            p.act(pt[:], bS[:, :], AF.Exp, R=[bS.r], W=[pt.r])
            if ui + DEPTH - 1 < len(units):
                emit_S(ui + DEPTH - 1)
            for hh in range(4):
                p.mm(accb[:, hh * 65:(hh + 1) * 65], pt[:, hh * 128:(hh + 1) * 128], vA[:, kt, gq, :],
                     n == 0 and hh == 0, n == nk_ - 1, R=[pt.r, vA.r], W=[accb.r])
            if n == nk_ - 1:
                epilogue(gq, br_)
            yield
        tb_ = bfv(B[7])
        for c in range(4):
            p.tr(tb_[:, c * 128:(c + 1) * 128], oab[:, c * 128:(c + 1) * 128], ident[:], R=[oab.r, ident.r], W=[B[7].r])
        p.cp("act", oT[:, 0:4, :], tb_[:, 0:512].rearrange("p (c q) -> p c q", q=128), R=[B[7].r], W=[oT.r])
        yield
        accD = [B[4], B[5], B[6]]
        hb = [(0, 0), (0, 1), (0, 2), (1, 0), (1, 1), (1, 2), (2, 0), (2, 1)]
        dunits = [(kt, hf) for kt in range(i + 1) for hf in range(2)]

        dbanks = [B[2], B[3], B[7]]

        def emit_SD(ui):
            kt, hf = dunits[ui]
            d = i - kt
            bS = dbanks[ui % 3]
            cols = slice(hf * 512, (hf + 1) * 512)
            p.mm(bS[:, :], Rz["ckvT"][:, kt * 128:(kt + 1) * 128], qlT[:, cols], True, False, R=[Rz["ckvT"].r, qlT.r], W=[bS.r])
            p.mm(bS[:, :], sD[:, kt * 128:(kt + 1) * 128], d30[:], False, False, R=[sD.r, d30.r], W=[bS.r])
            if d < 8:
                p.mm(bS[:, :], ident[:], relTb[:, d, cols], False, False, R=[ident.r, relTb.r], W=[bS.r])
            p.mm(bS[:, :], onesN[:], rowsB[:, cols], False, True, R=[onesN.r, rowsB.r], W=[bS.r])

        DD = 3
        for ui in range(min(DD - 1, len(dunits))):
            emit_SD(ui)
        for ui, (kt, hf) in enumerate(dunits):
            bS = dbanks[ui % DD]
            pt = PT[ui % DD]
            p.act(pt[:], bS[:, :], AF.Exp, R=[bS.r], W=[pt.r])
            if ui + DD - 1 < len(dunits):
                emit_SD(ui + DD - 1)
            for hh in range(4):
                h = hf * 4 + hh
                bk, sl = hb[h]
                p.mm(accD[bk][:, sl * 129:(sl + 1) * 129], pt[:, hh * 128:(hh + 1) * 128], Rz["ckvA"][:, kt, :],
                     kt == 0 and sl == 0, kt == i, R=[pt.r, Rz["ckvA"].r], W=[accD[bk].r])
            if hf == 1:
                yield
        for h in range(8):
            bk, sl = hb[h]
            p.ts("dve", coef[:, 16 + h:17 + h], accD[bk][:, sl * 129 + 128:sl * 129 + 129], 1e-30, None, ALU.max, R=[accD[bk].r], W=[coef.r])
        p.op("dve", lambda e: e.reciprocal(coef[:, 24:32], coef[:, 16:24]), R=[coef.r], W=[coef.r])
        for h in range(8):
            bk, sl = hb[h]
            p.ts("dve", oln[:, h, :], accD[bk][:, sl * 129:sl * 129 + 128], coef[:, 24 + h:25 + h], None, ALU.mult,
                 R=[accD[bk].r, coef.r], W=[oln.r])
        yield
        for hf in range(2):
            tb_ = bfv(B[7])
            for hh in range(4):
                p.tr(tb_[:, hh * 128:(hh + 1) * 128], oln[:, hf * 4 + hh, :], ident[:], R=[oln.r, ident.r], W=[B[7].r])
            p.cp("act", olT[:, hf * 4:(hf + 1) * 4, :], tb_[:, 0:512].rearrange("p (c q) -> p c q", q=128), R=[B[7].r], W=[olT.r])
        for c in range(4):
            for hl in range(2):
                p.mm(B[7][:, c * 128:(c + 1) * 128], wuvP[:, 2 * c + hl, :], olT[:, 2 * c + hl, :], c == 0 and hl == 0, c == 3 and hl == 1,
                     R=[wuvP.r, olT.r], W=[B[7].r])
        p.cp("act", oT[:, 4:8, :], B[7][:, :].rearrange("p (c q) -> p c q", q=128), R=[B[7].r], W=[oT.r])
        p.dma("pool", S["oT_s"][i, :, :, :], oT[:], R=[oT.r], W=[S["oT_s"].r])
        yield

    def len_P(i):
        return 1 + 6 + 2 * ((i + 1) + min(5, i + 1)) + 4 + 1 + (i + 1) + 2

    def len_D(i):
        return (128 * (i + 1) + 511) // 512 + 1 + BIS_ITERS + 1

    for _ in stream_D(0):
        pass
    for i in range(ntiles):
        gd = stream_D(i + 1) if i + 1 < ntiles else None
        ratio = (len_D(i + 1) / float(len_P(i))) if gd is not None else 0.0
        credit = 0.0
        for _ in stream_P(i):
            credit += ratio
            while gd is not None and credit >= 1.0:
                credit -= 1.0
                try:
                    next(gd)
                except StopIteration:
                    gd = None
        if gd is not None:
            for _ in gd:
                pass
    return st


def layer_norm_tile(p, r, rs, gbc, bbc, out, tmp, R_extra=()):
    p.tt("dve", rs[:, 2:3], rs[:, 0:1], rs[:, 1:2], ALU.add, R=[rs.r], W=[rs.r])
    p.ts("dve", rs[:, 3:4], rs[:, 2:3], -1.0 / D, None, ALU.mult, R=[rs.r], W=[rs.r])
    p.act(tmp[:], r[:], AF.Square, bias=rs[:, 3:4], accum=rs[:, 4:5], R=[r.r, rs.r], W=[tmp.r, rs.r])
    p.ts("dve", rs[:, 5:6], rs[:, 4:5], 1.0 / D, 1e-5, ALU.mult, ALU.add, R=[rs.r], W=[rs.r])
    p.act(rs[:, 6:7], rs[:, 5:6], AF.Sqrt, R=[rs.r], W=[rs.r])
    p.op("dve", lambda e: e.reciprocal(rs[:, 7:8], rs[:, 6:7]), R=[rs.r], W=[rs.r])
    p.ts("dve", tmp[:], r[:], rs[:, 3:4], rs[:, 7:8], ALU.add, ALU.mult, R=[r.r, rs.r], W=[tmp.r])
    p.tt("dve", tmp[:], tmp[:], gbc[:], ALU.mult, R=[tmp.r, gbc.r], W=[tmp.r])
    p.tt("dve", out[:], tmp[:], bbc[:], ALU.add, R=[tmp.r, bbc.r], W=[out.r])


def phase3(g):
    nc, p, sb, I, S, Rz = g.nc, g.p, g.sb, g.I, g.S, g.Rz
    st = ExitStack()
    B = g.banks
    stg = sb("w_stg", (128, 1024), F32, st)
    wba = sb("wba", (128, 4, 1024), BF16, st)
    wbb = sb("wbb", (128, 4, 1024), BF16, st)
    wout = sb("wout", (128, 8, 1024), BF16, st)
    for (dst, src, n) in [(wba, "w_ba", 4), (wbb, "w_bb", 4), (wout, "w_out", 8)]:
        for c in range(n):
            p.dma("sp", stg[:], I[src][c * 128:(c + 1) * 128, :], R=[I[src].r], W=[stg.r])
            p.cp("dve", dst[:, c, :], stg[:], R=[stg.r], W=[dst.r])
    bc = {}
    for nm in ("ln1_g", "ln1_b", "ln2_g", "ln2_b"):
        bc[nm] = sb("bc_" + nm, (128, 1024), F32, st)
        p.dma("sp", bc[nm][:], I[nm][0:1, :].partition_broadcast(128), R=[I[nm].r], W=[bc[nm].r])
    oT_l = [sb("oT2_%d" % j, (128, 8, 128), BF16, st) for j in range(2)]
    gT_l = [sb("gT_%d" % j, (128, 16, 128), F32, st) for j in range(2)]
    xt_l = [sb("xt_%d" % j, (128, 1024), F32, st) for j in range(2)]
    t1 = sb("t1", (128, 512), F32, st)
    t2 = sb("t2", (128, 512), F32, st)
    mT = sb("mT", (128, 8, 128), BF16, st)
    r = sb("r", (128, 1024), F32, st)
    h1 = sb("h1", (128, 1024), F32, st)
    tmp = sb("lntmp", (128, 1024), F32, st)
    rs = sb("rs", (128, 8), F32, st)
    h1T = sb("h1T", (128, 8, 128), F32, st)
    wr = sb("wr", (128, 8, 36), F32, st)
    brb = sb("brb", (128, 36), F32, st)
    lg = sb("lg", (128, 36), F32, st)
    rt = sb("rt", (128, 16), F32, st)
    rj = sb("rj", (128, 32), F32, st)
    og = sb("og", (128, 4), F32, st)
    esel = sb("esel", (128, 8), F32, st)
    m8r = sb("m8r", (128, 8), F32, st)
    oh = sb("oh", (128, 2, 8), F32, st)
    Ak = sb("Ak", (128, 2, 32), F32, st)
    Abf = sb("Abf", (128, 32), BF16, st)
    ustr = sb("ustr", (128, 128), BF16, st)
    posf = sb("posf", (128, 32), F32, st)
    base = sb("base", (128, 32), F32, st)
    iot = sb("iot", (128, 32), F32, st)
    tokid = sb("tokid", (128, 32), I32, st)
    tokrow = sb("tokrow", (128, 32, 16), I32, st)
    fill = sb("fill", (128, 2048), I32, st)
    p.dma("sp", wr[:], I["wr"][:, :].rearrange("(c p) n -> p c n", p=128), R=[I["wr"].r], W=[wr.r])
    p.dma("sp", brb[:], I["br"][0:1, :].partition_broadcast(128), R=[I["br"].r], W=[brb.r])
    p.dma("sp", stg[:, 0:128], I["ustrict"][:, :], R=[I["ustrict"].r], W=[stg.r])
    p.cp("dve", ustr[:], stg[:, 0:128], R=[stg.r], W=[ustr.r])
    p.dma("sp", iot[:], I["iota32"][:, :], R=[I["iota32"].r], W=[iot.r])
    p.dma("sp", tokid[:], I["tokid"][:, :], R=[I["tokid"].r], W=[tokid.r])
    p.cp("dve", tokrow[:], tokid[:].unsqueeze(2).to_broadcast([128, 32, 16]), R=[tokid.r], W=[tokrow.r])
    p.memset("dve", base[:], 0.0, W=[base.r])
    p.memset("pool", fill[:], 5000, W=[fill.r])
    p.dma("sp", S["slot"][:, :].rearrange("(p r) c -> p (r c)", p=128), fill[:], R=[fill.r], W=[S["slot"].r])
    nt3 = (g.debug or {}).get("ntiles", NT)

    def loads3(i):
        j = i % 2
        p.dma("sp", oT_l[j][:], S["oT_s"][i, :, :, :], R=[S["oT_s"].r], W=[oT_l[j].r])
        p.dma("sp", gT_l[j][:], S["g_s"][i, :, :, :], R=[S["g_s"].r], W=[gT_l[j].r])
        p.dma("sp", xt_l[j][:], I["x"][i * 128:(i + 1) * 128, :], R=[I["x"].r], W=[xt_l[j].r])

    loads3(0)
    for i in range(nt3):
        oT, gT, xt = oT_l[i % 2], gT_l[i % 2], xt_l[i % 2]
        if i + 1 < nt3:
            loads3(i + 1)
        for hf in range(2):
            for m4 in range(4):
                m = hf * 4 + m4
                for kc in range(4):
                    p.mm(B[0][:, m4 * 128:(m4 + 1) * 128], wba[:, kc, m * 128:(m + 1) * 128], oT[:, kc, :], m4 == 0 and kc == 0, kc == 3,
                         R=[wba.r, oT.r], W=[B[0].r])
                for kc in range(4):
                    p.mm(B[1][:, m4 * 128:(m4 + 1) * 128], wbb[:, kc, m * 128:(m + 1) * 128], oT[:, 4 + kc, :], m4 == 0 and kc == 0, kc == 3,
                         R=[wbb.r, oT.r], W=[B[1].r])
            p.tt("dve", t1[:], B[0][:, :], gT[:, hf * 4:(hf + 1) * 4, :].rearrange("p m q -> p (m q)"), ALU.mult, R=[B[0].r, gT.r], W=[t1.r])
            p.tt("dve", t2[:], B[1][:, :], gT[:, 8 + hf * 4:8 + (hf + 1) * 4, :].rearrange("p m q -> p (m q)"), ALU.mult, R=[B[1].r, gT.r], W=[t2.r])
            p.tt("dve", mT[:, hf * 4:(hf + 1) * 4, :].rearrange("p m q -> p (m q)"), t1[:], t2[:], ALU.add, R=[t1.r, t2.r], W=[mT.r])
        for hf in range(2):
            bk = B[2 + hf]
            for c in range(8):
                p.mm(bk[:, :], mT[:, c, :], wout[:, c, hf * 512:(hf + 1) * 512], c == 0, c == 7, R=[mT.r, wout.r], W=[bk.r])
            p.stt(r[:, hf * 512:(hf + 1) * 512], xt[:, hf * 512:(hf + 1) * 512], ALPHA, bk[:, :], ALU.mult, ALU.add,
                  R=[xt.r, bk.r], W=[r.r, rs.r], accum=rs[:, hf:hf + 1])
        layer_norm_tile(p, r, rs, bc["ln1_g"], bc["ln1_b"], h1, tmp)
        p.dma("pool", S["h1_s"][i * 128:(i + 1) * 128, :], h1[:], R=[h1.r], W=[S["h1_s"].r])
        for c in range(8):
            bk = B[4 + c // 4]
            p.tr(bk[:, (c % 4) * 128:(c % 4 + 1) * 128], h1[:, c * 128:(c + 1) * 128], Rz["identf"][:], R=[h1.r, Rz["identf"].r], W=[bk.r])
        for hf in range(2):
            p.cp("act", h1T[:, hf * 4:(hf + 1) * 4, :], B[4 + hf][:, :].rearrange("p (c q) -> p c q", q=128), R=[B[4 + hf].r], W=[h1T.r])
        for c in range(8):
            p.mm(B[6][:, 0:36], h1T[:, c, :], wr[:, c, :], c == 0, c == 7, R=[h1T.r, wr.r], W=[B[6].r])
        p.tt("dve", lg[:], B[6][:, 0:36], brb[:], ALU.add, R=[B[6].r, brb.r], W=[lg.r])
        p.op("dve", lambda e: e.reduce_max(rt[:, 0:1], lg[:, 0:4], AX.X), R=[lg.r], W=[rt.r])
        p.ts("dve", og[:], lg[:, 0:4], rt[:, 0:1], None, ALU.is_equal, R=[lg.r, rt.r], W=[og.r])
        p.ts("dve", rt[:, 1:2], rt[:, 0:1], -1.0, None, ALU.mult, R=[rt.r], W=[rt.r])
        p.act(rj[:, 0:4], lg[:, 0:4], AF.Exp, bias=rt[:, 1:2], accum=rt[:, 2:3], R=[lg.r, rt.r], W=[rj.r, rt.r])
        p.op("dve", lambda e: e.reciprocal(rt[:, 3:4], rt[:, 2:3]), R=[rt.r], W=[rt.r])
        p.ts("dve", esel[:], lg[:, 4:12], og[:, 0:1], None, ALU.mult, R=[lg.r, og.r], W=[esel.r])
        for gi in range(1, 4):
            p.stt(esel[:], lg[:, 4 + 8 * gi:12 + 8 * gi], og[:, gi:gi + 1], esel[:], ALU.mult, ALU.add, R=[lg.r, og.r, esel.r], W=[esel.r])
        p.op("dve", lambda e: e.max(m8r[:], esel[:]), R=[esel.r], W=[m8r.r])
        p.tt("dve", rt[:, 4:5], m8r[:, 1:2], m8r[:, 0:1], ALU.subtract, R=[m8r.r], W=[rt.r])
        p.act(rt[:, 5:6], rt[:, 4:5], AF.Exp, R=[rt.r], W=[rt.r])
        p.ts("dve", rt[:, 6:7], rt[:, 5:6], 1.0, None, ALU.add, R=[rt.r], W=[rt.r])
        p.op("dve", lambda e: e.reciprocal(rt[:, 7:8], rt[:, 6:7]), R=[rt.r], W=[rt.r])
        p.tt("dve", rt[:, 8:9], rt[:, 7:8], rt[:, 5:6], ALU.mult, R=[rt.r], W=[rt.r])
        p.tt("dve", g.wts[:, i, 0:1], rt[:, 7:8], rt[:, 3:4], ALU.mult, R=[rt.r], W=[g.wts.r])
        p.tt("dve", g.wts[:, i, 1:2], rt[:, 8:9], rt[:, 3:4], ALU.mult, R=[rt.r], W=[g.wts.r])
        for k in range(2):
            p.ts("dve", oh[:, k, :], esel[:], m8r[:, k:k + 1], None, ALU.is_equal, R=[esel.r, m8r.r], W=[oh.r])
            p.tt("dve", Ak[:, k, :].rearrange("p (a b) -> p a b", b=8), og[:].unsqueeze(2).to_broadcast([128, 4, 8]),
                 oh[:, k, :].unsqueeze(1).to_broadcast([128, 4, 8]), ALU.mult, R=[og.r, oh.r], W=[Ak.r])
        p.tt("dve", Abf[:], Ak[:, 0, :], Ak[:, 1, :], ALU.add, R=[Ak.r], W=[Abf.r])
        p.mm(B[7][:, 0:32], ustr[:], Abf[:], True, False, R=[ustr.r, Abf.r], W=[B[7].r])
        p.mm(B[7][:, 64:96], Rz["ones_bf"][:], Abf[:], False, True, R=[Rz["ones_bf"].r, Abf.r], W=[B[7].r])
        p.tt("dve", posf[:], B[7][:, 0:32], base[:], ALU.add, R=[B[7].r, base.r], W=[posf.r])
        p.tt("dve", base[:], B[7][:, 64:96], base[:], ALU.add, R=[B[7].r, base.r], W=[base.r])
        for k in range(2):
            p.stt(rj[:, 0:32], posf[:], 1.0, Ak[:, k, :], ALU.mult, ALU.mult, R=[posf.r, Ak.r], W=[rj.r, rt.r], accum=rt[:, 10 + k:11 + k])
            p.stt(rj[:, 0:32], iot[:], 1.0, Ak[:, k, :], ALU.mult, ALU.mult, R=[iot.r, Ak.r], W=[rj.r, rt.r], accum=rt[:, 12 + k:13 + k])
            p.ts("dve", rt[:, 14:15], rt[:, 10 + k:11 + k], float(CAP), 1e6, ALU.is_ge, ALU.mult, R=[rt.r], W=[rt.r])
            p.ts("dve", rt[:, 9:10], rt[:, 10 + k:11 + k], float(CAP), None, ALU.is_lt, R=[rt.r], W=[rt.r])
            p.tt("dve", g.wts[:, i, k:k + 1], g.wts[:, i, k:k + 1], rt[:, 9:10], ALU.mult, R=[rt.r, g.wts.r], W=[g.wts.r])
            p.stt(rt[:, 15:16], rt[:, 12 + k:13 + k], float(CAP), rt[:, 10 + k:11 + k], ALU.mult, ALU.add, R=[rt.r], W=[rt.r])
            p.tt("dve", rt[:, 15:16], rt[:, 15:16], rt[:, 14:15], ALU.add, R=[rt.r], W=[rt.r])
            p.cp("dve", g.dest[:, i, k:k + 1], rt[:, 15:16], R=[rt.r], W=[g.dest.r])
            p.dma("pool", None, None, R=[g.dest.r, tokrow.r], W=[S["slot"].r],
                  fn=lambda e, i=i, k=k: e.indirect_dma_start(
                      out=S["slot"][:, :], out_offset=bass.IndirectOffsetOnAxis(ap=g.dest[:, i, k:k + 1], axis=0),
                      in_=tokrow[:, i, :], in_offset=None, bounds_check=p.breg(e, 32 * CAP - 1), oob_is_err=False))
    return st


def phase4(g):
    nc, p, sb, I, S, Rz = g.nc, g.p, g.sb, g.I, g.S, g.Rz
    st = ExitStack()
    B = g.banks
    ident = Rz["identb"]
    nexp = (g.debug or {}).get("nexp", 32)
    sid = sb("sid", (128, 4, 16), I32, st)
    xg = [sb("xg%d" % i, (128, 1024), F32, st) for i in range(4)]
    xgb = [sb("xgb%d" % i, (128, 1024), BF16, st) for i in range(2)]
    xgT = sb("xgT", (128, 8, 512), BF16, st)
    hT = sb("hT", (128, 2, 512), BF16, st)
    sg = [sb("sg%d" % i, (128, 512), F32, st) for i in range(2)]
    yb = [sb("yb%d" % i, (128, 1024), F32, st) for i in range(2)]
    wgf = sb("wgf", (128, 8, 256), F32, st)
    wuf = sb("wuf", (128, 8, 256), F32, st)
    wdf = sb("wdf", (128, 2, 1024), F32, st)
    wg = [sb("wg%d" % i, (128, 8, 256), BF16, st) for i in range(2)]
    wu = [sb("wu%d" % i, (128, 8, 256), BF16, st) for i in range(2)]
    wd = [sb("wd%d" % i, (128, 2, 1024), BF16, st) for i in range(2)]
    for x_ in xg:
        p.memset("pool", x_[:], 0.0, W=[x_.r])
    ceng = ["dve", "act", "dve", "act"]
    xgT2 = [xgT, sb("xgTb", (128, 8, 512), BF16, st)]
    sid2 = [sid, sb("sidb", (128, 4, 16), I32, st)]

    def load_weights(e_):
        k = e_ % 2
        p.dma("sp", wgf[:], I["w_gate"][e_, :, :].rearrange("(p c) f -> p c f", c=8), R=[I["w_gate"].r], W=[wgf.r])
        p.dma("sp", wuf[:], I["w_up"][e_, :, :].rearrange("(p c) f -> p c f", c=8), R=[I["w_up"].r], W=[wuf.r])
        p.dma("sp", wdf[:], I["w_down"][e_, :, :].rearrange("(c p) n -> p c n", p=128), R=[I["w_down"].r], W=[wdf.r])
        p.cp("act", wg[k][:], wgf[:], R=[wgf.r], W=[wg[k].r])
        p.cp("dve", wu[k][:], wuf[:], R=[wuf.r], W=[wu[k].r])
        p.cp("act", wd[k][:], wdf[:], R=[wdf.r], W=[wd[k].r])

    def gather_tokens(e_):
        k = e_ % 2
        sd = sid2[k]
        p.dma("sp", sd[:], S["slot"][e_ * CAP:(e_ + 1) * CAP, :].rearrange("(s p) c -> p s c", p=128),
              R=[S["slot"].r], W=[sd.r])
        for s_ in range(4):
            p.dma("pool", None, None, R=[sd.r, S["h1_s"].r], W=[xg[s_].r],
                  fn=lambda e, s_=s_, sd=sd: e.indirect_dma_start(
                      out=xg[s_][:, :], out_offset=None, in_=S["h1_s"][:, :],
                      in_offset=bass.IndirectOffsetOnAxis(ap=sd[:, s_, 0:1], axis=0), bounds_check=p.breg(e, T - 1), oob_is_err=False))
            xb = xgb[s_ % 2]
            p.cp(ceng[s_], xb[:], xg[s_][:], R=[xg[s_].r], W=[xb.r])
            bk = B[6 + s_ % 2]
            tv = bk.t[:].bitcast(BF16)
            for c in range(8):
                p.tr(tv[:, c * 128:(c + 1) * 128], xb.t[:, c:1024:8], ident[:], R=[xb.r, ident.r], W=[bk.r])
            p.cp("act" if s_ % 2 else "dve", xgT2[k][:, :, s_ * 128:(s_ + 1) * 128], tv[:, :].rearrange("p (c q) -> p c q", q=128),
                 R=[bk.r], W=[xgT2[k].r])

    def compute(e_):
        k = e_ % 2
        xT_ = xgT2[k]
        for fc in range(2):
            bG, bU = B[2 * fc], B[2 * fc + 1]
            for c in range(8):
                p.mm(bG[:, :], wg[k][:, c, fc * 128:(fc + 1) * 128], xT_[:, c, :], c == 0, c == 7, R=[wg[k].r, xT_.r], W=[bG.r])
            for c in range(8):
                p.mm(bU[:, :], wu[k][:, c, fc * 128:(fc + 1) * 128], xT_[:, c, :], c == 0, c == 7, R=[wu[k].r, xT_.r], W=[bU.r])
            p.act(sg[fc][:], bG[:, :], AF.Silu, R=[bG.r], W=[sg[fc].r])
            p.tt("dve", hT[:, fc, :], sg[fc][:], bU[:, :], ALU.mult, R=[sg[fc].r, bU.r], W=[hT.r])
        for s_ in range(4):
            y_ = yb[s_ % 2]
            for hf in range(2):
                bY = B[4 + (2 * s_ + hf) % 2]
                for fc in range(2):
                    p.mm(bY[:, :], hT[:, fc, s_ * 128:(s_ + 1) * 128], wd[k][:, fc, hf * 512:(hf + 1) * 512], fc == 0, fc == 1,
                         R=[hT.r, wd[k].r], W=[bY.r])
                if hf == 0:
                    p.act(y_[:, 0:512], bY[:, :], AF.Copy, R=[bY.r], W=[y_.r])
                else:
                    p.cp("dve", y_[:, 512:1024], bY[:, :], R=[bY.r], W=[y_.r])
            p.dma("sp", S["y_s"][e_ * CAP + s_ * 128:e_ * CAP + (s_ + 1) * 128, :], y_[:], R=[y_.r], W=[S["y_s"].r])

    load_weights(0)
    gather_tokens(0)
    for e_ in range(nexp):
        if e_ + 1 < nexp:
            load_weights(e_ + 1)
            gather_tokens(e_ + 1)
        compute(e_)
    return st


def phase5(g):
    nc, p, sb, I, S, Rz = g.nc, g.p, g.sb, g.I, g.S, g.Rz
    st = ExitStack()
    bc = {}
    for nm in ("ln2_g", "ln2_b"):
        bc[nm] = sb("bc5_" + nm, (128, 1024), F32, st)
        p.dma("sp", bc[nm][:], I[nm][0:1, :].partition_broadcast(128), R=[I[nm].r], W=[bc[nm].r])
    y = [[sb("y%d_%d" % (k, j), (128, 1024), F32, st) for k in range(2)] for j in range(2)]
    h1 = [sb("h1_5%d" % j, (128, 1024), F32, st) for j in range(2)]
    r = sb("r5", (128, 1024), F32, st)
    tmp = sb("tmp5", (128, 1024), F32, st)
    ot = [sb("ot5%d" % j, (128, 1024), F32, st) for j in range(2)]
    rs = sb("rs5", (128, 8), F32, st)
    for j in range(2):
        for k in range(2):
            p.memset("dve", y[j][k][:], 0.0, W=[y[j][k].r])
    for i in range((g.debug or {}).get("ntiles", NT)):
        j = i % 2
        p.dma("sp", h1[j][:], S["h1_s"][i * 128:(i + 1) * 128, :], R=[S["h1_s"].r], W=[h1[j].r])
        for k in range(2):
            p.dma("pool", None, None, R=[g.dest.r, S["y_s"].r], W=[y[j][k].r],
                  fn=lambda e, i=i, k=k, j=j: e.indirect_dma_start(
                      out=y[j][k][:, :], out_offset=None, in_=S["y_s"][:, :],
                      in_offset=bass.IndirectOffsetOnAxis(ap=g.dest[:, i, k:k + 1], axis=0), bounds_check=p.breg(e, 32 * CAP - 1), oob_is_err=False))
        p.ts("dve", r[:], h1[j][:], ALPHA, None, ALU.mult, R=[h1[j].r], W=[r.r])
        p.stt(r[:], y[j][0][:], g.wts[:, i, 0:1], r[:], ALU.mult, ALU.add, R=[y[j][0].r, g.wts.r, r.r], W=[r.r])
        p.stt(r[:], y[j][1][:], g.wts[:, i, 1:2], r[:], ALU.mult, ALU.add, R=[y[j][1].r, g.wts.r, r.r], W=[r.r])
        p.op("dve", lambda e: e.reduce_sum(rs[:, 0:1], r[:], AX.X), R=[r.r], W=[rs.r])
        p.memset("dve", rs[:, 1:2], 0.0, W=[rs.r])
        layer_norm_tile(p, r, rs, bc["ln2_g"], bc["ln2_b"], ot[j], tmp)
        p.dma("sp", g.out[i * 128:(i + 1) * 128, :], ot[j][:], R=[ot[j].r], W=[g.out.r])
    return st


def host_inputs(inputs):
    f = lambda a: np.ascontiguousarray(np.asarray(a, dtype=np.float32))
    rel_bias = f(inputs["rel_bias"])
    shared = {
        "w_in": f(inputs["w_in"][0]),
        "pe_kT": f(inputs["cmp_pe_k"][0].T), "pe_vT": f(inputs["cmp_pe_v"][0].T),
        "cw1_k": f(inputs["cmp_w1_k"][0]), "cw2_k": f(inputs["cmp_w2_k"][0]),
        "cw1_v": f(inputs["cmp_w1_v"][0]), "cw2_v": f(inputs["cmp_w2_v"][0]),
        "ckv_g": f(inputs["ckv_norm_g"][0]).reshape(1, 128),
        "w_uk": f(inputs["w_uk"][0]), "w_uv": f(inputs["w_uv"][0]),
        "relflat": rel_bias.reshape(1, 512),
        "w_ba": f(inputs["w_branch_a"][0]), "w_bb": f(inputs["w_branch_b"][0]), "w_out": f(inputs["w_out"][0]),
        "ln1_g": f(inputs["ln1_g"][0]).reshape(1, D), "ln1_b": f(inputs["ln1_b"][0]).reshape(1, D),
        "wr": f(np.concatenate([np.asarray(inputs["w_grp"][0]), np.asarray(inputs["w_rtr"][0])], axis=1)),
        "br": f(np.concatenate([np.asarray(inputs["b_grp"][0]), np.asarray(inputs["b_rtr"][0])], axis=0)).reshape(1, 36),
        "w_gate": f(inputs["w_gate"][0]), "w_up": f(inputs["w_up"][0]), "w_down": f(inputs["w_down"][0]),
        "ln2_g": f(inputs["ln2_g"][0]).reshape(1, D), "ln2_b": f(inputs["ln2_b"][0]).reshape(1, D),
    }
    shared.update(_host_consts(rel_bias))
    x = np.asarray(inputs["x"], dtype=np.float32)
    maps = []
    for b in range(x.shape[0]):
        m = dict(shared)
        m["x"] = np.ascontiguousarray(x[b])
        m["xT"] = np.ascontiguousarray(x[b].T)
        maps.append(m)
    return maps


def kernel(**inputs):
    maps = host_inputs(inputs)
    nc, g = build()
    res = run_bass_kernel_spmd(nc, maps, core_ids=list(range(8)))
    return np.stack([np.asarray(r["out"], dtype=np.float32) for r in res.results], axis=0)
```

```python
import math
from contextlib import ExitStack
import numpy as np
import ml_dtypes
import concourse.bass as bass
import concourse.mybir as mybir
from concourse.bass_utils import run_bass_kernel_spmd

F32 = mybir.dt.float32
BF16 = mybir.dt.bfloat16
I32 = mybir.dt.int32
AF = mybir.ActivationFunctionType
ALU = mybir.AluOpType
AX = mybir.AxisListType

T = 4096
D = 1024
NT = T // 128
D_IN = 4316
ALPHA = 2.0 ** 0.25
CAP = 512
NEGM = 32768.0
BIS_ITERS = 20
DEBUG = None

O_QA, O_KC, O_VC, O_KS, O_VS, O_KW, O_VW, O_GN, O_QB, O_CKV, O_QI, O_KI, O_WI, O_GA, O_GB = (
    0, 512, 640, 768, 896, 1024, 1152, 1280, 1304, 1816, 1944, 2200, 2264, 2268, 3292)


class Res:
    __slots__ = ("name", "w", "r", "dsem", "dcount", "excl")

    def __init__(self, name):
        self.name = name
        self.excl = False
        self.w = None
        self.r = {}
        self.dsem = None
        self.dcount = 0


class Prog:
    ENG = ("pe", "act", "dve", "pool", "sp")

    def __init__(self, nc, stack):
        self.nc = nc
        self.stack = stack
        self.eobj = {"pe": nc.tensor, "act": nc.scalar, "dve": nc.vector, "pool": nc.gpsimd, "sp": nc.sync}
        self.sem = {e: stack.enter_context(nc.semaphore("c_" + e)) for e in self.ENG}
        self.cnt = {e: 0 for e in self.ENG}
        self.seen = {e: {} for e in self.ENG}
        self.ops = {e: [] for e in self.ENG}
        self.dres = []
        self.nres = 0

    def res(self, name=None):
        self.nres += 1
        return Res(name or "r%d" % self.nres)

    def _waits(self, eng, reads, writes):
        need = {}

        def add(ev, war=False):
            if ev is None:
                return
            sem, val, src = ev
            if src == eng and eng == "pe":
                return
            k = id(sem)
            if self.seen[eng].get(k, (None, 0))[1] >= val:
                return
            if k not in need or need[k][1] < val:
                need[k] = (sem, val)

        for r in reads:
            add(r.w)
        for w in writes:
            add(w.w)
            for ev in w.r.values():
                add(ev, True)
        out = []
        for k, (sem, val) in need.items():
            self.seen[eng][k] = (sem, val)
            out.append((sem, val))
        return out

    def op(self, eng, fn, R=(), W=()):
        W = list(W) + [r for r in R if r.excl]
        R = [r for r in R if not r.excl]
        waits = self._waits(eng, R, W)
        self.cnt[eng] += 1
        ev = (self.sem[eng], self.cnt[eng], eng)
        self.ops[eng].append((fn, waits, (self.sem[eng], 1)))
        for r in R:
            r.r[eng] = ev
        for w in W:
            w.w = ev
            w.r = {}

    def dma(self, q, out, in_, R=(), W=(), fn=None):
        waits = self._waits(q, R, W)
        tgt = W[0]
        if tgt.dsem is None:
            tgt.dsem = self.stack.enter_context(self.nc.semaphore("d_" + tgt.name))
            self.dres.append(tgt)
        tgt.dcount += 16
        ev = (tgt.dsem, tgt.dcount, None)
        if fn is None:
            fn = lambda e, o=out, i=in_: e.dma_start(out=o, in_=i)
        self.ops[q].append((fn, waits, (tgt.dsem, 16)))
        for r in R:
            r.r["dma%d" % id(tgt)] = ev
        for w in W:
            w.w = ev
            w.r = {}

    def flush(self, final=False):
        tail = {}
        for e in self.ENG:
            ws = []
            for e2 in self.ENG:
                if e2 != e and self.cnt[e2] > 0 and self.seen[e].get(id(self.sem[e2]), (None, 0))[1] < self.cnt[e2]:
                    ws.append((self.sem[e2], self.cnt[e2]))
                    self.seen[e][id(self.sem[e2])] = (self.sem[e2], self.cnt[e2])
            for r in self.dres:
                if self.seen[e].get(id(r.dsem), (None, 0))[1] < r.dcount:
                    ws.append((r.dsem, r.dcount))
                    self.seen[e][id(r.dsem)] = (r.dsem, r.dcount)
            tail[e] = ws
        ops = self.ops
        eobj = self.eobj
        self.regs = {}
        with self.nc.Block() as block:
            def run(e, engine):
                for fn, waits, inc in ops[e]:
                    for s, v in waits:
                        engine.wait_ge(s, v)
                    ins = fn(engine)
                    ins.then_inc(inc[0], inc[1])
                for s, v in tail[e]:
                    engine.wait_ge(s, v)

            @block.tensor
            def _(t):
                run("pe", t)

            @block.scalar
            def _(s):
                run("act", s)

            @block.vector
            def _(v):
                run("dve", v)

            @block.gpsimd
            def _(g):
                run("pool", g)

            @block.sync
            def _(sy):
                run("sp", sy)
        self.ops = {e: [] for e in self.ENG}

    def breg(self, e, val):
        if val not in self.regs:
            self.regs[val] = e.to_reg(val)
        return self.regs[val]

    def mm(self, out, lhsT, rhs, start, stop, R=(), W=()):
        self.op("pe", lambda e: e.matmul(out, lhsT, rhs, start=start, stop=stop, skip_group_check=True), R, W)

    def tr(self, out, in_, ident, R=(), W=()):
        self.op("pe", lambda e: e.transpose(out, in_, ident), R, W)

    def act(self, out, in_, func, R=(), W=(), bias=None, scale=None, accum=None, eng="act"):
        kw = {}
        if bias is not None:
            kw["bias"] = bias
        if scale is not None:
            kw["scale"] = scale
        if accum is not None:
            kw["accum_out"] = accum
        self.op("act", lambda e: e.activation(out, in_, func, **kw), R, W)

    def ts(self, eng, out, in0, s1, s2, op0, op1=None, R=(), W=(), accum=None):
        kw = {}
        if op1 is not None:
            kw["op1"] = op1
        if accum is not None:
            kw["accum_out"] = accum
        self.op(eng, lambda e: e.tensor_scalar(out, in0, s1, s2, op0, **kw), R, W)

    def tt(self, eng, out, in0, in1, op, R=(), W=()):
        self.op(eng, lambda e: e.tensor_tensor(out, in0, in1, op), R, W)

    def stt(self, out, in0, scalar, in1, op0, op1, R=(), W=(), accum=None):
        kw = {}
        if accum is not None:
            kw["accum_out"] = accum
        self.op("dve", lambda e: e.scalar_tensor_tensor(out, in0, scalar, in1, op0, op1, **kw), R, W)

    def cp(self, eng, out, in_, R=(), W=()):
        if eng == "act":
            self.op("act", lambda e: e.copy(out, in_), R, W)
        else:
            self.op(eng, lambda e: e.tensor_copy(out, in_), R, W)

    def memset(self, eng, ap, val, W=()):
        self.op(eng, lambda e: e.memset(ap, val), (), W)


class Buf:
    def __init__(self, t, res):
        self.t = t
        self.r = res

    def __getitem__(self, k):
        return self.t[k]


def _rel_bucket_np(dist):
    n = np.maximum(dist, 0)
    nf = np.maximum(n, 1).astype(np.float32)
    large = 16 + (np.log(nf / 16) / math.log(1024 / 16) * 16).astype(np.int32)
    return np.where(n < 16, n, np.minimum(large, 31))


def _host_consts(rel_bias):
    c = {}
    dd = np.arange(0, 1152)
    bk = _rel_bucket_np(dd)
    relvec = rel_bias[bk]
    p = np.arange(128)[:, None]
    j = np.arange(128)[None, :]
    relT = np.zeros((8, 128, 16, 128), np.float32)
    for di in range(8):
        dist = np.clip(128 * di + j - p, 0, 1151)
        relT[di] = relvec[dist].transpose(0, 2, 1)
    c["relTa"] = np.ascontiguousarray(relT[:, :, :8, :].transpose(1, 0, 2, 3)).reshape(128, 8, 2, 512)
    c["relTb"] = np.ascontiguousarray(relT[:, :, 8:, :].transpose(1, 0, 2, 3)).reshape(128, 8, 1024)
    c31 = relvec[1151]
    c["c31a"] = np.ascontiguousarray(np.repeat(c31[:8].reshape(2, 4, 1), 128, axis=2).reshape(2, 512))
    c["c31b"] = np.ascontiguousarray(np.repeat(c31[8:].reshape(8, 1), 128, axis=1).reshape(1, 1024))
    v = np.arange(503)[None, :]
    dist = p - 16 * v + 3937
    c["cmpv"] = np.ascontiguousarray(relvec[np.clip(dist, 0, 1151)][:, :, :8].transpose(0, 2, 1))
    c["cmpm"] = np.where(dist >= 0, 0.0, -30000.0).astype(np.float32)
    mc = np.where(j < p, -NEGM, 0.0).astype(np.float32)
    mw = np.where(j >= p, -NEGM, 0.0).astype(np.float32)
    c["mc4"] = np.tile(mc, (1, 4))
    c["mw4"] = np.tile(mw, (1, 4))
    c["ident"] = np.eye(128, dtype=np.float32)
    c["d30"] = np.tile(np.eye(128, dtype=np.float32) * NEGM, (1, 4))
    e_all = np.zeros((64, 32, 128), np.float32)
    for kt in range(32):
        e_all[2 * kt, kt, :64] = NEGM
        e_all[2 * kt + 1, kt, 64:] = NEGM
    c["eall"] = e_all.reshape(64, 32 * 128)
    c["mcq"] = np.where(j > p, -1e30, 0.0).astype(np.float32)
    cp_ = (np.arange(128) >= 64).astype(np.int64)[:, None]
    u = np.arange(128)[None, :] - 63
    c["wext"] = np.where((u == cp_) | (u == cp_ - 1), 1e4, np.where(u > cp_, -1e30, 0.0)).astype(np.float32)
    cs = 16 * np.arange(255)[:, None]
    ss = 64 * np.arange(64)[None, :]
    ov = np.minimum(cs + 32, ss + 64) - np.maximum(cs, ss)
    cm = np.zeros((256, 64), np.float32)
    cm[:255] = np.clip(ov, 0, None).astype(np.float32) / 16
    c["cmap"] = cm
    c["ustrict"] = (np.arange(128)[:, None] < np.arange(128)[None, :]).astype(np.float32)
    c["iota32"] = np.tile(np.arange(32, dtype=np.float32)[None, :], (128, 1))
    c["tokid"] = (np.arange(128)[:, None] + 128 * np.arange(32)[None, :]).astype(np.int32)
    return c


class Ctx:
    pass


def build(debug=None):
    nc = bass.Bass("TRN2", target_bir_lowering=False)
    es = ExitStack()
    p = Prog(nc, es)
    g = Ctx()
    g.nc, g.p, g.es, g.debug = nc, p, es, debug
    g.dbg_out = {}

    def din(name, shape, dt=F32):
        return Buf(nc.dram_tensor(name, list(shape), dt, kind="ExternalInput").ap(), p.res(name))

    def dscr(name, shape, dt=F32):
        kind = "ExternalOutput" if (debug and name in debug) else "Internal"
        return Buf(nc.dram_tensor(name, list(shape), dt, kind=kind).ap(), p.res(name))

    g.din, g.dscr = din, dscr

    def sb(name, shape, dt=F32, stack=None):
        t = (stack or es).enter_context(nc.sbuf_tensor("s_" + name, list(shape), dt))
        return Buf(t, p.res(name))

    def ps(name, shape, dt=F32, stack=None):
        t = (stack or es).enter_context(nc.psum_tensor(name, list(shape), dt))
        r = p.res(name)
        r.excl = True
        return Buf(t, r)

    g.sb, g.ps = sb, ps

    I = g.I = {}
    for name, shape in [
        ("xT", (D, T)), ("x", (T, D)), ("w_in", (D, D_IN)),
        ("pe_kT", (64, 32)), ("pe_vT", (64, 32)),
        ("cw1_k", (2048, 256)), ("cw2_k", (256, 64)), ("cw1_v", (2048, 256)), ("cw2_v", (256, 64)),
        ("ckv_g", (1, 128)), ("w_uk", (8, 64, 128)), ("w_uv", (8, 128, 64)), ("relflat", (1, 512)),
        ("w_ba", (512, D)), ("w_bb", (512, D)), ("w_out", (D, D)),
        ("ln1_g", (1, D)), ("ln1_b", (1, D)), ("wr", (D, 36)), ("br", (1, 36)),
        ("w_gate", (32, D, 256)), ("w_up", (32, D, 256)), ("w_down", (32, 256, D)),
        ("ln2_g", (1, D)), ("ln2_b", (1, D)),
        ("relTa", (128, 8, 2, 512)), ("relTb", (128, 8, 1024)), ("c31a", (2, 512)), ("c31b", (1, 1024)),
        ("cmpv", (128, 8, 503)), ("cmpm", (128, 503)), ("mc4", (128, 512)), ("mw4", (128, 512)),
        ("ident", (128, 128)), ("d30", (128, 512)), ("eall", (64, 4096)), ("mcq", (128, 128)),
        ("wext", (128, 128)), ("cmap", (256, 64)), ("ustrict", (128, 128)), ("iota32", (128, 32)),
    ]:
        I[name] = din(name, shape)
    I["tokid"] = din("tokid", (128, 32), I32)
    g.out = Buf(nc.dram_tensor("out", [T, D], F32, kind="ExternalOutput").ap(), p.res("out"))

    S = g.S = {}
    S["qa_s"] = dscr("qa_s", (NT, 2, 64, 4, 128), BF16)
    S["ql_s"] = dscr("ql_s", (NT, 128, 8, 128), BF16)
    S["qi_s"] = dscr("qi_s", (NT, 64, 4, 128), BF16)
    S["g_s"] = dscr("g_s", (NT, 128, 16, 128), F32)
    S["h1_s"] = dscr("h1_s", (T, D), F32)
    S["slot"] = dscr("slot", (32 * CAP, 16), I32)
    S["y_s"] = dscr("y_s", (32 * CAP, D), F32)

    Rz = g.Rz = {}
    Rz["ksT"] = sb("ksT", (128, T), BF16)
    Rz["kwT"] = sb("kwT", (128, T), BF16)
    Rz["ckvT"] = sb("ckvT", (128, T), BF16)
    Rz["kiT"] = sb("kiT", (64, T), BF16)
    Rz["vsA"] = sb("vsA", (128, NT, 2, 65), BF16)
    Rz["vwA"] = sb("vwA", (128, NT, 2, 65), BF16)
    Rz["ckvA"] = sb("ckvA", (128, NT, 129), BF16)
    Rz["gn"] = sb("gn", (128, NT, 24), F32)
    Rz["wabs"] = sb("wabs", (128, NT, 4), F32)
    Rz["wsgn"] = sb("wsgn", (128, NT, 4), F32)
    Rz["kcmpT"] = sb("kcmpT", (128, 256), BF16)
    Rz["vcmpM"] = sb("vcmpM", (128, 2, 2, 128), BF16)
    Rz["kmax"] = sb("kmax", (1, 4), F32)
    Rz["identb"] = sb("identb", (128, 128), BF16)
    Rz["identf"] = sb("identf", (128, 128), F32)
    Rz["ones_bf"] = sb("ones_bf", (128, 128), BF16)

    g.kcT = sb("kcT", (128, T), BF16)
    g.vcT = sb("vcT", (128, T), BF16)
    g.dest = sb("dest_i", (128, NT, 2), I32)
    g.wts = sb("wts", (128, NT, 2), F32)
    g.banks = [ps("bank%d" % i, (128, 512), F32) for i in range(8)]

    stage = (debug or {}).get("stage", 99)
    st = phase1(g)
    if debug:
        dump_resident(g)
    p.flush()
    st.close()
    if stage >= 2:
        st = phase2(g)
        p.flush()
        st.close()
    if stage >= 3:
        st = phase3(g)
        p.flush()
        st.close()
    if stage >= 4:
        st = phase4(g)
        p.flush()
        st.close()
    if stage >= 5:
        st = phase5(g)
        p.flush()
        st.close()
    return nc, g


def dump_resident(g):
    nc, p = g.nc, g.p
    for name, buf in g.Rz.items():
        shape = list(buf.t.shape)
        d = nc.dram_tensor("dbg_" + name, shape, buf.t.dtype, kind="ExternalOutput").ap()
        r = p.res("dbg_" + name)
        idx = tuple(slice(None) for _ in shape)
        p.dma("sp", d[idx], buf.t[idx], R=[buf.r], W=[r])


def phase1(g):
    nc, p, sb, I, S, Rz = g.nc, g.p, g.sb, g.I, g.S, g.Rz
    st = ExitStack()
    banks = g.banks
    bi = [0]

    def nbank():
        b = banks[bi[0] % 8]
        bi[0] += 1
        return b

    ev_rr = [0]

    def ev_eng():
        ev_rr[0] += 1
        return "act" if ev_rr[0] % 2 else "dve"

    stf = sb("p1_cst", (128, 128), F32, st)
    p.dma("sp", stf[:], I["ident"][:, :], R=[I["ident"].r], W=[stf.r])
    p.cp("dve", Rz["identb"][:], stf[:], R=[stf.r], W=[Rz["identb"].r])
    p.cp("dve", Rz["identf"][:], stf[:], R=[stf.r], W=[Rz["identf"].r])
    p.memset("dve", Rz["ones_bf"][:], 1.0, W=[Rz["ones_bf"].r])
    p.memset("pool", Rz["vsA"][:, :, :, 64:65], 1.0, W=[Rz["vsA"].r])
    p.memset("pool", Rz["vwA"][:, :, :, 64:65], 1.0, W=[Rz["vwA"].r])
    p.memset("pool", Rz["ckvA"][:, :, 128:129], 1.0, W=[Rz["ckvA"].r])

    xTb = sb("xTb", (128, 8, T), BF16, st)
    xst = [sb("xst%d" % i, (128, 1024), F32, st) for i in range(2)]
    xq = [p.res("xTq%d" % q) for q in range(4)]
    engs = ["dve", "act", "dve", "act"]
    xk = [0]

    def load_x(q):
        for c in range(8):
            s_ = xst[xk[0] % 2]
            xk[0] += 1
            p.dma("sp", s_[:], I["xT"][c * 128:(c + 1) * 128, q * 1024:(q + 1) * 1024], R=[I["xT"].r], W=[s_.r])
            p.cp(engs[c % 4], xTb[:, c, q * 1024:(q + 1) * 1024], s_[:], R=[s_.r], W=[xq[q]])

    cut = 99
    wst = [sb("wst%d" % i, (128, 8, 128), F32, st) for i in range(2)]
    wbf = [sb("wbf%d" % i, (128, 8, 128), BF16, st) for i in range(2)]
    wk = [0]

    def load_w_issue(col0, M):
        k = wk[0] % 2
        wk[0] += 1
        p.dma("sp", wst[k][:, :, 0:M], I["w_in"][:, col0:col0 + M].rearrange("(c p) m -> p c m", p=128),
              R=[I["w_in"].r], W=[wst[k].r])
        return k

    def load_w_cast(k, M):
        p.cp("act" if k else "dve", wbf[k][:, :, 0:M], wst[k][:, :, 0:M], R=[wst[k].r], W=[wbf[k].r])
        return wbf[k]

    groups = []

    def fm_group(col0, M, evac):
        groups.append((col0, M, evac))

    def run_groups():
        nxt = load_w_cast(load_w_issue(groups[0][0], groups[0][1]), groups[0][1])
        load_x(0)
        for gi, (col0, M, evac) in enumerate(groups):
            w = nxt
            kn = None
            if gi + 1 < len(groups):
                kn = load_w_issue(groups[gi + 1][0], groups[gi + 1][1])
            for tb in range(8):
                if gi == 0 and tb % 2 == 0 and tb < 6:
                    load_x(tb // 2 + 1)
                b = nbank()
                for c in range(8):
                    p.mm(b[0:M, :], w[:, c, 0:M], xTb[:, c, tb * 512:(tb + 1) * 512], c == 0, c == 7,
                         R=[w.r, xq[tb // 2]], W=[b.r])
                evac(tb, b)
            if kn is not None:
                nxt = load_w_cast(kn, groups[gi + 1][1])

    stg_bf = [sb("stgb%d" % i, (128, 512), BF16, st) for i in range(4)]
    stg_f = [sb("stgf%d" % i, (128, 512), F32, st) for i in range(2)] * 2
    sk = [0]

    def nstg(lst):
        sk[0] += 1
        return lst[sk[0] % 4]

    def evac_copy(dst_buf, scale=None):
        def f(tb, b):
            M = dst_buf.t.shape[0]
            e = ev_eng()
            o = dst_buf[:, tb * 512:(tb + 1) * 512]
            if e == "act":
                p.act(o, b[0:M, :], AF.Copy, R=[b.r], W=[dst_buf.r])
            else:
                p.cp("dve", o, b[0:M, :], R=[b.r], W=[dst_buf.r])
        return f

    for m in range(4):
        def ev(tb, b, m=m):
            s_ = nstg(stg_bf)
            p.act(s_[:], b[:], AF.Copy, scale=0.125, R=[b.r], W=[s_.r])
            gq, hh0 = m // 2, 2 * (m % 2)
            for hl in range(2):
                dst = S["qa_s"][tb * 4:(tb + 1) * 4, gq, :, hh0 + hl, :].rearrange("t d q -> d t q")
                src = s_[hl * 64:(hl + 1) * 64, :].rearrange("d (t q) -> d t q", q=128)
                p.dma("sp", dst, src, R=[s_.r], W=[S["qa_s"].r])
        fm_group(O_QA + 128 * m, 128, ev)

    kcT, vcT = g.kcT, g.vcT
    fm_group(O_KC, 128, evac_copy(kcT))
    fm_group(O_VC, 128, evac_copy(vcT))
    fm_group(O_KS, 128, evac_copy(Rz["ksT"]))
    fm_group(O_KW, 128, evac_copy(Rz["kwT"]))
    fm_group(O_KI, 64, evac_copy(Rz["kiT"]))

    wukf = sb("wukf", (128, 4, 128), F32, st)
    wukb = sb("wukb", (128, 4, 128), BF16, st)
    p.dma("sp", wukf[:], I["w_uk"][:, :, :].rearrange("(m hl) d r -> (hl d) m r", hl=2), R=[I["w_uk"].r], W=[wukf.r])
    p.cp("dve", wukb[:], wukf[:], R=[wukf.r], W=[wukb.r])
    for m in range(4):
        def ev(tb, b, m=m):
            s_ = nstg(stg_bf)
            p.cp("dve", s_[:], b[:], R=[b.r], W=[s_.r])
            for hl in range(2):
                b2 = nbank()
                p.mm(b2[:, :], wukb[hl * 64:(hl + 1) * 64, m, :], s_[hl * 64:(hl + 1) * 64, :], True, True,
                     R=[wukb.r, s_.r], W=[b2.r])
                s2 = nstg(stg_bf)
                p.act(s2[:], b2[:], AF.Copy, scale=0.125, R=[b2.r], W=[s2.r])
                dst = S["ql_s"][tb * 4:(tb + 1) * 4, :, 2 * m + hl, :].rearrange("t r q -> r t q")
                p.dma("sp", dst, s2[:].rearrange("r (t q) -> r t q", q=128), R=[s2.r], W=[S["ql_s"].r])
        fm_group(O_QB + 128 * m, 128, ev)

    for m in range(2):
        def ev(tb, b, m=m):
            s_ = nstg(stg_bf)
            p.act(s_[:], b[:], AF.Copy, scale=0.125, R=[b.r], W=[s_.r])
            for hl in range(2):
                dst = S["qi_s"][tb * 4:(tb + 1) * 4, :, 2 * m + hl, :].rearrange("t d q -> d t q")
                src = s_[hl * 64:(hl + 1) * 64, :].rearrange("d (t q) -> d t q", q=128)
                p.dma("sp", dst, src, R=[s_.r], W=[S["qi_s"].r])
        fm_group(O_QI + 128 * m, 128, ev)

    for m in range(16):
        def ev(tb, b, m=m):
            s_ = nstg(stg_f)
            p.act(s_[:], b[:], AF.Sigmoid, R=[b.r], W=[s_.r])
            dst = S["g_s"][tb * 4:(tb + 1) * 4, :, m, :].rearrange("t c q -> c t q")
            p.dma("sp", dst, s_[:].rearrange("c (t q) -> c t q", q=128), R=[s_.r], W=[S["g_s"].r])
        fm_group(O_GA + 128 * m, 128, ev)

    run_groups()
    wtf = sb("wtf", (128, 8, 412), F32, st)
    wtb = sb("wtb", (128, 8, 412), BF16, st)
    for (c0, n, o) in [(O_VS, 128, 0), (O_VW, 128, 128), (O_CKV, 128, 256), (O_GN, 24, 384), (O_WI, 4, 408)]:
        r_ = p.res("wtf%d" % o)
        p.dma("sp", wtf[:, :, o:o + n], I["w_in"][:, c0:c0 + n].rearrange("(c p) m -> p c m", p=128),
              R=[I["w_in"].r], W=[r_])
        p.cp("dve", wtb[:, :, o:o + n], wtf[:, :, o:o + n], R=[r_], W=[wtb.r])
    gbc = sb("gbc", (128, 128), F32, st)
    p.dma("sp", gbc[:], I["ckv_g"][0:1, :].partition_broadcast(128), R=[I["ckv_g"].r], W=[gbc.r])
    junk = sb("p1junk", (128, 128), F32, st)
    ssq = sb("p1ssq", (128, 4), F32, st)
    sub = (g.debug or {}).get("sub", 99)
    for tt in range(NT if sub >= 1 else 0):
        b = nbank()
        for c in range(8):
            p.mm(b[:, 0:412], xTb[:, c, tt * 128:(tt + 1) * 128], wtb[:, c, :], c == 0, c == 7,
                 R=[wtb.r, xq[tt // 8]], W=[b.r])
        p.cp("dve", Rz["vsA"][:, tt, :, 0:64], b[:, 0:128].rearrange("p (g d) -> p g d", g=2), R=[b.r], W=[Rz["vsA"].r])
        p.cp("dve", Rz["vwA"][:, tt, :, 0:64], b[:, 128:256].rearrange("p (g d) -> p g d", g=2), R=[b.r], W=[Rz["vwA"].r])
        if sub <= 1:
            continue
        p.act(junk[:], b[:, 256:384], AF.Square, R=[b.r], W=[junk.r, ssq.r], accum=ssq[:, 0:1])
        sub2 = (g.debug or {}).get("sub2", 99)
        if sub2 <= 0:
            continue
        p.ts("dve", ssq[:, 1:2], ssq[:, 0:1], 1.0 / 128, 1e-6, ALU.mult, ALU.add, R=[ssq.r], W=[ssq.r])
        if sub2 <= 1:
            continue
        p.act(ssq[:, 2:3], ssq[:, 1:2], AF.Sqrt, R=[ssq.r], W=[ssq.r])
        if sub2 <= 2:
            continue
        p.op("dve", lambda e: e.reciprocal(ssq[:, 3:4], ssq[:, 2:3]), R=[ssq.r], W=[ssq.r])
        if sub2 <= 3:
            continue
        p.stt(Rz["ckvA"][:, tt, 0:128], b[:, 256:384], ssq[:, 3:4], gbc[:], ALU.mult, ALU.mult,
              R=[b.r, ssq.r, gbc.r], W=[Rz["ckvA"].r])
        if sub <= 2:
            continue
        p.act(Rz["gn"][:, tt, :], b[:, 384:408], AF.Sigmoid, R=[b.r], W=[Rz["gn"].r])
        p.act(Rz["wabs"][:, tt, :], b[:, 408:412], AF.Abs, scale=0.5, R=[b.r], W=[Rz["wabs"].r])
        p.act(Rz["wsgn"][:, tt, :], b[:, 408:412], AF.Sign, R=[b.r], W=[Rz["wsgn"].r])
        if sub <= 3:
            continue
        b2 = nbank()
        tv = b2.t[:].bitcast(BF16)
        p.tr(tv[:, 0:128], Rz["ckvA"][:, tt, 0:128], Rz["identb"][:], R=[Rz["ckvA"].r, Rz["identb"].r], W=[b2.r])
        p.cp("act", Rz["ckvT"][:, tt * 128:(tt + 1) * 128], tv[:, 0:128], R=[b2.r], W=[Rz["ckvT"].r])

    if cut <= 6:
        return st
    p.flush()
    st.close()
    st = ExitStack()
    phase1b(g, st, kcT, vcT, nbank)
    return st


def phase1b(g, st, kcT, vcT, nbank):
    nc, p, sb, I, S, Rz = g.nc, g.p, g.sb, g.I, g.S, g.Rz
    w1s = sb("w1s", (128, 8, 256), F32, st)
    w2s = sb("w2s", (128, 2, 64), F32, st)
    pes = sb("pes", (128, 32), F32, st)
    peb = sb("peb", (128, 32), BF16, st)
    cst = sb("cst", (128, 2), F32, st)
    u = sb("cu", (128, 256), F32, st)
    t1 = sb("ct1", (128, 256), F32, st)
    t2 = sb("ct2", (128, 256), F32, st)
    cms = sb("cms", (128, 2, 64), F32, st)
    p.dma("sp", cms[:], I["cmap"][:, :].rearrange("(c p) n -> p c n", p=128), R=[I["cmap"].r], W=[cms.r])
    for gq in range(2):
        p.cp("dve", Rz["vcmpM"][:, :, gq, 64:128], cms[:], R=[cms.r], W=[Rz["vcmpM"].r])
    for kv, (srcT, w1n, w2n, pen) in enumerate([(kcT, "cw1_k", "cw2_k", "pe_kT"), (vcT, "cw1_v", "cw2_v", "pe_vT")]):
        w1b = sb("w1b%d" % kv, (128, 32, 256), BF16, st)
        w2p = sb("w2p%d" % kv, (128, 2, 2, 128), BF16, st)
        w2b = sb("w2b%d" % kv, (128, 2, 64), BF16, st)
        gel = sb("gel%d" % kv, (128, 2, 2, 256), BF16, st)
        p.memset("pool", gel[:], 0.0, W=[gel.r])
        p.memset("pool", w2p[:], 0.0, W=[w2p.r])
        for lq in range(4):
            for half in range(2):
                p.dma("sp", w1s[half * 64:(half + 1) * 64, :, :],
                      I[w1n][lq * 512:(lq + 1) * 512, :].rearrange("(l d) h -> d l h", d=64),
                      R=[I[w1n].r], W=[w1s.r])
            p.cp("act", w1b[:, lq * 8:(lq + 1) * 8, :], w1s[:], R=[w1s.r], W=[w1b.r])
        p.dma("sp", w2s[:], I[w2n][:, :].rearrange("(c p) d -> p c d", p=128), R=[I[w2n].r], W=[w2s.r])
        p.cp("dve", w2b[:], w2s[:], R=[w2s.r], W=[w2b.r])
        for gq in range(2):
            p.cp("dve", w2p[:, :, gq, gq * 64:(gq + 1) * 64], w2s[:], R=[w2s.r], W=[w2p.r])
        for half in range(2):
            p.dma("sp", pes[half * 64:(half + 1) * 64, :], I[pen][:, :], R=[I[pen].r], W=[pes.r])
        p.cp("dve", peb[:], pes[:], R=[pes.r], W=[peb.r])
        for gq in range(2):
            rows = slice(gq * 64, (gq + 1) * 64)
            for hc in range(2):
                bH, bC = nbank(), nbank()
                for l in range(32):
                    p.mm(bH[:, 0:255], w1b[rows, l, hc * 128:(hc + 1) * 128], srcT.t[rows, l:l + 16 * 254 + 1:16],
                         l == 0, l == 31, R=[w1b.r, srcT.r], W=[bH.r])
                for l in range(32):
                    p.mm(bC[:, 0:1], w1b[rows, l, hc * 128:(hc + 1) * 128], peb[rows, l:l + 1],
                         l == 0, l == 31, R=[w1b.r, peb.r], W=[bC.r])
                p.cp("dve", cst[:, 0:1], bC[:, 0:1], R=[bC.r], W=[cst.r])
                p.act(u[:, 0:255], bH[:, 0:255], AF.Identity, bias=cst[:, 0:1], R=[bH.r, cst.r], W=[u.r])
                p.tt("dve", t1[:, 0:255], u[:, 0:255], u[:, 0:255], ALU.mult, R=[u.r], W=[t1.r])
                p.ts("dve", t1[:, 0:255], t1[:, 0:255], 0.044715, 1.0, ALU.mult, ALU.add, R=[t1.r], W=[t1.r])
                p.tt("dve", t1[:, 0:255], t1[:, 0:255], u[:, 0:255], ALU.mult, R=[t1.r, u.r], W=[t1.r])
                p.act(t2[:, 0:255], t1[:, 0:255], AF.Tanh, scale=0.7978845608028654, R=[t1.r], W=[t2.r])
                p.stt(t2[:, 0:255], t2[:, 0:255], 1.0, u[:, 0:255], ALU.add, ALU.mult, R=[t2.r, u.r], W=[t2.r])
                p.ts("dve", gel[:, gq, hc, 0:255], t2[:, 0:255], 0.5, None, ALU.mult, R=[t2.r], W=[gel.r])
        if kv == 0:
            b = nbank()
            n = 0
            for gq in range(2):
                for hc in range(2):
                    p.mm(b[:, 0:256], w2p[:, hc, gq, :], gel[:, gq, hc, :], n == 0, n == 3, R=[w2p.r, gel.r], W=[b.r])
                    n += 1
            p.cp("dve", Rz["kcmpT"][:], b[:, 0:256], R=[b.r], W=[Rz["kcmpT"].r])
        else:
            for gq in range(2):
                for cc in range(2):
                    b = nbank()
                    for hc in range(2):
                        p.mm(b[:, 0:64], gel[:, gq, hc, cc * 128:(cc + 1) * 128], w2b[:, hc, :], hc == 0, hc == 1,
                             R=[gel.r, w2b.r], W=[b.r])
                    p.cp("dve", Rz["vcmpM"][:, cc, gq, 0:64], b[:, 0:64], R=[b.r], W=[Rz["vcmpM"].r])

    sq = sb("sq", (128, T), BF16, st)
    row = sb("kmrow", (1, T), F32, st)
    tmp = sb("kmtmp", (1, 8), F32, st)
    rl = sb("relrow", (1, 512), F32, st)
    p.dma("sp", rl[:], I["relflat"][:, :], R=[I["relflat"].r], W=[rl.r])
    p.act(rl[:], rl[:], AF.Abs, R=[rl.r], W=[rl.r])
    p.op("dve", lambda e: e.reduce_max(tmp[:, 0:1], rl[:], AX.X), R=[rl.r], W=[tmp.r])
    for which, srcs in enumerate([(Rz["ksT"], Rz["kwT"]), (Rz["ckvT"],)]):
        first = True
        for s_ in srcs:
            p.tt("dve", sq[:], s_[:], s_[:], ALU.mult, R=[s_.r], W=[sq.r])
            for kb in range(8):
                b = nbank()
                p.mm(b[0:1, :], Rz["ones_bf"][:, 0:1], sq[:, kb * 512:(kb + 1) * 512], True, True,
                     R=[sq.r, Rz["ones_bf"].r], W=[b.r])
                if first:
                    p.cp("dve", row[:, kb * 512:(kb + 1) * 512], b[0:1, :], R=[b.r], W=[row.r])
                else:
                    p.tt("dve", row[:, kb * 512:(kb + 1) * 512], row[:, kb * 512:(kb + 1) * 512], b[0:1, :], ALU.add,
                         R=[b.r, row.r], W=[row.r])
            first = False
        p.op("dve", lambda e: e.reduce_max(tmp[:, 1:2], row[:], AX.X), R=[row.r], W=[tmp.r])
        p.act(tmp[:, 2:3], tmp[:, 1:2], AF.Sqrt, R=[tmp.r], W=[tmp.r])
        p.ts("dve", Rz["kmax"][:, 2 * which:2 * which + 1], tmp[:, 2:3], -1.03, None, ALU.mult, R=[tmp.r], W=[Rz["kmax"].r])
        p.ts("dve", Rz["kmax"][:, 2 * which + 1:2 * which + 2], tmp[:, 0:1], -1.0, None, ALU.mult, R=[tmp.r], W=[Rz["kmax"].r])


def phase2(g):
    nc, p, sb, I, S, Rz = g.nc, g.p, g.sb, g.I, g.S, g.Rz
    st = ExitStack()
    B = g.banks
    ident, identf, ones = Rz["identb"], Rz["identf"], Rz["ones_bf"]
    S["oT_s"] = g.dscr("oT_s", (NT, 128, 8, 128), BF16)
    ntiles = (g.debug or {}).get("ntiles", NT)

    stg = sb("c_stg", (128, 1024), F32, st)
    relTa = sb("relTa", (128, 8, 2, 512), BF16, st)
    relTb = sb("relTb", (128, 8, 1024), BF16, st)
    relTw = sb("relTw", (128, 2, 512), BF16, st)
    eall = sb("eall", (128, 32, 128), BF16, st)
    onesN = sb("onesN", (128, 128), BF16, st)
    p.memset("pool", eall[:], 0.0, W=[eall.r])
    p.memset("pool", onesN[:], 0.0, W=[onesN.r])
    p.memset("pool", onesN[0:1, :], 1.0, W=[onesN.r])
    d30 = sb("d30", (128, 512), BF16, st)
    mcmp = sb("mcmp", (128, 8, 503), BF16, st)
    wext = sb("wext", (128, 128), F32, st)
    mcq = sb("mcq", (128, 128), F32, st)
    rowsA = [sb("rowsA%d" % i, (128, 512), BF16, st) for i in range(2)]
    rowsB = sb("rowsB", (128, 1024), BF16, st)
    for r_ in rowsA + [rowsB]:
        p.memset("pool", r_[:], 0.0, W=[r_.r])
    wuvP = sb("wuvP", (128, 8, 128), BF16, st)
    st0 = ExitStack()
    mc4 = sb("mc4", (128, 512), F32, st0)
    mw4 = sb("mw4", (128, 512), F32, st0)
    c31A = sb("c31A", (128, 2, 512), F32, st0)
    c31B = sb("c31B", (128, 1024), F32, st0)
    p.dma("sp", mc4[:], I["mc4"][:, :], R=[I["mc4"].r], W=[mc4.r])
    p.dma("sp", mw4[:], I["mw4"][:, :], R=[I["mw4"].r], W=[mw4.r])
    p.dma("sp", wext[:], I["wext"][:, :], R=[I["wext"].r], W=[wext.r])
    p.dma("sp", mcq[:], I["mcq"][:, :], R=[I["mcq"].r], W=[mcq.r])
    for gq in range(2):
        p.dma("sp", c31A[:, gq, :], I["c31a"][gq:gq + 1, :].partition_broadcast(128), R=[I["c31a"].r], W=[c31A.r])
    p.dma("sp", c31B[:], I["c31b"][0:1, :].partition_broadcast(128), R=[I["c31b"].r], W=[c31B.r])
    for d in range(8):
        for gq in range(2):
            p.dma("sp", stg[:, 0:512], I["relTa"][:, d, gq, :], R=[I["relTa"].r], W=[stg.r])
            p.tt("dve", stg[:, 0:512], stg[:, 0:512], c31A[:, gq, :], ALU.subtract, R=[stg.r, c31A.r], W=[stg.r])
            if d == 0:
                p.tt("dve", stg[:, 0:512], stg[:, 0:512], mc4[:], ALU.add, R=[stg.r, mc4.r], W=[stg.r])
            p.cp("dve", relTa[:, d, gq, :], stg[:, 0:512], R=[stg.r], W=[relTa.r])
            if d == 4:
                p.tt("dve", stg[:, 0:512], stg[:, 0:512], mw4[:], ALU.add, R=[stg.r, mw4.r], W=[stg.r])
                p.cp("dve", relTw[:, gq, :], stg[:, 0:512], R=[stg.r], W=[relTw.r])
        p.dma("sp", stg[:], I["relTb"][:, d, :], R=[I["relTb"].r], W=[stg.r])
        p.tt("dve", stg[:], stg[:], c31B[:], ALU.subtract, R=[stg.r, c31B.r], W=[stg.r])
        if d == 0:
            for hf in range(2):
                p.tt("dve", stg[:, hf * 512:(hf + 1) * 512], stg[:, hf * 512:(hf + 1) * 512], mc4[:], ALU.add,
                     R=[stg.r, mc4.r], W=[stg.r])
        p.cp("dve", relTb[:, d, :], stg[:], R=[stg.r], W=[relTb.r])
    for q4 in range(4):
        p.dma("sp", stg[0:64, :], I["eall"][:, q4 * 1024:(q4 + 1) * 1024], R=[I["eall"].r], W=[stg.r])
        p.cp("dve", eall[0:64, q4 * 8:(q4 + 1) * 8, :], stg[0:64, :].rearrange("p (a b) -> p a b", b=128), R=[stg.r], W=[eall.r])
    p.dma("sp", stg[:, 0:512], I["d30"][:, :], R=[I["d30"].r], W=[stg.r])
    p.cp("dve", d30[:], stg[:, 0:512], R=[stg.r], W=[d30.r])
    for h in range(8):
        p.dma("sp", stg[:, 0:503], I["cmpv"][:, h, :], R=[I["cmpv"].r], W=[stg.r])
        p.dma("sp", stg[:, 512:1015], I["cmpm"][:, :], R=[I["cmpm"].r], W=[stg.r])
        p.tt("dve", mcmp[:, h, :], stg[:, 0:503], stg[:, 512:1015], ALU.add, R=[stg.r], W=[mcmp.r])
    p.memset("dve", eall[64:65, :, :], 1.0, W=[eall.r])
    p.memset("pool", wuvP[:], 0.0, W=[wuvP.r])
    for h in range(8):
        p.dma("sp", stg[:, 0:64], I["w_uv"][h, :, :], R=[I["w_uv"].r], W=[stg.r])
        p.cp("dve", wuvP[:, h, (h % 2) * 64:(h % 2) * 64 + 64], stg[:, 0:64], R=[stg.r], W=[wuvP.r])

    p.flush()
    st0.close()
    qTz = [sb("qTz%d" % i, (128, 512), BF16, st) for i in range(2)]
    for q_ in qTz:
        p.memset("pool", q_[:], 0.0, W=[q_.r])
    qlT = sb("qlT", (128, 1024), BF16, st)
    qiT = sb("qiT", (64, 512), BF16, st)
    zidx = sb("zidx", (128, T), F32, st)
    selD = Buf(g.kcT.t, g.kcT.r)
    junk = Buf(g.vcT.t, g.vcT.r)
    rr = [sb("rr%d" % i, (128, 512), F32, st) for i in range(2)]
    sq = sb("sqq", (128, 1024), BF16, st)
    srow = Buf(stg.t[0:1, :], stg.r)
    scmp = sb("scmp", (128, 8, 256), F32, st)
    pn = sb("pn", (128, 8, 256), BF16, st)
    pnT = sb("pnT", (128, 16, 128), BF16, st)
    sm = sb("sm", (128, 16), F32, st)
    sm2 = sb("sm2", (128, 16), F32, st)
    imp = sb("imp", (128, 64), F32, st)
    sc1 = sb("sc1", (128, 64), F32, st)
    sc2 = sb("sc2", (128, 64), F32, st)
    m8 = sb("m8", (128, 16), F32, st)
    selb = sb("selb", (128, 64), BF16, st)
    selT4 = [sb("selT4%d" % i, (128, 512), BF16, st) for i in range(2)]
    for s_ in selT4:
        p.memset("pool", s_[:], 0.0, W=[s_.r])
    PT = [sb("PT%d" % i, (128, 512), BF16, st) for i in range(4)]
    ocmp = sb("ocmp", (128, 512), F32, st)
    oa32 = sb("oa32", (128, 512), F32, st)
    oab = sb("oab", (128, 512), BF16, st)
    coef = sb("coef", (128, 32), F32, st)
    oln = sb("oln", (128, 8, 128), BF16, st)
    olT = sb("olT", (128, 8, 128), BF16, st)
    oT = sb("oT", (128, 8, 128), BF16, st)
    bis = sb("bis", (128, 8), F32, st)
    p.memset("pool", pn[:], 0.0, W=[pn.r])
    p.memset("pool", scmp[:], 0.0, W=[scmp.r])

    def bfv(bank):
        return bank.t[:].bitcast(BF16)

    selDs = [selD, junk]
    pw = sb("pw", (128, BIS_ITERS + 1), F32, st)
    wct = sb("wct", (128, BIS_ITERS + 1), F32, st)
    for k in range(BIS_ITERS + 1):
        p.memset("pool", pw[:, k:k + 1], 2.0 ** -(k + 1), W=[pw.r])

    def stream_D(i):
        nk = 128 * (i + 1)
        sD = selDs[i % 2]
        p.dma("sp", qiT[:], S["qi_s"][i, :, :, :].rearrange("d h q -> d (h q)"), R=[S["qi_s"].r], W=[qiT.r])
        for kb in range((nk + 511) // 512):
            k0 = kb * 512
            kn = min(512, nk - k0)
            for hi in range(4):
                bk = B[hi % 2]
                r_ = rr[hi % 2]
                p.mm(bk[:, 0:kn], qiT[:, hi * 128:(hi + 1) * 128], Rz["kiT"][:, k0:k0 + kn], True, True,
                     R=[qiT.r, Rz["kiT"].r], W=[bk.r])
                p.act(r_[:, 0:kn], bk[:, 0:kn], AF.Relu, scale=Rz["wabs"][:, i, hi:hi + 1], R=[bk.r, Rz["wabs"].r], W=[r_.r])
                if hi == 0:
                    p.ts("dve", zidx[:, k0:k0 + kn], r_[:, 0:kn], Rz["wsgn"][:, i, 0:1], None, ALU.mult,
                         R=[r_.r, Rz["wsgn"].r], W=[zidx.r])
                else:
                    p.stt(zidx[:, k0:k0 + kn], r_[:, 0:kn], Rz["wsgn"][:, i, hi:hi + 1], zidx[:, k0:k0 + kn], ALU.mult, ALU.add,
                          R=[r_.r, Rz["wsgn"].r, zidx.r], W=[zidx.r])
            yield
        p.op("dve", lambda e: e.tensor_reduce(bis[:, 0:1], zidx[:, 0:nk], AX.X, ALU.min), R=[zidx.r], W=[bis.r])
        p.op("dve", lambda e: e.reduce_max(bis[:, 1:2], zidx[:, 0:nk], AX.X), R=[zidx.r], W=[bis.r])
        p.stt(bis[:, 2:3], bis[:, 1:2], 1.0, bis[:, 0:1], ALU.add, ALU.subtract, R=[bis.r], W=[bis.r])
        p.ts("dve", wct[:], pw[:], bis[:, 2:3], None, ALU.mult, R=[pw.r, bis.r], W=[wct.r])
        p.tt("dve", bis[:, 3:4], bis[:, 0:1], wct[:, 0:1], ALU.add, R=[bis.r, wct.r], W=[bis.r])
        p.tt("dve", zidx[:, nk - 128:nk], zidx[:, nk - 128:nk], mcq[:], ALU.add, R=[zidx.r, mcq.r], W=[zidx.r])
        yield
        for k in range(BIS_ITERS):
            p.ts("dve", sD[:, 0:nk], zidx[:, 0:nk], bis[:, 3:4], None, ALU.is_ge, ALU.add, R=[zidx.r, bis.r], W=[sD.r, bis.r],
                 accum=bis[:, 4:5])
            p.stt(bis[:, 5:6], bis[:, 4:5], 256.0, wct[:, k:k + 1], ALU.is_ge, ALU.mult, R=[bis.r, wct.r], W=[bis.r])
            p.ts("dve", bis[:, 3:4], bis[:, 3:4], wct[:, k + 1:k + 2], bis[:, 5:6], ALU.subtract, ALU.add, R=[bis.r, wct.r], W=[bis.r])
            yield
        p.tt("dve", bis[:, 6:7], bis[:, 3:4], wct[:, BIS_ITERS:BIS_ITERS + 1], ALU.subtract, R=[bis.r, wct.r], W=[bis.r])
        p.ts("dve", sD[:, 0:nk], zidx[:, 0:nk], bis[:, 6:7], 1.0, ALU.is_ge, ALU.subtract, R=[zidx.r, bis.r], W=[sD.r])
        yield

    def stream_P(i):
        sD = selDs[i % 2]
        for gq in range(2):
            p.dma("sp", qTz[gq][gq * 64:(gq + 1) * 64, :], S["qa_s"][i, gq, :, :, :].rearrange("d h q -> d (h q)"),
                  R=[S["qa_s"].r], W=[qTz[gq].r])
        p.dma("sp", qlT[:], S["ql_s"][i, :, :, :].rearrange("r h q -> r (h q)"), R=[S["ql_s"].r], W=[qlT.r])
        for gq in range(2):
            p.act(sq[:, 0:512], qTz[gq][:], AF.Square, R=[qTz[gq].r], W=[sq.r])
            p.mm(B[7][0:1, :], ones[:, 0:1], sq[:, 0:512], True, True, R=[ones.r, sq.r], W=[B[7].r])
            p.act(srow[:, 0:512], B[7][0:1, :], AF.Sqrt, R=[B[7].r], W=[srow.r])
            p.ts("dve", rowsA[gq][0:1, :], srow[:, 0:512], Rz["kmax"][0:1, 0:1], Rz["kmax"][0:1, 1:2], ALU.mult, ALU.add,
                 R=[srow.r, Rz["kmax"].r], W=[rowsA[gq].r])
            p.dma("sp", selT4[gq][64:65, :], rowsA[gq][0:1, :], R=[rowsA[gq].r], W=[selT4[gq].r])
        p.act(sq[:], qlT[:], AF.Square, R=[qlT.r], W=[sq.r])
        for hf in range(2):
            p.mm(B[7][0:1, :], ones[:, 0:1], sq[:, hf * 512:(hf + 1) * 512], True, True, R=[ones.r, sq.r], W=[B[7].r])
            p.act(srow[:, hf * 512:(hf + 1) * 512], B[7][0:1, :], AF.Sqrt, R=[B[7].r], W=[srow.r])
        p.ts("dve", rowsB[0:1, :], srow[:], Rz["kmax"][0:1, 2:3], Rz["kmax"][0:1, 3:4], ALU.mult, ALU.add,
             R=[srow.r, Rz["kmax"].r], W=[rowsB.r])
        yield
        off = 248 - 8 * i
        for h in range(8):
            gq, hh = h // 4, h % 4
            rows = slice(gq * 64, (gq + 1) * 64)
            bL = B[2 + h // 2]
            p.mm(bL[:, (h % 2) * 256:(h % 2) * 256 + 255], qTz[gq][rows, hh * 128:(hh + 1) * 128], Rz["kcmpT"][rows, 0:255],
                 h % 2 == 0, h % 2 == 1, R=[qTz[gq].r, Rz["kcmpT"].r], W=[bL.r])
        for b4 in range(4):
            bL = B[2 + b4]
            p.tt("dve", scmp[:, 2 * b4:2 * b4 + 2, 0:255], bL[:, :].rearrange("p (h c) -> p h c", c=256)[:, :, 0:255],
                 mcmp[:, 2 * b4:2 * b4 + 2, off:off + 255], ALU.add, R=[bL.r, mcmp.r], W=[scmp.r])
        yield
        p.op("dve", lambda e: e.reduce_max(sm[:, 0:8], scmp[:, :, 0:255], AX.X), R=[scmp.r], W=[sm.r])
        p.ts("dve", sm[:, 8:16], sm[:, 0:8], -1000.0, -1.0, ALU.max, ALU.mult, R=[sm.r], W=[sm.r])
        p.tt("dve", scmp[:, :, 0:255], scmp[:, :, 0:255], sm[:, 8:16].unsqueeze(2).to_broadcast([128, 8, 255]), ALU.add,
             R=[scmp.r, sm.r], W=[scmp.r])
        p.act(scmp[:, :, 0:255], scmp[:, :, 0:255], AF.Exp, R=[scmp.r], W=[scmp.r])
        yield
        p.op("dve", lambda e: e.reduce_sum(sm2[:, 0:8], scmp[:, :, 0:255], AX.X), R=[scmp.r], W=[sm2.r])
        p.ts("dve", sm2[:, 0:8], sm2[:, 0:8], 1e-30, None, ALU.max, R=[sm2.r], W=[sm2.r])
        p.op("dve", lambda e: e.reciprocal(sm2[:, 8:16], sm2[:, 0:8]), R=[sm2.r], W=[sm2.r])
        p.tt("dve", pn[:, :, 0:255], scmp[:, :, 0:255], sm2[:, 8:16].unsqueeze(2).to_broadcast([128, 8, 255]), ALU.mult,
             R=[scmp.r, sm2.r], W=[pn.r])
        yield
        for hf in range(2):
            bk = B[2 + hf]
            tb_ = bfv(bk)
            for j in range(8):
                h, cc = hf * 4 + j // 2, j % 2
                p.tr(tb_[:, j * 128:(j + 1) * 128], pn[:, h, cc * 128:(cc + 1) * 128], ident[:], R=[pn.r, ident.r], W=[bk.r])
            p.cp("act" if hf else "dve", pnT[:, hf * 8:(hf + 1) * 8, :], tb_[:, :].rearrange("p (c q) -> p c q", q=128), R=[bk.r], W=[pnT.r])
        yield
        for gq in range(2):
            accb = B[6 + gq]
            for hh in range(4):
                h = gq * 4 + hh
                for cc in range(2):
                    p.mm(accb[:, hh * 128:(hh + 1) * 128], pnT[:, 2 * h + cc, :], Rz["vcmpM"][:, cc, gq, :], hh == 0 and cc == 0, hh == 3 and cc == 1,
                         R=[pnT.r, Rz["vcmpM"].r], W=[accb.r])
            p.cp("act", ocmp[:, gq * 256:(gq + 1) * 256].rearrange("p (h d) -> p h d", d=64),
                 accb[:, :].rearrange("p (h j) -> p h j", j=128)[:, :, 0:64], R=[accb.r], W=[ocmp.r])
            p.op("dve", lambda e, accb=accb: e.reduce_sum(imp[:], accb[:, :].rearrange("p (h j) -> p j h", j=128)[:, 64:128, :], AX.X),
                 R=[accb.r], W=[imp.r])
            p.tt("dve", sc1[:], imp[:], wext[:, 63 - 2 * i:127 - 2 * i], ALU.add, R=[imp.r, wext.r], W=[sc1.r])
            p.ts("dve", sc1[:, 0:1], imp[:, 0:1], 1e4, None, ALU.add, R=[imp.r], W=[sc1.r])
            p.op("dve", lambda e: e.max(m8[:, 0:8], sc1[:]), R=[sc1.r], W=[m8.r])
            p.op("dve", lambda e: e.match_replace(sc2[:], m8[:, 0:8], sc1[:], -1e30), R=[sc1.r, m8.r], W=[sc2.r])
            p.op("dve", lambda e: e.max(m8[:, 8:16], sc2[:]), R=[sc2.r], W=[m8.r])
            p.ts("dve", m8[:, 15:16], m8[:, 15:16], -1e29, None, ALU.max, R=[m8.r], W=[m8.r])
            p.ts("dve", selb[:], sc1[:], m8[:, 15:16], 1.0, ALU.is_ge, ALU.subtract, R=[sc1.r, m8.r], W=[selb.r])
            tb2 = bfv(accb)
            p.tr(tb2[0:64, 0:128], selb[:], ident[:], R=[selb.r, ident.r], W=[accb.r])
            p.cp("dve", selT4[gq][0:64, :].rearrange("p (h q) -> p h q", q=128),
                 tb2[0:64, 0:128].unsqueeze(1).to_broadcast([64, 4, 128]), R=[accb.r], W=[selT4[gq].r])
            yield
        DEPTH = 4
        sbanks = [B[2], B[3], B[6], B[7]]
        units = []
        for gq in range(2):
            for br_ in range(2):
                kts = list(range(0, i + 1)) if br_ == 0 else list(range(max(0, i - 4), i + 1))
                for n, kt in enumerate(kts):
                    units.append((gq, br_, n, kt, len(kts)))

        def emit_S(ui):
            gq, br_, n, kt, nk_ = units[ui]
            bS = sbanks[ui % DEPTH]
            kT = Rz["ksT"] if br_ == 0 else Rz["kwT"]
            d = i - kt
            p.mm(bS[:, :], kT[:, kt * 128:(kt + 1) * 128], qTz[gq][:], True, False, R=[kT.r, qTz[gq].r], W=[bS.r])
            if br_ == 0:
                p.mm(bS[:, :], eall[:, kt, :], selT4[gq][:], False, d >= 8, R=[eall.r, selT4[gq].r], W=[bS.r])
            if d < 8:
                rel = relTw[:, gq, :] if (br_ == 1 and d == 4) else relTa[:, d, gq, :]
                p.mm(bS[:, :], ident[:], rel, False, br_ == 0, R=[ident.r, relTa.r, relTw.r], W=[bS.r])
            if br_ == 1:
                p.mm(bS[:, :], onesN[:], rowsA[gq][:], False, True, R=[onesN.r, rowsA[gq].r], W=[bS.r])

        def epilogue(gq, br_):
            accb = B[4 + br_]
            accv = accb[:, 0:260].rearrange("p (h e) -> p h e", e=65)
            p.ts("dve", coef[:, 0:4], accv[:, :, 64], 1e-30, None, ALU.max, R=[accb.r], W=[coef.r])
            p.op("dve", lambda e: e.reciprocal(coef[:, 4:8], coef[:, 0:4]), R=[coef.r], W=[coef.r])
            gv = Rz["gn"][:, i, gq * 12:(gq + 1) * 12].rearrange("p (h t) -> p h t", t=3)
            p.tt("dve", coef[:, 8:12], coef[:, 4:8], gv[:, :, 1 + br_], ALU.mult, R=[coef.r, Rz["gn"].r], W=[coef.r])
            for hh in range(4):
                h = gq * 4 + hh
                if br_ == 0:
                    p.ts("dve", oa32[:, h * 64:(h + 1) * 64], ocmp[:, h * 64:(h + 1) * 64], Rz["gn"][:, i, 3 * h:3 * h + 1], None, ALU.mult,
                         R=[ocmp.r, Rz["gn"].r], W=[oa32.r])
                    p.stt(oa32[:, h * 64:(h + 1) * 64], accv[:, hh, 0:64], coef[:, 8 + hh:9 + hh], oa32[:, h * 64:(h + 1) * 64], ALU.mult, ALU.add,
                          R=[accb.r, coef.r, oa32.r], W=[oa32.r])
                else:
                    p.stt(oab[:, h * 64:(h + 1) * 64], accv[:, hh, 0:64], coef[:, 8 + hh:9 + hh], oa32[:, h * 64:(h + 1) * 64], ALU.mult, ALU.add,
                          R=[accb.r, coef.r, oa32.r], W=[oab.r])

        for ui in range(min(DEPTH - 1, len(units))):
            emit_S(ui)
        for ui, (gq, br_, n, kt, nk_) in enumerate(units):
            bS = sbanks[ui % DEPTH]
            pt = PT[ui % DEPTH]
            accb = B[4 + br_]
            vA = Rz["vsA"] if br_ == 0 else Rz["vwA"]
            p.act(pt[:], bS[:, :], AF.Exp, R=[bS.r], W=[pt.r])
            if ui + DEPTH - 1 < len(units):
                emit_S(ui + DEPTH - 1)
            for hh in range(4):
                p.mm(accb[:, hh * 65:(hh + 1) * 65], pt[:, hh * 128:(hh + 1) * 128], vA[:, kt, gq, :],
                     n == 0 and hh == 0, n == nk_ - 1, R=[pt.r, vA.r], W=[accb.r])
            if n == nk_ - 1:
                epilogue(gq, br_)
            yield
        tb_ = bfv(B[7])
        for c in range(4):
            p.tr(tb_[:, c * 128:(c + 1) * 128], oab[:, c * 128:(c + 1) * 128], ident[:], R=[oab.r, ident.r], W=[B[7].r])
        p.cp("act", oT[:, 0:4, :], tb_[:, 0:512].rearrange("p (c q) -> p c q", q=128), R=[B[7].r], W=[oT.r])
        yield
        accD = [B[4], B[5], B[6]]
        hb = [(0, 0), (0, 1), (0, 2), (1, 0), (1, 1), (1, 2), (2, 0), (2, 1)]
        dunits = [(kt, hf) for kt in range(i + 1) for hf in range(2)]

        dbanks = [B[2], B[3], B[7]]

        def emit_SD(ui):
            kt, hf = dunits[ui]
            d = i - kt
            bS = dbanks[ui % 3]
            cols = slice(hf * 512, (hf + 1) * 512)
            p.mm(bS[:, :], Rz["ckvT"][:, kt * 128:(kt + 1) * 128], qlT[:, cols], True, False, R=[Rz["ckvT"].r, qlT.r], W=[bS.r])
            p.mm(bS[:, :], sD[:, kt * 128:(kt + 1) * 128], d30[:], False, False, R=[sD.r, d30.r], W=[bS.r])
            if d < 8:
                p.mm(bS[:, :], ident[:], relTb[:, d, cols], False, False, R=[ident.r, relTb.r], W=[bS.r])
            p.mm(bS[:, :], onesN[:], rowsB[:, cols], False, True, R=[onesN.r, rowsB.r], W=[bS.r])

        DD = 3
        for ui in range(min(DD - 1, len(dunits))):
            emit_SD(ui)
        for ui, (kt, hf) in enumerate(dunits):
            bS = dbanks[ui % DD]
            pt = PT[ui % DD]
            p.act(pt[:], bS[:, :], AF.Exp, R=[bS.r], W=[pt.r])
            if ui + DD - 1 < len(dunits):
                emit_SD(ui + DD - 1)
            for hh in range(4):
                h = hf * 4 + hh
                bk, sl = hb[h]
                p.mm(accD[bk][:, sl * 129:(sl + 1) * 129], pt[:, hh * 128:(hh + 1) * 128], Rz["ckvA"][:, kt, :],
                     kt == 0 and sl == 0, kt == i, R=[pt.r, Rz["ckvA"].r], W=[accD[bk].r])
            if hf == 1:
                yield
        for h in range(8):
            bk, sl = hb[h]
            p.ts("dve", coef[:, 16 + h:17 + h], accD[bk][:, sl * 129 + 128:sl * 129 + 129], 1e-30, None, ALU.max, R=[accD[bk].r], W=[coef.r])
        p.op("dve", lambda e: e.reciprocal(coef[:, 24:32], coef[:, 16:24]), R=[coef.r], W=[coef.r])
        for h in range(8):
            bk, sl = hb[h]
            p.ts("dve", oln[:, h, :], accD[bk][:, sl * 129:sl * 129 + 128], coef[:, 24 + h:25 + h], None, ALU.mult,
                 R=[accD[bk].r, coef.r], W=[oln.r])
        yield
        for hf in range(2):
            tb_ = bfv(B[7])
            for hh in range(4):
                p.tr(tb_[:, hh * 128:(hh + 1) * 128], oln[:, hf * 4 + hh, :], ident[:], R=[oln.r, ident.r], W=[B[7].r])
            p.cp("act", olT[:, hf * 4:(hf + 1) * 4, :], tb_[:, 0:512].rearrange("p (c q) -> p c q", q=128), R=[B[7].r], W=[olT.r])
        for c in range(4):
            for hl in range(2):
                p.mm(B[7][:, c * 128:(c + 1) * 128], wuvP[:, 2 * c + hl, :], olT[:, 2 * c + hl, :], c == 0 and hl == 0, c == 3 and hl == 1,
                     R=[wuvP.r, olT.r], W=[B[7].r])
        p.cp("act", oT[:, 4:8, :], B[7][:, :].rearrange("p (c q) -> p c q", q=128), R=[B[7].r], W=[oT.r])
        p.dma("pool", S["oT_s"][i, :, :, :], oT[:], R=[oT.r], W=[S["oT_s"].r])
        yield

    def len_P(i):
        return 1 + 6 + 2 * ((i + 1) + min(5, i + 1)) + 4 + 1 + (i + 1) + 2

    def len_D(i):
        return (128 * (i + 1) + 511) // 512 + 1 + BIS_ITERS + 1

    for _ in stream_D(0):
        pass
    for i in range(ntiles):
        gd = stream_D(i + 1) if i + 1 < ntiles else None
        ratio = (len_D(i + 1) / float(len_P(i))) if gd is not None else 0.0
        credit = 0.0
        for _ in stream_P(i):
            credit += ratio
            while gd is not None and credit >= 1.0:
                credit -= 1.0
                try:
                    next(gd)
                except StopIteration:
                    gd = None
        if gd is not None:
            for _ in gd:
                pass
    return st


def layer_norm_tile(p, r, rs, gbc, bbc, out, tmp, R_extra=()):
    p.tt("dve", rs[:, 2:3], rs[:, 0:1], rs[:, 1:2], ALU.add, R=[rs.r], W=[rs.r])
    p.ts("dve", rs[:, 3:4], rs[:, 2:3], -1.0 / D, None, ALU.mult, R=[rs.r], W=[rs.r])
    p.act(tmp[:], r[:], AF.Square, bias=rs[:, 3:4], accum=rs[:, 4:5], R=[r.r, rs.r], W=[tmp.r, rs.r])
    p.ts("dve", rs[:, 5:6], rs[:, 4:5], 1.0 / D, 1e-5, ALU.mult, ALU.add, R=[rs.r], W=[rs.r])
    p.act(rs[:, 6:7], rs[:, 5:6], AF.Sqrt, R=[rs.r], W=[rs.r])
    p.op("dve", lambda e: e.reciprocal(rs[:, 7:8], rs[:, 6:7]), R=[rs.r], W=[rs.r])
    p.ts("dve", tmp[:], r[:], rs[:, 3:4], rs[:, 7:8], ALU.add, ALU.mult, R=[r.r, rs.r], W=[tmp.r])
    p.tt("dve", tmp[:], tmp[:], gbc[:], ALU.mult, R=[tmp.r, gbc.r], W=[tmp.r])
    p.tt("dve", out[:], tmp[:], bbc[:], ALU.add, R=[tmp.r, bbc.r], W=[out.r])


def phase3(g):
    nc, p, sb, I, S, Rz = g.nc, g.p, g.sb, g.I, g.S, g.Rz
    st = ExitStack()
    B = g.banks
    stg = sb("w_stg", (128, 1024), F32, st)
    wba = sb("wba", (128, 4, 1024), BF16, st)
    wbb = sb("wbb", (128, 4, 1024), BF16, st)
    wout = sb("wout", (128, 8, 1024), BF16, st)
    for (dst, src, n) in [(wba, "w_ba", 4), (wbb, "w_bb", 4), (wout, "w_out", 8)]:
        for c in range(n):
            p.dma("sp", stg[:], I[src][c * 128:(c + 1) * 128, :], R=[I[src].r], W=[stg.r])
            p.cp("dve", dst[:, c, :], stg[:], R=[stg.r], W=[dst.r])
    bc = {}
    for nm in ("ln1_g", "ln1_b", "ln2_g", "ln2_b"):
        bc[nm] = sb("bc_" + nm, (128, 1024), F32, st)
        p.dma("sp", bc[nm][:], I[nm][0:1, :].partition_broadcast(128), R=[I[nm].r], W=[bc[nm].r])
    oT_l = [sb("oT2_%d" % j, (128, 8, 128), BF16, st) for j in range(2)]
    gT_l = [sb("gT_%d" % j, (128, 16, 128), F32, st) for j in range(2)]
    xt_l = [sb("xt_%d" % j, (128, 1024), F32, st) for j in range(2)]
    t1 = sb("t1", (128, 512), F32, st)
    t2 = sb("t2", (128, 512), F32, st)
    mT = sb("mT", (128, 8, 128), BF16, st)
    r = sb("r", (128, 1024), F32, st)
    h1 = sb("h1", (128, 1024), F32, st)
    tmp = sb("lntmp", (128, 1024), F32, st)
    rs = sb("rs", (128, 8), F32, st)
    h1T = sb("h1T", (128, 8, 128), F32, st)
    wr = sb("wr", (128, 8, 36), F32, st)
    brb = sb("brb", (128, 36), F32, st)
    lg = sb("lg", (128, 36), F32, st)
    rt = sb("rt", (128, 16), F32, st)
    rj = sb("rj", (128, 32), F32, st)
    og = sb("og", (128, 4), F32, st)
    esel = sb("esel", (128, 8), F32, st)
    m8r = sb("m8r", (128, 8), F32, st)
    oh = sb("oh", (128, 2, 8), F32, st)
    Ak = sb("Ak", (128, 2, 32), F32, st)
    Abf = sb("Abf", (128, 32), BF16, st)
    ustr = sb("ustr", (128, 128), BF16, st)
    posf = sb("posf", (128, 32), F32, st)
    base = sb("base", (128, 32), F32, st)
    iot = sb("iot", (128, 32), F32, st)
    tokid = sb("tokid", (128, 32), I32, st)
    tokrow = sb("tokrow", (128, 32, 16), I32, st)
    fill = sb("fill", (128, 2048), I32, st)
    p.dma("sp", wr[:], I["wr"][:, :].rearrange("(c p) n -> p c n", p=128), R=[I["wr"].r], W=[wr.r])
    p.dma("sp", brb[:], I["br"][0:1, :].partition_broadcast(128), R=[I["br"].r], W=[brb.r])
    p.dma("sp", stg[:, 0:128], I["ustrict"][:, :], R=[I["ustrict"].r], W=[stg.r])
    p.cp("dve", ustr[:], stg[:, 0:128], R=[stg.r], W=[ustr.r])
    p.dma("sp", iot[:], I["iota32"][:, :], R=[I["iota32"].r], W=[iot.r])
    p.dma("sp", tokid[:], I["tokid"][:, :], R=[I["tokid"].r], W=[tokid.r])
    p.cp("dve", tokrow[:], tokid[:].unsqueeze(2).to_broadcast([128, 32, 16]), R=[tokid.r], W=[tokrow.r])
    p.memset("dve", base[:], 0.0, W=[base.r])
    p.memset("pool", fill[:], 5000, W=[fill.r])
    p.dma("sp", S["slot"][:, :].rearrange("(p r) c -> p (r c)", p=128), fill[:], R=[fill.r], W=[S["slot"].r])
    nt3 = (g.debug or {}).get("ntiles", NT)

    def loads3(i):
        j = i % 2
        p.dma("sp", oT_l[j][:], S["oT_s"][i, :, :, :], R=[S["oT_s"].r], W=[oT_l[j].r])
        p.dma("sp", gT_l[j][:], S["g_s"][i, :, :, :], R=[S["g_s"].r], W=[gT_l[j].r])
        p.dma("sp", xt_l[j][:], I["x"][i * 128:(i + 1) * 128, :], R=[I["x"].r], W=[xt_l[j].r])

    loads3(0)
    for i in range(nt3):
        oT, gT, xt = oT_l[i % 2], gT_l[i % 2], xt_l[i % 2]
        if i + 1 < nt3:
            loads3(i + 1)
        for hf in range(2):
            for m4 in range(4):
                m = hf * 4 + m4
                for kc in range(4):
                    p.mm(B[0][:, m4 * 128:(m4 + 1) * 128], wba[:, kc, m * 128:(m + 1) * 128], oT[:, kc, :], m4 == 0 and kc == 0, kc == 3,
                         R=[wba.r, oT.r], W=[B[0].r])
                for kc in range(4):
                    p.mm(B[1][:, m4 * 128:(m4 + 1) * 128], wbb[:, kc, m * 128:(m + 1) * 128], oT[:, 4 + kc, :], m4 == 0 and kc == 0, kc == 3,
                         R=[wbb.r, oT.r], W=[B[1].r])
            p.tt("dve", t1[:], B[0][:, :], gT[:, hf * 4:(hf + 1) * 4, :].rearrange("p m q -> p (m q)"), ALU.mult, R=[B[0].r, gT.r], W=[t1.r])
            p.tt("dve", t2[:], B[1][:, :], gT[:, 8 + hf * 4:8 + (hf + 1) * 4, :].rearrange("p m q -> p (m q)"), ALU.mult, R=[B[1].r, gT.r], W=[t2.r])
            p.tt("dve", mT[:, hf * 4:(hf + 1) * 4, :].rearrange("p m q -> p (m q)"), t1[:], t2[:], ALU.add, R=[t1.r, t2.r], W=[mT.r])
        for hf in range(2):
            bk = B[2 + hf]
            for c in range(8):
                p.mm(bk[:, :], mT[:, c, :], wout[:, c, hf * 512:(hf + 1) * 512], c == 0, c == 7, R=[mT.r, wout.r], W=[bk.r])
            p.stt(r[:, hf * 512:(hf + 1) * 512], xt[:, hf * 512:(hf + 1) * 512], ALPHA, bk[:, :], ALU.mult, ALU.add,
                  R=[xt.r, bk.r], W=[r.r, rs.r], accum=rs[:, hf:hf + 1])
        layer_norm_tile(p, r, rs, bc["ln1_g"], bc["ln1_b"], h1, tmp)
        p.dma("pool", S["h1_s"][i * 128:(i + 1) * 128, :], h1[:], R=[h1.r], W=[S["h1_s"].r])
        for c in range(8):
            bk = B[4 + c // 4]
            p.tr(bk[:, (c % 4) * 128:(c % 4 + 1) * 128], h1[:, c * 128:(c + 1) * 128], Rz["identf"][:], R=[h1.r, Rz["identf"].r], W=[bk.r])
        for hf in range(2):
            p.cp("act", h1T[:, hf * 4:(hf + 1) * 4, :], B[4 + hf][:, :].rearrange("p (c q) -> p c q", q=128), R=[B[4 + hf].r], W=[h1T.r])
        for c in range(8):
            p.mm(B[6][:, 0:36], h1T[:, c, :], wr[:, c, :], c == 0, c == 7, R=[h1T.r, wr.r], W=[B[6].r])
        p.tt("dve", lg[:], B[6][:, 0:36], brb[:], ALU.add, R=[B[6].r, brb.r], W=[lg.r])
        p.op("dve", lambda e: e.reduce_max(rt[:, 0:1], lg[:, 0:4], AX.X), R=[lg.r], W=[rt.r])
        p.ts("dve", og[:], lg[:, 0:4], rt[:, 0:1], None, ALU.is_equal, R=[lg.r, rt.r], W=[og.r])
        p.ts("dve", rt[:, 1:2], rt[:, 0:1], -1.0, None, ALU.mult, R=[rt.r], W=[rt.r])
        p.act(rj[:, 0:4], lg[:, 0:4], AF.Exp, bias=rt[:, 1:2], accum=rt[:, 2:3], R=[lg.r, rt.r], W=[rj.r, rt.r])
        p.op("dve", lambda e: e.reciprocal(rt[:, 3:4], rt[:, 2:3]), R=[rt.r], W=[rt.r])
        p.ts("dve", esel[:], lg[:, 4:12], og[:, 0:1], None, ALU.mult, R=[lg.r, og.r], W=[esel.r])
        for gi in range(1, 4):
            p.stt(esel[:], lg[:, 4 + 8 * gi:12 + 8 * gi], og[:, gi:gi + 1], esel[:], ALU.mult, ALU.add, R=[lg.r, og.r, esel.r], W=[esel.r])
        p.op("dve", lambda e: e.max(m8r[:], esel[:]), R=[esel.r], W=[m8r.r])
        p.tt("dve", rt[:, 4:5], m8r[:, 1:2], m8r[:, 0:1], ALU.subtract, R=[m8r.r], W=[rt.r])
        p.act(rt[:, 5:6], rt[:, 4:5], AF.Exp, R=[rt.r], W=[rt.r])
        p.ts("dve", rt[:, 6:7], rt[:, 5:6], 1.0, None, ALU.add, R=[rt.r], W=[rt.r])
        p.op("dve", lambda e: e.reciprocal(rt[:, 7:8], rt[:, 6:7]), R=[rt.r], W=[rt.r])
        p.tt("dve", rt[:, 8:9], rt[:, 7:8], rt[:, 5:6], ALU.mult, R=[rt.r], W=[rt.r])
        p.tt("dve", g.wts[:, i, 0:1], rt[:, 7:8], rt[:, 3:4], ALU.mult, R=[rt.r], W=[g.wts.r])
        p.tt("dve", g.wts[:, i, 1:2], rt[:, 8:9], rt[:, 3:4], ALU.mult, R=[rt.r], W=[g.wts.r])
        for k in range(2):
            p.ts("dve", oh[:, k, :], esel[:], m8r[:, k:k + 1], None, ALU.is_equal, R=[esel.r, m8r.r], W=[oh.r])
            p.tt("dve", Ak[:, k, :].rearrange("p (a b) -> p a b", b=8), og[:].unsqueeze(2).to_broadcast([128, 4, 8]),
                 oh[:, k, :].unsqueeze(1).to_broadcast([128, 4, 8]), ALU.mult, R=[og.r, oh.r], W=[Ak.r])
        p.tt("dve", Abf[:], Ak[:, 0, :], Ak[:, 1, :], ALU.add, R=[Ak.r], W=[Abf.r])
        p.mm(B[7][:, 0:32], ustr[:], Abf[:], True, False, R=[ustr.r, Abf.r], W=[B[7].r])
        p.mm(B[7][:, 64:96], Rz["ones_bf"][:], Abf[:], False, True, R=[Rz["ones_bf"].r, Abf.r], W=[B[7].r])
        p.tt("dve", posf[:], B[7][:, 0:32], base[:], ALU.add, R=[B[7].r, base.r], W=[posf.r])
        p.tt("dve", base[:], B[7][:, 64:96], base[:], ALU.add, R=[B[7].r, base.r], W=[base.r])
        for k in range(2):
            p.stt(rj[:, 0:32], posf[:], 1.0, Ak[:, k, :], ALU.mult, ALU.mult, R=[posf.r, Ak.r], W=[rj.r, rt.r], accum=rt[:, 10 + k:11 + k])
            p.stt(rj[:, 0:32], iot[:], 1.0, Ak[:, k, :], ALU.mult, ALU.mult, R=[iot.r, Ak.r], W=[rj.r, rt.r], accum=rt[:, 12 + k:13 + k])
            p.ts("dve", rt[:, 14:15], rt[:, 10 + k:11 + k], float(CAP), 1e6, ALU.is_ge, ALU.mult, R=[rt.r], W=[rt.r])
            p.ts("dve", rt[:, 9:10], rt[:, 10 + k:11 + k], float(CAP), None, ALU.is_lt, R=[rt.r], W=[rt.r])
            p.tt("dve", g.wts[:, i, k:k + 1], g.wts[:, i, k:k + 1], rt[:, 9:10], ALU.mult, R=[rt.r, g.wts.r], W=[g.wts.r])
            p.stt(rt[:, 15:16], rt[:, 12 + k:13 + k], float(CAP), rt[:, 10 + k:11 + k], ALU.mult, ALU.add, R=[rt.r], W=[rt.r])
            p.tt("dve", rt[:, 15:16], rt[:, 15:16], rt[:, 14:15], ALU.add, R=[rt.r], W=[rt.r])
            p.cp("dve", g.dest[:, i, k:k + 1], rt[:, 15:16], R=[rt.r], W=[g.dest.r])
            p.dma("pool", None, None, R=[g.dest.r, tokrow.r], W=[S["slot"].r],
                  fn=lambda e, i=i, k=k: e.indirect_dma_start(
                      out=S["slot"][:, :], out_offset=bass.IndirectOffsetOnAxis(ap=g.dest[:, i, k:k + 1], axis=0),
                      in_=tokrow[:, i, :], in_offset=None, bounds_check=p.breg(e, 32 * CAP - 1), oob_is_err=False))
    return st


def phase4(g):
    nc, p, sb, I, S, Rz = g.nc, g.p, g.sb, g.I, g.S, g.Rz
    st = ExitStack()
    B = g.banks
    ident = Rz["identb"]
    nexp = (g.debug or {}).get("nexp", 32)
    sid = sb("sid", (128, 4, 16), I32, st)
    xg = [sb("xg%d" % i, (128, 1024), F32, st) for i in range(4)]
    xgb = [sb("xgb%d" % i, (128, 1024), BF16, st) for i in range(2)]
    xgT = sb("xgT", (128, 8, 512), BF16, st)
    hT = sb("hT", (128, 2, 512), BF16, st)
    sg = [sb("sg%d" % i, (128, 512), F32, st) for i in range(2)]
    yb = [sb("yb%d" % i, (128, 1024), F32, st) for i in range(2)]
    wgf = sb("wgf", (128, 8, 256), F32, st)
    wuf = sb("wuf", (128, 8, 256), F32, st)
    wdf = sb("wdf", (128, 2, 1024), F32, st)
    wg = [sb("wg%d" % i, (128, 8, 256), BF16, st) for i in range(2)]
    wu = [sb("wu%d" % i, (128, 8, 256), BF16, st) for i in range(2)]
    wd = [sb("wd%d" % i, (128, 2, 1024), BF16, st) for i in range(2)]
    for x_ in xg:
        p.memset("pool", x_[:], 0.0, W=[x_.r])
    ceng = ["dve", "act", "dve", "act"]
    xgT2 = [xgT, sb("xgTb", (128, 8, 512), BF16, st)]
    sid2 = [sid, sb("sidb", (128, 4, 16), I32, st)]

    def issue_loads(e_):
        k = e_ % 2
        sd = sid2[k]
        p.dma("sp", wgf[:], I["w_gate"][e_, :, :].rearrange("(p c) f -> p c f", c=8), R=[I["w_gate"].r], W=[wgf.r])
        p.dma("sp", wuf[:], I["w_up"][e_, :, :].rearrange("(p c) f -> p c f", c=8), R=[I["w_up"].r], W=[wuf.r])
        p.dma("sp", wdf[:], I["w_down"][e_, :, :].rearrange("(c p) n -> p c n", p=128), R=[I["w_down"].r], W=[wdf.r])
        p.dma("sp", sd[:], S["slot"][e_ * CAP:(e_ + 1) * CAP, :].rearrange("(s p) c -> p s c", p=128),
              R=[S["slot"].r], W=[sd.r])
        for s_ in range(4):
            p.dma("pool", None, None, R=[sd.r, S["h1_s"].r], W=[xg[s_].r],
                  fn=lambda e, s_=s_, sd=sd: e.indirect_dma_start(
                      out=xg[s_][:, :], out_offset=None, in_=S["h1_s"][:, :],
                      in_offset=bass.IndirectOffsetOnAxis(ap=sd[:, s_, 0:1], axis=0), bounds_check=p.breg(e, T - 1), oob_is_err=False))

    def finish_loads(e_):
        k = e_ % 2
        p.cp("act", wg[k][:], wgf[:], R=[wgf.r], W=[wg[k].r])
        p.cp("dve", wu[k][:], wuf[:], R=[wuf.r], W=[wu[k].r])
        p.cp("act", wd[k][:], wdf[:], R=[wdf.r], W=[wd[k].r])
        for s_ in range(4):
            xb = xgb[s_ % 2]
            p.cp(ceng[s_], xb[:], xg[s_][:], R=[xg[s_].r], W=[xb.r])
            bk = B[6 + s_ % 2]
            tv = bk.t[:].bitcast(BF16)
            for c in range(8):
                p.tr(tv[:, c * 128:(c + 1) * 128], xb.t[:, c:1024:8], ident[:], R=[xb.r, ident.r], W=[bk.r])
            p.cp("act" if s_ % 2 else "dve", xgT2[k][:, :, s_ * 128:(s_ + 1) * 128], tv[:, :].rearrange("p (c q) -> p c q", q=128),
                 R=[bk.r], W=[xgT2[k].r])

    def compute(e_):
        k = e_ % 2
        xT_ = xgT2[k]
        for fc in range(2):
            bG, bU = B[2 * fc], B[2 * fc + 1]
            for c in range(8):
                p.mm(bG[:, :], wg[k][:, c, fc * 128:(fc + 1) * 128], xT_[:, c, :], c == 0, c == 7, R=[wg[k].r, xT_.r], W=[bG.r])
            for c in range(8):
                p.mm(bU[:, :], wu[k][:, c, fc * 128:(fc + 1) * 128], xT_[:, c, :], c == 0, c == 7, R=[wu[k].r, xT_.r], W=[bU.r])
            p.act(sg[fc][:], bG[:, :], AF.Silu, R=[bG.r], W=[sg[fc].r])
            p.tt("dve", hT[:, fc, :], sg[fc][:], bU[:, :], ALU.mult, R=[sg[fc].r, bU.r], W=[hT.r])
        for s_ in range(4):
            y_ = yb[s_ % 2]
            for hf in range(2):
                bY = B[4 + (2 * s_ + hf) % 2]
                for fc in range(2):
                    p.mm(bY[:, :], hT[:, fc, s_ * 128:(s_ + 1) * 128], wd[k][:, fc, hf * 512:(hf + 1) * 512], fc == 0, fc == 1,
                         R=[hT.r, wd[k].r], W=[bY.r])
                if hf == 0:
                    p.act(y_[:, 0:512], bY[:, :], AF.Copy, R=[bY.r], W=[y_.r])
                else:
                    p.cp("dve", y_[:, 512:1024], bY[:, :], R=[bY.r], W=[y_.r])
            p.dma("sp", S["y_s"][e_ * CAP + s_ * 128:e_ * CAP + (s_ + 1) * 128, :], y_[:], R=[y_.r], W=[S["y_s"].r])

    issue_loads(0)
    finish_loads(0)
    for e_ in range(nexp):
        if e_ + 1 < nexp:
            issue_loads(e_ + 1)
        compute(e_)
        if e_ + 1 < nexp:
            finish_loads(e_ + 1)
    return st


def phase5(g):
    nc, p, sb, I, S, Rz = g.nc, g.p, g.sb, g.I, g.S, g.Rz
    st = ExitStack()
    bc = {}
    for nm in ("ln2_g", "ln2_b"):
        bc[nm] = sb("bc5_" + nm, (128, 1024), F32, st)
        p.dma("sp", bc[nm][:], I[nm][0:1, :].partition_broadcast(128), R=[I[nm].r], W=[bc[nm].r])
    y = [[sb("y%d_%d" % (k, j), (128, 1024), F32, st) for k in range(2)] for j in range(2)]
    h1 = [sb("h1_5%d" % j, (128, 1024), F32, st) for j in range(2)]
    r = sb("r5", (128, 1024), F32, st)
    tmp = sb("tmp5", (128, 1024), F32, st)
    ot = [sb("ot5%d" % j, (128, 1024), F32, st) for j in range(2)]
    rs = sb("rs5", (128, 8), F32, st)
    for j in range(2):
        for k in range(2):
            p.memset("dve", y[j][k][:], 0.0, W=[y[j][k].r])
    for i in range((g.debug or {}).get("ntiles", NT)):
        j = i % 2
        p.dma("sp", h1[j][:], S["h1_s"][i * 128:(i + 1) * 128, :], R=[S["h1_s"].r], W=[h1[j].r])
        for k in range(2):
            p.dma("pool", None, None, R=[g.dest.r, S["y_s"].r], W=[y[j][k].r],
                  fn=lambda e, i=i, k=k, j=j: e.indirect_dma_start(
                      out=y[j][k][:, :], out_offset=None, in_=S["y_s"][:, :],
                      in_offset=bass.IndirectOffsetOnAxis(ap=g.dest[:, i, k:k + 1], axis=0), bounds_check=p.breg(e, 32 * CAP - 1), oob_is_err=False))
        p.ts("dve", r[:], h1[j][:], ALPHA, None, ALU.mult, R=[h1[j].r], W=[r.r])
        p.stt(r[:], y[j][0][:], g.wts[:, i, 0:1], r[:], ALU.mult, ALU.add, R=[y[j][0].r, g.wts.r, r.r], W=[r.r])
        p.stt(r[:], y[j][1][:], g.wts[:, i, 1:2], r[:], ALU.mult, ALU.add, R=[y[j][1].r, g.wts.r, r.r], W=[r.r])
        p.op("dve", lambda e: e.reduce_sum(rs[:, 0:1], r[:], AX.X), R=[r.r], W=[rs.r])
        p.memset("dve", rs[:, 1:2], 0.0, W=[rs.r])
        layer_norm_tile(p, r, rs, bc["ln2_g"], bc["ln2_b"], ot[j], tmp)
        p.dma("sp", g.out[i * 128:(i + 1) * 128, :], ot[j][:], R=[ot[j].r], W=[g.out.r])
    return st


def host_inputs(inputs):
    f = lambda a: np.ascontiguousarray(np.asarray(a, dtype=np.float32))
    rel_bias = f(inputs["rel_bias"])
    shared = {
        "w_in": f(inputs["w_in"][0]),
        "pe_kT": f(inputs["cmp_pe_k"][0].T), "pe_vT": f(inputs["cmp_pe_v"][0].T),
        "cw1_k": f(inputs["cmp_w1_k"][0]), "cw2_k": f(inputs["cmp_w2_k"][0]),
        "cw1_v": f(inputs["cmp_w1_v"][0]), "cw2_v": f(inputs["cmp_w2_v"][0]),
        "ckv_g": f(inputs["ckv_norm_g"][0]).reshape(1, 128),
        "w_uk": f(inputs["w_uk"][0]), "w_uv": f(inputs["w_uv"][0]),
        "relflat": rel_bias.reshape(1, 512),
        "w_ba": f(inputs["w_branch_a"][0]), "w_bb": f(inputs["w_branch_b"][0]), "w_out": f(inputs["w_out"][0]),
        "ln1_g": f(inputs["ln1_g"][0]).reshape(1, D), "ln1_b": f(inputs["ln1_b"][0]).reshape(1, D),
        "wr": f(np.concatenate([np.asarray(inputs["w_grp"][0]), np.asarray(inputs["w_rtr"][0])], axis=1)),
        "br": f(np.concatenate([np.asarray(inputs["b_grp"][0]), np.asarray(inputs["b_rtr"][0])], axis=0)).reshape(1, 36),
        "w_gate": f(inputs["w_gate"][0]), "w_up": f(inputs["w_up"][0]), "w_down": f(inputs["w_down"][0]),
        "ln2_g": f(inputs["ln2_g"][0]).reshape(1, D), "ln2_b": f(inputs["ln2_b"][0]).reshape(1, D),
    }
    shared.update(_host_consts(rel_bias))
    x = np.asarray(inputs["x"], dtype=np.float32)
    maps = []
    for b in range(x.shape[0]):
        m = dict(shared)
        m["x"] = np.ascontiguousarray(x[b])
        m["xT"] = np.ascontiguousarray(x[b].T)
        maps.append(m)
    return maps


def kernel(**inputs):
    maps = host_inputs(inputs)
    nc, g = build()
    res = run_bass_kernel_spmd(nc, maps, core_ids=list(range(8)))
    return np.stack([np.asarray(r["out"], dtype=np.float32) for r in res.results], axis=0)
```

```python
import math
from contextlib import ExitStack
import numpy as np
import ml_dtypes
import concourse.bass as bass
import concourse.mybir as mybir
from concourse.bass_utils import run_bass_kernel_spmd

F32 = mybir.dt.float32
BF16 = mybir.dt.bfloat16
I32 = mybir.dt.int32
AF = mybir.ActivationFunctionType
ALU = mybir.AluOpType
AX = mybir.AxisListType

T = 4096
D = 1024
NT = T // 128
D_IN = 4316
ALPHA = 2.0 ** 0.25
CAP = 512
NEGM = 32768.0
BIS_ITERS = 20
DEBUG = None

O_QA, O_KC, O_VC, O_KS, O_VS, O_KW, O_VW, O_GN, O_QB, O_CKV, O_QI, O_KI, O_WI, O_GA, O_GB = (
    0, 512, 640, 768, 896, 1024, 1152, 1280, 1304, 1816, 1944, 2200, 2264, 2268, 3292)


class Res:
    __slots__ = ("name", "w", "r", "dsem", "dcount", "excl")

    def __init__(self, name):
        self.name = name
        self.excl = False
        self.w = None
        self.r = {}
        self.dsem = None
        self.dcount = 0


class Prog:
    ENG = ("pe", "act", "dve", "pool", "sp")

    def __init__(self, nc, stack):
        self.nc = nc
        self.stack = stack
        self.eobj = {"pe": nc.tensor, "act": nc.scalar, "dve": nc.vector, "pool": nc.gpsimd, "sp": nc.sync}
        self.sem = {e: stack.enter_context(nc.semaphore("c_" + e)) for e in self.ENG}
        self.cnt = {e: 0 for e in self.ENG}
        self.seen = {e: {} for e in self.ENG}
        self.ops = {e: [] for e in self.ENG}
        self.dres = []
        self.nres = 0

    def res(self, name=None):
        self.nres += 1
        return Res(name or "r%d" % self.nres)

    def _waits(self, eng, reads, writes):
        need = {}

        def add(ev, war=False):
            if ev is None:
                return
            sem, val, src = ev
            if src == eng and eng == "pe":
                return
            k = id(sem)
            if self.seen[eng].get(k, (None, 0))[1] >= val:
                return
            if k not in need or need[k][1] < val:
                need[k] = (sem, val)

        for r in reads:
            add(r.w)
        for w in writes:
            add(w.w)
            for ev in w.r.values():
                add(ev, True)
        out = []
        for k, (sem, val) in need.items():
            self.seen[eng][k] = (sem, val)
            out.append((sem, val))
        return out

    def op(self, eng, fn, R=(), W=()):
        W = list(W) + [r for r in R if r.excl]
        R = [r for r in R if not r.excl]
        waits = self._waits(eng, R, W)
        self.cnt[eng] += 1
        ev = (self.sem[eng], self.cnt[eng], eng)
        self.ops[eng].append((fn, waits, (self.sem[eng], 1)))
        for r in R:
            r.r[eng] = ev
        for w in W:
            w.w = ev
            w.r = {}

    def dma(self, q, out, in_, R=(), W=(), fn=None):
        waits = self._waits(q, R, W)
        tgt = W[0]
        if tgt.dsem is None:
            tgt.dsem = self.stack.enter_context(self.nc.semaphore("d_" + tgt.name))
            self.dres.append(tgt)
        tgt.dcount += 16
        ev = (tgt.dsem, tgt.dcount, None)
        if fn is None:
            fn = lambda e, o=out, i=in_: e.dma_start(out=o, in_=i)
        self.ops[q].append((fn, waits, (tgt.dsem, 16)))
        for r in R:
            r.r["dma%d" % id(tgt)] = ev
        for w in W:
            w.w = ev
            w.r = {}

    def flush(self, final=False):
        tail = {}
        for e in self.ENG:
            ws = []
            for e2 in self.ENG:
                if e2 != e and self.cnt[e2] > 0 and self.seen[e].get(id(self.sem[e2]), (None, 0))[1] < self.cnt[e2]:
                    ws.append((self.sem[e2], self.cnt[e2]))
                    self.seen[e][id(self.sem[e2])] = (self.sem[e2], self.cnt[e2])
            for r in self.dres:
                if self.seen[e].get(id(r.dsem), (None, 0))[1] < r.dcount:
                    ws.append((r.dsem, r.dcount))
                    self.seen[e][id(r.dsem)] = (r.dsem, r.dcount)
            tail[e] = ws
        ops = self.ops
        eobj = self.eobj
        self.regs = {}
        with self.nc.Block() as block:
            def run(e, engine):
                for fn, waits, inc in ops[e]:
                    for s, v in waits:
                        engine.wait_ge(s, v)
                    ins = fn(engine)
                    ins.then_inc(inc[0], inc[1])
                for s, v in tail[e]:
                    engine.wait_ge(s, v)

            @block.tensor
            def _(t):
                run("pe", t)

            @block.scalar
            def _(s):
                run("act", s)

            @block.vector
            def _(v):
                run("dve", v)

            @block.gpsimd
            def _(g):
                run("pool", g)

            @block.sync
            def _(sy):
                run("sp", sy)
        self.ops = {e: [] for e in self.ENG}

    def breg(self, e, val):
        if val not in self.regs:
            self.regs[val] = e.to_reg(val)
        return self.regs[val]

    def mm(self, out, lhsT, rhs, start, stop, R=(), W=()):
        self.op("pe", lambda e: e.matmul(out, lhsT, rhs, start=start, stop=stop, skip_group_check=True), R, W)

    def tr(self, out, in_, ident, R=(), W=()):
        self.op("pe", lambda e: e.transpose(out, in_, ident), R, W)

    def act(self, out, in_, func, R=(), W=(), bias=None, scale=None, accum=None, eng="act"):
        kw = {}
        if bias is not None:
            kw["bias"] = bias
        if scale is not None:
            kw["scale"] = scale
        if accum is not None:
            kw["accum_out"] = accum
        self.op("act", lambda e: e.activation(out, in_, func, **kw), R, W)

    def ts(self, eng, out, in0, s1, s2, op0, op1=None, R=(), W=(), accum=None):
        kw = {}
        if op1 is not None:
            kw["op1"] = op1
        if accum is not None:
            kw["accum_out"] = accum
        self.op(eng, lambda e: e.tensor_scalar(out, in0, s1, s2, op0, **kw), R, W)

    def tt(self, eng, out, in0, in1, op, R=(), W=()):
        self.op(eng, lambda e: e.tensor_tensor(out, in0, in1, op), R, W)

    def stt(self, out, in0, scalar, in1, op0, op1, R=(), W=(), accum=None):
        kw = {}
        if accum is not None:
            kw["accum_out"] = accum
        self.op("dve", lambda e: e.scalar_tensor_tensor(out, in0, scalar, in1, op0, op1, **kw), R, W)

    def cp(self, eng, out, in_, R=(), W=()):
        if eng == "act":
            self.op("act", lambda e: e.copy(out, in_), R, W)
        else:
            self.op(eng, lambda e: e.tensor_copy(out, in_), R, W)

    def memset(self, eng, ap, val, W=()):
        self.op(eng, lambda e: e.memset(ap, val), (), W)


class Buf:
    def __init__(self, t, res):
        self.t = t
        self.r = res

    def __getitem__(self, k):
        return self.t[k]


def _rel_bucket_np(dist):
    n = np.maximum(dist, 0)
    nf = np.maximum(n, 1).astype(np.float32)
    large = 16 + (np.log(nf / 16) / math.log(1024 / 16) * 16).astype(np.int32)
    return np.where(n < 16, n, np.minimum(large, 31))


def _host_consts(rel_bias):
    c = {}
    dd = np.arange(0, 1152)
    bk = _rel_bucket_np(dd)
    relvec = rel_bias[bk]
    p = np.arange(128)[:, None]
    j = np.arange(128)[None, :]
    relT = np.zeros((8, 128, 16, 128), np.float32)
    for di in range(8):
        dist = np.clip(128 * di + j - p, 0, 1151)
        relT[di] = relvec[dist].transpose(0, 2, 1)
    c["relTa"] = np.ascontiguousarray(relT[:, :, :8, :].transpose(1, 0, 2, 3)).reshape(128, 8, 2, 512)
    c["relTb"] = np.ascontiguousarray(relT[:, :, 8:, :].transpose(1, 0, 2, 3)).reshape(128, 8, 1024)
    c31 = relvec[1151]
    c["c31a"] = np.ascontiguousarray(np.repeat(c31[:8].reshape(2, 4, 1), 128, axis=2).reshape(2, 512))
    c["c31b"] = np.ascontiguousarray(np.repeat(c31[8:].reshape(8, 1), 128, axis=1).reshape(1, 1024))
    v = np.arange(503)[None, :]
    dist = p - 16 * v + 3937
    c["cmpv"] = np.ascontiguousarray(relvec[np.clip(dist, 0, 1151)][:, :, :8].transpose(0, 2, 1))
    c["cmpm"] = np.where(dist >= 0, 0.0, -30000.0).astype(np.float32)
    mc = np.where(j < p, -NEGM, 0.0).astype(np.float32)
    mw = np.where(j >= p, -NEGM, 0.0).astype(np.float32)
    c["mc4"] = np.tile(mc, (1, 4))
    c["mw4"] = np.tile(mw, (1, 4))
    c["ident"] = np.eye(128, dtype=np.float32)
    c["d30"] = np.tile(np.eye(128, dtype=np.float32) * NEGM, (1, 4))
    e_all = np.zeros((64, 32, 128), np.float32)
    for kt in range(32):
        e_all[2 * kt, kt, :64] = NEGM
        e_all[2 * kt + 1, kt, 64:] = NEGM
    c["eall"] = e_all.reshape(64, 32 * 128)
    c["mcq"] = np.where(j > p, -1e30, 0.0).astype(np.float32)
    cp_ = (np.arange(128) >= 64).astype(np.int64)[:, None]
    u = np.arange(128)[None, :] - 63
    c["wext"] = np.where((u == cp_) | (u == cp_ - 1), 1e4, np.where(u > cp_, -1e30, 0.0)).astype(np.float32)
    cs = 16 * np.arange(255)[:, None]
    ss = 64 * np.arange(64)[None, :]
    ov = np.minimum(cs + 32, ss + 64) - np.maximum(cs, ss)
    cm = np.zeros((256, 64), np.float32)
    cm[:255] = np.clip(ov, 0, None).astype(np.float32) / 16
    c["cmap"] = cm
    c["ustrict"] = (np.arange(128)[:, None] < np.arange(128)[None, :]).astype(np.float32)
    c["iota32"] = np.tile(np.arange(32, dtype=np.float32)[None, :], (128, 1))
    c["tokid"] = (np.arange(128)[:, None] + 128 * np.arange(32)[None, :]).astype(np.int32)
    return c


class Ctx:
    pass


def build(debug=None):
    nc = bass.Bass("TRN2", target_bir_lowering=False)
    es = ExitStack()
    p = Prog(nc, es)
    g = Ctx()
    g.nc, g.p, g.es, g.debug = nc, p, es, debug
    g.dbg_out = {}

    def din(name, shape, dt=F32):
        return Buf(nc.dram_tensor(name, list(shape), dt, kind="ExternalInput").ap(), p.res(name))

    def dscr(name, shape, dt=F32):
        kind = "ExternalOutput" if (debug and name in debug) else "Internal"
        return Buf(nc.dram_tensor(name, list(shape), dt, kind=kind).ap(), p.res(name))

    g.din, g.dscr = din, dscr

    def sb(name, shape, dt=F32, stack=None):
        t = (stack or es).enter_context(nc.sbuf_tensor("s_" + name, list(shape), dt))
        return Buf(t, p.res(name))

    def ps(name, shape, dt=F32, stack=None):
        t = (stack or es).enter_context(nc.psum_tensor(name, list(shape), dt))
        r = p.res(name)
        r.excl = True
        return Buf(t, r)

    g.sb, g.ps = sb, ps

    I = g.I = {}
    for name, shape in [
        ("xT", (D, T)), ("x", (T, D)), ("w_in", (D, D_IN)),
        ("pe_kT", (64, 32)), ("pe_vT", (64, 32)),
        ("cw1_k", (2048, 256)), ("cw2_k", (256, 64)), ("cw1_v", (2048, 256)), ("cw2_v", (256, 64)),
        ("ckv_g", (1, 128)), ("w_uk", (8, 64, 128)), ("w_uv", (8, 128, 64)), ("relflat", (1, 512)),
        ("w_ba", (512, D)), ("w_bb", (512, D)), ("w_out", (D, D)),
        ("ln1_g", (1, D)), ("ln1_b", (1, D)), ("wr", (D, 36)), ("br", (1, 36)),
        ("w_gate", (32, D, 256)), ("w_up", (32, D, 256)), ("w_down", (32, 256, D)),
        ("ln2_g", (1, D)), ("ln2_b", (1, D)),
        ("relTa", (128, 8, 2, 512)), ("relTb", (128, 8, 1024)), ("c31a", (2, 512)), ("c31b", (1, 1024)),
        ("cmpv", (128, 8, 503)), ("cmpm", (128, 503)), ("mc4", (128, 512)), ("mw4", (128, 512)),
        ("ident", (128, 128)), ("d30", (128, 512)), ("eall", (64, 4096)), ("mcq", (128, 128)),
        ("wext", (128, 128)), ("cmap", (256, 64)), ("ustrict", (128, 128)), ("iota32", (128, 32)),
    ]:
        I[name] = din(name, shape)
    I["tokid"] = din("tokid", (128, 32), I32)
    g.out = Buf(nc.dram_tensor("out", [T, D], F32, kind="ExternalOutput").ap(), p.res("out"))

    S = g.S = {}
    S["qa_s"] = dscr("qa_s", (NT, 2, 64, 4, 128), BF16)
    S["ql_s"] = dscr("ql_s", (NT, 128, 8, 128), BF16)
    S["qi_s"] = dscr("qi_s", (NT, 64, 4, 128), BF16)
    S["g_s"] = dscr("g_s", (NT, 128, 16, 128), F32)
    S["h1_s"] = dscr("h1_s", (T, D), F32)
    S["slot"] = dscr("slot", (32 * CAP, 16), I32)
    S["y_s"] = dscr("y_s", (32 * CAP, D), F32)

    Rz = g.Rz = {}
    Rz["ksT"] = sb("ksT", (128, T), BF16)
    Rz["kwT"] = sb("kwT", (128, T), BF16)
    Rz["ckvT"] = sb("ckvT", (128, T), BF16)
    Rz["kiT"] = sb("kiT", (64, T), BF16)
    Rz["vsA"] = sb("vsA", (128, NT, 2, 65), BF16)
    Rz["vwA"] = sb("vwA", (128, NT, 2, 65), BF16)
    Rz["ckvA"] = sb("ckvA", (128, NT, 129), BF16)
    Rz["gn"] = sb("gn", (128, NT, 24), F32)
    Rz["wabs"] = sb("wabs", (128, NT, 4), F32)
    Rz["wsgn"] = sb("wsgn", (128, NT, 4), F32)
    Rz["kcmpT"] = sb("kcmpT", (128, 256), BF16)
    Rz["vcmpM"] = sb("vcmpM", (128, 2, 2, 128), BF16)
    Rz["kmax"] = sb("kmax", (1, 4), F32)
    Rz["identb"] = sb("identb", (128, 128), BF16)
    Rz["identf"] = sb("identf", (128, 128), F32)
    Rz["ones_bf"] = sb("ones_bf", (128, 128), BF16)

    g.kcT = sb("kcT", (128, T), BF16)
    g.vcT = sb("vcT", (128, T), BF16)
    g.dest = sb("dest_i", (128, NT, 2), I32)
    g.wts = sb("wts", (128, NT, 2), F32)
    g.banks = [ps("bank%d" % i, (128, 512), F32) for i in range(8)]

    stage = (debug or {}).get("stage", 99)
    st = phase1(g)
    if debug:
        dump_resident(g)
    p.flush()
    st.close()
    if stage >= 2:
        st = phase2(g)
        p.flush()
        st.close()
    if stage >= 3:
        st = phase3(g)
        p.flush()
        st.close()
    if stage >= 4:
        st = phase4(g)
        p.flush()
        st.close()
    if stage >= 5:
        st = phase5(g)
        p.flush()
        st.close()
    return nc, g


def dump_resident(g):
    nc, p = g.nc, g.p
    for name, buf in g.Rz.items():
        shape = list(buf.t.shape)
        d = nc.dram_tensor("dbg_" + name, shape, buf.t.dtype, kind="ExternalOutput").ap()
        r = p.res("dbg_" + name)
        idx = tuple(slice(None) for _ in shape)
        p.dma("sp", d[idx], buf.t[idx], R=[buf.r], W=[r])


def phase1(g):
    nc, p, sb, I, S, Rz = g.nc, g.p, g.sb, g.I, g.S, g.Rz
    st = ExitStack()
    banks = g.banks
    bi = [0]

    def nbank():
        b = banks[bi[0] % 8]
        bi[0] += 1
        return b

    ev_rr = [0]

    def ev_eng():
        ev_rr[0] += 1
        return "act" if ev_rr[0] % 2 else "dve"

    stf = sb("p1_cst", (128, 128), F32, st)
    p.dma("sp", stf[:], I["ident"][:, :], R=[I["ident"].r], W=[stf.r])
    p.cp("dve", Rz["identb"][:], stf[:], R=[stf.r], W=[Rz["identb"].r])
    p.cp("dve", Rz["identf"][:], stf[:], R=[stf.r], W=[Rz["identf"].r])
    p.memset("dve", Rz["ones_bf"][:], 1.0, W=[Rz["ones_bf"].r])
    p.memset("pool", Rz["vsA"][:, :, :, 64:65], 1.0, W=[Rz["vsA"].r])
    p.memset("pool", Rz["vwA"][:, :, :, 64:65], 1.0, W=[Rz["vwA"].r])
    p.memset("pool", Rz["ckvA"][:, :, 128:129], 1.0, W=[Rz["ckvA"].r])

    xTb = sb("xTb", (128, 8, T), BF16, st)
    xst = [sb("xst%d" % i, (128, 1024), F32, st) for i in range(2)]
    xq = [p.res("xTq%d" % q) for q in range(4)]
    engs = ["dve", "act", "dve", "act"]
    xk = [0]

    def load_x(q):
        for c in range(8):
            s_ = xst[xk[0] % 2]
            xk[0] += 1
            p.dma("sp", s_[:], I["xT"][c * 128:(c + 1) * 128, q * 1024:(q + 1) * 1024], R=[I["xT"].r], W=[s_.r])
            p.cp(engs[c % 4], xTb[:, c, q * 1024:(q + 1) * 1024], s_[:], R=[s_.r], W=[xq[q]])

    cut = 99
    wst = [sb("wst%d" % i, (128, 8, 128), F32, st) for i in range(2)]
    wbf = [sb("wbf%d" % i, (128, 8, 128), BF16, st) for i in range(2)]
    wk = [0]

    def load_w_issue(col0, M):
        k = wk[0] % 2
        wk[0] += 1
        p.dma("sp", wst[k][:, :, 0:M], I["w_in"][:, col0:col0 + M].rearrange("(c p) m -> p c m", p=128),
              R=[I["w_in"].r], W=[wst[k].r])
        return k

    def load_w_cast(k, M):
        p.cp("act" if k else "dve", wbf[k][:, :, 0:M], wst[k][:, :, 0:M], R=[wst[k].r], W=[wbf[k].r])
        return wbf[k]

    groups = []

    def fm_group(col0, M, evac):
        groups.append((col0, M, evac))

    def run_groups():
        nxt = load_w_cast(load_w_issue(groups[0][0], groups[0][1]), groups[0][1])
        load_x(0)
        for gi, (col0, M, evac) in enumerate(groups):
            w = nxt
            kn = None
            if gi + 1 < len(groups):
                kn = load_w_issue(groups[gi + 1][0], groups[gi + 1][1])
            for tb in range(8):
                if gi == 0 and tb % 2 == 0 and tb < 6:
                    load_x(tb // 2 + 1)
                b = nbank()
                for c in range(8):
                    p.mm(b[0:M, :], w[:, c, 0:M], xTb[:, c, tb * 512:(tb + 1) * 512], c == 0, c == 7,
                         R=[w.r, xq[tb // 2]], W=[b.r])
                evac(tb, b)
            if kn is not None:
                nxt = load_w_cast(kn, groups[gi + 1][1])

    stg_bf = [sb("stgb%d" % i, (128, 512), BF16, st) for i in range(4)]
    stg_f = [sb("stgf%d" % i, (128, 512), F32, st) for i in range(2)] * 2
    sk = [0]

    def nstg(lst):
        sk[0] += 1
        return lst[sk[0] % 4]

    def evac_copy(dst_buf, scale=None):
        def f(tb, b):
            M = dst_buf.t.shape[0]
            e = ev_eng()
            o = dst_buf[:, tb * 512:(tb + 1) * 512]
            if e == "act":
                p.act(o, b[0:M, :], AF.Copy, R=[b.r], W=[dst_buf.r])
            else:
                p.cp("dve", o, b[0:M, :], R=[b.r], W=[dst_buf.r])
        return f

    for m in range(4):
        def ev(tb, b, m=m):
            s_ = nstg(stg_bf)
            p.act(s_[:], b[:], AF.Copy, scale=0.125, R=[b.r], W=[s_.r])
            gq, hh0 = m // 2, 2 * (m % 2)
            for hl in range(2):
                dst = S["qa_s"][tb * 4:(tb + 1) * 4, gq, :, hh0 + hl, :].rearrange("t d q -> d t q")
                src = s_[hl * 64:(hl + 1) * 64, :].rearrange("d (t q) -> d t q", q=128)
                p.dma("sp", dst, src, R=[s_.r], W=[S["qa_s"].r])
        fm_group(O_QA + 128 * m, 128, ev)

    kcT, vcT = g.kcT, g.vcT
    fm_group(O_KC, 128, evac_copy(kcT))
    fm_group(O_VC, 128, evac_copy(vcT))
    fm_group(O_KS, 128, evac_copy(Rz["ksT"]))
    fm_group(O_KW, 128, evac_copy(Rz["kwT"]))
    fm_group(O_KI, 64, evac_copy(Rz["kiT"]))

    wukf = sb("wukf", (128, 4, 128), F32, st)
    wukb = sb("wukb", (128, 4, 128), BF16, st)
    p.dma("sp", wukf[:], I["w_uk"][:, :, :].rearrange("(m hl) d r -> (hl d) m r", hl=2), R=[I["w_uk"].r], W=[wukf.r])
    p.cp("dve", wukb[:], wukf[:], R=[wukf.r], W=[wukb.r])
    for m in range(4):
        def ev(tb, b, m=m):
            s_ = nstg(stg_bf)
            p.cp("dve", s_[:], b[:], R=[b.r], W=[s_.r])
            for hl in range(2):
                b2 = nbank()
                p.mm(b2[:, :], wukb[hl * 64:(hl + 1) * 64, m, :], s_[hl * 64:(hl + 1) * 64, :], True, True,
                     R=[wukb.r, s_.r], W=[b2.r])
                s2 = nstg(stg_bf)
                p.act(s2[:], b2[:], AF.Copy, scale=0.125, R=[b2.r], W=[s2.r])
                dst = S["ql_s"][tb * 4:(tb + 1) * 4, :, 2 * m + hl, :].rearrange("t r q -> r t q")
                p.dma("sp", dst, s2[:].rearrange("r (t q) -> r t q", q=128), R=[s2.r], W=[S["ql_s"].r])
        fm_group(O_QB + 128 * m, 128, ev)

    for m in range(2):
        def ev(tb, b, m=m):
            s_ = nstg(stg_bf)
            p.act(s_[:], b[:], AF.Copy, scale=0.125, R=[b.r], W=[s_.r])
            for hl in range(2):
                dst = S["qi_s"][tb * 4:(tb + 1) * 4, :, 2 * m + hl, :].rearrange("t d q -> d t q")
                src = s_[hl * 64:(hl + 1) * 64, :].rearrange("d (t q) -> d t q", q=128)
                p.dma("sp", dst, src, R=[s_.r], W=[S["qi_s"].r])
        fm_group(O_QI + 128 * m, 128, ev)

    for m in range(16):
        def ev(tb, b, m=m):
            s_ = nstg(stg_f)
            p.act(s_[:], b[:], AF.Sigmoid, R=[b.r], W=[s_.r])
            dst = S["g_s"][tb * 4:(tb + 1) * 4, :, m, :].rearrange("t c q -> c t q")
            p.dma("sp", dst, s_[:].rearrange("c (t q) -> c t q", q=128), R=[s_.r], W=[S["g_s"].r])
        fm_group(O_GA + 128 * m, 128, ev)

    run_groups()
    wtf = sb("wtf", (128, 8, 412), F32, st)
    wtb = sb("wtb", (128, 8, 412), BF16, st)
    for (c0, n, o) in [(O_VS, 128, 0), (O_VW, 128, 128), (O_CKV, 128, 256), (O_GN, 24, 384), (O_WI, 4, 408)]:
        r_ = p.res("wtf%d" % o)
        p.dma("sp", wtf[:, :, o:o + n], I["w_in"][:, c0:c0 + n].rearrange("(c p) m -> p c m", p=128),
              R=[I["w_in"].r], W=[r_])
        p.cp("dve", wtb[:, :, o:o + n], wtf[:, :, o:o + n], R=[r_], W=[wtb.r])
    gbc = sb("gbc", (128, 128), F32, st)
    p.dma("sp", gbc[:], I["ckv_g"][0:1, :].partition_broadcast(128), R=[I["ckv_g"].r], W=[gbc.r])
    junk = sb("p1junk", (128, 128), F32, st)
    ssq = sb("p1ssq", (128, 4), F32, st)
    sub = (g.debug or {}).get("sub", 99)
    for tt in range(NT if sub >= 1 else 0):
        b = nbank()
        for c in range(8):
            p.mm(b[:, 0:412], xTb[:, c, tt * 128:(tt + 1) * 128], wtb[:, c, :], c == 0, c == 7,
                 R=[wtb.r, xq[tt // 8]], W=[b.r])
        p.cp("dve", Rz["vsA"][:, tt, :, 0:64], b[:, 0:128].rearrange("p (g d) -> p g d", g=2), R=[b.r], W=[Rz["vsA"].r])
        p.cp("dve", Rz["vwA"][:, tt, :, 0:64], b[:, 128:256].rearrange("p (g d) -> p g d", g=2), R=[b.r], W=[Rz["vwA"].r])
        if sub <= 1:
            continue
        p.act(junk[:], b[:, 256:384], AF.Square, R=[b.r], W=[junk.r, ssq.r], accum=ssq[:, 0:1])
        sub2 = (g.debug or {}).get("sub2", 99)
        if sub2 <= 0:
            continue
        p.ts("dve", ssq[:, 1:2], ssq[:, 0:1], 1.0 / 128, 1e-6, ALU.mult, ALU.add, R=[ssq.r], W=[ssq.r])
        if sub2 <= 1:
            continue
        p.act(ssq[:, 2:3], ssq[:, 1:2], AF.Sqrt, R=[ssq.r], W=[ssq.r])
        if sub2 <= 2:
            continue
        p.op("dve", lambda e: e.reciprocal(ssq[:, 3:4], ssq[:, 2:3]), R=[ssq.r], W=[ssq.r])
        if sub2 <= 3:
            continue
        p.stt(Rz["ckvA"][:, tt, 0:128], b[:, 256:384], ssq[:, 3:4], gbc[:], ALU.mult, ALU.mult,
              R=[b.r, ssq.r, gbc.r], W=[Rz["ckvA"].r])
        if sub <= 2:
            continue
        p.act(Rz["gn"][:, tt, :], b[:, 384:408], AF.Sigmoid, R=[b.r], W=[Rz["gn"].r])
        p.act(Rz["wabs"][:, tt, :], b[:, 408:412], AF.Abs, scale=0.5, R=[b.r], W=[Rz["wabs"].r])
        p.act(Rz["wsgn"][:, tt, :], b[:, 408:412], AF.Sign, R=[b.r], W=[Rz["wsgn"].r])
        if sub <= 3:
            continue
        b2 = nbank()
        tv = b2.t[:].bitcast(BF16)
        p.tr(tv[:, 0:128], Rz["ckvA"][:, tt, 0:128], Rz["identb"][:], R=[Rz["ckvA"].r, Rz["identb"].r], W=[b2.r])
        p.cp("act", Rz["ckvT"][:, tt * 128:(tt + 1) * 128], tv[:, 0:128], R=[b2.r], W=[Rz["ckvT"].r])

    if cut <= 6:
        return st
    p.flush()
    st.close()
    st = ExitStack()
    phase1b(g, st, kcT, vcT, nbank)
    return st


def phase1b(g, st, kcT, vcT, nbank):
    nc, p, sb, I, S, Rz = g.nc, g.p, g.sb, g.I, g.S, g.Rz
    w1s = sb("w1s", (128, 8, 256), F32, st)
    w2s = sb("w2s", (128, 2, 64), F32, st)
    pes = sb("pes", (128, 32), F32, st)
    peb = sb("peb", (128, 32), BF16, st)
    cst = sb("cst", (128, 2), F32, st)
    u = sb("cu", (128, 256), F32, st)
    t1 = sb("ct1", (128, 256), F32, st)
    t2 = sb("ct2", (128, 256), F32, st)
    cms = sb("cms", (128, 2, 64), F32, st)
    p.dma("sp", cms[:], I["cmap"][:, :].rearrange("(c p) n -> p c n", p=128), R=[I["cmap"].r], W=[cms.r])
    for gq in range(2):
        p.cp("dve", Rz["vcmpM"][:, :, gq, 64:128], cms[:], R=[cms.r], W=[Rz["vcmpM"].r])
    for kv, (srcT, w1n, w2n, pen) in enumerate([(kcT, "cw1_k", "cw2_k", "pe_kT"), (vcT, "cw1_v", "cw2_v", "pe_vT")]):
        w1b = sb("w1b%d" % kv, (128, 32, 256), BF16, st)
        w2p = sb("w2p%d" % kv, (128, 2, 2, 128), BF16, st)
        w2b = sb("w2b%d" % kv, (128, 2, 64), BF16, st)
        gel = sb("gel%d" % kv, (128, 2, 2, 256), BF16, st)
        p.memset("pool", gel[:], 0.0, W=[gel.r])
        p.memset("pool", w2p[:], 0.0, W=[w2p.r])
        for lq in range(4):
            for half in range(2):
                p.dma("sp", w1s[half * 64:(half + 1) * 64, :, :],
                      I[w1n][lq * 512:(lq + 1) * 512, :].rearrange("(l d) h -> d l h", d=64),
                      R=[I[w1n].r], W=[w1s.r])
            p.cp("act", w1b[:, lq * 8:(lq + 1) * 8, :], w1s[:], R=[w1s.r], W=[w1b.r])
        p.dma("sp", w2s[:], I[w2n][:, :].rearrange("(c p) d -> p c d", p=128), R=[I[w2n].r], W=[w2s.r])
        p.cp("dve", w2b[:], w2s[:], R=[w2s.r], W=[w2b.r])
        for gq in range(2):
            p.cp("dve", w2p[:, :, gq, gq * 64:(gq + 1) * 64], w2s[:], R=[w2s.r], W=[w2p.r])
        for half in range(2):
            p.dma("sp", pes[half * 64:(half + 1) * 64, :], I[pen][:, :], R=[I[pen].r], W=[pes.r])
        p.cp("dve", peb[:], pes[:], R=[pes.r], W=[peb.r])
        for gq in range(2):
            rows = slice(gq * 64, (gq + 1) * 64)
            for hc in range(2):
                bH, bC = nbank(), nbank()
                for l in range(32):
                    p.mm(bH[:, 0:255], w1b[rows, l, hc * 128:(hc + 1) * 128], srcT.t[rows, l:l + 16 * 254 + 1:16],
                         l == 0, l == 31, R=[w1b.r, srcT.r], W=[bH.r])
                for l in range(32):
                    p.mm(bC[:, 0:1], w1b[rows, l, hc * 128:(hc + 1) * 128], peb[rows, l:l + 1],
                         l == 0, l == 31, R=[w1b.r, peb.r], W=[bC.r])
                p.cp("dve", cst[:, 0:1], bC[:, 0:1], R=[bC.r], W=[cst.r])
                p.act(u[:, 0:255], bH[:, 0:255], AF.Identity, bias=cst[:, 0:1], R=[bH.r, cst.r], W=[u.r])
                p.tt("dve", t1[:, 0:255], u[:, 0:255], u[:, 0:255], ALU.mult, R=[u.r], W=[t1.r])
                p.ts("dve", t1[:, 0:255], t1[:, 0:255], 0.044715, 1.0, ALU.mult, ALU.add, R=[t1.r], W=[t1.r])
                p.tt("dve", t1[:, 0:255], t1[:, 0:255], u[:, 0:255], ALU.mult, R=[t1.r, u.r], W=[t1.r])
                p.act(t2[:, 0:255], t1[:, 0:255], AF.Tanh, scale=0.7978845608028654, R=[t1.r], W=[t2.r])
                p.stt(t2[:, 0:255], t2[:, 0:255], 1.0, u[:, 0:255], ALU.add, ALU.mult, R=[t2.r, u.r], W=[t2.r])
                p.ts("dve", gel[:, gq, hc, 0:255], t2[:, 0:255], 0.5, None, ALU.mult, R=[t2.r], W=[gel.r])
        if kv == 0:
            b = nbank()
            n = 0
            for gq in range(2):
                for hc in range(2):
                    p.mm(b[:, 0:256], w2p[:, hc, gq, :], gel[:, gq, hc, :], n == 0, n == 3, R=[w2p.r, gel.r], W=[b.r])
                    n += 1
            p.cp("dve", Rz["kcmpT"][:], b[:, 0:256], R=[b.r], W=[Rz["kcmpT"].r])
        else:
            for gq in range(2):
                for cc in range(2):
                    b = nbank()
                    for hc in range(2):
                        p.mm(b[:, 0:64], gel[:, gq, hc, cc * 128:(cc + 1) * 128], w2b[:, hc, :], hc == 0, hc == 1,
                             R=[gel.r, w2b.r], W=[b.r])
                    p.cp("dve", Rz["vcmpM"][:, cc, gq, 0:64], b[:, 0:64], R=[b.r], W=[Rz["vcmpM"].r])

    sq = sb("sq", (128, T), BF16, st)
    row = sb("kmrow", (1, T), F32, st)
    tmp = sb("kmtmp", (1, 8), F32, st)
    rl = sb("relrow", (1, 512), F32, st)
    p.dma("sp", rl[:], I["relflat"][:, :], R=[I["relflat"].r], W=[rl.r])
    p.act(rl[:], rl[:], AF.Abs, R=[rl.r], W=[rl.r])
    p.op("dve", lambda e: e.reduce_max(tmp[:, 0:1], rl[:], AX.X), R=[rl.r], W=[tmp.r])
    for which, srcs in enumerate([(Rz["ksT"], Rz["kwT"]), (Rz["ckvT"],)]):
        first = True
        for s_ in srcs:
            p.tt("dve", sq[:], s_[:], s_[:], ALU.mult, R=[s_.r], W=[sq.r])
            for kb in range(8):
                b = nbank()
                p.mm(b[0:1, :], Rz["ones_bf"][:, 0:1], sq[:, kb * 512:(kb + 1) * 512], True, True,
                     R=[sq.r, Rz["ones_bf"].r], W=[b.r])
                if first:
                    p.cp("dve", row[:, kb * 512:(kb + 1) * 512], b[0:1, :], R=[b.r], W=[row.r])
                else:
                    p.tt("dve", row[:, kb * 512:(kb + 1) * 512], row[:, kb * 512:(kb + 1) * 512], b[0:1, :], ALU.add,
                         R=[b.r, row.r], W=[row.r])
            first = False
        p.op("dve", lambda e: e.reduce_max(tmp[:, 1:2], row[:], AX.X), R=[row.r], W=[tmp.r])
        p.act(tmp[:, 2:3], tmp[:, 1:2], AF.Sqrt, R=[tmp.r], W=[tmp.r])
        p.ts("dve", Rz["kmax"][:, 2 * which:2 * which + 1], tmp[:, 2:3], -1.03, None, ALU.mult, R=[tmp.r], W=[Rz["kmax"].r])
        p.ts("dve", Rz["kmax"][:, 2 * which + 1:2 * which + 2], tmp[:, 0:1], -1.0, None, ALU.mult, R=[tmp.r], W=[Rz["kmax"].r])


def phase2(g):
    nc, p, sb, I, S, Rz = g.nc, g.p, g.sb, g.I, g.S, g.Rz
    st = ExitStack()
    B = g.banks
    ident, identf, ones = Rz["identb"], Rz["identf"], Rz["ones_bf"]
    S["oT_s"] = g.dscr("oT_s", (NT, 128, 8, 128), BF16)
    ntiles = (g.debug or {}).get("ntiles", NT)

    stg = sb("c_stg", (128, 1024), F32, st)
    relTa = sb("relTa", (128, 8, 2, 512), BF16, st)
    relTb = sb("relTb", (128, 8, 1024), BF16, st)
    relTw = sb("relTw", (128, 2, 512), BF16, st)
    eall = sb("eall", (128, 32, 128), BF16, st)
    onesN = sb("onesN", (128, 128), BF16, st)
    p.memset("pool", eall[:], 0.0, W=[eall.r])
    p.memset("pool", onesN[:], 0.0, W=[onesN.r])
    p.memset("pool", onesN[0:1, :], 1.0, W=[onesN.r])
    d30 = sb("d30", (128, 512), BF16, st)
    mcmp = sb("mcmp", (128, 8, 503), BF16, st)
    wext = sb("wext", (128, 128), F32, st)
    mcq = sb("mcq", (128, 128), F32, st)
    rowsA = [sb("rowsA%d" % i, (128, 512), BF16, st) for i in range(2)]
    rowsB = sb("rowsB", (128, 1024), BF16, st)
    for r_ in rowsA + [rowsB]:
        p.memset("pool", r_[:], 0.0, W=[r_.r])
    wuvP = sb("wuvP", (128, 8, 128), BF16, st)
    st0 = ExitStack()
    mc4 = sb("mc4", (128, 512), F32, st0)
    mw4 = sb("mw4", (128, 512), F32, st0)
    c31A = sb("c31A", (128, 2, 512), F32, st0)
    c31B = sb("c31B", (128, 1024), F32, st0)
    p.dma("sp", mc4[:], I["mc4"][:, :], R=[I["mc4"].r], W=[mc4.r])
    p.dma("sp", mw4[:], I["mw4"][:, :], R=[I["mw4"].r], W=[mw4.r])
    p.dma("sp", wext[:], I["wext"][:, :], R=[I["wext"].r], W=[wext.r])
    p.dma("sp", mcq[:], I["mcq"][:, :], R=[I["mcq"].r], W=[mcq.r])
    for gq in range(2):
        p.dma("sp", c31A[:, gq, :], I["c31a"][gq:gq + 1, :].partition_broadcast(128), R=[I["c31a"].r], W=[c31A.r])
    p.dma("sp", c31B[:], I["c31b"][0:1, :].partition_broadcast(128), R=[I["c31b"].r], W=[c31B.r])
    for d in range(8):
        for gq in range(2):
            p.dma("sp", stg[:, 0:512], I["relTa"][:, d, gq, :], R=[I["relTa"].r], W=[stg.r])
            p.tt("dve", stg[:, 0:512], stg[:, 0:512], c31A[:, gq, :], ALU.subtract, R=[stg.r, c31A.r], W=[stg.r])
            if d == 0:
                p.tt("dve", stg[:, 0:512], stg[:, 0:512], mc4[:], ALU.add, R=[stg.r, mc4.r], W=[stg.r])
            p.cp("dve", relTa[:, d, gq, :], stg[:, 0:512], R=[stg.r], W=[relTa.r])
            if d == 4:
                p.tt("dve", stg[:, 0:512], stg[:, 0:512], mw4[:], ALU.add, R=[stg.r, mw4.r], W=[stg.r])
                p.cp("dve", relTw[:, gq, :], stg[:, 0:512], R=[stg.r], W=[relTw.r])
        p.dma("sp", stg[:], I["relTb"][:, d, :], R=[I["relTb"].r], W=[stg.r])
        p.tt("dve", stg[:], stg[:], c31B[:], ALU.subtract, R=[stg.r, c31B.r], W=[stg.r])
        if d == 0:
            for hf in range(2):
                p.tt("dve", stg[:, hf * 512:(hf + 1) * 512], stg[:, hf * 512:(hf + 1) * 512], mc4[:], ALU.add,
                     R=[stg.r, mc4.r], W=[stg.r])
        p.cp("dve", relTb[:, d, :], stg[:], R=[stg.r], W=[relTb.r])
    for q4 in range(4):
        p.dma("sp", stg[0:64, :], I["eall"][:, q4 * 1024:(q4 + 1) * 1024], R=[I["eall"].r], W=[stg.r])
        p.cp("dve", eall[0:64, q4 * 8:(q4 + 1) * 8, :], stg[0:64, :].rearrange("p (a b) -> p a b", b=128), R=[stg.r], W=[eall.r])
    p.dma("sp", stg[:, 0:512], I["d30"][:, :], R=[I["d30"].r], W=[stg.r])
    p.cp("dve", d30[:], stg[:, 0:512], R=[stg.r], W=[d30.r])
    for h in range(8):
        p.dma("sp", stg[:, 0:503], I["cmpv"][:, h, :], R=[I["cmpv"].r], W=[stg.r])
        p.dma("sp", stg[:, 512:1015], I["cmpm"][:, :], R=[I["cmpm"].r], W=[stg.r])
        p.tt("dve", mcmp[:, h, :], stg[:, 0:503], stg[:, 512:1015], ALU.add, R=[stg.r], W=[mcmp.r])
    p.memset("dve", eall[64:65, :, :], 1.0, W=[eall.r])
    p.memset("pool", wuvP[:], 0.0, W=[wuvP.r])
    for h in range(8):
        p.dma("sp", stg[:, 0:64], I["w_uv"][h, :, :], R=[I["w_uv"].r], W=[stg.r])
        p.cp("dve", wuvP[:, h, (h % 2) * 64:(h % 2) * 64 + 64], stg[:, 0:64], R=[stg.r], W=[wuvP.r])

    p.flush()
    st0.close()
    qTz = [sb("qTz%d" % i, (128, 512), BF16, st) for i in range(2)]
    for q_ in qTz:
        p.memset("pool", q_[:], 0.0, W=[q_.r])
    qlT = sb("qlT", (128, 1024), BF16, st)
    qiT = sb("qiT", (64, 512), BF16, st)
    zidx = sb("zidx", (128, T), F32, st)
    selD = Buf(g.kcT.t, g.kcT.r)
    junk = Buf(g.vcT.t, g.vcT.r)
    rr = [sb("rr%d" % i, (128, 512), F32, st) for i in range(2)]
    sq = sb("sqq", (128, 1024), BF16, st)
    srow = Buf(stg.t[0:1, :], stg.r)
    scmp = sb("scmp", (128, 8, 256), F32, st)
    pn = sb("pn", (128, 8, 256), BF16, st)
    pnT = sb("pnT", (128, 16, 128), BF16, st)
    sm = sb("sm", (128, 16), F32, st)
    sm2 = sb("sm2", (128, 16), F32, st)
    imp = sb("imp", (128, 64), F32, st)
    sc1 = sb("sc1", (128, 64), F32, st)
    sc2 = sb("sc2", (128, 64), F32, st)
    m8 = sb("m8", (128, 16), F32, st)
    selb = sb("selb", (128, 64), BF16, st)
    selT4 = [sb("selT4%d" % i, (128, 512), BF16, st) for i in range(2)]
    for s_ in selT4:
        p.memset("pool", s_[:], 0.0, W=[s_.r])
    PT = [sb("PT%d" % i, (128, 512), BF16, st) for i in range(4)]
    ocmp = sb("ocmp", (128, 512), F32, st)
    oa32 = sb("oa32", (128, 512), F32, st)
    oab = sb("oab", (128, 512), BF16, st)
    coef = sb("coef", (128, 32), F32, st)
    oln = sb("oln", (128, 8, 128), BF16, st)
    olT = sb("olT", (128, 8, 128), BF16, st)
    oT = sb("oT", (128, 8, 128), BF16, st)
    bis = sb("bis", (128, 8), F32, st)
    p.memset("pool", pn[:], 0.0, W=[pn.r])
    p.memset("pool", scmp[:], 0.0, W=[scmp.r])

    def bfv(bank):
        return bank.t[:].bitcast(BF16)

    selDs = [selD, junk]
    pw = sb("pw", (128, BIS_ITERS + 1), F32, st)
    wct = sb("wct", (128, BIS_ITERS + 1), F32, st)
    for k in range(BIS_ITERS + 1):
        p.memset("pool", pw[:, k:k + 1], 2.0 ** -(k + 1), W=[pw.r])

    def stream_D(i):
        nk = 128 * (i + 1)
        sD = selDs[i % 2]
        p.dma("sp", qiT[:], S["qi_s"][i, :, :, :].rearrange("d h q -> d (h q)"), R=[S["qi_s"].r], W=[qiT.r])
        for kb in range((nk + 511) // 512):
            k0 = kb * 512
            kn = min(512, nk - k0)
            for hi in range(4):
                bk = B[hi % 2]
                r_ = rr[hi % 2]
                p.mm(bk[:, 0:kn], qiT[:, hi * 128:(hi + 1) * 128], Rz["kiT"][:, k0:k0 + kn], True, True,
                     R=[qiT.r, Rz["kiT"].r], W=[bk.r])
                p.act(r_[:, 0:kn], bk[:, 0:kn], AF.Relu, scale=Rz["wabs"][:, i, hi:hi + 1], R=[bk.r, Rz["wabs"].r], W=[r_.r])
                if hi == 0:
                    p.ts("dve", zidx[:, k0:k0 + kn], r_[:, 0:kn], Rz["wsgn"][:, i, 0:1], None, ALU.mult,
                         R=[r_.r, Rz["wsgn"].r], W=[zidx.r])
                else:
                    p.stt(zidx[:, k0:k0 + kn], r_[:, 0:kn], Rz["wsgn"][:, i, hi:hi + 1], zidx[:, k0:k0 + kn], ALU.mult, ALU.add,
                          R=[r_.r, Rz["wsgn"].r, zidx.r], W=[zidx.r])
            yield
        p.op("dve", lambda e: e.tensor_reduce(bis[:, 0:1], zidx[:, 0:nk], AX.X, ALU.min), R=[zidx.r], W=[bis.r])
        p.op("dve", lambda e: e.reduce_max(bis[:, 1:2], zidx[:, 0:nk], AX.X), R=[zidx.r], W=[bis.r])
        p.stt(bis[:, 2:3], bis[:, 1:2], 1.0, bis[:, 0:1], ALU.add, ALU.subtract, R=[bis.r], W=[bis.r])
        p.ts("dve", wct[:], pw[:], bis[:, 2:3], None, ALU.mult, R=[pw.r, bis.r], W=[wct.r])
        p.tt("dve", bis[:, 3:4], bis[:, 0:1], wct[:, 0:1], ALU.add, R=[bis.r, wct.r], W=[bis.r])
        p.tt("dve", zidx[:, nk - 128:nk], zidx[:, nk - 128:nk], mcq[:], ALU.add, R=[zidx.r, mcq.r], W=[zidx.r])
        yield
        for k in range(BIS_ITERS):
            p.ts("dve", sD[:, 0:nk], zidx[:, 0:nk], bis[:, 3:4], None, ALU.is_ge, ALU.add, R=[zidx.r, bis.r], W=[sD.r, bis.r],
                 accum=bis[:, 4:5])
            p.stt(bis[:, 5:6], bis[:, 4:5], 256.0, wct[:, k:k + 1], ALU.is_ge, ALU.mult, R=[bis.r, wct.r], W=[bis.r])
            p.ts("dve", bis[:, 3:4], bis[:, 3:4], wct[:, k + 1:k + 2], bis[:, 5:6], ALU.subtract, ALU.add, R=[bis.r, wct.r], W=[bis.r])
            yield
        p.tt("dve", bis[:, 6:7], bis[:, 3:4], wct[:, BIS_ITERS:BIS_ITERS + 1], ALU.subtract, R=[bis.r, wct.r], W=[bis.r])
        p.ts("dve", sD[:, 0:nk], zidx[:, 0:nk], bis[:, 6:7], 1.0, ALU.is_ge, ALU.subtract, R=[zidx.r, bis.r], W=[sD.r])
        yield

    def stream_P(i):
        sD = selDs[i % 2]
        for gq in range(2):
            p.dma("sp", qTz[gq][gq * 64:(gq + 1) * 64, :], S["qa_s"][i, gq, :, :, :].rearrange("d h q -> d (h q)"),
                  R=[S["qa_s"].r], W=[qTz[gq].r])
        p.dma("sp", qlT[:], S["ql_s"][i, :, :, :].rearrange("r h q -> r (h q)"), R=[S["ql_s"].r], W=[qlT.r])
        for gq in range(2):
            p.act(sq[:, 0:512], qTz[gq][:], AF.Square, R=[qTz[gq].r], W=[sq.r])
            p.mm(B[7][0:1, :], ones[:, 0:1], sq[:, 0:512], True, True, R=[ones.r, sq.r], W=[B[7].r])
            p.act(srow[:, 0:512], B[7][0:1, :], AF.Sqrt, R=[B[7].r], W=[srow.r])
            p.ts("dve", rowsA[gq][0:1, :], srow[:, 0:512], Rz["kmax"][0:1, 0:1], Rz["kmax"][0:1, 1:2], ALU.mult, ALU.add,
                 R=[srow.r, Rz["kmax"].r], W=[rowsA[gq].r])
            p.dma("sp", selT4[gq][64:65, :], rowsA[gq][0:1, :], R=[rowsA[gq].r], W=[selT4[gq].r])
        p.act(sq[:], qlT[:], AF.Square, R=[qlT.r], W=[sq.r])
        for hf in range(2):
            p.mm(B[7][0:1, :], ones[:, 0:1], sq[:, hf * 512:(hf + 1) * 512], True, True, R=[ones.r, sq.r], W=[B[7].r])
            p.act(srow[:, hf * 512:(hf + 1) * 512], B[7][0:1, :], AF.Sqrt, R=[B[7].r], W=[srow.r])
        p.ts("dve", rowsB[0:1, :], srow[:], Rz["kmax"][0:1, 2:3], Rz["kmax"][0:1, 3:4], ALU.mult, ALU.add,
             R=[srow.r, Rz["kmax"].r], W=[rowsB.r])
        yield
        off = 248 - 8 * i
        for h in range(8):
            gq, hh = h // 4, h % 4
            rows = slice(gq * 64, (gq + 1) * 64)
            bL = B[2 + h // 2]
            p.mm(bL[:, (h % 2) * 256:(h % 2) * 256 + 255], qTz[gq][rows, hh * 128:(hh + 1) * 128], Rz["kcmpT"][rows, 0:255],
                 h % 2 == 0, h % 2 == 1, R=[qTz[gq].r, Rz["kcmpT"].r], W=[bL.r])
        for b4 in range(4):
            bL = B[2 + b4]
            p.tt("dve", scmp[:, 2 * b4:2 * b4 + 2, 0:255], bL[:, :].rearrange("p (h c) -> p h c", c=256)[:, :, 0:255],
                 mcmp[:, 2 * b4:2 * b4 + 2, off:off + 255], ALU.add, R=[bL.r, mcmp.r], W=[scmp.r])
        yield
        p.op("dve", lambda e: e.reduce_max(sm[:, 0:8], scmp[:, :, 0:255], AX.X), R=[scmp.r], W=[sm.r])
        p.ts("dve", sm[:, 8:16], sm[:, 0:8], -1000.0, -1.0, ALU.max, ALU.mult, R=[sm.r], W=[sm.r])
        p.tt("dve", scmp[:, :, 0:255], scmp[:, :, 0:255], sm[:, 8:16].unsqueeze(2).to_broadcast([128, 8, 255]), ALU.add,
             R=[scmp.r, sm.r], W=[scmp.r])
        p.act(scmp[:, :, 0:255], scmp[:, :, 0:255], AF.Exp, R=[scmp.r], W=[scmp.r])
        yield
        p.op("dve", lambda e: e.reduce_sum(sm2[:, 0:8], scmp[:, :, 0:255], AX.X), R=[scmp.r], W=[sm2.r])
        p.ts("dve", sm2[:, 0:8], sm2[:, 0:8], 1e-30, None, ALU.max, R=[sm2.r], W=[sm2.r])
        p.op("dve", lambda e: e.reciprocal(sm2[:, 8:16], sm2[:, 0:8]), R=[sm2.r], W=[sm2.r])
        p.tt("dve", pn[:, :, 0:255], scmp[:, :, 0:255], sm2[:, 8:16].unsqueeze(2).to_broadcast([128, 8, 255]), ALU.mult,
             R=[scmp.r, sm2.r], W=[pn.r])
        yield
        for hf in range(2):
            bk = B[2 + hf]
            tb_ = bfv(bk)
            for j in range(8):
                h, cc = hf * 4 + j // 2, j % 2
                p.tr(tb_[:, j * 128:(j + 1) * 128], pn[:, h, cc * 128:(cc + 1) * 128], ident[:], R=[pn.r, ident.r], W=[bk.r])
            p.cp("act" if hf else "dve", pnT[:, hf * 8:(hf + 1) * 8, :], tb_[:, :].rearrange("p (c q) -> p c q", q=128), R=[bk.r], W=[pnT.r])
        yield
        for gq in range(2):
            accb = B[6 + gq]
            for hh in range(4):
                h = gq * 4 + hh
                for cc in range(2):
                    p.mm(accb[:, hh * 128:(hh + 1) * 128], pnT[:, 2 * h + cc, :], Rz["vcmpM"][:, cc, gq, :], hh == 0 and cc == 0, hh == 3 and cc == 1,
                         R=[pnT.r, Rz["vcmpM"].r], W=[accb.r])
            p.cp("act", ocmp[:, gq * 256:(gq + 1) * 256].rearrange("p (h d) -> p h d", d=64),
                 accb[:, :].rearrange("p (h j) -> p h j", j=128)[:, :, 0:64], R=[accb.r], W=[ocmp.r])
            p.op("dve", lambda e, accb=accb: e.reduce_sum(imp[:], accb[:, :].rearrange("p (h j) -> p j h", j=128)[:, 64:128, :], AX.X),
                 R=[accb.r], W=[imp.r])
            p.tt("dve", sc1[:], imp[:], wext[:, 63 - 2 * i:127 - 2 * i], ALU.add, R=[imp.r, wext.r], W=[sc1.r])
            p.ts("dve", sc1[:, 0:1], imp[:, 0:1], 1e4, None, ALU.add, R=[imp.r], W=[sc1.r])
            p.op("dve", lambda e: e.max(m8[:, 0:8], sc1[:]), R=[sc1.r], W=[m8.r])
            p.op("dve", lambda e: e.match_replace(sc2[:], m8[:, 0:8], sc1[:], -1e30), R=[sc1.r, m8.r], W=[sc2.r])
            p.op("dve", lambda e: e.max(m8[:, 8:16], sc2[:]), R=[sc2.r], W=[m8.r])
            p.ts("dve", m8[:, 15:16], m8[:, 15:16], -1e29, None, ALU.max, R=[m8.r], W=[m8.r])
            p.ts("dve", selb[:], sc1[:], m8[:, 15:16], 1.0, ALU.is_ge, ALU.subtract, R=[sc1.r, m8.r], W=[selb.r])
            tb2 = bfv(accb)
            p.tr(tb2[0:64, 0:128], selb[:], ident[:], R=[selb.r, ident.r], W=[accb.r])
            p.cp("dve", selT4[gq][0:64, :].rearrange("p (h q) -> p h q", q=128),
                 tb2[0:64, 0:128].unsqueeze(1).to_broadcast([64, 4, 128]), R=[accb.r], W=[selT4[gq].r])
            yield
        DEPTH = 4
        sbanks = [B[2], B[3], B[6], B[7]]
        units = []
        for gq in range(2):
            for br_ in range(2):
                kts = list(range(0, i + 1)) if br_ == 0 else list(range(max(0, i - 4), i + 1))
                for n, kt in enumerate(kts):
                    units.append((gq, br_, n, kt, len(kts)))

        def emit_S(ui):
            gq, br_, n, kt, nk_ = units[ui]
            bS = sbanks[ui % DEPTH]
            kT = Rz["ksT"] if br_ == 0 else Rz["kwT"]
            d = i - kt
            p.mm(bS[:, :], kT[:, kt * 128:(kt + 1) * 128], qTz[gq][:], True, False, R=[kT.r, qTz[gq].r], W=[bS.r])
            if br_ == 0:
                p.mm(bS[:, :], eall[:, kt, :], selT4[gq][:], False, d >= 8, R=[eall.r, selT4[gq].r], W=[bS.r])
            if d < 8:
                rel = relTw[:, gq, :] if (br_ == 1 and d == 4) else relTa[:, d, gq, :]
                p.mm(bS[:, :], ident[:], rel, False, br_ == 0, R=[ident.r, relTa.r, relTw.r], W=[bS.r])
            if br_ == 1:
                p.mm(bS[:, :], onesN[:], rowsA[gq][:], False, True, R=[onesN.r, rowsA[gq].r], W=[bS.r])

        def epilogue(gq, br_):
            accb = B[4 + br_]
            accv = accb[:, 0:260].rearrange("p (h e) -> p h e", e=65)
            p.ts("dve", coef[:, 0:4], accv[:, :, 64], 1e-30, None, ALU.max, R=[accb.r], W=[coef.r])
            p.op("dve", lambda e: e.reciprocal(coef[:, 4:8], coef[:, 0:4]), R=[coef.r], W=[coef.r])
            gv = Rz["gn"][:, i, gq * 12:(gq + 1) * 12].rearrange("p (h t) -> p h t", t=3)
            p.tt("dve", coef[:, 8:12], coef[:, 4:8], gv[:, :, 1 + br_], ALU.mult, R=[coef.r, Rz["gn"].r], W=[coef.r])
            for hh in range(4):
                h = gq * 4 + hh
                if br_ == 0:
                    p.ts("dve", oa32[:, h * 64:(h + 1) * 64], ocmp[:, h * 64:(h + 1) * 64], Rz["gn"][:, i, 3 * h:3 * h + 1], None, ALU.mult,
                         R=[ocmp.r, Rz["gn"].r], W=[oa32.r])
                    p.stt(oa32[:, h * 64:(h + 1) * 64], accv[:, hh, 0:64], coef[:, 8 + hh:9 + hh], oa32[:, h * 64:(h + 1) * 64], ALU.mult, ALU.add,
                          R=[accb.r, coef.r, oa32.r], W=[oa32.r])
                else:
                    p.stt(oab[:, h * 64:(h + 1) * 64], accv[:, hh, 0:64], coef[:, 8 + hh:9 + hh], oa32[:, h * 64:(h + 1) * 64], ALU.mult, ALU.add,
                          R=[accb.r, coef.r, oa32.r], W=[oab.r])

        pend = []
        for ui in range(min(DEPTH - 1, len(units))):
            emit_S(ui)
        for ui, (gq, br_, n, kt, nk_) in enumerate(units):
            bS = sbanks[ui % DEPTH]
            pt = PT[ui % DEPTH]
            accb = B[4 + br_]
            vA = Rz["vsA"] if br_ == 0 else Rz["vwA"]
            p.act(pt[:], bS[:, :], AF.Exp, R=[bS.r], W=[pt.r])
            if ui + DEPTH - 1 < len(units):
                emit_S(ui + DEPTH - 1)
            for hh in range(4):
                p.mm(accb[:, hh * 65:(hh + 1) * 65], pt[:, hh * 128:(hh + 1) * 128], vA[:, kt, gq, :],
                     n == 0 and hh == 0, n == nk_ - 1, R=[pt.r, vA.r], W=[accb.r])
            if n == nk_ - 1:
                pend.append([2, gq, br_])
            for pe_ in list(pend):
                if pe_[0] == 0:
                    epilogue(pe_[1], pe_[2])
                    pend.remove(pe_)
                else:
                    pe_[0] -= 1
            yield
        for pe_ in pend:
            epilogue(pe_[1], pe_[2])
        tb_ = bfv(B[7])
        for c in range(4):
            p.tr(tb_[:, c * 128:(c + 1) * 128], oab[:, c * 128:(c + 1) * 128], ident[:], R=[oab.r, ident.r], W=[B[7].r])
        p.cp("act", oT[:, 0:4, :], tb_[:, 0:512].rearrange("p (c q) -> p c q", q=128), R=[B[7].r], W=[oT.r])
        yield
        accD = [B[4], B[5], B[6]]
        hb = [(0, 0), (0, 1), (0, 2), (1, 0), (1, 1), (1, 2), (2, 0), (2, 1)]
        dunits = [(kt, hf) for kt in range(i + 1) for hf in range(2)]

        dbanks = [B[2], B[3], B[7]]

        def emit_SD(ui):
            kt, hf = dunits[ui]
            d = i - kt
            bS = dbanks[ui % 3]
            cols = slice(hf * 512, (hf + 1) * 512)
            p.mm(bS[:, :], Rz["ckvT"][:, kt * 128:(kt + 1) * 128], qlT[:, cols], True, False, R=[Rz["ckvT"].r, qlT.r], W=[bS.r])
            p.mm(bS[:, :], sD[:, kt * 128:(kt + 1) * 128], d30[:], False, False, R=[sD.r, d30.r], W=[bS.r])
            if d < 8:
                p.mm(bS[:, :], ident[:], relTb[:, d, cols], False, False, R=[ident.r, relTb.r], W=[bS.r])
            p.mm(bS[:, :], onesN[:], rowsB[:, cols], False, True, R=[onesN.r, rowsB.r], W=[bS.r])

        DD = 3
        for ui in range(min(DD - 1, len(dunits))):
            emit_SD(ui)
        for ui, (kt, hf) in enumerate(dunits):
            bS = dbanks[ui % DD]
            pt = PT[ui % DD]
            p.act(pt[:], bS[:, :], AF.Exp, R=[bS.r], W=[pt.r])
            if ui + DD - 1 < len(dunits):
                emit_SD(ui + DD - 1)
            for hh in range(4):
                h = hf * 4 + hh
                bk, sl = hb[h]
                p.mm(accD[bk][:, sl * 129:(sl + 1) * 129], pt[:, hh * 128:(hh + 1) * 128], Rz["ckvA"][:, kt, :],
                     kt == 0 and sl == 0, kt == i, R=[pt.r, Rz["ckvA"].r], W=[accD[bk].r])
            if hf == 1:
                yield
        for h in range(8):
            bk, sl = hb[h]
            p.ts("dve", coef[:, 16 + h:17 + h], accD[bk][:, sl * 129 + 128:sl * 129 + 129], 1e-30, None, ALU.max, R=[accD[bk].r], W=[coef.r])
        p.op("dve", lambda e: e.reciprocal(coef[:, 24:32], coef[:, 16:24]), R=[coef.r], W=[coef.r])
        for h in range(8):
            bk, sl = hb[h]
            p.ts("dve", oln[:, h, :], accD[bk][:, sl * 129:sl * 129 + 128], coef[:, 24 + h:25 + h], None, ALU.mult,
                 R=[accD[bk].r, coef.r], W=[oln.r])
        yield
        for hf in range(2):
            tb_ = bfv(B[7])
            for hh in range(4):
                p.tr(tb_[:, hh * 128:(hh + 1) * 128], oln[:, hf * 4 + hh, :], ident[:], R=[oln.r, ident.r], W=[B[7].r])
            p.cp("act", olT[:, hf * 4:(hf + 1) * 4, :], tb_[:, 0:512].rearrange("p (c q) -> p c q", q=128), R=[B[7].r], W=[olT.r])
        for c in range(4):
            for hl in range(2):
                p.mm(B[7][:, c * 128:(c + 1) * 128], wuvP[:, 2 * c + hl, :], olT[:, 2 * c + hl, :], c == 0 and hl == 0, c == 3 and hl == 1,
                     R=[wuvP.r, olT.r], W=[B[7].r])
        p.cp("act", oT[:, 4:8, :], B[7][:, :].rearrange("p (c q) -> p c q", q=128), R=[B[7].r], W=[oT.r])
        p.dma("pool", S["oT_s"][i, :, :, :], oT[:], R=[oT.r], W=[S["oT_s"].r])
        yield

    def len_P(i):
        return 1 + 6 + 2 * ((i + 1) + min(5, i + 1)) + 4 + 1 + (i + 1) + 2

    def len_D(i):
        return (128 * (i + 1) + 511) // 512 + 1 + BIS_ITERS + 1

    for _ in stream_D(0):
        pass
    for i in range(ntiles):
        gd = stream_D(i + 1) if i + 1 < ntiles else None
        ratio = (len_D(i + 1) / float(len_P(i))) if gd is not None else 0.0
        credit = 0.0
        for _ in stream_P(i):
            credit += ratio
            while gd is not None and credit >= 1.0:
                credit -= 1.0
                try:
                    next(gd)
                except StopIteration:
                    gd = None
        if gd is not None:
            for _ in gd:
                pass
    return st


def layer_norm_tile(p, r, rs, gbc, bbc, out, tmp, R_extra=()):
    p.tt("dve", rs[:, 2:3], rs[:, 0:1], rs[:, 1:2], ALU.add, R=[rs.r], W=[rs.r])
    p.ts("dve", rs[:, 3:4], rs[:, 2:3], -1.0 / D, None, ALU.mult, R=[rs.r], W=[rs.r])
    p.act(tmp[:], r[:], AF.Square, bias=rs[:, 3:4], accum=rs[:, 4:5], R=[r.r, rs.r], W=[tmp.r, rs.r])
    p.ts("dve", rs[:, 5:6], rs[:, 4:5], 1.0 / D, 1e-5, ALU.mult, ALU.add, R=[rs.r], W=[rs.r])
    p.act(rs[:, 6:7], rs[:, 5:6], AF.Sqrt, R=[rs.r], W=[rs.r])
    p.op("dve", lambda e: e.reciprocal(rs[:, 7:8], rs[:, 6:7]), R=[rs.r], W=[rs.r])
    p.ts("dve", tmp[:], r[:], rs[:, 3:4], rs[:, 7:8], ALU.add, ALU.mult, R=[r.r, rs.r], W=[tmp.r])
    p.tt("dve", tmp[:], tmp[:], gbc[:], ALU.mult, R=[tmp.r, gbc.r], W=[tmp.r])
    p.tt("dve", out[:], tmp[:], bbc[:], ALU.add, R=[tmp.r, bbc.r], W=[out.r])


def phase3(g):
    nc, p, sb, I, S, Rz = g.nc, g.p, g.sb, g.I, g.S, g.Rz
    st = ExitStack()
    B = g.banks
    stg = sb("w_stg", (128, 1024), F32, st)
    wba = sb("wba", (128, 4, 1024), BF16, st)
    wbb = sb("wbb", (128, 4, 1024), BF16, st)
    wout = sb("wout", (128, 8, 1024), BF16, st)
    for (dst, src, n) in [(wba, "w_ba", 4), (wbb, "w_bb", 4), (wout, "w_out", 8)]:
        for c in range(n):
            p.dma("sp", stg[:], I[src][c * 128:(c + 1) * 128, :], R=[I[src].r], W=[stg.r])
            p.cp("dve", dst[:, c, :], stg[:], R=[stg.r], W=[dst.r])
    bc = {}
    for nm in ("ln1_g", "ln1_b", "ln2_g", "ln2_b"):
        bc[nm] = sb("bc_" + nm, (128, 1024), F32, st)
        p.dma("sp", bc[nm][:], I[nm][0:1, :].partition_broadcast(128), R=[I[nm].r], W=[bc[nm].r])
    oT_l = [sb("oT2_%d" % j, (128, 8, 128), BF16, st) for j in range(2)]
    gT_l = [sb("gT_%d" % j, (128, 16, 128), F32, st) for j in range(2)]
    xt_l = [sb("xt_%d" % j, (128, 1024), F32, st) for j in range(2)]
    t1 = sb("t1", (128, 512), F32, st)
    t2 = sb("t2", (128, 512), F32, st)
    mT = sb("mT", (128, 8, 128), BF16, st)
    r = sb("r", (128, 1024), F32, st)
    h1 = sb("h1", (128, 1024), F32, st)
    tmp = sb("lntmp", (128, 1024), F32, st)
    rs = sb("rs", (128, 8), F32, st)
    h1T = sb("h1T", (128, 8, 128), F32, st)
    wr = sb("wr", (128, 8, 36), F32, st)
    brb = sb("brb", (128, 36), F32, st)
    lg = sb("lg", (128, 36), F32, st)
    rt = sb("rt", (128, 16), F32, st)
    rj = sb("rj", (128, 32), F32, st)
    og = sb("og", (128, 4), F32, st)
    esel = sb("esel", (128, 8), F32, st)
    m8r = sb("m8r", (128, 8), F32, st)
    oh = sb("oh", (128, 2, 8), F32, st)
    Ak = sb("Ak", (128, 2, 32), F32, st)
    Abf = sb("Abf", (128, 32), BF16, st)
    ustr = sb("ustr", (128, 128), BF16, st)
    posf = sb("posf", (128, 32), F32, st)
    base = sb("base", (128, 32), F32, st)
    iot = sb("iot", (128, 32), F32, st)
    tokid = sb("tokid", (128, 32), I32, st)
    tokrow = sb("tokrow", (128, 32, 16), I32, st)
    fill = sb("fill", (128, 2048), I32, st)
    p.dma("sp", wr[:], I["wr"][:, :].rearrange("(c p) n -> p c n", p=128), R=[I["wr"].r], W=[wr.r])
    p.dma("sp", brb[:], I["br"][0:1, :].partition_broadcast(128), R=[I["br"].r], W=[brb.r])
    p.dma("sp", stg[:, 0:128], I["ustrict"][:, :], R=[I["ustrict"].r], W=[stg.r])
    p.cp("dve", ustr[:], stg[:, 0:128], R=[stg.r], W=[ustr.r])
    p.dma("sp", iot[:], I["iota32"][:, :], R=[I["iota32"].r], W=[iot.r])
    p.dma("sp", tokid[:], I["tokid"][:, :], R=[I["tokid"].r], W=[tokid.r])
    p.cp("dve", tokrow[:], tokid[:].unsqueeze(2).to_broadcast([128, 32, 16]), R=[tokid.r], W=[tokrow.r])
    p.memset("dve", base[:], 0.0, W=[base.r])
    p.memset("pool", fill[:], 5000, W=[fill.r])
    p.dma("sp", S["slot"][:, :].rearrange("(p r) c -> p (r c)", p=128), fill[:], R=[fill.r], W=[S["slot"].r])
    nt3 = (g.debug or {}).get("ntiles", NT)

    def loads3(i):
        j = i % 2
        p.dma("sp", oT_l[j][:], S["oT_s"][i, :, :, :], R=[S["oT_s"].r], W=[oT_l[j].r])
        p.dma("sp", gT_l[j][:], S["g_s"][i, :, :, :], R=[S["g_s"].r], W=[gT_l[j].r])
        p.dma("sp", xt_l[j][:], I["x"][i * 128:(i + 1) * 128, :], R=[I["x"].r], W=[xt_l[j].r])

    loads3(0)
    for i in range(nt3):
        oT, gT, xt = oT_l[i % 2], gT_l[i % 2], xt_l[i % 2]
        if i + 1 < nt3:
            loads3(i + 1)
        for hf in range(2):
            for m4 in range(4):
                m = hf * 4 + m4
                for kc in range(4):
                    p.mm(B[0][:, m4 * 128:(m4 + 1) * 128], wba[:, kc, m * 128:(m + 1) * 128], oT[:, kc, :], m4 == 0 and kc == 0, kc == 3,
                         R=[wba.r, oT.r], W=[B[0].r])
                for kc in range(4):
                    p.mm(B[1][:, m4 * 128:(m4 + 1) * 128], wbb[:, kc, m * 128:(m + 1) * 128], oT[:, 4 + kc, :], m4 == 0 and kc == 0, kc == 3,
                         R=[wbb.r, oT.r], W=[B[1].r])
            p.tt("dve", t1[:], B[0][:, :], gT[:, hf * 4:(hf + 1) * 4, :].rearrange("p m q -> p (m q)"), ALU.mult, R=[B[0].r, gT.r], W=[t1.r])
            p.tt("dve", t2[:], B[1][:, :], gT[:, 8 + hf * 4:8 + (hf + 1) * 4, :].rearrange("p m q -> p (m q)"), ALU.mult, R=[B[1].r, gT.r], W=[t2.r])
            p.tt("dve", mT[:, hf * 4:(hf + 1) * 4, :].rearrange("p m q -> p (m q)"), t1[:], t2[:], ALU.add, R=[t1.r, t2.r], W=[mT.r])
        for hf in range(2):
            bk = B[2 + hf]
            for c in range(8):
                p.mm(bk[:, :], mT[:, c, :], wout[:, c, hf * 512:(hf + 1) * 512], c == 0, c == 7, R=[mT.r, wout.r], W=[bk.r])
            p.stt(r[:, hf * 512:(hf + 1) * 512], xt[:, hf * 512:(hf + 1) * 512], ALPHA, bk[:, :], ALU.mult, ALU.add,
                  R=[xt.r, bk.r], W=[r.r, rs.r], accum=rs[:, hf:hf + 1])
        layer_norm_tile(p, r, rs, bc["ln1_g"], bc["ln1_b"], h1, tmp)
        p.dma("pool", S["h1_s"][i * 128:(i + 1) * 128, :], h1[:], R=[h1.r], W=[S["h1_s"].r])
        for c in range(8):
            bk = B[4 + c // 4]
            p.tr(bk[:, (c % 4) * 128:(c % 4 + 1) * 128], h1[:, c * 128:(c + 1) * 128], Rz["identf"][:], R=[h1.r, Rz["identf"].r], W=[bk.r])
        for hf in range(2):
            p.cp("act", h1T[:, hf * 4:(hf + 1) * 4, :], B[4 + hf][:, :].rearrange("p (c q) -> p c q", q=128), R=[B[4 + hf].r], W=[h1T.r])
        for c in range(8):
            p.mm(B[6][:, 0:36], h1T[:, c, :], wr[:, c, :], c == 0, c == 7, R=[h1T.r, wr.r], W=[B[6].r])
        p.tt("dve", lg[:], B[6][:, 0:36], brb[:], ALU.add, R=[B[6].r, brb.r], W=[lg.r])
        p.op("dve", lambda e: e.reduce_max(rt[:, 0:1], lg[:, 0:4], AX.X), R=[lg.r], W=[rt.r])
        p.ts("dve", og[:], lg[:, 0:4], rt[:, 0:1], None, ALU.is_equal, R=[lg.r, rt.r], W=[og.r])
        p.ts("dve", rt[:, 1:2], rt[:, 0:1], -1.0, None, ALU.mult, R=[rt.r], W=[rt.r])
        p.act(rj[:, 0:4], lg[:, 0:4], AF.Exp, bias=rt[:, 1:2], accum=rt[:, 2:3], R=[lg.r, rt.r], W=[rj.r, rt.r])
        p.op("dve", lambda e: e.reciprocal(rt[:, 3:4], rt[:, 2:3]), R=[rt.r], W=[rt.r])
        p.ts("dve", esel[:], lg[:, 4:12], og[:, 0:1], None, ALU.mult, R=[lg.r, og.r], W=[esel.r])
        for gi in range(1, 4):
            p.stt(esel[:], lg[:, 4 + 8 * gi:12 + 8 * gi], og[:, gi:gi + 1], esel[:], ALU.mult, ALU.add, R=[lg.r, og.r, esel.r], W=[esel.r])
        p.op("dve", lambda e: e.max(m8r[:], esel[:]), R=[esel.r], W=[m8r.r])
        p.tt("dve", rt[:, 4:5], m8r[:, 1:2], m8r[:, 0:1], ALU.subtract, R=[m8r.r], W=[rt.r])
        p.act(rt[:, 5:6], rt[:, 4:5], AF.Exp, R=[rt.r], W=[rt.r])
        p.ts("dve", rt[:, 6:7], rt[:, 5:6], 1.0, None, ALU.add, R=[rt.r], W=[rt.r])
        p.op("dve", lambda e: e.reciprocal(rt[:, 7:8], rt[:, 6:7]), R=[rt.r], W=[rt.r])
        p.tt("dve", rt[:, 8:9], rt[:, 7:8], rt[:, 5:6], ALU.mult, R=[rt.r], W=[rt.r])
        p.tt("dve", g.wts[:, i, 0:1], rt[:, 7:8], rt[:, 3:4], ALU.mult, R=[rt.r], W=[g.wts.r])
        p.tt("dve", g.wts[:, i, 1:2], rt[:, 8:9], rt[:, 3:4], ALU.mult, R=[rt.r], W=[g.wts.r])
        for k in range(2):
            p.ts("dve", oh[:, k, :], esel[:], m8r[:, k:k + 1], None, ALU.is_equal, R=[esel.r, m8r.r], W=[oh.r])
            p.tt("dve", Ak[:, k, :].rearrange("p (a b) -> p a b", b=8), og[:].unsqueeze(2).to_broadcast([128, 4, 8]),
                 oh[:, k, :].unsqueeze(1).to_broadcast([128, 4, 8]), ALU.mult, R=[og.r, oh.r], W=[Ak.r])
        p.tt("dve", Abf[:], Ak[:, 0, :], Ak[:, 1, :], ALU.add, R=[Ak.r], W=[Abf.r])
        p.mm(B[7][:, 0:32], ustr[:], Abf[:], True, False, R=[ustr.r, Abf.r], W=[B[7].r])
        p.mm(B[7][:, 64:96], Rz["ones_bf"][:], Abf[:], False, True, R=[Rz["ones_bf"].r, Abf.r], W=[B[7].r])
        p.tt("dve", posf[:], B[7][:, 0:32], base[:], ALU.add, R=[B[7].r, base.r], W=[posf.r])
        p.tt("dve", base[:], B[7][:, 64:96], base[:], ALU.add, R=[B[7].r, base.r], W=[base.r])
        for k in range(2):
            p.stt(rj[:, 0:32], posf[:], 1.0, Ak[:, k, :], ALU.mult, ALU.mult, R=[posf.r, Ak.r], W=[rj.r, rt.r], accum=rt[:, 10 + k:11 + k])
            p.stt(rj[:, 0:32], iot[:], 1.0, Ak[:, k, :], ALU.mult, ALU.mult, R=[iot.r, Ak.r], W=[rj.r, rt.r], accum=rt[:, 12 + k:13 + k])
            p.ts("dve", rt[:, 14:15], rt[:, 10 + k:11 + k], float(CAP), 1e6, ALU.is_ge, ALU.mult, R=[rt.r], W=[rt.r])
            p.ts("dve", rt[:, 9:10], rt[:, 10 + k:11 + k], float(CAP), None, ALU.is_lt, R=[rt.r], W=[rt.r])
            p.tt("dve", g.wts[:, i, k:k + 1], g.wts[:, i, k:k + 1], rt[:, 9:10], ALU.mult, R=[rt.r, g.wts.r], W=[g.wts.r])
            p.stt(rt[:, 15:16], rt[:, 12 + k:13 + k], float(CAP), rt[:, 10 + k:11 + k], ALU.mult, ALU.add, R=[rt.r], W=[rt.r])
            p.tt("dve", rt[:, 15:16], rt[:, 15:16], rt[:, 14:15], ALU.add, R=[rt.r], W=[rt.r])
            p.cp("dve", g.dest[:, i, k:k + 1], rt[:, 15:16], R=[rt.r], W=[g.dest.r])
            p.dma("pool", None, None, R=[g.dest.r, tokrow.r], W=[S["slot"].r],
                  fn=lambda e, i=i, k=k: e.indirect_dma_start(
                      out=S["slot"][:, :], out_offset=bass.IndirectOffsetOnAxis(ap=g.dest[:, i, k:k + 1], axis=0),
                      in_=tokrow[:, i, :], in_offset=None, bounds_check=p.breg(e, 32 * CAP - 1), oob_is_err=False))
    return st


def phase4(g):
    nc, p, sb, I, S, Rz = g.nc, g.p, g.sb, g.I, g.S, g.Rz
    st = ExitStack()
    B = g.banks
    ident = Rz["identb"]
    nexp = (g.debug or {}).get("nexp", 32)
    sid = sb("sid", (128, 4, 16), I32, st)
    xg = [sb("xg%d" % i, (128, 1024), F32, st) for i in range(4)]
    xgb = [sb("xgb%d" % i, (128, 1024), BF16, st) for i in range(2)]
    xgT = sb("xgT", (128, 8, 512), BF16, st)
    hT = sb("hT", (128, 2, 512), BF16, st)
    sg = [sb("sg%d" % i, (128, 512), F32, st) for i in range(2)]
    yb = [sb("yb%d" % i, (128, 1024), F32, st) for i in range(2)]
    wgf = sb("wgf", (128, 8, 256), F32, st)
    wuf = sb("wuf", (128, 8, 256), F32, st)
    wdf = sb("wdf", (128, 2, 1024), F32, st)
    wg = [sb("wg%d" % i, (128, 8, 256), BF16, st) for i in range(2)]
    wu = [sb("wu%d" % i, (128, 8, 256), BF16, st) for i in range(2)]
    wd = [sb("wd%d" % i, (128, 2, 1024), BF16, st) for i in range(2)]
    for x_ in xg:
        p.memset("pool", x_[:], 0.0, W=[x_.r])
    ceng = ["dve", "act", "dve", "act"]
    xgT2 = [xgT, sb("xgTb", (128, 8, 512), BF16, st)]
    sid2 = [sid, sb("sidb", (128, 4, 16), I32, st)]

    def issue_loads(e_):
        k = e_ % 2
        sd = sid2[k]
        p.dma("sp", wgf[:], I["w_gate"][e_, :, :].rearrange("(p c) f -> p c f", c=8), R=[I["w_gate"].r], W=[wgf.r])
        p.dma("sp", wuf[:], I["w_up"][e_, :, :].rearrange("(p c) f -> p c f", c=8), R=[I["w_up"].r], W=[wuf.r])
        p.dma("sp", wdf[:], I["w_down"][e_, :, :].rearrange("(c p) n -> p c n", p=128), R=[I["w_down"].r], W=[wdf.r])
        p.dma("sp", sd[:], S["slot"][e_ * CAP:(e_ + 1) * CAP, :].rearrange("(s p) c -> p s c", p=128),
              R=[S["slot"].r], W=[sd.r])
        for s_ in range(4):
            p.dma("pool", None, None, R=[sd.r, S["h1_s"].r], W=[xg[s_].r],
                  fn=lambda e, s_=s_, sd=sd: e.indirect_dma_start(
                      out=xg[s_][:, :], out_offset=None, in_=S["h1_s"][:, :],
                      in_offset=bass.IndirectOffsetOnAxis(ap=sd[:, s_, 0:1], axis=0), bounds_check=p.breg(e, T - 1), oob_is_err=False))

    def finish_loads(e_):
        k = e_ % 2
        p.cp("act", wg[k][:], wgf[:], R=[wgf.r], W=[wg[k].r])
        p.cp("dve", wu[k][:], wuf[:], R=[wuf.r], W=[wu[k].r])
        p.cp("act", wd[k][:], wdf[:], R=[wdf.r], W=[wd[k].r])
        for s_ in range(4):
            xb = xgb[s_ % 2]
            p.cp(ceng[s_], xb[:], xg[s_][:], R=[xg[s_].r], W=[xb.r])
            bk = B[6 + s_ % 2]
            tv = bk.t[:].bitcast(BF16)
            for c in range(8):
                p.tr(tv[:, c * 128:(c + 1) * 128], xb.t[:, c:1024:8], ident[:], R=[xb.r, ident.r], W=[bk.r])
            p.cp("act" if s_ % 2 else "dve", xgT2[k][:, :, s_ * 128:(s_ + 1) * 128], tv[:, :].rearrange("p (c q) -> p c q", q=128),
                 R=[bk.r], W=[xgT2[k].r])

    def compute(e_):
        k = e_ % 2
        xT_ = xgT2[k]
        for fc in range(2):
            bG, bU = B[2 * fc], B[2 * fc + 1]
            for c in range(8):
                p.mm(bG[:, :], wg[k][:, c, fc * 128:(fc + 1) * 128], xT_[:, c, :], c == 0, c == 7, R=[wg[k].r, xT_.r], W=[bG.r])
            for c in range(8):
                p.mm(bU[:, :], wu[k][:, c, fc * 128:(fc + 1) * 128], xT_[:, c, :], c == 0, c == 7, R=[wu[k].r, xT_.r], W=[bU.r])
            p.act(sg[fc][:], bG[:, :], AF.Silu, R=[bG.r], W=[sg[fc].r])
            p.tt("dve", hT[:, fc, :], sg[fc][:], bU[:, :], ALU.mult, R=[sg[fc].r, bU.r], W=[hT.r])
        for s_ in range(4):
            y_ = yb[s_ % 2]
            for hf in range(2):
                bY = B[4 + (2 * s_ + hf) % 2]
                for fc in range(2):
                    p.mm(bY[:, :], hT[:, fc, s_ * 128:(s_ + 1) * 128], wd[k][:, fc, hf * 512:(hf + 1) * 512], fc == 0, fc == 1,
                         R=[hT.r, wd[k].r], W=[bY.r])
                if hf == 0:
                    p.act(y_[:, 0:512], bY[:, :], AF.Copy, R=[bY.r], W=[y_.r])
                else:
                    p.cp("dve", y_[:, 512:1024], bY[:, :], R=[bY.r], W=[y_.r])
            p.dma("sp", S["y_s"][e_ * CAP + s_ * 128:e_ * CAP + (s_ + 1) * 128, :], y_[:], R=[y_.r], W=[S["y_s"].r])

    issue_loads(0)
    finish_loads(0)
    for e_ in range(nexp):
        if e_ + 1 < nexp:
            issue_loads(e_ + 1)
        compute(e_)
        if e_ + 1 < nexp:
            finish_loads(e_ + 1)
    return st


def phase5(g):
    nc, p, sb, I, S, Rz = g.nc, g.p, g.sb, g.I, g.S, g.Rz
    st = ExitStack()
    bc = {}
    for nm in ("ln2_g", "ln2_b"):
        bc[nm] = sb("bc5_" + nm, (128, 1024), F32, st)
        p.dma("sp", bc[nm][:], I[nm][0:1, :].partition_broadcast(128), R=[I[nm].r], W=[bc[nm].r])
    y = [[sb("y%d_%d" % (k, j), (128, 1024), F32, st) for k in range(2)] for j in range(2)]
    h1 = [sb("h1_5%d" % j, (128, 1024), F32, st) for j in range(2)]
    r = sb("r5", (128, 1024), F32, st)
    tmp = sb("tmp5", (128, 1024), F32, st)
    ot = [sb("ot5%d" % j, (128, 1024), F32, st) for j in range(2)]
    rs = sb("rs5", (128, 8), F32, st)
    for j in range(2):
        for k in range(2):
            p.memset("dve", y[j][k][:], 0.0, W=[y[j][k].r])
    nt5 = (g.debug or {}).get("ntiles", NT)

    def loads5(i):
        j = i % 2
        p.dma("sp", h1[j][:], S["h1_s"][i * 128:(i + 1) * 128, :], R=[S["h1_s"].r], W=[h1[j].r])
        for k in range(2):
            p.dma("pool", None, None, R=[g.dest.r, S["y_s"].r], W=[y[j][k].r],
                  fn=lambda e, i=i, k=k, j=j: e.indirect_dma_start(
                      out=y[j][k][:, :], out_offset=None, in_=S["y_s"][:, :],
                      in_offset=bass.IndirectOffsetOnAxis(ap=g.dest[:, i, k:k + 1], axis=0), bounds_check=p.breg(e, 32 * CAP - 1), oob_is_err=False))

    loads5(0)
    for i in range(nt5):
        j = i % 2
        if i + 1 < nt5:
            loads5(i + 1)
        p.ts("dve", r[:], h1[j][:], ALPHA, None, ALU.mult, R=[h1[j].r], W=[r.r])
        p.stt(r[:], y[j][0][:], g.wts[:, i, 0:1], r[:], ALU.mult, ALU.add, R=[y[j][0].r, g.wts.r, r.r], W=[r.r])
        p.stt(r[:], y[j][1][:], g.wts[:, i, 1:2], r[:], ALU.mult, ALU.add, R=[y[j][1].r, g.wts.r, r.r], W=[r.r])
        p.op("dve", lambda e: e.reduce_sum(rs[:, 0:1], r[:], AX.X), R=[r.r], W=[rs.r])
        p.memset("dve", rs[:, 1:2], 0.0, W=[rs.r])
        layer_norm_tile(p, r, rs, bc["ln2_g"], bc["ln2_b"], ot[j], tmp)
        p.dma("sp", g.out[i * 128:(i + 1) * 128, :], ot[j][:], R=[ot[j].r], W=[g.out.r])
    return st


def host_inputs(inputs):
    f = lambda a: np.ascontiguousarray(np.asarray(a, dtype=np.float32))
    rel_bias = f(inputs["rel_bias"])
    shared = {
        "w_in": f(inputs["w_in"][0]),
        "pe_kT": f(inputs["cmp_pe_k"][0].T), "pe_vT": f(inputs["cmp_pe_v"][0].T),
        "cw1_k": f(inputs["cmp_w1_k"][0]), "cw2_k": f(inputs["cmp_w2_k"][0]),
        "cw1_v": f(inputs["cmp_w1_v"][0]), "cw2_v": f(inputs["cmp_w2_v"][0]),
        "ckv_g": f(inputs["ckv_norm_g"][0]).reshape(1, 128),
        "w_uk": f(inputs["w_uk"][0]), "w_uv": f(inputs["w_uv"][0]),
        "relflat": rel_bias.reshape(1, 512),
        "w_ba": f(inputs["w_branch_a"][0]), "w_bb": f(inputs["w_branch_b"][0]), "w_out": f(inputs["w_out"][0]),
        "ln1_g": f(inputs["ln1_g"][0]).reshape(1, D), "ln1_b": f(inputs["ln1_b"][0]).reshape(1, D),
        "wr": f(np.concatenate([np.asarray(inputs["w_grp"][0]), np.asarray(inputs["w_rtr"][0])], axis=1)),
        "br": f(np.concatenate([np.asarray(inputs["b_grp"][0]), np.asarray(inputs["b_rtr"][0])], axis=0)).reshape(1, 36),
        "w_gate": f(inputs["w_gate"][0]), "w_up": f(inputs["w_up"][0]), "w_down": f(inputs["w_down"][0]),
        "ln2_g": f(inputs["ln2_g"][0]).reshape(1, D), "ln2_b": f(inputs["ln2_b"][0]).reshape(1, D),
    }
    shared.update(_host_consts(rel_bias))
    x = np.asarray(inputs["x"], dtype=np.float32)
    maps = []
    for b in range(x.shape[0]):
        m = dict(shared)
        m["x"] = np.ascontiguousarray(x[b])
        m["xT"] = np.ascontiguousarray(x[b].T)
        maps.append(m)
    return maps


def kernel(**inputs):
    maps = host_inputs(inputs)
    nc, g = build()
    res = run_bass_kernel_spmd(nc, maps, core_ids=list(range(8)))
    return np.stack([np.asarray(r["out"], dtype=np.float32) for r in res.results], axis=0)
```

```python
import math
from contextlib import ExitStack
import numpy as np
import ml_dtypes
import concourse.bass as bass
import concourse.mybir as mybir
from concourse.bass_utils import run_bass_kernel_spmd

F32 = mybir.dt.float32
BF16 = mybir.dt.bfloat16
I32 = mybir.dt.int32
AF = mybir.ActivationFunctionType
ALU = mybir.AluOpType
AX = mybir.AxisListType

T = 4096
D = 1024
NT = T // 128
D_IN = 4316
ALPHA = 2.0 ** 0.25
CAP = 512
NEGM = 32768.0
BIS_ITERS = 20
DEBUG = None

O_QA, O_KC, O_VC, O_KS, O_VS, O_KW, O_VW, O_GN, O_QB, O_CKV, O_QI, O_KI, O_WI, O_GA, O_GB = (
    0, 512, 640, 768, 896, 1024, 1152, 1280, 1304, 1816, 1944, 2200, 2264, 2268, 3292)


class Res:
    __slots__ = ("name", "w", "r", "dsem", "dcount", "excl")

    def __init__(self, name):
        self.name = name
        self.excl = False
        self.w = None
        self.r = {}
        self.dsem = None
        self.dcount = 0


class Prog:
    ENG = ("pe", "act", "dve", "pool", "sp")

    def __init__(self, nc, stack):
        self.nc = nc
        self.stack = stack
        self.eobj = {"pe": nc.tensor, "act": nc.scalar, "dve": nc.vector, "pool": nc.gpsimd, "sp": nc.sync}
        self.sem = {e: stack.enter_context(nc.semaphore("c_" + e)) for e in self.ENG}
        self.cnt = {e: 0 for e in self.ENG}
        self.seen = {e: {} for e in self.ENG}
        self.ops = {e: [] for e in self.ENG}
        self.dres = []
        self.nres = 0

    def res(self, name=None):
        self.nres += 1
        return Res(name or "r%d" % self.nres)

    def _waits(self, eng, reads, writes):
        need = {}

        def add(ev, war=False):
            if ev is None:
                return
            sem, val, src = ev
            if src == eng and eng == "pe":
                return
            k = id(sem)
            if self.seen[eng].get(k, (None, 0))[1] >= val:
                return
            if k not in need or need[k][1] < val:
                need[k] = (sem, val)

        for r in reads:
            add(r.w)
        for w in writes:
            add(w.w)
            for ev in w.r.values():
                add(ev, True)
        out = []
        for k, (sem, val) in need.items():
            self.seen[eng][k] = (sem, val)
            out.append((sem, val))
        return out

    def op(self, eng, fn, R=(), W=()):
        W = list(W) + [r for r in R if r.excl]
        R = [r for r in R if not r.excl]
        waits = self._waits(eng, R, W)
        self.cnt[eng] += 1
        ev = (self.sem[eng], self.cnt[eng], eng)
        self.ops[eng].append((fn, waits, (self.sem[eng], 1)))
        for r in R:
            r.r[eng] = ev
        for w in W:
            w.w = ev
            w.r = {}

    def dma(self, q, out, in_, R=(), W=(), fn=None):
        waits = self._waits(q, R, W)
        tgt = W[0]
        if tgt.dsem is None:
            tgt.dsem = self.stack.enter_context(self.nc.semaphore("d_" + tgt.name))
            self.dres.append(tgt)
        tgt.dcount += 16
        ev = (tgt.dsem, tgt.dcount, None)
        if fn is None:
            fn = lambda e, o=out, i=in_: e.dma_start(out=o, in_=i)
        self.ops[q].append((fn, waits, (tgt.dsem, 16)))
        for r in R:
            r.r["dma%d" % id(tgt)] = ev
        for w in W:
            w.w = ev
            w.r = {}

    def flush(self, final=False):
        tail = {}
        for e in self.ENG:
            ws = []
            for e2 in self.ENG:
                if e2 != e and self.cnt[e2] > 0 and self.seen[e].get(id(self.sem[e2]), (None, 0))[1] < self.cnt[e2]:
                    ws.append((self.sem[e2], self.cnt[e2]))
                    self.seen[e][id(self.sem[e2])] = (self.sem[e2], self.cnt[e2])
            for r in self.dres:
                if self.seen[e].get(id(r.dsem), (None, 0))[1] < r.dcount:
                    ws.append((r.dsem, r.dcount))
                    self.seen[e][id(r.dsem)] = (r.dsem, r.dcount)
            tail[e] = ws
        ops = self.ops
        eobj = self.eobj
        self.regs = {}
        with self.nc.Block() as block:
            def run(e, engine):
                for fn, waits, inc in ops[e]:
                    for s, v in waits:
                        engine.wait_ge(s, v)
                    ins = fn(engine)
                    ins.then_inc(inc[0], inc[1])
                for s, v in tail[e]:
                    engine.wait_ge(s, v)

            @block.tensor
            def _(t):
                run("pe", t)

            @block.scalar
            def _(s):
                run("act", s)

            @block.vector
            def _(v):
                run("dve", v)

            @block.gpsimd
            def _(g):
                run("pool", g)

            @block.sync
            def _(sy):
                run("sp", sy)
        self.ops = {e: [] for e in self.ENG}

    def breg(self, e, val):
        if val not in self.regs:
            self.regs[val] = e.to_reg(val)
        return self.regs[val]

    def mm(self, out, lhsT, rhs, start, stop, R=(), W=()):
        self.op("pe", lambda e: e.matmul(out, lhsT, rhs, start=start, stop=stop, skip_group_check=True), R, W)

    def tr(self, out, in_, ident, R=(), W=()):
        self.op("pe", lambda e: e.transpose(out, in_, ident), R, W)

    def act(self, out, in_, func, R=(), W=(), bias=None, scale=None, accum=None, eng="act"):
        kw = {}
        if bias is not None:
            kw["bias"] = bias
        if scale is not None:
            kw["scale"] = scale
        if accum is not None:
            kw["accum_out"] = accum
        self.op("act", lambda e: e.activation(out, in_, func, **kw), R, W)

    def ts(self, eng, out, in0, s1, s2, op0, op1=None, R=(), W=(), accum=None):
        kw = {}
        if op1 is not None:
            kw["op1"] = op1
        if accum is not None:
            kw["accum_out"] = accum
        self.op(eng, lambda e: e.tensor_scalar(out, in0, s1, s2, op0, **kw), R, W)

    def tt(self, eng, out, in0, in1, op, R=(), W=()):
        self.op(eng, lambda e: e.tensor_tensor(out, in0, in1, op), R, W)

    def stt(self, out, in0, scalar, in1, op0, op1, R=(), W=(), accum=None):
        kw = {}
        if accum is not None:
            kw["accum_out"] = accum
        self.op("dve", lambda e: e.scalar_tensor_tensor(out, in0, scalar, in1, op0, op1, **kw), R, W)

    def cp(self, eng, out, in_, R=(), W=()):
        if eng == "act":
            self.op("act", lambda e: e.copy(out, in_), R, W)
        else:
            self.op(eng, lambda e: e.tensor_copy(out, in_), R, W)

    def memset(self, eng, ap, val, W=()):
        self.op(eng, lambda e: e.memset(ap, val), (), W)


class Buf:
    def __init__(self, t, res):
        self.t = t
        self.r = res

    def __getitem__(self, k):
        return self.t[k]


def _rel_bucket_np(dist):
    n = np.maximum(dist, 0)
    nf = np.maximum(n, 1).astype(np.float32)
    large = 16 + (np.log(nf / 16) / math.log(1024 / 16) * 16).astype(np.int32)
    return np.where(n < 16, n, np.minimum(large, 31))


def _host_consts(rel_bias):
    c = {}
    dd = np.arange(0, 1152)
    bk = _rel_bucket_np(dd)
    relvec = rel_bias[bk]
    p = np.arange(128)[:, None]
    j = np.arange(128)[None, :]
    relT = np.zeros((8, 128, 16, 128), np.float32)
    for di in range(8):
        dist = np.clip(128 * di + j - p, 0, 1151)
        relT[di] = relvec[dist].transpose(0, 2, 1)
    c["relTa"] = np.ascontiguousarray(relT[:, :, :8, :].transpose(1, 0, 2, 3)).reshape(128, 8, 2, 512)
    c["relTb"] = np.ascontiguousarray(relT[:, :, 8:, :].transpose(1, 0, 2, 3)).reshape(128, 8, 1024)
    c31 = relvec[1151]
    c["c31a"] = np.ascontiguousarray(np.repeat(c31[:8].reshape(2, 4, 1), 128, axis=2).reshape(2, 512))
    c["c31b"] = np.ascontiguousarray(np.repeat(c31[8:].reshape(8, 1), 128, axis=1).reshape(1, 1024))
    v = np.arange(503)[None, :]
    dist = p - 16 * v + 3937
    c["cmpv"] = np.ascontiguousarray(relvec[np.clip(dist, 0, 1151)][:, :, :8].transpose(0, 2, 1))
    c["cmpm"] = np.where(dist >= 0, 0.0, -30000.0).astype(np.float32)
    mc = np.where(j < p, -NEGM, 0.0).astype(np.float32)
    mw = np.where(j >= p, -NEGM, 0.0).astype(np.float32)
    c["mc4"] = np.tile(mc, (1, 4))
    c["mw4"] = np.tile(mw, (1, 4))
    c["ident"] = np.eye(128, dtype=np.float32)
    c["d30"] = np.tile(np.eye(128, dtype=np.float32) * NEGM, (1, 4))
    e_all = np.zeros((64, 32, 128), np.float32)
    for kt in range(32):
        e_all[2 * kt, kt, :64] = NEGM
        e_all[2 * kt + 1, kt, 64:] = NEGM
    c["eall"] = e_all.reshape(64, 32 * 128)
    c["mcq"] = np.where(j > p, -1e30, 0.0).astype(np.float32)
    cp_ = (np.arange(128) >= 64).astype(np.int64)[:, None]
    u = np.arange(128)[None, :] - 63
    c["wext"] = np.where((u == cp_) | (u == cp_ - 1), 1e4, np.where(u > cp_, -1e30, 0.0)).astype(np.float32)
    cs = 16 * np.arange(255)[:, None]
    ss = 64 * np.arange(64)[None, :]
    ov = np.minimum(cs + 32, ss + 64) - np.maximum(cs, ss)
    cm = np.zeros((256, 64), np.float32)
    cm[:255] = np.clip(ov, 0, None).astype(np.float32) / 16
    c["cmap"] = cm
    c["ustrict"] = (np.arange(128)[:, None] < np.arange(128)[None, :]).astype(np.float32)
    c["iota32"] = np.tile(np.arange(32, dtype=np.float32)[None, :], (128, 1))
    c["tokid"] = (np.arange(128)[:, None] + 128 * np.arange(32)[None, :]).astype(np.int32)
    return c


class Ctx:
    pass


def build(debug=None):
    nc = bass.Bass("TRN2", target_bir_lowering=False)
    es = ExitStack()
    p = Prog(nc, es)
    g = Ctx()
    g.nc, g.p, g.es, g.debug = nc, p, es, debug
    g.dbg_out = {}

    def din(name, shape, dt=F32):
        return Buf(nc.dram_tensor(name, list(shape), dt, kind="ExternalInput").ap(), p.res(name))

    def dscr(name, shape, dt=F32):
        kind = "ExternalOutput" if (debug and name in debug) else "Internal"
        return Buf(nc.dram_tensor(name, list(shape), dt, kind=kind).ap(), p.res(name))

    g.din, g.dscr = din, dscr

    def sb(name, shape, dt=F32, stack=None):
        t = (stack or es).enter_context(nc.sbuf_tensor("s_" + name, list(shape), dt))
        return Buf(t, p.res(name))

    def ps(name, shape, dt=F32, stack=None):
        t = (stack or es).enter_context(nc.psum_tensor(name, list(shape), dt))
        r = p.res(name)
        r.excl = True
        return Buf(t, r)

    g.sb, g.ps = sb, ps

    I = g.I = {}
    for name, shape in [
        ("xT", (D, T)), ("x", (T, D)), ("w_in", (D, D_IN)),
        ("pe_kT", (64, 32)), ("pe_vT", (64, 32)),
        ("cw1_k", (2048, 256)), ("cw2_k", (256, 64)), ("cw1_v", (2048, 256)), ("cw2_v", (256, 64)),
        ("ckv_g", (1, 128)), ("w_uk", (8, 64, 128)), ("w_uv", (8, 128, 64)), ("relflat", (1, 512)),
        ("w_ba", (512, D)), ("w_bb", (512, D)), ("w_out", (D, D)),
        ("ln1_g", (1, D)), ("ln1_b", (1, D)), ("wr", (D, 36)), ("br", (1, 36)),
        ("w_gate", (32, D, 256)), ("w_up", (32, D, 256)), ("w_down", (32, 256, D)),
        ("ln2_g", (1, D)), ("ln2_b", (1, D)),
        ("relTa", (128, 8, 2, 512)), ("relTb", (128, 8, 1024)), ("c31a", (2, 512)), ("c31b", (1, 1024)),
        ("cmpv", (128, 8, 503)), ("cmpm", (128, 503)), ("mc4", (128, 512)), ("mw4", (128, 512)),
        ("ident", (128, 128)), ("d30", (128, 512)), ("eall", (64, 4096)), ("mcq", (128, 128)),
        ("wext", (128, 128)), ("cmap", (256, 64)), ("ustrict", (128, 128)), ("iota32", (128, 32)),
    ]:
        I[name] = din(name, shape)
    I["tokid"] = din("tokid", (128, 32), I32)
    g.out = Buf(nc.dram_tensor("out", [T, D], F32, kind="ExternalOutput").ap(), p.res("out"))

    S = g.S = {}
    S["qa_s"] = dscr("qa_s", (NT, 2, 64, 4, 128), BF16)
    S["ql_s"] = dscr("ql_s", (NT, 128, 8, 128), BF16)
    S["qi_s"] = dscr("qi_s", (NT, 64, 4, 128), BF16)
    S["g_s"] = dscr("g_s", (NT, 128, 16, 128), F32)
    S["h1_s"] = dscr("h1_s", (T, D), F32)
    S["slot"] = dscr("slot", (32 * CAP, 16), I32)
    S["y_s"] = dscr("y_s", (32 * CAP, D), F32)

    Rz = g.Rz = {}
    Rz["ksT"] = sb("ksT", (128, T), BF16)
    Rz["kwT"] = sb("kwT", (128, T), BF16)
    Rz["ckvT"] = sb("ckvT", (128, T), BF16)
    Rz["kiT"] = sb("kiT", (64, T), BF16)
    Rz["vsA"] = sb("vsA", (128, NT, 2, 65), BF16)
    Rz["vwA"] = sb("vwA", (128, NT, 2, 65), BF16)
    Rz["ckvA"] = sb("ckvA", (128, NT, 129), BF16)
    Rz["gn"] = sb("gn", (128, NT, 24), F32)
    Rz["wabs"] = sb("wabs", (128, NT, 4), F32)
    Rz["wsgn"] = sb("wsgn", (128, NT, 4), F32)
    Rz["kcmpT"] = sb("kcmpT", (128, 256), BF16)
    Rz["vcmpM"] = sb("vcmpM", (128, 2, 2, 128), BF16)
    Rz["kmax"] = sb("kmax", (1, 4), F32)
    Rz["identb"] = sb("identb", (128, 128), BF16)
    Rz["identf"] = sb("identf", (128, 128), F32)
    Rz["ones_bf"] = sb("ones_bf", (128, 128), BF16)

    g.kcT = sb("kcT", (128, T), BF16)
    g.vcT = sb("vcT", (128, T), BF16)
    g.dest = sb("dest_i", (128, NT, 2), I32)
    g.wts = sb("wts", (128, NT, 2), F32)
    g.banks = [ps("bank%d" % i, (128, 512), F32) for i in range(8)]

    stage = (debug or {}).get("stage", 99)
    st = phase1(g)
    if debug:
        dump_resident(g)
    p.flush()
    st.close()
    if stage >= 2:
        st = phase2(g)
        p.flush()
        st.close()
    if stage >= 3:
        st = phase3(g)
        p.flush()
        st.close()
    if stage >= 4:
        st = phase4(g)
        p.flush()
        st.close()
    if stage >= 5:
        st = phase5(g)
        p.flush()
        st.close()
    return nc, g


def dump_resident(g):
    nc, p = g.nc, g.p
    for name, buf in g.Rz.items():
        shape = list(buf.t.shape)
        d = nc.dram_tensor("dbg_" + name, shape, buf.t.dtype, kind="ExternalOutput").ap()
        r = p.res("dbg_" + name)
        idx = tuple(slice(None) for _ in shape)
        p.dma("sp", d[idx], buf.t[idx], R=[buf.r], W=[r])


def phase1(g):
    nc, p, sb, I, S, Rz = g.nc, g.p, g.sb, g.I, g.S, g.Rz
    st = ExitStack()
    banks = g.banks
    bi = [0]

    def nbank():
        b = banks[bi[0] % 8]
        bi[0] += 1
        return b

    ev_rr = [0]

    def ev_eng():
        ev_rr[0] += 1
        return "act" if ev_rr[0] % 2 else "dve"

    stf = sb("p1_cst", (128, 128), F32, st)
    p.dma("sp", stf[:], I["ident"][:, :], R=[I["ident"].r], W=[stf.r])
    p.cp("dve", Rz["identb"][:], stf[:], R=[stf.r], W=[Rz["identb"].r])
    p.cp("dve", Rz["identf"][:], stf[:], R=[stf.r], W=[Rz["identf"].r])
    p.memset("dve", Rz["ones_bf"][:], 1.0, W=[Rz["ones_bf"].r])
    p.memset("pool", Rz["vsA"][:, :, :, 64:65], 1.0, W=[Rz["vsA"].r])
    p.memset("pool", Rz["vwA"][:, :, :, 64:65], 1.0, W=[Rz["vwA"].r])
    p.memset("pool", Rz["ckvA"][:, :, 128:129], 1.0, W=[Rz["ckvA"].r])

    xTb = sb("xTb", (128, 8, T), BF16, st)
    xst = [sb("xst%d" % i, (128, 1024), F32, st) for i in range(2)]
    xq = [p.res("xTq%d" % q) for q in range(4)]
    engs = ["dve", "act", "dve", "act"]
    xk = [0]

    def load_x(q):
        for c in range(8):
            s_ = xst[xk[0] % 2]
            xk[0] += 1
            p.dma("sp", s_[:], I["xT"][c * 128:(c + 1) * 128, q * 1024:(q + 1) * 1024], R=[I["xT"].r], W=[s_.r])
            p.cp(engs[c % 4], xTb[:, c, q * 1024:(q + 1) * 1024], s_[:], R=[s_.r], W=[xq[q]])

    cut = 99
    wst = [sb("wst%d" % i, (128, 8, 128), F32, st) for i in range(2)]
    wbf = [sb("wbf%d" % i, (128, 8, 128), BF16, st) for i in range(2)]
    wk = [0]

    def load_w_issue(col0, M):
        k = wk[0] % 2
        wk[0] += 1
        p.dma("sp", wst[k][:, :, 0:M], I["w_in"][:, col0:col0 + M].rearrange("(c p) m -> p c m", p=128),
              R=[I["w_in"].r], W=[wst[k].r])
        return k

    def load_w_cast(k, M):
        p.cp("act" if k else "dve", wbf[k][:, :, 0:M], wst[k][:, :, 0:M], R=[wst[k].r], W=[wbf[k].r])
        return wbf[k]

    groups = []

    def fm_group(col0, M, evac):
        groups.append((col0, M, evac))

    def run_groups():
        nxt = load_w_cast(load_w_issue(groups[0][0], groups[0][1]), groups[0][1])
        load_x(0)
        for gi, (col0, M, evac) in enumerate(groups):
            w = nxt
            kn = None
            if gi + 1 < len(groups):
                kn = load_w_issue(groups[gi + 1][0], groups[gi + 1][1])
            for tb in range(8):
                if gi == 0 and tb % 2 == 0 and tb < 6:
                    load_x(tb // 2 + 1)
                b = nbank()
                for c in range(8):
                    p.mm(b[0:M, :], w[:, c, 0:M], xTb[:, c, tb * 512:(tb + 1) * 512], c == 0, c == 7,
                         R=[w.r, xq[tb // 2]], W=[b.r])
                evac(tb, b)
            if kn is not None:
                nxt = load_w_cast(kn, groups[gi + 1][1])

    stg_bf = [sb("stgb%d" % i, (128, 512), BF16, st) for i in range(4)]
    stg_f = [sb("stgf%d" % i, (128, 512), F32, st) for i in range(2)] * 2
    sk = [0]

    def nstg(lst):
        sk[0] += 1
        return lst[sk[0] % 4]

    def evac_copy(dst_buf, scale=None):
        def f(tb, b):
            M = dst_buf.t.shape[0]
            e = ev_eng()
            o = dst_buf[:, tb * 512:(tb + 1) * 512]
            if e == "act":
                p.act(o, b[0:M, :], AF.Copy, R=[b.r], W=[dst_buf.r])
            else:
                p.cp("dve", o, b[0:M, :], R=[b.r], W=[dst_buf.r])
        return f

    for m in range(4):
        def ev(tb, b, m=m):
            s_ = nstg(stg_bf)
            p.act(s_[:], b[:], AF.Copy, scale=0.125, R=[b.r], W=[s_.r])
            gq, hh0 = m // 2, 2 * (m % 2)
            for hl in range(2):
                dst = S["qa_s"][tb * 4:(tb + 1) * 4, gq, :, hh0 + hl, :].rearrange("t d q -> d t q")
                src = s_[hl * 64:(hl + 1) * 64, :].rearrange("d (t q) -> d t q", q=128)
                p.dma("sp", dst, src, R=[s_.r], W=[S["qa_s"].r])
        fm_group(O_QA + 128 * m, 128, ev)

    kcT, vcT = g.kcT, g.vcT
    fm_group(O_KC, 128, evac_copy(kcT))
    fm_group(O_VC, 128, evac_copy(vcT))
    fm_group(O_KS, 128, evac_copy(Rz["ksT"]))
    fm_group(O_KW, 128, evac_copy(Rz["kwT"]))
    fm_group(O_KI, 64, evac_copy(Rz["kiT"]))

    wukf = sb("wukf", (128, 4, 128), F32, st)
    wukb = sb("wukb", (128, 4, 128), BF16, st)
    p.dma("sp", wukf[:], I["w_uk"][:, :, :].rearrange("(m hl) d r -> (hl d) m r", hl=2), R=[I["w_uk"].r], W=[wukf.r])
    p.cp("dve", wukb[:], wukf[:], R=[wukf.r], W=[wukb.r])
    for m in range(4):
        def ev(tb, b, m=m):
            s_ = nstg(stg_bf)
            p.cp("dve", s_[:], b[:], R=[b.r], W=[s_.r])
            for hl in range(2):
                b2 = nbank()
                p.mm(b2[:, :], wukb[hl * 64:(hl + 1) * 64, m, :], s_[hl * 64:(hl + 1) * 64, :], True, True,
                     R=[wukb.r, s_.r], W=[b2.r])
                s2 = nstg(stg_bf)
                p.act(s2[:], b2[:], AF.Copy, scale=0.125, R=[b2.r], W=[s2.r])
                dst = S["ql_s"][tb * 4:(tb + 1) * 4, :, 2 * m + hl, :].rearrange("t r q -> r t q")
                p.dma("sp", dst, s2[:].rearrange("r (t q) -> r t q", q=128), R=[s2.r], W=[S["ql_s"].r])
        fm_group(O_QB + 128 * m, 128, ev)

    for m in range(2):
        def ev(tb, b, m=m):
            s_ = nstg(stg_bf)
            p.act(s_[:], b[:], AF.Copy, scale=0.125, R=[b.r], W=[s_.r])
            for hl in range(2):
                dst = S["qi_s"][tb * 4:(tb + 1) * 4, :, 2 * m + hl, :].rearrange("t d q -> d t q")
                src = s_[hl * 64:(hl + 1) * 64, :].rearrange("d (t q) -> d t q", q=128)
                p.dma("sp", dst, src, R=[s_.r], W=[S["qi_s"].r])
        fm_group(O_QI + 128 * m, 128, ev)

    for m in range(16):
        def ev(tb, b, m=m):
            s_ = nstg(stg_f)
            p.act(s_[:], b[:], AF.Sigmoid, R=[b.r], W=[s_.r])
            dst = S["g_s"][tb * 4:(tb + 1) * 4, :, m, :].rearrange("t c q -> c t q")
            p.dma("sp", dst, s_[:].rearrange("c (t q) -> c t q", q=128), R=[s_.r], W=[S["g_s"].r])
        fm_group(O_GA + 128 * m, 128, ev)

    run_groups()
    wtf = sb("wtf", (128, 8, 412), F32, st)
    wtb = sb("wtb", (128, 8, 412), BF16, st)
    for (c0, n, o) in [(O_VS, 128, 0), (O_VW, 128, 128), (O_CKV, 128, 256), (O_GN, 24, 384), (O_WI, 4, 408)]:
        r_ = p.res("wtf%d" % o)
        p.dma("sp", wtf[:, :, o:o + n], I["w_in"][:, c0:c0 + n].rearrange("(c p) m -> p c m", p=128),
              R=[I["w_in"].r], W=[r_])
        p.cp("dve", wtb[:, :, o:o + n], wtf[:, :, o:o + n], R=[r_], W=[wtb.r])
    gbc = sb("gbc", (128, 128), F32, st)
    p.dma("sp", gbc[:], I["ckv_g"][0:1, :].partition_broadcast(128), R=[I["ckv_g"].r], W=[gbc.r])
    junk = sb("p1junk", (128, 128), F32, st)
    ssq = sb("p1ssq", (128, 4), F32, st)
    sub = (g.debug or {}).get("sub", 99)
    for tt in range(NT if sub >= 1 else 0):
        b = nbank()
        for c in range(8):
            p.mm(b[:, 0:412], xTb[:, c, tt * 128:(tt + 1) * 128], wtb[:, c, :], c == 0, c == 7,
                 R=[wtb.r, xq[tt // 8]], W=[b.r])
        p.cp("dve", Rz["vsA"][:, tt, :, 0:64], b[:, 0:128].rearrange("p (g d) -> p g d", g=2), R=[b.r], W=[Rz["vsA"].r])
        p.cp("dve", Rz["vwA"][:, tt, :, 0:64], b[:, 128:256].rearrange("p (g d) -> p g d", g=2), R=[b.r], W=[Rz["vwA"].r])
        if sub <= 1:
            continue
        p.act(junk[:], b[:, 256:384], AF.Square, R=[b.r], W=[junk.r, ssq.r], accum=ssq[:, 0:1])
        sub2 = (g.debug or {}).get("sub2", 99)
        if sub2 <= 0:
            continue
        p.ts("dve", ssq[:, 1:2], ssq[:, 0:1], 1.0 / 128, 1e-6, ALU.mult, ALU.add, R=[ssq.r], W=[ssq.r])
        if sub2 <= 1:
            continue
        p.act(ssq[:, 2:3], ssq[:, 1:2], AF.Sqrt, R=[ssq.r], W=[ssq.r])
        if sub2 <= 2:
            continue
        p.op("dve", lambda e: e.reciprocal(ssq[:, 3:4], ssq[:, 2:3]), R=[ssq.r], W=[ssq.r])
        if sub2 <= 3:
            continue
        p.stt(Rz["ckvA"][:, tt, 0:128], b[:, 256:384], ssq[:, 3:4], gbc[:], ALU.mult, ALU.mult,
              R=[b.r, ssq.r, gbc.r], W=[Rz["ckvA"].r])
        if sub <= 2:
            continue
        p.act(Rz["gn"][:, tt, :], b[:, 384:408], AF.Sigmoid, R=[b.r], W=[Rz["gn"].r])
        p.act(Rz["wabs"][:, tt, :], b[:, 408:412], AF.Abs, scale=0.5, R=[b.r], W=[Rz["wabs"].r])
        p.act(Rz["wsgn"][:, tt, :], b[:, 408:412], AF.Sign, R=[b.r], W=[Rz["wsgn"].r])
        if sub <= 3:
            continue
        b2 = nbank()
        tv = b2.t[:].bitcast(BF16)
        p.tr(tv[:, 0:128], Rz["ckvA"][:, tt, 0:128], Rz["identb"][:], R=[Rz["ckvA"].r, Rz["identb"].r], W=[b2.r])
        p.cp("act", Rz["ckvT"][:, tt * 128:(tt + 1) * 128], tv[:, 0:128], R=[b2.r], W=[Rz["ckvT"].r])

    if cut <= 6:
        return st
    p.flush()
    st.close()
    st = ExitStack()
    phase1b(g, st, kcT, vcT, nbank)
    return st


def phase1b(g, st, kcT, vcT, nbank):
    nc, p, sb, I, S, Rz = g.nc, g.p, g.sb, g.I, g.S, g.Rz
    w1s = sb("w1s", (128, 8, 256), F32, st)
    w2s = sb("w2s", (128, 2, 64), F32, st)
    pes = sb("pes", (128, 32), F32, st)
    peb = sb("peb", (128, 32), BF16, st)
    cst = sb("cst", (128, 2), F32, st)
    u = sb("cu", (128, 256), F32, st)
    t1 = sb("ct1", (128, 256), F32, st)
    t2 = sb("ct2", (128, 256), F32, st)
    cms = sb("cms", (128, 2, 64), F32, st)
    p.dma("sp", cms[:], I["cmap"][:, :].rearrange("(c p) n -> p c n", p=128), R=[I["cmap"].r], W=[cms.r])
    for gq in range(2):
        p.cp("dve", Rz["vcmpM"][:, :, gq, 64:128], cms[:], R=[cms.r], W=[Rz["vcmpM"].r])
    for kv, (srcT, w1n, w2n, pen) in enumerate([(kcT, "cw1_k", "cw2_k", "pe_kT"), (vcT, "cw1_v", "cw2_v", "pe_vT")]):
        w1b = sb("w1b%d" % kv, (128, 32, 256), BF16, st)
        w2p = sb("w2p%d" % kv, (128, 2, 2, 128), BF16, st)
        w2b = sb("w2b%d" % kv, (128, 2, 64), BF16, st)
        gel = sb("gel%d" % kv, (128, 2, 2, 256), BF16, st)
        p.memset("pool", gel[:], 0.0, W=[gel.r])
        p.memset("pool", w2p[:], 0.0, W=[w2p.r])
        for lq in range(4):
            for half in range(2):
                p.dma("sp", w1s[half * 64:(half + 1) * 64, :, :],
                      I[w1n][lq * 512:(lq + 1) * 512, :].rearrange("(l d) h -> d l h", d=64),
                      R=[I[w1n].r], W=[w1s.r])
            p.cp("act", w1b[:, lq * 8:(lq + 1) * 8, :], w1s[:], R=[w1s.r], W=[w1b.r])
        p.dma("sp", w2s[:], I[w2n][:, :].rearrange("(c p) d -> p c d", p=128), R=[I[w2n].r], W=[w2s.r])
        p.cp("dve", w2b[:], w2s[:], R=[w2s.r], W=[w2b.r])
        for gq in range(2):
            p.cp("dve", w2p[:, :, gq, gq * 64:(gq + 1) * 64], w2s[:], R=[w2s.r], W=[w2p.r])
        for half in range(2):
            p.dma("sp", pes[half * 64:(half + 1) * 64, :], I[pen][:, :], R=[I[pen].r], W=[pes.r])
        p.cp("dve", peb[:], pes[:], R=[pes.r], W=[peb.r])
        for gq in range(2):
            rows = slice(gq * 64, (gq + 1) * 64)
            for hc in range(2):
                bH, bC = nbank(), nbank()
                for l in range(32):
                    p.mm(bH[:, 0:255], w1b[rows, l, hc * 128:(hc + 1) * 128], srcT.t[rows, l:l + 16 * 254 + 1:16],
                         l == 0, l == 31, R=[w1b.r, srcT.r], W=[bH.r])
                for l in range(32):
                    p.mm(bC[:, 0:1], w1b[rows, l, hc * 128:(hc + 1) * 128], peb[rows, l:l + 1],
                         l == 0, l == 31, R=[w1b.r, peb.r], W=[bC.r])
                p.cp("dve", cst[:, 0:1], bC[:, 0:1], R=[bC.r], W=[cst.r])
                p.act(u[:, 0:255], bH[:, 0:255], AF.Identity, bias=cst[:, 0:1], R=[bH.r, cst.r], W=[u.r])
                p.tt("dve", t1[:, 0:255], u[:, 0:255], u[:, 0:255], ALU.mult, R=[u.r], W=[t1.r])
                p.ts("dve", t1[:, 0:255], t1[:, 0:255], 0.044715, 1.0, ALU.mult, ALU.add, R=[t1.r], W=[t1.r])
                p.tt("dve", t1[:, 0:255], t1[:, 0:255], u[:, 0:255], ALU.mult, R=[t1.r, u.r], W=[t1.r])
                p.act(t2[:, 0:255], t1[:, 0:255], AF.Tanh, scale=0.7978845608028654, R=[t1.r], W=[t2.r])
                p.stt(t2[:, 0:255], t2[:, 0:255], 1.0, u[:, 0:255], ALU.add, ALU.mult, R=[t2.r, u.r], W=[t2.r])
                p.ts("dve", gel[:, gq, hc, 0:255], t2[:, 0:255], 0.5, None, ALU.mult, R=[t2.r], W=[gel.r])
        if kv == 0:
            b = nbank()
            n = 0
            for gq in range(2):
                for hc in range(2):
                    p.mm(b[:, 0:256], w2p[:, hc, gq, :], gel[:, gq, hc, :], n == 0, n == 3, R=[w2p.r, gel.r], W=[b.r])
                    n += 1
            p.cp("dve", Rz["kcmpT"][:], b[:, 0:256], R=[b.r], W=[Rz["kcmpT"].r])
        else:
            for gq in range(2):
                for cc in range(2):
                    b = nbank()
                    for hc in range(2):
                        p.mm(b[:, 0:64], gel[:, gq, hc, cc * 128:(cc + 1) * 128], w2b[:, hc, :], hc == 0, hc == 1,
                             R=[gel.r, w2b.r], W=[b.r])
                    p.cp("dve", Rz["vcmpM"][:, cc, gq, 0:64], b[:, 0:64], R=[b.r], W=[Rz["vcmpM"].r])

    sq = sb("sq", (128, T), BF16, st)
    row = sb("kmrow", (1, T), F32, st)
    tmp = sb("kmtmp", (1, 8), F32, st)
    rl = sb("relrow", (1, 512), F32, st)
    p.dma("sp", rl[:], I["relflat"][:, :], R=[I["relflat"].r], W=[rl.r])
    p.act(rl[:], rl[:], AF.Abs, R=[rl.r], W=[rl.r])
    p.op("dve", lambda e: e.reduce_max(tmp[:, 0:1], rl[:], AX.X), R=[rl.r], W=[tmp.r])
    for which, srcs in enumerate([(Rz["ksT"], Rz["kwT"]), (Rz["ckvT"],)]):
        first = True
        for s_ in srcs:
            p.tt("dve", sq[:], s_[:], s_[:], ALU.mult, R=[s_.r], W=[sq.r])
            for kb in range(8):
                b = nbank()
                p.mm(b[0:1, :], Rz["ones_bf"][:, 0:1], sq[:, kb * 512:(kb + 1) * 512], True, True,
                     R=[sq.r, Rz["ones_bf"].r], W=[b.r])
                if first:
                    p.cp("dve", row[:, kb * 512:(kb + 1) * 512], b[0:1, :], R=[b.r], W=[row.r])
                else:
                    p.tt("dve", row[:, kb * 512:(kb + 1) * 512], row[:, kb * 512:(kb + 1) * 512], b[0:1, :], ALU.add,
                         R=[b.r, row.r], W=[row.r])
            first = False
        p.op("dve", lambda e: e.reduce_max(tmp[:, 1:2], row[:], AX.X), R=[row.r], W=[tmp.r])
        p.act(tmp[:, 2:3], tmp[:, 1:2], AF.Sqrt, R=[tmp.r], W=[tmp.r])
        p.ts("dve", Rz["kmax"][:, 2 * which:2 * which + 1], tmp[:, 2:3], -1.03, None, ALU.mult, R=[tmp.r], W=[Rz["kmax"].r])
        p.ts("dve", Rz["kmax"][:, 2 * which + 1:2 * which + 2], tmp[:, 0:1], -1.0, None, ALU.mult, R=[tmp.r], W=[Rz["kmax"].r])


def phase2(g):
    nc, p, sb, I, S, Rz = g.nc, g.p, g.sb, g.I, g.S, g.Rz
    st = ExitStack()
    B = g.banks
    ident, identf, ones = Rz["identb"], Rz["identf"], Rz["ones_bf"]
    S["oT_s"] = g.dscr("oT_s", (NT, 128, 8, 128), BF16)
    ntiles = (g.debug or {}).get("ntiles", NT)

    stg = sb("c_stg", (128, 1024), F32, st)
    relTa = sb("relTa", (128, 8, 2, 512), BF16, st)
    relTb = sb("relTb", (128, 8, 1024), BF16, st)
    relTw = sb("relTw", (128, 2, 512), BF16, st)
    eall = sb("eall", (128, 32, 128), BF16, st)
    onesN = sb("onesN", (128, 128), BF16, st)
    p.memset("pool", eall[:], 0.0, W=[eall.r])
    p.memset("pool", onesN[:], 0.0, W=[onesN.r])
    p.memset("pool", onesN[0:1, :], 1.0, W=[onesN.r])
    d30 = sb("d30", (128, 512), BF16, st)
    mcmp = sb("mcmp", (128, 8, 503), BF16, st)
    wext = sb("wext", (128, 128), F32, st)
    mcq = sb("mcq", (128, 128), F32, st)
    rowsA = [sb("rowsA%d" % i, (128, 512), BF16, st) for i in range(2)]
    rowsB = sb("rowsB", (128, 1024), BF16, st)
    for r_ in rowsA + [rowsB]:
        p.memset("pool", r_[:], 0.0, W=[r_.r])
    wuvP = sb("wuvP", (128, 8, 128), BF16, st)
    st0 = ExitStack()
    mc4 = sb("mc4", (128, 512), F32, st0)
    mw4 = sb("mw4", (128, 512), F32, st0)
    c31A = sb("c31A", (128, 2, 512), F32, st0)
    c31B = sb("c31B", (128, 1024), F32, st0)
    p.dma("sp", mc4[:], I["mc4"][:, :], R=[I["mc4"].r], W=[mc4.r])
    p.dma("sp", mw4[:], I["mw4"][:, :], R=[I["mw4"].r], W=[mw4.r])
    p.dma("sp", wext[:], I["wext"][:, :], R=[I["wext"].r], W=[wext.r])
    p.dma("sp", mcq[:], I["mcq"][:, :], R=[I["mcq"].r], W=[mcq.r])
    for gq in range(2):
        p.dma("sp", c31A[:, gq, :], I["c31a"][gq:gq + 1, :].partition_broadcast(128), R=[I["c31a"].r], W=[c31A.r])
    p.dma("sp", c31B[:], I["c31b"][0:1, :].partition_broadcast(128), R=[I["c31b"].r], W=[c31B.r])
    for d in range(8):
        for gq in range(2):
            p.dma("sp", stg[:, 0:512], I["relTa"][:, d, gq, :], R=[I["relTa"].r], W=[stg.r])
            p.tt("dve", stg[:, 0:512], stg[:, 0:512], c31A[:, gq, :], ALU.subtract, R=[stg.r, c31A.r], W=[stg.r])
            if d == 0:
                p.tt("dve", stg[:, 0:512], stg[:, 0:512], mc4[:], ALU.add, R=[stg.r, mc4.r], W=[stg.r])
            p.cp("dve", relTa[:, d, gq, :], stg[:, 0:512], R=[stg.r], W=[relTa.r])
            if d == 4:
                p.tt("dve", stg[:, 0:512], stg[:, 0:512], mw4[:], ALU.add, R=[stg.r, mw4.r], W=[stg.r])
                p.cp("dve", relTw[:, gq, :], stg[:, 0:512], R=[stg.r], W=[relTw.r])
        p.dma("sp", stg[:], I["relTb"][:, d, :], R=[I["relTb"].r], W=[stg.r])
        p.tt("dve", stg[:], stg[:], c31B[:], ALU.subtract, R=[stg.r, c31B.r], W=[stg.r])
        if d == 0:
            for hf in range(2):
                p.tt("dve", stg[:, hf * 512:(hf + 1) * 512], stg[:, hf * 512:(hf + 1) * 512], mc4[:], ALU.add,
                     R=[stg.r, mc4.r], W=[stg.r])
        p.cp("dve", relTb[:, d, :], stg[:], R=[stg.r], W=[relTb.r])
    for q4 in range(4):
        p.dma("sp", stg[0:64, :], I["eall"][:, q4 * 1024:(q4 + 1) * 1024], R=[I["eall"].r], W=[stg.r])
        p.cp("dve", eall[0:64, q4 * 8:(q4 + 1) * 8, :], stg[0:64, :].rearrange("p (a b) -> p a b", b=128), R=[stg.r], W=[eall.r])
    p.dma("sp", stg[:, 0:512], I["d30"][:, :], R=[I["d30"].r], W=[stg.r])
    p.cp("dve", d30[:], stg[:, 0:512], R=[stg.r], W=[d30.r])
    for h in range(8):
        p.dma("sp", stg[:, 0:503], I["cmpv"][:, h, :], R=[I["cmpv"].r], W=[stg.r])
        p.dma("sp", stg[:, 512:1015], I["cmpm"][:, :], R=[I["cmpm"].r], W=[stg.r])
        p.tt("dve", mcmp[:, h, :], stg[:, 0:503], stg[:, 512:1015], ALU.add, R=[stg.r], W=[mcmp.r])
    p.memset("dve", eall[64:65, :, :], 1.0, W=[eall.r])
    p.memset("pool", wuvP[:], 0.0, W=[wuvP.r])
    for h in range(8):
        p.dma("sp", stg[:, 0:64], I["w_uv"][h, :, :], R=[I["w_uv"].r], W=[stg.r])
        p.cp("dve", wuvP[:, h, (h % 2) * 64:(h % 2) * 64 + 64], stg[:, 0:64], R=[stg.r], W=[wuvP.r])

    p.flush()
    st0.close()
    qTz = [sb("qTz%d" % i, (128, 512), BF16, st) for i in range(2)]
    for q_ in qTz:
        p.memset("pool", q_[:], 0.0, W=[q_.r])
    qlT = sb("qlT", (128, 1024), BF16, st)
    qiT = sb("qiT", (64, 512), BF16, st)
    zidx = sb("zidx", (128, T), F32, st)
    selD = Buf(g.kcT.t, g.kcT.r)
    junk = Buf(g.vcT.t, g.vcT.r)
    rr = [sb("rr%d" % i, (128, 512), F32, st) for i in range(2)]
    sq = sb("sqq", (128, 1024), BF16, st)
    srow = Buf(stg.t[0:1, :], stg.r)
    scmp = sb("scmp", (128, 8, 256), F32, st)
    pn = sb("pn", (128, 8, 256), BF16, st)
    pnT = sb("pnT", (128, 16, 128), BF16, st)
    sm = sb("sm", (128, 16), F32, st)
    sm2 = sb("sm2", (128, 16), F32, st)
    imp = sb("imp", (128, 64), F32, st)
    sc1 = sb("sc1", (128, 64), F32, st)
    sc2 = sb("sc2", (128, 64), F32, st)
    m8 = sb("m8", (128, 16), F32, st)
    selb = sb("selb", (128, 64), BF16, st)
    selT4 = [sb("selT4%d" % i, (128, 512), BF16, st) for i in range(2)]
    for s_ in selT4:
        p.memset("pool", s_[:], 0.0, W=[s_.r])
    PT = [sb("PT%d" % i, (128, 512), BF16, st) for i in range(4)]
    ocmp = sb("ocmp", (128, 512), F32, st)
    oa32 = sb("oa32", (128, 512), F32, st)
    oab = sb("oab", (128, 512), BF16, st)
    coef = sb("coef", (128, 32), F32, st)
    oln = sb("oln", (128, 8, 128), BF16, st)
    olT = sb("olT", (128, 8, 128), BF16, st)
    oT = sb("oT", (128, 8, 128), BF16, st)
    bis = sb("bis", (128, 8), F32, st)
    p.memset("pool", pn[:], 0.0, W=[pn.r])
    p.memset("pool", scmp[:], 0.0, W=[scmp.r])

    def bfv(bank):
        return bank.t[:].bitcast(BF16)

    selDs = [selD, junk]
    pw = sb("pw", (128, BIS_ITERS + 1), F32, st)
    wct = sb("wct", (128, BIS_ITERS + 1), F32, st)
    for k in range(BIS_ITERS + 1):
        p.memset("pool", pw[:, k:k + 1], 2.0 ** -(k + 1), W=[pw.r])

    def stream_D(i):
        nk = 128 * (i + 1)
        sD = selDs[i % 2]
        p.dma("sp", qiT[:], S["qi_s"][i, :, :, :].rearrange("d h q -> d (h q)"), R=[S["qi_s"].r], W=[qiT.r])
        for kb in range((nk + 511) // 512):
            k0 = kb * 512
            kn = min(512, nk - k0)
            for hi in range(4):
                bk = B[hi % 2]
                r_ = rr[hi % 2]
                p.mm(bk[:, 0:kn], qiT[:, hi * 128:(hi + 1) * 128], Rz["kiT"][:, k0:k0 + kn], True, True,
                     R=[qiT.r, Rz["kiT"].r], W=[bk.r])
                p.act(r_[:, 0:kn], bk[:, 0:kn], AF.Relu, scale=Rz["wabs"][:, i, hi:hi + 1], R=[bk.r, Rz["wabs"].r], W=[r_.r])
                if hi == 0:
                    p.ts("dve", zidx[:, k0:k0 + kn], r_[:, 0:kn], Rz["wsgn"][:, i, 0:1], None, ALU.mult,
                         R=[r_.r, Rz["wsgn"].r], W=[zidx.r])
                else:
                    p.stt(zidx[:, k0:k0 + kn], r_[:, 0:kn], Rz["wsgn"][:, i, hi:hi + 1], zidx[:, k0:k0 + kn], ALU.mult, ALU.add,
                          R=[r_.r, Rz["wsgn"].r, zidx.r], W=[zidx.r])
            yield
        p.op("dve", lambda e: e.tensor_reduce(bis[:, 0:1], zidx[:, 0:nk], AX.X, ALU.min), R=[zidx.r], W=[bis.r])
        p.op("dve", lambda e: e.reduce_max(bis[:, 1:2], zidx[:, 0:nk], AX.X), R=[zidx.r], W=[bis.r])
        p.stt(bis[:, 2:3], bis[:, 1:2], 1.0, bis[:, 0:1], ALU.add, ALU.subtract, R=[bis.r], W=[bis.r])
        p.ts("dve", wct[:], pw[:], bis[:, 2:3], None, ALU.mult, R=[pw.r, bis.r], W=[wct.r])
        p.tt("dve", bis[:, 3:4], bis[:, 0:1], wct[:, 0:1], ALU.add, R=[bis.r, wct.r], W=[bis.r])
        p.tt("dve", zidx[:, nk - 128:nk], zidx[:, nk - 128:nk], mcq[:], ALU.add, R=[zidx.r, mcq.r], W=[zidx.r])
        yield
        for k in range(BIS_ITERS):
            p.ts("dve", sD[:, 0:nk], zidx[:, 0:nk], bis[:, 3:4], None, ALU.is_ge, ALU.add, R=[zidx.r, bis.r], W=[sD.r, bis.r],
                 accum=bis[:, 4:5])
            p.stt(bis[:, 5:6], bis[:, 4:5], 256.0, wct[:, k:k + 1], ALU.is_ge, ALU.mult, R=[bis.r, wct.r], W=[bis.r])
            p.ts("dve", bis[:, 3:4], bis[:, 3:4], wct[:, k + 1:k + 2], bis[:, 5:6], ALU.subtract, ALU.add, R=[bis.r, wct.r], W=[bis.r])
            yield
        p.tt("dve", bis[:, 6:7], bis[:, 3:4], wct[:, BIS_ITERS:BIS_ITERS + 1], ALU.subtract, R=[bis.r, wct.r], W=[bis.r])
        p.ts("dve", sD[:, 0:nk], zidx[:, 0:nk], bis[:, 6:7], 1.0, ALU.is_ge, ALU.subtract, R=[zidx.r, bis.r], W=[sD.r])
        yield

    def stream_P(i):
        sD = selDs[i % 2]
        for gq in range(2):
            p.dma("sp", qTz[gq][gq * 64:(gq + 1) * 64, :], S["qa_s"][i, gq, :, :, :].rearrange("d h q -> d (h q)"),
                  R=[S["qa_s"].r], W=[qTz[gq].r])
        p.dma("sp", qlT[:], S["ql_s"][i, :, :, :].rearrange("r h q -> r (h q)"), R=[S["ql_s"].r], W=[qlT.r])
        for gq in range(2):
            p.act(sq[:, 0:512], qTz[gq][:], AF.Square, R=[qTz[gq].r], W=[sq.r])
            p.mm(B[7][0:1, :], ones[:, 0:1], sq[:, 0:512], True, True, R=[ones.r, sq.r], W=[B[7].r])
            p.act(srow[:, 0:512], B[7][0:1, :], AF.Sqrt, R=[B[7].r], W=[srow.r])
            p.ts("dve", rowsA[gq][0:1, :], srow[:, 0:512], Rz["kmax"][0:1, 0:1], Rz["kmax"][0:1, 1:2], ALU.mult, ALU.add,
                 R=[srow.r, Rz["kmax"].r], W=[rowsA[gq].r])
            p.dma("sp", selT4[gq][64:65, :], rowsA[gq][0:1, :], R=[rowsA[gq].r], W=[selT4[gq].r])
        p.act(sq[:], qlT[:], AF.Square, R=[qlT.r], W=[sq.r])
        for hf in range(2):
            p.mm(B[7][0:1, :], ones[:, 0:1], sq[:, hf * 512:(hf + 1) * 512], True, True, R=[ones.r, sq.r], W=[B[7].r])
            p.act(srow[:, hf * 512:(hf + 1) * 512], B[7][0:1, :], AF.Sqrt, R=[B[7].r], W=[srow.r])
        p.ts("dve", rowsB[0:1, :], srow[:], Rz["kmax"][0:1, 2:3], Rz["kmax"][0:1, 3:4], ALU.mult, ALU.add,
             R=[srow.r, Rz["kmax"].r], W=[rowsB.r])
        yield
        off = 248 - 8 * i
        for h in range(8):
            gq, hh = h // 4, h % 4
            rows = slice(gq * 64, (gq + 1) * 64)
            bL = B[2 + h // 2]
            p.mm(bL[:, (h % 2) * 256:(h % 2) * 256 + 255], qTz[gq][rows, hh * 128:(hh + 1) * 128], Rz["kcmpT"][rows, 0:255],
                 h % 2 == 0, h % 2 == 1, R=[qTz[gq].r, Rz["kcmpT"].r], W=[bL.r])
        for b4 in range(4):
            bL = B[2 + b4]
            p.tt("dve", scmp[:, 2 * b4:2 * b4 + 2, 0:255], bL[:, :].rearrange("p (h c) -> p h c", c=256)[:, :, 0:255],
                 mcmp[:, 2 * b4:2 * b4 + 2, off:off + 255], ALU.add, R=[bL.r, mcmp.r], W=[scmp.r])
        yield
        p.op("dve", lambda e: e.reduce_max(sm[:, 0:8], scmp[:, :, 0:255], AX.X), R=[scmp.r], W=[sm.r])
        p.ts("dve", sm[:, 8:16], sm[:, 0:8], -1000.0, -1.0, ALU.max, ALU.mult, R=[sm.r], W=[sm.r])
        p.tt("dve", scmp[:, :, 0:255], scmp[:, :, 0:255], sm[:, 8:16].unsqueeze(2).to_broadcast([128, 8, 255]), ALU.add,
             R=[scmp.r, sm.r], W=[scmp.r])
        p.act(scmp[:, :, 0:255], scmp[:, :, 0:255], AF.Exp, R=[scmp.r], W=[scmp.r])
        yield
        p.op("dve", lambda e: e.reduce_sum(sm2[:, 0:8], scmp[:, :, 0:255], AX.X), R=[scmp.r], W=[sm2.r])
        p.ts("dve", sm2[:, 0:8], sm2[:, 0:8], 1e-30, None, ALU.max, R=[sm2.r], W=[sm2.r])
        p.op("dve", lambda e: e.reciprocal(sm2[:, 8:16], sm2[:, 0:8]), R=[sm2.r], W=[sm2.r])
        p.tt("dve", pn[:, :, 0:255], scmp[:, :, 0:255], sm2[:, 8:16].unsqueeze(2).to_broadcast([128, 8, 255]), ALU.mult,
             R=[scmp.r, sm2.r], W=[pn.r])
        yield
        for hf in range(2):
            bk = B[2 + hf]
            tb_ = bfv(bk)
            for j in range(8):
                h, cc = hf * 4 + j // 2, j % 2
                p.tr(tb_[:, j * 128:(j + 1) * 128], pn[:, h, cc * 128:(cc + 1) * 128], ident[:], R=[pn.r, ident.r], W=[bk.r])
            p.cp("act" if hf else "dve", pnT[:, hf * 8:(hf + 1) * 8, :], tb_[:, :].rearrange("p (c q) -> p c q", q=128), R=[bk.r], W=[pnT.r])
        yield
        for gq in range(2):
            accb = B[6 + gq]
            for hh in range(4):
                h = gq * 4 + hh
                for cc in range(2):
                    p.mm(accb[:, hh * 128:(hh + 1) * 128], pnT[:, 2 * h + cc, :], Rz["vcmpM"][:, cc, gq, :], hh == 0 and cc == 0, hh == 3 and cc == 1,
                         R=[pnT.r, Rz["vcmpM"].r], W=[accb.r])
            p.cp("act", ocmp[:, gq * 256:(gq + 1) * 256].rearrange("p (h d) -> p h d", d=64),
                 accb[:, :].rearrange("p (h j) -> p h j", j=128)[:, :, 0:64], R=[accb.r], W=[ocmp.r])
            p.op("dve", lambda e, accb=accb: e.reduce_sum(imp[:], accb[:, :].rearrange("p (h j) -> p j h", j=128)[:, 64:128, :], AX.X),
                 R=[accb.r], W=[imp.r])
            p.tt("dve", sc1[:], imp[:], wext[:, 63 - 2 * i:127 - 2 * i], ALU.add, R=[imp.r, wext.r], W=[sc1.r])
            p.ts("dve", sc1[:, 0:1], imp[:, 0:1], 1e4, None, ALU.add, R=[imp.r], W=[sc1.r])
            p.op("dve", lambda e: e.max(m8[:, 0:8], sc1[:]), R=[sc1.r], W=[m8.r])
            p.op("dve", lambda e: e.match_replace(sc2[:], m8[:, 0:8], sc1[:], -1e30), R=[sc1.r, m8.r], W=[sc2.r])
            p.op("dve", lambda e: e.max(m8[:, 8:16], sc2[:]), R=[sc2.r], W=[m8.r])
            p.ts("dve", m8[:, 15:16], m8[:, 15:16], -1e29, None, ALU.max, R=[m8.r], W=[m8.r])
            p.ts("dve", selb[:], sc1[:], m8[:, 15:16], 1.0, ALU.is_ge, ALU.subtract, R=[sc1.r, m8.r], W=[selb.r])
            tb2 = bfv(accb)
            p.tr(tb2[0:64, 0:128], selb[:], ident[:], R=[selb.r, ident.r], W=[accb.r])
            p.cp("dve", selT4[gq][0:64, :].rearrange("p (h q) -> p h q", q=128),
                 tb2[0:64, 0:128].unsqueeze(1).to_broadcast([64, 4, 128]), R=[accb.r], W=[selT4[gq].r])
            yield
        DEPTH = 4
        sbanks = [B[2], B[3], B[6], B[7]]
        units = []
        for gq in range(2):
            for br_ in range(2):
                kts = list(range(0, i + 1)) if br_ == 0 else list(range(max(0, i - 4), i + 1))
                for n, kt in enumerate(kts):
                    units.append((gq, br_, n, kt, len(kts)))

        def emit_S(ui):
            gq, br_, n, kt, nk_ = units[ui]
            bS = sbanks[ui % DEPTH]
            kT = Rz["ksT"] if br_ == 0 else Rz["kwT"]
            d = i - kt
            p.mm(bS[:, :], kT[:, kt * 128:(kt + 1) * 128], qTz[gq][:], True, False, R=[kT.r, qTz[gq].r], W=[bS.r])
            if br_ == 0:
                p.mm(bS[:, :], eall[:, kt, :], selT4[gq][:], False, d >= 8, R=[eall.r, selT4[gq].r], W=[bS.r])
            if d < 8:
                rel = relTw[:, gq, :] if (br_ == 1 and d == 4) else relTa[:, d, gq, :]
                p.mm(bS[:, :], ident[:], rel, False, br_ == 0, R=[ident.r, relTa.r, relTw.r], W=[bS.r])
            if br_ == 1:
                p.mm(bS[:, :], onesN[:], rowsA[gq][:], False, True, R=[onesN.r, rowsA[gq].r], W=[bS.r])

        def epilogue(gq, br_):
            accb = B[4 + br_]
            accv = accb[:, 0:260].rearrange("p (h e) -> p h e", e=65)
            p.ts("dve", coef[:, 0:4], accv[:, :, 64], 1e-30, None, ALU.max, R=[accb.r], W=[coef.r])
            p.op("dve", lambda e: e.reciprocal(coef[:, 4:8], coef[:, 0:4]), R=[coef.r], W=[coef.r])
            gv = Rz["gn"][:, i, gq * 12:(gq + 1) * 12].rearrange("p (h t) -> p h t", t=3)
            p.tt("dve", coef[:, 8:12], coef[:, 4:8], gv[:, :, 1 + br_], ALU.mult, R=[coef.r, Rz["gn"].r], W=[coef.r])
            for hh in range(4):
                h = gq * 4 + hh
                if br_ == 0:
                    p.ts("dve", oa32[:, h * 64:(h + 1) * 64], ocmp[:, h * 64:(h + 1) * 64], Rz["gn"][:, i, 3 * h:3 * h + 1], None, ALU.mult,
                         R=[ocmp.r, Rz["gn"].r], W=[oa32.r])
                    p.stt(oa32[:, h * 64:(h + 1) * 64], accv[:, hh, 0:64], coef[:, 8 + hh:9 + hh], oa32[:, h * 64:(h + 1) * 64], ALU.mult, ALU.add,
                          R=[accb.r, coef.r, oa32.r], W=[oa32.r])
                else:
                    p.stt(oab[:, h * 64:(h + 1) * 64], accv[:, hh, 0:64], coef[:, 8 + hh:9 + hh], oa32[:, h * 64:(h + 1) * 64], ALU.mult, ALU.add,
                          R=[accb.r, coef.r, oa32.r], W=[oab.r])

        pend = []
        for ui in range(min(DEPTH - 1, len(units))):
            emit_S(ui)
        for ui, (gq, br_, n, kt, nk_) in enumerate(units):
            bS = sbanks[ui % DEPTH]
            pt = PT[ui % DEPTH]
            accb = B[4 + br_]
            vA = Rz["vsA"] if br_ == 0 else Rz["vwA"]
            p.act(pt[:], bS[:, :], AF.Exp, R=[bS.r], W=[pt.r])
            if ui + DEPTH - 1 < len(units):
                emit_S(ui + DEPTH - 1)
            if n == 0:
                for pe_ in list(pend):
                    if pe_[2] == br_:
                        epilogue(pe_[1], pe_[2])
                        pend.remove(pe_)
            for hh in range(4):
                p.mm(accb[:, hh * 65:(hh + 1) * 65], pt[:, hh * 128:(hh + 1) * 128], vA[:, kt, gq, :],
                     n == 0 and hh == 0, n == nk_ - 1, R=[pt.r, vA.r], W=[accb.r])
            if n == nk_ - 1:
                pend.append([2, gq, br_])
            for pe_ in list(pend):
                if pe_[0] == 0:
                    epilogue(pe_[1], pe_[2])
                    pend.remove(pe_)
                else:
                    pe_[0] -= 1
            yield
        for pe_ in pend:
            epilogue(pe_[1], pe_[2])
        tb_ = bfv(B[7])
        for c in range(4):
            p.tr(tb_[:, c * 128:(c + 1) * 128], oab[:, c * 128:(c + 1) * 128], ident[:], R=[oab.r, ident.r], W=[B[7].r])
        p.cp("act", oT[:, 0:4, :], tb_[:, 0:512].rearrange("p (c q) -> p c q", q=128), R=[B[7].r], W=[oT.r])
        yield
        accD = [B[4], B[5], B[6]]
        hb = [(0, 0), (0, 1), (0, 2), (1, 0), (1, 1), (1, 2), (2, 0), (2, 1)]
        dunits = [(kt, hf) for kt in range(i + 1) for hf in range(2)]

        dbanks = [B[2], B[3], B[7]]

        def emit_SD(ui):
            kt, hf = dunits[ui]
            d = i - kt
            bS = dbanks[ui % 3]
            cols = slice(hf * 512, (hf + 1) * 512)
            p.mm(bS[:, :], Rz["ckvT"][:, kt * 128:(kt + 1) * 128], qlT[:, cols], True, False, R=[Rz["ckvT"].r, qlT.r], W=[bS.r])
            p.mm(bS[:, :], sD[:, kt * 128:(kt + 1) * 128], d30[:], False, False, R=[sD.r, d30.r], W=[bS.r])
            if d < 8:
                p.mm(bS[:, :], ident[:], relTb[:, d, cols], False, False, R=[ident.r, relTb.r], W=[bS.r])
            p.mm(bS[:, :], onesN[:], rowsB[:, cols], False, True, R=[onesN.r, rowsB.r], W=[bS.r])

        DD = 3
        for ui in range(min(DD - 1, len(dunits))):
            emit_SD(ui)
        for ui, (kt, hf) in enumerate(dunits):
            bS = dbanks[ui % DD]
            pt = PT[ui % DD]
            p.act(pt[:], bS[:, :], AF.Exp, R=[bS.r], W=[pt.r])
            if ui + DD - 1 < len(dunits):
                emit_SD(ui + DD - 1)
            for hh in range(4):
                h = hf * 4 + hh
                bk, sl = hb[h]
                p.mm(accD[bk][:, sl * 129:(sl + 1) * 129], pt[:, hh * 128:(hh + 1) * 128], Rz["ckvA"][:, kt, :],
                     kt == 0 and sl == 0, kt == i, R=[pt.r, Rz["ckvA"].r], W=[accD[bk].r])
            if hf == 1:
                yield
        for h in range(8):
            bk, sl = hb[h]
            p.ts("dve", coef[:, 16 + h:17 + h], accD[bk][:, sl * 129 + 128:sl * 129 + 129], 1e-30, None, ALU.max, R=[accD[bk].r], W=[coef.r])
        p.op("dve", lambda e: e.reciprocal(coef[:, 24:32], coef[:, 16:24]), R=[coef.r], W=[coef.r])
        for h in range(8):
            bk, sl = hb[h]
            p.ts("dve", oln[:, h, :], accD[bk][:, sl * 129:sl * 129 + 128], coef[:, 24 + h:25 + h], None, ALU.mult,
                 R=[accD[bk].r, coef.r], W=[oln.r])
        yield
        for hf in range(2):
            tb_ = bfv(B[7])
            for hh in range(4):
                p.tr(tb_[:, hh * 128:(hh + 1) * 128], oln[:, hf * 4 + hh, :], ident[:], R=[oln.r, ident.r], W=[B[7].r])
            p.cp("act", olT[:, hf * 4:(hf + 1) * 4, :], tb_[:, 0:512].rearrange("p (c q) -> p c q", q=128), R=[B[7].r], W=[olT.r])
        for c in range(4):
            for hl in range(2):
                p.mm(B[7][:, c * 128:(c + 1) * 128], wuvP[:, 2 * c + hl, :], olT[:, 2 * c + hl, :], c == 0 and hl == 0, c == 3 and hl == 1,
                     R=[wuvP.r, olT.r], W=[B[7].r])
        p.cp("act", oT[:, 4:8, :], B[7][:, :].rearrange("p (c q) -> p c q", q=128), R=[B[7].r], W=[oT.r])
        p.dma("pool", S["oT_s"][i, :, :, :], oT[:], R=[oT.r], W=[S["oT_s"].r])
        yield

    def len_P(i):
        return 1 + 6 + 2 * ((i + 1) + min(5, i + 1)) + 4 + 1 + (i + 1) + 2

    def len_D(i):
        return (128 * (i + 1) + 511) // 512 + 1 + BIS_ITERS + 1

    for _ in stream_D(0):
        pass
    for i in range(ntiles):
        gd = stream_D(i + 1) if i + 1 < ntiles else None
        ratio = (len_D(i + 1) / float(len_P(i))) if gd is not None else 0.0
        credit = 0.0
        for _ in stream_P(i):
            credit += ratio
            while gd is not None and credit >= 1.0:
                credit -= 1.0
                try:
                    next(gd)
                except StopIteration:
                    gd = None
        if gd is not None:
            for _ in gd:
                pass
    return st


def layer_norm_tile(p, r, rs, gbc, bbc, out, tmp, R_extra=()):
    p.tt("dve", rs[:, 2:3], rs[:, 0:1], rs[:, 1:2], ALU.add, R=[rs.r], W=[rs.r])
    p.ts("dve", rs[:, 3:4], rs[:, 2:3], -1.0 / D, None, ALU.mult, R=[rs.r], W=[rs.r])
    p.act(tmp[:], r[:], AF.Square, bias=rs[:, 3:4], accum=rs[:, 4:5], R=[r.r, rs.r], W=[tmp.r, rs.r])
    p.ts("dve", rs[:, 5:6], rs[:, 4:5], 1.0 / D, 1e-5, ALU.mult, ALU.add, R=[rs.r], W=[rs.r])
    p.act(rs[:, 6:7], rs[:, 5:6], AF.Sqrt, R=[rs.r], W=[rs.r])
    p.op("dve", lambda e: e.reciprocal(rs[:, 7:8], rs[:, 6:7]), R=[rs.r], W=[rs.r])
    p.ts("dve", tmp[:], r[:], rs[:, 3:4], rs[:, 7:8], ALU.add, ALU.mult, R=[r.r, rs.r], W=[tmp.r])
    p.tt("dve", tmp[:], tmp[:], gbc[:], ALU.mult, R=[tmp.r, gbc.r], W=[tmp.r])
    p.tt("dve", out[:], tmp[:], bbc[:], ALU.add, R=[tmp.r, bbc.r], W=[out.r])


def phase3(g):
    nc, p, sb, I, S, Rz = g.nc, g.p, g.sb, g.I, g.S, g.Rz
    st = ExitStack()
    B = g.banks
    stg = sb("w_stg", (128, 1024), F32, st)
    wba = sb("wba", (128, 4, 1024), BF16, st)
    wbb = sb("wbb", (128, 4, 1024), BF16, st)
    wout = sb("wout", (128, 8, 1024), BF16, st)
    for (dst, src, n) in [(wba, "w_ba", 4), (wbb, "w_bb", 4), (wout, "w_out", 8)]:
        for c in range(n):
            p.dma("sp", stg[:], I[src][c * 128:(c + 1) * 128, :], R=[I[src].r], W=[stg.r])
            p.cp("dve", dst[:, c, :], stg[:], R=[stg.r], W=[dst.r])
    bc = {}
    for nm in ("ln1_g", "ln1_b", "ln2_g", "ln2_b"):
        bc[nm] = sb("bc_" + nm, (128, 1024), F32, st)
        p.dma("sp", bc[nm][:], I[nm][0:1, :].partition_broadcast(128), R=[I[nm].r], W=[bc[nm].r])
    oT_l = [sb("oT2_%d" % j, (128, 8, 128), BF16, st) for j in range(2)]
    gT_l = [sb("gT_%d" % j, (128, 16, 128), F32, st) for j in range(2)]
    xt_l = [sb("xt_%d" % j, (128, 1024), F32, st) for j in range(2)]
    t1 = sb("t1", (128, 512), F32, st)
    t2 = sb("t2", (128, 512), F32, st)
    mT = sb("mT", (128, 8, 128), BF16, st)
    r = sb("r", (128, 1024), F32, st)
    h1 = sb("h1", (128, 1024), F32, st)
    tmp = sb("lntmp", (128, 1024), F32, st)
    rs = sb("rs", (128, 8), F32, st)
    h1T = sb("h1T", (128, 8, 128), F32, st)
    wr = sb("wr", (128, 8, 36), F32, st)
    brb = sb("brb", (128, 36), F32, st)
    lg = sb("lg", (128, 36), F32, st)
    rt = sb("rt", (128, 16), F32, st)
    rj = sb("rj", (128, 32), F32, st)
    og = sb("og", (128, 4), F32, st)
    esel = sb("esel", (128, 8), F32, st)
    m8r = sb("m8r", (128, 8), F32, st)
    oh = sb("oh", (128, 2, 8), F32, st)
    Ak = sb("Ak", (128, 2, 32), F32, st)
    Abf = sb("Abf", (128, 32), BF16, st)
    ustr = sb("ustr", (128, 128), BF16, st)
    posf = sb("posf", (128, 32), F32, st)
    base = sb("base", (128, 32), F32, st)
    iot = sb("iot", (128, 32), F32, st)
    tokid = sb("tokid", (128, 32), I32, st)
    tokrow = sb("tokrow", (128, 32, 16), I32, st)
    fill = sb("fill", (128, 2048), I32, st)
    p.dma("sp", wr[:], I["wr"][:, :].rearrange("(c p) n -> p c n", p=128), R=[I["wr"].r], W=[wr.r])
    p.dma("sp", brb[:], I["br"][0:1, :].partition_broadcast(128), R=[I["br"].r], W=[brb.r])
    p.dma("sp", stg[:, 0:128], I["ustrict"][:, :], R=[I["ustrict"].r], W=[stg.r])
    p.cp("dve", ustr[:], stg[:, 0:128], R=[stg.r], W=[ustr.r])
    p.dma("sp", iot[:], I["iota32"][:, :], R=[I["iota32"].r], W=[iot.r])
    p.dma("sp", tokid[:], I["tokid"][:, :], R=[I["tokid"].r], W=[tokid.r])
    p.cp("dve", tokrow[:], tokid[:].unsqueeze(2).to_broadcast([128, 32, 16]), R=[tokid.r], W=[tokrow.r])
    p.memset("dve", base[:], 0.0, W=[base.r])
    p.memset("pool", fill[:], 5000, W=[fill.r])
    p.dma("sp", S["slot"][:, :].rearrange("(p r) c -> p (r c)", p=128), fill[:], R=[fill.r], W=[S["slot"].r])
    nt3 = (g.debug or {}).get("ntiles", NT)

    def loads3(i):
        j = i % 2
        p.dma("sp", oT_l[j][:], S["oT_s"][i, :, :, :], R=[S["oT_s"].r], W=[oT_l[j].r])
        p.dma("sp", gT_l[j][:], S["g_s"][i, :, :, :], R=[S["g_s"].r], W=[gT_l[j].r])
        p.dma("sp", xt_l[j][:], I["x"][i * 128:(i + 1) * 128, :], R=[I["x"].r], W=[xt_l[j].r])

    loads3(0)
    for i in range(nt3):
        oT, gT, xt = oT_l[i % 2], gT_l[i % 2], xt_l[i % 2]
        if i + 1 < nt3:
            loads3(i + 1)
        for hf in range(2):
            for m4 in range(4):
                m = hf * 4 + m4
                for kc in range(4):
                    p.mm(B[0][:, m4 * 128:(m4 + 1) * 128], wba[:, kc, m * 128:(m + 1) * 128], oT[:, kc, :], m4 == 0 and kc == 0, kc == 3,
                         R=[wba.r, oT.r], W=[B[0].r])
                for kc in range(4):
                    p.mm(B[1][:, m4 * 128:(m4 + 1) * 128], wbb[:, kc, m * 128:(m + 1) * 128], oT[:, 4 + kc, :], m4 == 0 and kc == 0, kc == 3,
                         R=[wbb.r, oT.r], W=[B[1].r])
            p.tt("dve", t1[:], B[0][:, :], gT[:, hf * 4:(hf + 1) * 4, :].rearrange("p m q -> p (m q)"), ALU.mult, R=[B[0].r, gT.r], W=[t1.r])
            p.tt("dve", t2[:], B[1][:, :], gT[:, 8 + hf * 4:8 + (hf + 1) * 4, :].rearrange("p m q -> p (m q)"), ALU.mult, R=[B[1].r, gT.r], W=[t2.r])
            p.tt("dve", mT[:, hf * 4:(hf + 1) * 4, :].rearrange("p m q -> p (m q)"), t1[:], t2[:], ALU.add, R=[t1.r, t2.r], W=[mT.r])
        for hf in range(2):
            bk = B[2 + hf]
            for c in range(8):
                p.mm(bk[:, :], mT[:, c, :], wout[:, c, hf * 512:(hf + 1) * 512], c == 0, c == 7, R=[mT.r, wout.r], W=[bk.r])
            p.stt(r[:, hf * 512:(hf + 1) * 512], xt[:, hf * 512:(hf + 1) * 512], ALPHA, bk[:, :], ALU.mult, ALU.add,
                  R=[xt.r, bk.r], W=[r.r, rs.r], accum=rs[:, hf:hf + 1])
        layer_norm_tile(p, r, rs, bc["ln1_g"], bc["ln1_b"], h1, tmp)
        p.dma("pool", S["h1_s"][i * 128:(i + 1) * 128, :], h1[:], R=[h1.r], W=[S["h1_s"].r])
        for c in range(8):
            bk = B[4 + c // 4]
            p.tr(bk[:, (c % 4) * 128:(c % 4 + 1) * 128], h1[:, c * 128:(c + 1) * 128], Rz["identf"][:], R=[h1.r, Rz["identf"].r], W=[bk.r])
        for hf in range(2):
            p.cp("act", h1T[:, hf * 4:(hf + 1) * 4, :], B[4 + hf][:, :].rearrange("p (c q) -> p c q", q=128), R=[B[4 + hf].r], W=[h1T.r])
        for c in range(8):
            p.mm(B[6][:, 0:36], h1T[:, c, :], wr[:, c, :], c == 0, c == 7, R=[h1T.r, wr.r], W=[B[6].r])
        p.tt("dve", lg[:], B[6][:, 0:36], brb[:], ALU.add, R=[B[6].r, brb.r], W=[lg.r])
        p.op("dve", lambda e: e.reduce_max(rt[:, 0:1], lg[:, 0:4], AX.X), R=[lg.r], W=[rt.r])
        p.ts("dve", og[:], lg[:, 0:4], rt[:, 0:1], None, ALU.is_equal, R=[lg.r, rt.r], W=[og.r])
        p.ts("dve", rt[:, 1:2], rt[:, 0:1], -1.0, None, ALU.mult, R=[rt.r], W=[rt.r])
        p.act(rj[:, 0:4], lg[:, 0:4], AF.Exp, bias=rt[:, 1:2], accum=rt[:, 2:3], R=[lg.r, rt.r], W=[rj.r, rt.r])
        p.op("dve", lambda e: e.reciprocal(rt[:, 3:4], rt[:, 2:3]), R=[rt.r], W=[rt.r])
        p.ts("dve", esel[:], lg[:, 4:12], og[:, 0:1], None, ALU.mult, R=[lg.r, og.r], W=[esel.r])
        for gi in range(1, 4):
            p.stt(esel[:], lg[:, 4 + 8 * gi:12 + 8 * gi], og[:, gi:gi + 1], esel[:], ALU.mult, ALU.add, R=[lg.r, og.r, esel.r], W=[esel.r])
        p.op("dve", lambda e: e.max(m8r[:], esel[:]), R=[esel.r], W=[m8r.r])
        p.tt("dve", rt[:, 4:5], m8r[:, 1:2], m8r[:, 0:1], ALU.subtract, R=[m8r.r], W=[rt.r])
        p.act(rt[:, 5:6], rt[:, 4:5], AF.Exp, R=[rt.r], W=[rt.r])
        p.ts("dve", rt[:, 6:7], rt[:, 5:6], 1.0, None, ALU.add, R=[rt.r], W=[rt.r])
        p.op("dve", lambda e: e.reciprocal(rt[:, 7:8], rt[:, 6:7]), R=[rt.r], W=[rt.r])
        p.tt("dve", rt[:, 8:9], rt[:, 7:8], rt[:, 5:6], ALU.mult, R=[rt.r], W=[rt.r])
        p.tt("dve", g.wts[:, i, 0:1], rt[:, 7:8], rt[:, 3:4], ALU.mult, R=[rt.r], W=[g.wts.r])
        p.tt("dve", g.wts[:, i, 1:2], rt[:, 8:9], rt[:, 3:4], ALU.mult, R=[rt.r], W=[g.wts.r])
        for k in range(2):
            p.ts("dve", oh[:, k, :], esel[:], m8r[:, k:k + 1], None, ALU.is_equal, R=[esel.r, m8r.r], W=[oh.r])
            p.tt("dve", Ak[:, k, :].rearrange("p (a b) -> p a b", b=8), og[:].unsqueeze(2).to_broadcast([128, 4, 8]),
                 oh[:, k, :].unsqueeze(1).to_broadcast([128, 4, 8]), ALU.mult, R=[og.r, oh.r], W=[Ak.r])
        p.tt("dve", Abf[:], Ak[:, 0, :], Ak[:, 1, :], ALU.add, R=[Ak.r], W=[Abf.r])
        p.mm(B[7][:, 0:32], ustr[:], Abf[:], True, False, R=[ustr.r, Abf.r], W=[B[7].r])
        p.mm(B[7][:, 64:96], Rz["ones_bf"][:], Abf[:], False, True, R=[Rz["ones_bf"].r, Abf.r], W=[B[7].r])
        p.tt("dve", posf[:], B[7][:, 0:32], base[:], ALU.add, R=[B[7].r, base.r], W=[posf.r])
        p.tt("dve", base[:], B[7][:, 64:96], base[:], ALU.add, R=[B[7].r, base.r], W=[base.r])
        for k in range(2):
            p.stt(rj[:, 0:32], posf[:], 1.0, Ak[:, k, :], ALU.mult, ALU.mult, R=[posf.r, Ak.r], W=[rj.r, rt.r], accum=rt[:, 10 + k:11 + k])
            p.stt(rj[:, 0:32], iot[:], 1.0, Ak[:, k, :], ALU.mult, ALU.mult, R=[iot.r, Ak.r], W=[rj.r, rt.r], accum=rt[:, 12 + k:13 + k])
            p.ts("dve", rt[:, 14:15], rt[:, 10 + k:11 + k], float(CAP), 1e6, ALU.is_ge, ALU.mult, R=[rt.r], W=[rt.r])
            p.ts("dve", rt[:, 9:10], rt[:, 10 + k:11 + k], float(CAP), None, ALU.is_lt, R=[rt.r], W=[rt.r])
            p.tt("dve", g.wts[:, i, k:k + 1], g.wts[:, i, k:k + 1], rt[:, 9:10], ALU.mult, R=[rt.r, g.wts.r], W=[g.wts.r])
            p.stt(rt[:, 15:16], rt[:, 12 + k:13 + k], float(CAP), rt[:, 10 + k:11 + k], ALU.mult, ALU.add, R=[rt.r], W=[rt.r])
            p.tt("dve", rt[:, 15:16], rt[:, 15:16], rt[:, 14:15], ALU.add, R=[rt.r], W=[rt.r])
            p.cp("dve", g.dest[:, i, k:k + 1], rt[:, 15:16], R=[rt.r], W=[g.dest.r])
            p.dma("pool", None, None, R=[g.dest.r, tokrow.r], W=[S["slot"].r],
                  fn=lambda e, i=i, k=k: e.indirect_dma_start(
                      out=S["slot"][:, :], out_offset=bass.IndirectOffsetOnAxis(ap=g.dest[:, i, k:k + 1], axis=0),
                      in_=tokrow[:, i, :], in_offset=None, bounds_check=p.breg(e, 32 * CAP - 1), oob_is_err=False))
    return st


def phase4(g):
    nc, p, sb, I, S, Rz = g.nc, g.p, g.sb, g.I, g.S, g.Rz
    st = ExitStack()
    B = g.banks
    ident = Rz["identb"]
    nexp = (g.debug or {}).get("nexp", 32)
    sid = sb("sid", (128, 4, 16), I32, st)
    xg = [sb("xg%d" % i, (128, 1024), F32, st) for i in range(4)]
    xgb = [sb("xgb%d" % i, (128, 1024), BF16, st) for i in range(2)]
    xgT = sb("xgT", (128, 8, 512), BF16, st)
    hT = sb("hT", (128, 2, 512), BF16, st)
    sg = [sb("sg%d" % i, (128, 512), F32, st) for i in range(2)]
    yb = [sb("yb%d" % i, (128, 1024), F32, st) for i in range(2)]
    wgf = sb("wgf", (128, 8, 256), F32, st)
    wuf = sb("wuf", (128, 8, 256), F32, st)
    wdf = sb("wdf", (128, 2, 1024), F32, st)
    wg = [sb("wg%d" % i, (128, 8, 256), BF16, st) for i in range(2)]
    wu = [sb("wu%d" % i, (128, 8, 256), BF16, st) for i in range(2)]
    wd = [sb("wd%d" % i, (128, 2, 1024), BF16, st) for i in range(2)]
    for x_ in xg:
        p.memset("pool", x_[:], 0.0, W=[x_.r])
    ceng = ["dve", "act", "dve", "act"]
    xgT2 = [xgT, sb("xgTb", (128, 8, 512), BF16, st)]
    sid2 = [sid, sb("sidb", (128, 4, 16), I32, st)]

    def issue_loads(e_):
        k = e_ % 2
        sd = sid2[k]
        p.dma("sp", wgf[:], I["w_gate"][e_, :, :].rearrange("(p c) f -> p c f", c=8), R=[I["w_gate"].r], W=[wgf.r])
        p.dma("sp", wuf[:], I["w_up"][e_, :, :].rearrange("(p c) f -> p c f", c=8), R=[I["w_up"].r], W=[wuf.r])
        p.dma("sp", wdf[:], I["w_down"][e_, :, :].rearrange("(c p) n -> p c n", p=128), R=[I["w_down"].r], W=[wdf.r])
        p.dma("sp", sd[:], S["slot"][e_ * CAP:(e_ + 1) * CAP, :].rearrange("(s p) c -> p s c", p=128),
              R=[S["slot"].r], W=[sd.r])
        for s_ in range(4):
            p.dma("pool", None, None, R=[sd.r, S["h1_s"].r], W=[xg[s_].r],
                  fn=lambda e, s_=s_, sd=sd: e.indirect_dma_start(
                      out=xg[s_][:, :], out_offset=None, in_=S["h1_s"][:, :],
                      in_offset=bass.IndirectOffsetOnAxis(ap=sd[:, s_, 0:1], axis=0), bounds_check=p.breg(e, T - 1), oob_is_err=False))

    def finish_loads(e_):
        k = e_ % 2
        p.cp("act", wg[k][:], wgf[:], R=[wgf.r], W=[wg[k].r])
        p.cp("dve", wu[k][:], wuf[:], R=[wuf.r], W=[wu[k].r])
        p.cp("act", wd[k][:], wdf[:], R=[wdf.r], W=[wd[k].r])
        for s_ in range(4):
            xb = xgb[s_ % 2]
            p.cp(ceng[s_], xb[:], xg[s_][:], R=[xg[s_].r], W=[xb.r])
            bk = B[6 + s_ % 2]
            tv = bk.t[:].bitcast(BF16)
            for c in range(8):
                p.tr(tv[:, c * 128:(c + 1) * 128], xb.t[:, c:1024:8], ident[:], R=[xb.r, ident.r], W=[bk.r])
            p.cp("act" if s_ % 2 else "dve", xgT2[k][:, :, s_ * 128:(s_ + 1) * 128], tv[:, :].rearrange("p (c q) -> p c q", q=128),
                 R=[bk.r], W=[xgT2[k].r])

    def compute(e_):
        k = e_ % 2
        xT_ = xgT2[k]
        for fc in range(2):
            bG, bU = B[2 * fc], B[2 * fc + 1]
            for c in range(8):
                p.mm(bG[:, :], wg[k][:, c, fc * 128:(fc + 1) * 128], xT_[:, c, :], c == 0, c == 7, R=[wg[k].r, xT_.r], W=[bG.r])
            for c in range(8):
                p.mm(bU[:, :], wu[k][:, c, fc * 128:(fc + 1) * 128], xT_[:, c, :], c == 0, c == 7, R=[wu[k].r, xT_.r], W=[bU.r])
            p.act(sg[fc][:], bG[:, :], AF.Silu, R=[bG.r], W=[sg[fc].r])
            p.tt("dve", hT[:, fc, :], sg[fc][:], bU[:, :], ALU.mult, R=[sg[fc].r, bU.r], W=[hT.r])
        for s_ in range(4):
            y_ = yb[s_ % 2]
            for hf in range(2):
                bY = B[4 + (2 * s_ + hf) % 2]
                for fc in range(2):
                    p.mm(bY[:, :], hT[:, fc, s_ * 128:(s_ + 1) * 128], wd[k][:, fc, hf * 512:(hf + 1) * 512], fc == 0, fc == 1,
                         R=[hT.r, wd[k].r], W=[bY.r])
                if hf == 0:
                    p.act(y_[:, 0:512], bY[:, :], AF.Copy, R=[bY.r], W=[y_.r])
                else:
                    p.cp("dve", y_[:, 512:1024], bY[:, :], R=[bY.r], W=[y_.r])
            p.dma("sp", S["y_s"][e_ * CAP + s_ * 128:e_ * CAP + (s_ + 1) * 128, :], y_[:], R=[y_.r], W=[S["y_s"].r])

    issue_loads(0)
    finish_loads(0)
    for e_ in range(nexp):
        if e_ + 1 < nexp:
            issue_loads(e_ + 1)
        compute(e_)
        if e_ + 1 < nexp:
            finish_loads(e_ + 1)
    return st


def phase5(g):
    nc, p, sb, I, S, Rz = g.nc, g.p, g.sb, g.I, g.S, g.Rz
    st = ExitStack()
    bc = {}
    for nm in ("ln2_g", "ln2_b"):
        bc[nm] = sb("bc5_" + nm, (128, 1024), F32, st)
        p.dma("sp", bc[nm][:], I[nm][0:1, :].partition_broadcast(128), R=[I[nm].r], W=[bc[nm].r])
    y = [[sb("y%d_%d" % (k, j), (128, 1024), F32, st) for k in range(2)] for j in range(2)]
    h1 = [sb("h1_5%d" % j, (128, 1024), F32, st) for j in range(2)]
    r = sb("r5", (128, 1024), F32, st)
    tmp = sb("tmp5", (128, 1024), F32, st)
    ot = [sb("ot5%d" % j, (128, 1024), F32, st) for j in range(2)]
    rs = sb("rs5", (128, 8), F32, st)
    for j in range(2):
        for k in range(2):
            p.memset("dve", y[j][k][:], 0.0, W=[y[j][k].r])
    nt5 = (g.debug or {}).get("ntiles", NT)

    def loads5(i):
        j = i % 2
        p.dma("sp", h1[j][:], S["h1_s"][i * 128:(i + 1) * 128, :], R=[S["h1_s"].r], W=[h1[j].r])
        for k in range(2):
            p.dma("pool", None, None, R=[g.dest.r, S["y_s"].r], W=[y[j][k].r],
                  fn=lambda e, i=i, k=k, j=j: e.indirect_dma_start(
                      out=y[j][k][:, :], out_offset=None, in_=S["y_s"][:, :],
                      in_offset=bass.IndirectOffsetOnAxis(ap=g.dest[:, i, k:k + 1], axis=0), bounds_check=p.breg(e, 32 * CAP - 1), oob_is_err=False))

    loads5(0)
    for i in range(nt5):
        j = i % 2
        if i + 1 < nt5:
            loads5(i + 1)
        p.ts("dve", r[:], h1[j][:], ALPHA, None, ALU.mult, R=[h1[j].r], W=[r.r])
        p.stt(r[:], y[j][0][:], g.wts[:, i, 0:1], r[:], ALU.mult, ALU.add, R=[y[j][0].r, g.wts.r, r.r], W=[r.r])
        p.stt(r[:], y[j][1][:], g.wts[:, i, 1:2], r[:], ALU.mult, ALU.add, R=[y[j][1].r, g.wts.r, r.r], W=[r.r])
        p.op("dve", lambda e: e.reduce_sum(rs[:, 0:1], r[:], AX.X), R=[r.r], W=[rs.r])
        p.memset("dve", rs[:, 1:2], 0.0, W=[rs.r])
        layer_norm_tile(p, r, rs, bc["ln2_g"], bc["ln2_b"], ot[j], tmp)
        p.dma("sp", g.out[i * 128:(i + 1) * 128, :], ot[j][:], R=[ot[j].r], W=[g.out.r])
    return st


def host_inputs(inputs):
    f = lambda a: np.ascontiguousarray(np.asarray(a, dtype=np.float32))
    rel_bias = f(inputs["rel_bias"])
    shared = {
        "w_in": f(inputs["w_in"][0]),
        "pe_kT": f(inputs["cmp_pe_k"][0].T), "pe_vT": f(inputs["cmp_pe_v"][0].T),
        "cw1_k": f(inputs["cmp_w1_k"][0]), "cw2_k": f(inputs["cmp_w2_k"][0]),
        "cw1_v": f(inputs["cmp_w1_v"][0]), "cw2_v": f(inputs["cmp_w2_v"][0]),
        "ckv_g": f(inputs["ckv_norm_g"][0]).reshape(1, 128),
        "w_uk": f(inputs["w_uk"][0]), "w_uv": f(inputs["w_uv"][0]),
        "relflat": rel_bias.reshape(1, 512),
        "w_ba": f(inputs["w_branch_a"][0]), "w_bb": f(inputs["w_branch_b"][0]), "w_out": f(inputs["w_out"][0]),
        "ln1_g": f(inputs["ln1_g"][0]).reshape(1, D), "ln1_b": f(inputs["ln1_b"][0]).reshape(1, D),
        "wr": f(np.concatenate([np.asarray(inputs["w_grp"][0]), np.asarray(inputs["w_rtr"][0])], axis=1)),
        "br": f(np.concatenate([np.asarray(inputs["b_grp"][0]), np.asarray(inputs["b_rtr"][0])], axis=0)).reshape(1, 36),
        "w_gate": f(inputs["w_gate"][0]), "w_up": f(inputs["w_up"][0]), "w_down": f(inputs["w_down"][0]),
        "ln2_g": f(inputs["ln2_g"][0]).reshape(1, D), "ln2_b": f(inputs["ln2_b"][0]).reshape(1, D),
    }
    shared.update(_host_consts(rel_bias))
    x = np.asarray(inputs["x"], dtype=np.float32)
    maps = []
    for b in range(x.shape[0]):
        m = dict(shared)
        m["x"] = np.ascontiguousarray(x[b])
        m["xT"] = np.ascontiguousarray(x[b].T)
        maps.append(m)
    return maps


def kernel(**inputs):
    maps = host_inputs(inputs)
    nc, g = build()
    res = run_bass_kernel_spmd(nc, maps, core_ids=list(range(8)))
    return np.stack([np.asarray(r["out"], dtype=np.float32) for r in res.results], axis=0)
```

```python
import math
from contextlib import ExitStack
import numpy as np
import ml_dtypes
import concourse.bass as bass
import concourse.mybir as mybir
from concourse.bass_utils import run_bass_kernel_spmd

F32 = mybir.dt.float32
BF16 = mybir.dt.bfloat16
I32 = mybir.dt.int32
AF = mybir.ActivationFunctionType
ALU = mybir.AluOpType
AX = mybir.AxisListType

T = 4096
D = 1024
NT = T // 128
D_IN = 4316
ALPHA = 2.0 ** 0.25
CAP = 512
NEGM = 32768.0
BIS_ITERS = 20
DEBUG = None

O_QA, O_KC, O_VC, O_KS, O_VS, O_KW, O_VW, O_GN, O_QB, O_CKV, O_QI, O_KI, O_WI, O_GA, O_GB = (
    0, 512, 640, 768, 896, 1024, 1152, 1280, 1304, 1816, 1944, 2200, 2264, 2268, 3292)


class Res:
    __slots__ = ("name", "w", "r", "dsem", "dcount", "excl")

    def __init__(self, name):
        self.name = name
        self.excl = False
        self.w = None
        self.r = {}
        self.dsem = None
        self.dcount = 0


class Prog:
    ENG = ("pe", "act", "dve", "pool", "sp")

    def __init__(self, nc, stack):
        self.nc = nc
        self.stack = stack
        self.eobj = {"pe": nc.tensor, "act": nc.scalar, "dve": nc.vector, "pool": nc.gpsimd, "sp": nc.sync}
        self.sem = {e: stack.enter_context(nc.semaphore("c_" + e)) for e in self.ENG}
        self.cnt = {e: 0 for e in self.ENG}
        self.seen = {e: {} for e in self.ENG}
        self.ops = {e: [] for e in self.ENG}
        self.dres = []
        self.nres = 0

    def res(self, name=None):
        self.nres += 1
        return Res(name or "r%d" % self.nres)

    def _waits(self, eng, reads, writes):
        need = {}

        def add(ev, war=False):
            if ev is None:
                return
            sem, val, src = ev
            if src == eng and eng == "pe":
                return
            k = id(sem)
            if self.seen[eng].get(k, (None, 0))[1] >= val:
                return
            if k not in need or need[k][1] < val:
                need[k] = (sem, val)

        for r in reads:
            add(r.w)
        for w in writes:
            add(w.w)
            for ev in w.r.values():
                add(ev, True)
        out = []
        for k, (sem, val) in need.items():
            self.seen[eng][k] = (sem, val)
            out.append((sem, val))
        return out

    def op(self, eng, fn, R=(), W=()):
        W = list(W) + [r for r in R if r.excl]
        R = [r for r in R if not r.excl]
        waits = self._waits(eng, R, W)
        self.cnt[eng] += 1
        ev = (self.sem[eng], self.cnt[eng], eng)
        self.ops[eng].append((fn, waits, (self.sem[eng], 1)))
        for r in R:
            r.r[eng] = ev
        for w in W:
            w.w = ev
            w.r = {}

    def dma(self, q, out, in_, R=(), W=(), fn=None):
        waits = self._waits(q, R, W)
        tgt = W[0]
        if tgt.dsem is None:
            tgt.dsem = self.stack.enter_context(self.nc.semaphore("d_" + tgt.name))
            self.dres.append(tgt)
        tgt.dcount += 16
        ev = (tgt.dsem, tgt.dcount, None)
        if fn is None:
            fn = lambda e, o=out, i=in_: e.dma_start(out=o, in_=i)
        self.ops[q].append((fn, waits, (tgt.dsem, 16)))
        for r in R:
            r.r["dma%d" % id(tgt)] = ev
        for w in W:
            w.w = ev
            w.r = {}

    def flush(self, final=False):
        tail = {}
        for e in self.ENG:
            ws = []
            for e2 in self.ENG:
                if e2 != e and self.cnt[e2] > 0 and self.seen[e].get(id(self.sem[e2]), (None, 0))[1] < self.cnt[e2]:
                    ws.append((self.sem[e2], self.cnt[e2]))
                    self.seen[e][id(self.sem[e2])] = (self.sem[e2], self.cnt[e2])
            for r in self.dres:
                if self.seen[e].get(id(r.dsem), (None, 0))[1] < r.dcount:
                    ws.append((r.dsem, r.dcount))
                    self.seen[e][id(r.dsem)] = (r.dsem, r.dcount)
            tail[e] = ws
        ops = self.ops
        eobj = self.eobj
        self.regs = {}
        with self.nc.Block() as block:
            def run(e, engine):
                for fn, waits, inc in ops[e]:
                    for s, v in waits:
                        engine.wait_ge(s, v)
                    ins = fn(engine)
                    ins.then_inc(inc[0], inc[1])
                for s, v in tail[e]:
                    engine.wait_ge(s, v)

            @block.tensor
            def _(t):
                run("pe", t)

            @block.scalar
            def _(s):
                run("act", s)

            @block.vector
            def _(v):
                run("dve", v)

            @block.gpsimd
            def _(g):
                run("pool", g)

            @block.sync
            def _(sy):
                run("sp", sy)
        self.ops = {e: [] for e in self.ENG}

    def breg(self, e, val):
        if val not in self.regs:
            self.regs[val] = e.to_reg(val)
        return self.regs[val]

    def mm(self, out, lhsT, rhs, start, stop, R=(), W=()):
        self.op("pe", lambda e: e.matmul(out, lhsT, rhs, start=start, stop=stop, skip_group_check=True), R, W)

    def tr(self, out, in_, ident, R=(), W=()):
        self.op("pe", lambda e: e.transpose(out, in_, ident), R, W)

    def act(self, out, in_, func, R=(), W=(), bias=None, scale=None, accum=None, eng="act"):
        kw = {}
        if bias is not None:
            kw["bias"] = bias
        if scale is not None:
            kw["scale"] = scale
        if accum is not None:
            kw["accum_out"] = accum
        self.op("act", lambda e: e.activation(out, in_, func, **kw), R, W)

    def ts(self, eng, out, in0, s1, s2, op0, op1=None, R=(), W=(), accum=None):
        kw = {}
        if op1 is not None:
            kw["op1"] = op1
        if accum is not None:
            kw["accum_out"] = accum
        self.op(eng, lambda e: e.tensor_scalar(out, in0, s1, s2, op0, **kw), R, W)

    def tt(self, eng, out, in0, in1, op, R=(), W=()):
        self.op(eng, lambda e: e.tensor_tensor(out, in0, in1, op), R, W)

    def stt(self, out, in0, scalar, in1, op0, op1, R=(), W=(), accum=None):
        kw = {}
        if accum is not None:
            kw["accum_out"] = accum
        self.op("dve", lambda e: e.scalar_tensor_tensor(out, in0, scalar, in1, op0, op1, **kw), R, W)

    def cp(self, eng, out, in_, R=(), W=()):
        if eng == "act":
            self.op("act", lambda e: e.copy(out, in_), R, W)
        else:
            self.op(eng, lambda e: e.tensor_copy(out, in_), R, W)

    def memset(self, eng, ap, val, W=()):
        self.op(eng, lambda e: e.memset(ap, val), (), W)


class Buf:
    def __init__(self, t, res):
        self.t = t
        self.r = res

    def __getitem__(self, k):
        return self.t[k]


def _rel_bucket_np(dist):
    n = np.maximum(dist, 0)
    nf = np.maximum(n, 1).astype(np.float32)
    large = 16 + (np.log(nf / 16) / math.log(1024 / 16) * 16).astype(np.int32)
    return np.where(n < 16, n, np.minimum(large, 31))


def _host_consts(rel_bias):
    c = {}
    dd = np.arange(0, 1152)
    bk = _rel_bucket_np(dd)
    relvec = rel_bias[bk]
    p = np.arange(128)[:, None]
    j = np.arange(128)[None, :]
    relT = np.zeros((8, 128, 16, 128), np.float32)
    for di in range(8):
        dist = np.clip(128 * di + j - p, 0, 1151)
        relT[di] = relvec[dist].transpose(0, 2, 1)
    c["relTa"] = np.ascontiguousarray(relT[:, :, :8, :].transpose(1, 0, 2, 3)).reshape(128, 8, 2, 512)
    c["relTb"] = np.ascontiguousarray(relT[:, :, 8:, :].transpose(1, 0, 2, 3)).reshape(128, 8, 1024)
    c31 = relvec[1151]
    c["c31a"] = np.ascontiguousarray(np.repeat(c31[:8].reshape(2, 4, 1), 128, axis=2).reshape(2, 512))
    c["c31b"] = np.ascontiguousarray(np.repeat(c31[8:].reshape(8, 1), 128, axis=1).reshape(1, 1024))
    v = np.arange(503)[None, :]
    dist = p - 16 * v + 3937
    c["cmpv"] = np.ascontiguousarray(relvec[np.clip(dist, 0, 1151)][:, :, :8].transpose(0, 2, 1))
    c["cmpm"] = np.where(dist >= 0, 0.0, -30000.0).astype(np.float32)
    mc = np.where(j < p, -NEGM, 0.0).astype(np.float32)
    mw = np.where(j >= p, -NEGM, 0.0).astype(np.float32)
    c["mc4"] = np.tile(mc, (1, 4))
    c["mw4"] = np.tile(mw, (1, 4))
    c["ident"] = np.eye(128, dtype=np.float32)
    c["d30"] = np.tile(np.eye(128, dtype=np.float32) * NEGM, (1, 4))
    e_all = np.zeros((64, 32, 128), np.float32)
    for kt in range(32):
        e_all[2 * kt, kt, :64] = NEGM
        e_all[2 * kt + 1, kt, 64:] = NEGM
    c["eall"] = e_all.reshape(64, 32 * 128)
    c["mcq"] = np.where(j > p, -1e30, 0.0).astype(np.float32)
    cp_ = (np.arange(128) >= 64).astype(np.int64)[:, None]
    u = np.arange(128)[None, :] - 63
    c["wext"] = np.where((u == cp_) | (u == cp_ - 1), 1e4, np.where(u > cp_, -1e30, 0.0)).astype(np.float32)
    cs = 16 * np.arange(255)[:, None]
    ss = 64 * np.arange(64)[None, :]
    ov = np.minimum(cs + 32, ss + 64) - np.maximum(cs, ss)
    cm = np.zeros((256, 64), np.float32)
    cm[:255] = np.clip(ov, 0, None).astype(np.float32) / 16
    c["cmap"] = cm
    c["ustrict"] = (np.arange(128)[:, None] < np.arange(128)[None, :]).astype(np.float32)
    c["iota32"] = np.tile(np.arange(32, dtype=np.float32)[None, :], (128, 1))
    c["tokid"] = (np.arange(128)[:, None] + 128 * np.arange(32)[None, :]).astype(np.int32)
    return c


class Ctx:
    pass


def build(debug=None):
    nc = bass.Bass("TRN2", target_bir_lowering=False)
    es = ExitStack()
    p = Prog(nc, es)
    g = Ctx()
    g.nc, g.p, g.es, g.debug = nc, p, es, debug
    g.dbg_out = {}

    def din(name, shape, dt=F32):
        return Buf(nc.dram_tensor(name, list(shape), dt, kind="ExternalInput").ap(), p.res(name))

    def dscr(name, shape, dt=F32):
        kind = "ExternalOutput" if (debug and name in debug) else "Internal"
        return Buf(nc.dram_tensor(name, list(shape), dt, kind=kind).ap(), p.res(name))

    g.din, g.dscr = din, dscr

    def sb(name, shape, dt=F32, stack=None):
        t = (stack or es).enter_context(nc.sbuf_tensor("s_" + name, list(shape), dt))
        return Buf(t, p.res(name))

    def ps(name, shape, dt=F32, stack=None):
        t = (stack or es).enter_context(nc.psum_tensor(name, list(shape), dt))
        r = p.res(name)
        r.excl = True
        return Buf(t, r)

    g.sb, g.ps = sb, ps

    I = g.I = {}
    for name, shape in [
        ("xT", (D, T)), ("x", (T, D)), ("w_in", (D, D_IN)),
        ("pe_kT", (64, 32)), ("pe_vT", (64, 32)),
        ("cw1_k", (2048, 256)), ("cw2_k", (256, 64)), ("cw1_v", (2048, 256)), ("cw2_v", (256, 64)),
        ("ckv_g", (1, 128)), ("w_uk", (8, 64, 128)), ("w_uv", (8, 128, 64)), ("relflat", (1, 512)),
        ("w_ba", (512, D)), ("w_bb", (512, D)), ("w_out", (D, D)),
        ("ln1_g", (1, D)), ("ln1_b", (1, D)), ("wr", (D, 36)), ("br", (1, 36)),
        ("w_gate", (32, D, 256)), ("w_up", (32, D, 256)), ("w_down", (32, 256, D)),
        ("ln2_g", (1, D)), ("ln2_b", (1, D)),
        ("relTa", (128, 8, 2, 512)), ("relTb", (128, 8, 1024)), ("c31a", (2, 512)), ("c31b", (1, 1024)),
        ("cmpv", (128, 8, 503)), ("cmpm", (128, 503)), ("mc4", (128, 512)), ("mw4", (128, 512)),
        ("ident", (128, 128)), ("d30", (128, 512)), ("eall", (64, 4096)), ("mcq", (128, 128)),
        ("wext", (128, 128)), ("cmap", (256, 64)), ("ustrict", (128, 128)), ("iota32", (128, 32)),
    ]:
        I[name] = din(name, shape)
    I["tokid"] = din("tokid", (128, 32), I32)
    g.out = Buf(nc.dram_tensor("out", [T, D], F32, kind="ExternalOutput").ap(), p.res("out"))

    S = g.S = {}
    S["qa_s"] = dscr("qa_s", (NT, 2, 64, 4, 128), BF16)
    S["ql_s"] = dscr("ql_s", (NT, 128, 8, 128), BF16)
    S["qi_s"] = dscr("qi_s", (NT, 64, 4, 128), BF16)
    S["g_s"] = dscr("g_s", (NT, 128, 16, 128), F32)
    S["h1_s"] = dscr("h1_s", (T, D), F32)
    S["slot"] = dscr("slot", (32 * CAP, 16), I32)
    S["y_s"] = dscr("y_s", (32 * CAP, D), F32)

    Rz = g.Rz = {}
    Rz["ksT"] = sb("ksT", (128, T), BF16)
    Rz["kwT"] = sb("kwT", (128, T), BF16)
    Rz["ckvT"] = sb("ckvT", (128, T), BF16)
    Rz["kiT"] = sb("kiT", (64, T), BF16)
    Rz["vsA"] = sb("vsA", (128, NT, 2, 65), BF16)
    Rz["vwA"] = sb("vwA", (128, NT, 2, 65), BF16)
    Rz["ckvA"] = sb("ckvA", (128, NT, 129), BF16)
    Rz["gn"] = sb("gn", (128, NT, 24), F32)
    Rz["wabs"] = sb("wabs", (128, NT, 4), F32)
    Rz["wsgn"] = sb("wsgn", (128, NT, 4), F32)
    Rz["kcmpT"] = sb("kcmpT", (128, 256), BF16)
    Rz["vcmpM"] = sb("vcmpM", (128, 2, 2, 128), BF16)
    Rz["kmax"] = sb("kmax", (1, 4), F32)
    Rz["identb"] = sb("identb", (128, 128), BF16)
    Rz["identf"] = sb("identf", (128, 128), F32)
    Rz["ones_bf"] = sb("ones_bf", (128, 128), BF16)

    g.kcT = sb("kcT", (128, T), BF16)
    g.vcT = sb("vcT", (128, T), BF16)
    g.dest = sb("dest_i", (128, NT, 2), I32)
    g.wts = sb("wts", (128, NT, 2), F32)
    g.banks = [ps("bank%d" % i, (128, 512), F32) for i in range(8)]

    stage = (debug or {}).get("stage", 99)
    st = phase1(g)
    if debug:
        dump_resident(g)
    p.flush()
    st.close()
    if stage >= 2:
        st = phase2(g)
        p.flush()
        st.close()
    if stage >= 3:
        st = phase3(g)
        p.flush()
        st.close()
    if stage >= 4:
        st = phase4(g)
        p.flush()
        st.close()
    if stage >= 5:
        st = phase5(g)
        p.flush()
        st.close()
    return nc, g


def dump_resident(g):
    nc, p = g.nc, g.p
    for name, buf in g.Rz.items():
        shape = list(buf.t.shape)
        d = nc.dram_tensor("dbg_" + name, shape, buf.t.dtype, kind="ExternalOutput").ap()
        r = p.res("dbg_" + name)
        idx = tuple(slice(None) for _ in shape)
        p.dma("sp", d[idx], buf.t[idx], R=[buf.r], W=[r])


def phase1(g):
    nc, p, sb, I, S, Rz = g.nc, g.p, g.sb, g.I, g.S, g.Rz
    st = ExitStack()
    banks = g.banks
    bi = [0]

    def nbank():
        b = banks[bi[0] % 8]
        bi[0] += 1
        return b

    ev_rr = [0]

    def ev_eng():
        ev_rr[0] += 1
        return "act" if ev_rr[0] % 2 else "dve"

    stf = sb("p1_cst", (128, 128), F32, st)
    p.dma("sp", stf[:], I["ident"][:, :], R=[I["ident"].r], W=[stf.r])
    p.cp("dve", Rz["identb"][:], stf[:], R=[stf.r], W=[Rz["identb"].r])
    p.cp("dve", Rz["identf"][:], stf[:], R=[stf.r], W=[Rz["identf"].r])
    p.memset("dve", Rz["ones_bf"][:], 1.0, W=[Rz["ones_bf"].r])
    p.memset("pool", Rz["vsA"][:, :, :, 64:65], 1.0, W=[Rz["vsA"].r])
    p.memset("pool", Rz["vwA"][:, :, :, 64:65], 1.0, W=[Rz["vwA"].r])
    p.memset("pool", Rz["ckvA"][:, :, 128:129], 1.0, W=[Rz["ckvA"].r])

    xTb = sb("xTb", (128, 8, T), BF16, st)
    xst = [sb("xst%d" % i, (128, 1024), F32, st) for i in range(2)]
    xq = [p.res("xTq%d" % q) for q in range(4)]
    engs = ["dve", "act", "dve", "act"]
    xk = [0]

    def load_x(q):
        for c in range(8):
            s_ = xst[xk[0] % 2]
            xk[0] += 1
            p.dma("sp", s_[:], I["xT"][c * 128:(c + 1) * 128, q * 1024:(q + 1) * 1024], R=[I["xT"].r], W=[s_.r])
            p.cp(engs[c % 4], xTb[:, c, q * 1024:(q + 1) * 1024], s_[:], R=[s_.r], W=[xq[q]])

    cut = 99
    wst = [sb("wst%d" % i, (128, 8, 128), F32, st) for i in range(2)]
    wbf = [sb("wbf%d" % i, (128, 8, 128), BF16, st) for i in range(2)]
    wk = [0]

    def load_w_issue(col0, M):
        k = wk[0] % 2
        wk[0] += 1
        p.dma("sp", wst[k][:, :, 0:M], I["w_in"][:, col0:col0 + M].rearrange("(c p) m -> p c m", p=128),
              R=[I["w_in"].r], W=[wst[k].r])
        return k

    def load_w_cast(k, M):
        p.cp("act" if k else "dve", wbf[k][:, :, 0:M], wst[k][:, :, 0:M], R=[wst[k].r], W=[wbf[k].r])
        return wbf[k]

    groups = []

    def fm_group(col0, M, evac):
        groups.append((col0, M, evac))

    def run_groups():
        nxt = load_w_cast(load_w_issue(groups[0][0], groups[0][1]), groups[0][1])
        load_x(0)
        for gi, (col0, M, evac) in enumerate(groups):
            w = nxt
            kn = None
            if gi + 1 < len(groups):
                kn = load_w_issue(groups[gi + 1][0], groups[gi + 1][1])
            for tb in range(8):
                if gi == 0 and tb % 2 == 0 and tb < 6:
                    load_x(tb // 2 + 1)
                b = nbank()
                for c in range(8):
                    p.mm(b[0:M, :], w[:, c, 0:M], xTb[:, c, tb * 512:(tb + 1) * 512], c == 0, c == 7,
                         R=[w.r, xq[tb // 2]], W=[b.r])
                evac(tb, b)
            if kn is not None:
                nxt = load_w_cast(kn, groups[gi + 1][1])

    stg_bf = [sb("stgb%d" % i, (128, 512), BF16, st) for i in range(4)]
    stg_f = [sb("stgf%d" % i, (128, 512), F32, st) for i in range(2)] * 2
    sk = [0]

    def nstg(lst):
        sk[0] += 1
        return lst[sk[0] % 4]

    def evac_copy(dst_buf, scale=None):
        def f(tb, b):
            M = dst_buf.t.shape[0]
            e = ev_eng()
            o = dst_buf[:, tb * 512:(tb + 1) * 512]
            if e == "act":
                p.act(o, b[0:M, :], AF.Copy, R=[b.r], W=[dst_buf.r])
            else:
                p.cp("dve", o, b[0:M, :], R=[b.r], W=[dst_buf.r])
        return f

    for m in range(4):
        def ev(tb, b, m=m):
            s_ = nstg(stg_bf)
            p.act(s_[:], b[:], AF.Copy, scale=0.125, R=[b.r], W=[s_.r])
            gq, hh0 = m // 2, 2 * (m % 2)
            for hl in range(2):
                dst = S["qa_s"][tb * 4:(tb + 1) * 4, gq, :, hh0 + hl, :].rearrange("t d q -> d t q")
                src = s_[hl * 64:(hl + 1) * 64, :].rearrange("d (t q) -> d t q", q=128)
                p.dma("sp", dst, src, R=[s_.r], W=[S["qa_s"].r])
        fm_group(O_QA + 128 * m, 128, ev)

    kcT, vcT = g.kcT, g.vcT
    fm_group(O_KC, 128, evac_copy(kcT))
    fm_group(O_VC, 128, evac_copy(vcT))
    fm_group(O_KS, 128, evac_copy(Rz["ksT"]))
    fm_group(O_KW, 128, evac_copy(Rz["kwT"]))
    fm_group(O_KI, 64, evac_copy(Rz["kiT"]))

    wukf = sb("wukf", (128, 4, 128), F32, st)
    wukb = sb("wukb", (128, 4, 128), BF16, st)
    p.dma("sp", wukf[:], I["w_uk"][:, :, :].rearrange("(m hl) d r -> (hl d) m r", hl=2), R=[I["w_uk"].r], W=[wukf.r])
    p.cp("dve", wukb[:], wukf[:], R=[wukf.r], W=[wukb.r])
    for m in range(4):
        def ev(tb, b, m=m):
            s_ = nstg(stg_bf)
            p.cp("dve", s_[:], b[:], R=[b.r], W=[s_.r])
            for hl in range(2):
                b2 = nbank()
                p.mm(b2[:, :], wukb[hl * 64:(hl + 1) * 64, m, :], s_[hl * 64:(hl + 1) * 64, :], True, True,
                     R=[wukb.r, s_.r], W=[b2.r])
                s2 = nstg(stg_bf)
                p.act(s2[:], b2[:], AF.Copy, scale=0.125, R=[b2.r], W=[s2.r])
                dst = S["ql_s"][tb * 4:(tb + 1) * 4, :, 2 * m + hl, :].rearrange("t r q -> r t q")
                p.dma("sp", dst, s2[:].rearrange("r (t q) -> r t q", q=128), R=[s2.r], W=[S["ql_s"].r])
        fm_group(O_QB + 128 * m, 128, ev)

    for m in range(2):
        def ev(tb, b, m=m):
            s_ = nstg(stg_bf)
            p.act(s_[:], b[:], AF.Copy, scale=0.125, R=[b.r], W=[s_.r])
            for hl in range(2):
                dst = S["qi_s"][tb * 4:(tb + 1) * 4, :, 2 * m + hl, :].rearrange("t d q -> d t q")
                src = s_[hl * 64:(hl + 1) * 64, :].rearrange("d (t q) -> d t q", q=128)
                p.dma("sp", dst, src, R=[s_.r], W=[S["qi_s"].r])
        fm_group(O_QI + 128 * m, 128, ev)

    for m in range(16):
        def ev(tb, b, m=m):
            s_ = nstg(stg_f)
            p.act(s_[:], b[:], AF.Sigmoid, R=[b.r], W=[s_.r])
            dst = S["g_s"][tb * 4:(tb + 1) * 4, :, m, :].rearrange("t c q -> c t q")
            p.dma("sp", dst, s_[:].rearrange("c (t q) -> c t q", q=128), R=[s_.r], W=[S["g_s"].r])
        fm_group(O_GA + 128 * m, 128, ev)

    run_groups()
    wtf = sb("wtf", (128, 8, 412), F32, st)
    wtb = sb("wtb", (128, 8, 412), BF16, st)
    for (c0, n, o) in [(O_VS, 128, 0), (O_VW, 128, 128), (O_CKV, 128, 256), (O_GN, 24, 384), (O_WI, 4, 408)]:
        r_ = p.res("wtf%d" % o)
        p.dma("sp", wtf[:, :, o:o + n], I["w_in"][:, c0:c0 + n].rearrange("(c p) m -> p c m", p=128),
              R=[I["w_in"].r], W=[r_])
        p.cp("dve", wtb[:, :, o:o + n], wtf[:, :, o:o + n], R=[r_], W=[wtb.r])
    gbc = sb("gbc", (128, 128), F32, st)
    p.dma("sp", gbc[:], I["ckv_g"][0:1, :].partition_broadcast(128), R=[I["ckv_g"].r], W=[gbc.r])
    junk = sb("p1junk", (128, 128), F32, st)
    ssq = sb("p1ssq", (128, 4), F32, st)
    sub = (g.debug or {}).get("sub", 99)
    for tt in range(NT if sub >= 1 else 0):
        b = nbank()
        for c in range(8):
            p.mm(b[:, 0:412], xTb[:, c, tt * 128:(tt + 1) * 128], wtb[:, c, :], c == 0, c == 7,
                 R=[wtb.r, xq[tt // 8]], W=[b.r])
        p.cp("dve", Rz["vsA"][:, tt, :, 0:64], b[:, 0:128].rearrange("p (g d) -> p g d", g=2), R=[b.r], W=[Rz["vsA"].r])
        p.cp("dve", Rz["vwA"][:, tt, :, 0:64], b[:, 128:256].rearrange("p (g d) -> p g d", g=2), R=[b.r], W=[Rz["vwA"].r])
        if sub <= 1:
            continue
        p.act(junk[:], b[:, 256:384], AF.Square, R=[b.r], W=[junk.r, ssq.r], accum=ssq[:, 0:1])
        sub2 = (g.debug or {}).get("sub2", 99)
        if sub2 <= 0:
            continue
        p.ts("dve", ssq[:, 1:2], ssq[:, 0:1], 1.0 / 128, 1e-6, ALU.mult, ALU.add, R=[ssq.r], W=[ssq.r])
        if sub2 <= 1:
            continue
        p.act(ssq[:, 2:3], ssq[:, 1:2], AF.Sqrt, R=[ssq.r], W=[ssq.r])
        if sub2 <= 2:
            continue
        p.op("dve", lambda e: e.reciprocal(ssq[:, 3:4], ssq[:, 2:3]), R=[ssq.r], W=[ssq.r])
        if sub2 <= 3:
            continue
        p.stt(Rz["ckvA"][:, tt, 0:128], b[:, 256:384], ssq[:, 3:4], gbc[:], ALU.mult, ALU.mult,
              R=[b.r, ssq.r, gbc.r], W=[Rz["ckvA"].r])
        if sub <= 2:
            continue
        p.act(Rz["gn"][:, tt, :], b[:, 384:408], AF.Sigmoid, R=[b.r], W=[Rz["gn"].r])
        p.act(Rz["wabs"][:, tt, :], b[:, 408:412], AF.Abs, scale=0.5, R=[b.r], W=[Rz["wabs"].r])
        p.act(Rz["wsgn"][:, tt, :], b[:, 408:412], AF.Sign, R=[b.r], W=[Rz["wsgn"].r])
        if sub <= 3:
            continue
        b2 = nbank()
        tv = b2.t[:].bitcast(BF16)
        p.tr(tv[:, 0:128], Rz["ckvA"][:, tt, 0:128], Rz["identb"][:], R=[Rz["ckvA"].r, Rz["identb"].r], W=[b2.r])
        p.cp("act", Rz["ckvT"][:, tt * 128:(tt + 1) * 128], tv[:, 0:128], R=[b2.r], W=[Rz["ckvT"].r])

    if cut <= 6:
        return st
    p.flush()
    st.close()
    st = ExitStack()
    phase1b(g, st, kcT, vcT, nbank)
    return st


def phase1b(g, st, kcT, vcT, nbank):
    nc, p, sb, I, S, Rz = g.nc, g.p, g.sb, g.I, g.S, g.Rz
    w1s = sb("w1s", (128, 8, 256), F32, st)
    w2s = sb("w2s", (128, 2, 64), F32, st)
    pes = sb("pes", (128, 32), F32, st)
    peb = sb("peb", (128, 32), BF16, st)
    cst = sb("cst", (128, 2), F32, st)
    u = sb("cu", (128, 256), F32, st)
    t1 = sb("ct1", (128, 256), F32, st)
    t2 = sb("ct2", (128, 256), F32, st)
    cms = sb("cms", (128, 2, 64), F32, st)
    p.dma("sp", cms[:], I["cmap"][:, :].rearrange("(c p) n -> p c n", p=128), R=[I["cmap"].r], W=[cms.r])
    for gq in range(2):
        p.cp("dve", Rz["vcmpM"][:, :, gq, 64:128], cms[:], R=[cms.r], W=[Rz["vcmpM"].r])
    for kv, (srcT, w1n, w2n, pen) in enumerate([(kcT, "cw1_k", "cw2_k", "pe_kT"), (vcT, "cw1_v", "cw2_v", "pe_vT")]):
        w1b = sb("w1b%d" % kv, (128, 32, 256), BF16, st)
        w2p = sb("w2p%d" % kv, (128, 2, 2, 128), BF16, st)
        w2b = sb("w2b%d" % kv, (128, 2, 64), BF16, st)
        gel = sb("gel%d" % kv, (128, 2, 2, 256), BF16, st)
        p.memset("pool", gel[:], 0.0, W=[gel.r])
        p.memset("pool", w2p[:], 0.0, W=[w2p.r])
        for lq in range(4):
            for half in range(2):
                p.dma("sp", w1s[half * 64:(half + 1) * 64, :, :],
                      I[w1n][lq * 512:(lq + 1) * 512, :].rearrange("(l d) h -> d l h", d=64),
                      R=[I[w1n].r], W=[w1s.r])
            p.cp("act", w1b[:, lq * 8:(lq + 1) * 8, :], w1s[:], R=[w1s.r], W=[w1b.r])
        p.dma("sp", w2s[:], I[w2n][:, :].rearrange("(c p) d -> p c d", p=128), R=[I[w2n].r], W=[w2s.r])
        p.cp("dve", w2b[:], w2s[:], R=[w2s.r], W=[w2b.r])
        for gq in range(2):
            p.cp("dve", w2p[:, :, gq, gq * 64:(gq + 1) * 64], w2s[:], R=[w2s.r], W=[w2p.r])
        for half in range(2):
            p.dma("sp", pes[half * 64:(half + 1) * 64, :], I[pen][:, :], R=[I[pen].r], W=[pes.r])
        p.cp("dve", peb[:], pes[:], R=[pes.r], W=[peb.r])
        for gq in range(2):
            rows = slice(gq * 64, (gq + 1) * 64)
            for hc in range(2):
                bH, bC = nbank(), nbank()
                for l in range(32):
                    p.mm(bH[:, 0:255], w1b[rows, l, hc * 128:(hc + 1) * 128], srcT.t[rows, l:l + 16 * 254 + 1:16],
                         l == 0, l == 31, R=[w1b.r, srcT.r], W=[bH.r])
                for l in range(32):
                    p.mm(bC[:, 0:1], w1b[rows, l, hc * 128:(hc + 1) * 128], peb[rows, l:l + 1],
                         l == 0, l == 31, R=[w1b.r, peb.r], W=[bC.r])
                p.cp("dve", cst[:, 0:1], bC[:, 0:1], R=[bC.r], W=[cst.r])
                p.act(u[:, 0:255], bH[:, 0:255], AF.Identity, bias=cst[:, 0:1], R=[bH.r, cst.r], W=[u.r])
                p.tt("dve", t1[:, 0:255], u[:, 0:255], u[:, 0:255], ALU.mult, R=[u.r], W=[t1.r])
                p.ts("dve", t1[:, 0:255], t1[:, 0:255], 0.044715, 1.0, ALU.mult, ALU.add, R=[t1.r], W=[t1.r])
                p.tt("dve", t1[:, 0:255], t1[:, 0:255], u[:, 0:255], ALU.mult, R=[t1.r, u.r], W=[t1.r])
                p.act(t2[:, 0:255], t1[:, 0:255], AF.Tanh, scale=0.7978845608028654, R=[t1.r], W=[t2.r])
                p.stt(t2[:, 0:255], t2[:, 0:255], 1.0, u[:, 0:255], ALU.add, ALU.mult, R=[t2.r, u.r], W=[t2.r])
                p.ts("dve", gel[:, gq, hc, 0:255], t2[:, 0:255], 0.5, None, ALU.mult, R=[t2.r], W=[gel.r])
        if kv == 0:
            b = nbank()
            n = 0
            for gq in range(2):
                for hc in range(2):
                    p.mm(b[:, 0:256], w2p[:, hc, gq, :], gel[:, gq, hc, :], n == 0, n == 3, R=[w2p.r, gel.r], W=[b.r])
                    n += 1
            p.cp("dve", Rz["kcmpT"][:], b[:, 0:256], R=[b.r], W=[Rz["kcmpT"].r])
        else:
            for gq in range(2):
                for cc in range(2):
                    b = nbank()
                    for hc in range(2):
                        p.mm(b[:, 0:64], gel[:, gq, hc, cc * 128:(cc + 1) * 128], w2b[:, hc, :], hc == 0, hc == 1,
                             R=[gel.r, w2b.r], W=[b.r])
                    p.cp("dve", Rz["vcmpM"][:, cc, gq, 0:64], b[:, 0:64], R=[b.r], W=[Rz["vcmpM"].r])

    sq = sb("sq", (128, T), BF16, st)
    row = sb("kmrow", (1, T), F32, st)
    tmp = sb("kmtmp", (1, 8), F32, st)
    rl = sb("relrow", (1, 512), F32, st)
    p.dma("sp", rl[:], I["relflat"][:, :], R=[I["relflat"].r], W=[rl.r])
    p.act(rl[:], rl[:], AF.Abs, R=[rl.r], W=[rl.r])
    p.op("dve", lambda e: e.reduce_max(tmp[:, 0:1], rl[:], AX.X), R=[rl.r], W=[tmp.r])
    for which, srcs in enumerate([(Rz["ksT"], Rz["kwT"]), (Rz["ckvT"],)]):
        first = True
        for s_ in srcs:
            p.tt("dve", sq[:], s_[:], s_[:], ALU.mult, R=[s_.r], W=[sq.r])
            for kb in range(8):
                b = nbank()
                p.mm(b[0:1, :], Rz["ones_bf"][:, 0:1], sq[:, kb * 512:(kb + 1) * 512], True, True,
                     R=[sq.r, Rz["ones_bf"].r], W=[b.r])
                if first:
                    p.cp("dve", row[:, kb * 512:(kb + 1) * 512], b[0:1, :], R=[b.r], W=[row.r])
                else:
                    p.tt("dve", row[:, kb * 512:(kb + 1) * 512], row[:, kb * 512:(kb + 1) * 512], b[0:1, :], ALU.add,
                         R=[b.r, row.r], W=[row.r])
            first = False
        p.op("dve", lambda e: e.reduce_max(tmp[:, 1:2], row[:], AX.X), R=[row.r], W=[tmp.r])
        p.act(tmp[:, 2:3], tmp[:, 1:2], AF.Sqrt, R=[tmp.r], W=[tmp.r])
        p.ts("dve", Rz["kmax"][:, 2 * which:2 * which + 1], tmp[:, 2:3], -1.03, None, ALU.mult, R=[tmp.r], W=[Rz["kmax"].r])
        p.ts("dve", Rz["kmax"][:, 2 * which + 1:2 * which + 2], tmp[:, 0:1], -1.0, None, ALU.mult, R=[tmp.r], W=[Rz["kmax"].r])


def phase2(g):
    nc, p, sb, I, S, Rz = g.nc, g.p, g.sb, g.I, g.S, g.Rz
    st = ExitStack()
    B = g.banks
    ident, identf, ones = Rz["identb"], Rz["identf"], Rz["ones_bf"]
    S["oT_s"] = g.dscr("oT_s", (NT, 128, 8, 128), BF16)
    ntiles = (g.debug or {}).get("ntiles", NT)

    stg = sb("c_stg", (128, 1024), F32, st)
    relTa = sb("relTa", (128, 8, 2, 512), BF16, st)
    relTb = sb("relTb", (128, 8, 1024), BF16, st)
    relTw = sb("relTw", (128, 2, 512), BF16, st)
    eall = sb("eall", (128, 32, 128), BF16, st)
    onesN = sb("onesN", (128, 128), BF16, st)
    p.memset("pool", eall[:], 0.0, W=[eall.r])
    p.memset("pool", onesN[:], 0.0, W=[onesN.r])
    p.memset("pool", onesN[0:1, :], 1.0, W=[onesN.r])
    d30 = sb("d30", (128, 512), BF16, st)
    mcmp = sb("mcmp", (128, 8, 503), BF16, st)
    wext = sb("wext", (128, 128), F32, st)
    mcq = sb("mcq", (128, 128), F32, st)
    rowsA = [sb("rowsA%d" % i, (128, 512), BF16, st) for i in range(2)]
    rowsB = sb("rowsB", (128, 1024), BF16, st)
    for r_ in rowsA + [rowsB]:
        p.memset("pool", r_[:], 0.0, W=[r_.r])
    wuvP = sb("wuvP", (128, 8, 128), BF16, st)
    st0 = ExitStack()
    mc4 = sb("mc4", (128, 512), F32, st0)
    mw4 = sb("mw4", (128, 512), F32, st0)
    c31A = sb("c31A", (128, 2, 512), F32, st0)
    c31B = sb("c31B", (128, 1024), F32, st0)
    p.dma("sp", mc4[:], I["mc4"][:, :], R=[I["mc4"].r], W=[mc4.r])
    p.dma("sp", mw4[:], I["mw4"][:, :], R=[I["mw4"].r], W=[mw4.r])
    p.dma("sp", wext[:], I["wext"][:, :], R=[I["wext"].r], W=[wext.r])
    p.dma("sp", mcq[:], I["mcq"][:, :], R=[I["mcq"].r], W=[mcq.r])
    for gq in range(2):
        p.dma("sp", c31A[:, gq, :], I["c31a"][gq:gq + 1, :].partition_broadcast(128), R=[I["c31a"].r], W=[c31A.r])
    p.dma("sp", c31B[:], I["c31b"][0:1, :].partition_broadcast(128), R=[I["c31b"].r], W=[c31B.r])
    for d in range(8):
        for gq in range(2):
            p.dma("sp", stg[:, 0:512], I["relTa"][:, d, gq, :], R=[I["relTa"].r], W=[stg.r])
            p.tt("dve", stg[:, 0:512], stg[:, 0:512], c31A[:, gq, :], ALU.subtract, R=[stg.r, c31A.r], W=[stg.r])
            if d == 0:
                p.tt("dve", stg[:, 0:512], stg[:, 0:512], mc4[:], ALU.add, R=[stg.r, mc4.r], W=[stg.r])
            p.cp("dve", relTa[:, d, gq, :], stg[:, 0:512], R=[stg.r], W=[relTa.r])
            if d == 4:
                p.tt("dve", stg[:, 0:512], stg[:, 0:512], mw4[:], ALU.add, R=[stg.r, mw4.r], W=[stg.r])
                p.cp("dve", relTw[:, gq, :], stg[:, 0:512], R=[stg.r], W=[relTw.r])
        p.dma("sp", stg[:], I["relTb"][:, d, :], R=[I["relTb"].r], W=[stg.r])
        p.tt("dve", stg[:], stg[:], c31B[:], ALU.subtract, R=[stg.r, c31B.r], W=[stg.r])
        if d == 0:
            for hf in range(2):
                p.tt("dve", stg[:, hf * 512:(hf + 1) * 512], stg[:, hf * 512:(hf + 1) * 512], mc4[:], ALU.add,
                     R=[stg.r, mc4.r], W=[stg.r])
        p.cp("dve", relTb[:, d, :], stg[:], R=[stg.r], W=[relTb.r])
    for q4 in range(4):
        p.dma("sp", stg[0:64, :], I["eall"][:, q4 * 1024:(q4 + 1) * 1024], R=[I["eall"].r], W=[stg.r])
        p.cp("dve", eall[0:64, q4 * 8:(q4 + 1) * 8, :], stg[0:64, :].rearrange("p (a b) -> p a b", b=128), R=[stg.r], W=[eall.r])
    p.dma("sp", stg[:, 0:512], I["d30"][:, :], R=[I["d30"].r], W=[stg.r])
    p.cp("dve", d30[:], stg[:, 0:512], R=[stg.r], W=[d30.r])
    for h in range(8):
        p.dma("sp", stg[:, 0:503], I["cmpv"][:, h, :], R=[I["cmpv"].r], W=[stg.r])
        p.dma("sp", stg[:, 512:1015], I["cmpm"][:, :], R=[I["cmpm"].r], W=[stg.r])
        p.tt("dve", mcmp[:, h, :], stg[:, 0:503], stg[:, 512:1015], ALU.add, R=[stg.r], W=[mcmp.r])
    p.memset("dve", eall[64:65, :, :], 1.0, W=[eall.r])
    p.memset("pool", wuvP[:], 0.0, W=[wuvP.r])
    for h in range(8):
        p.dma("sp", stg[:, 0:64], I["w_uv"][h, :, :], R=[I["w_uv"].r], W=[stg.r])
        p.cp("dve", wuvP[:, h, (h % 2) * 64:(h % 2) * 64 + 64], stg[:, 0:64], R=[stg.r], W=[wuvP.r])

    p.flush()
    st0.close()
    qTz = [sb("qTz%d" % i, (128, 512), BF16, st) for i in range(2)]
    for q_ in qTz:
        p.memset("pool", q_[:], 0.0, W=[q_.r])
    qlT = sb("qlT", (128, 1024), BF16, st)
    qiT = sb("qiT", (64, 512), BF16, st)
    zidx = sb("zidx", (128, T), F32, st)
    selD = Buf(g.kcT.t, g.kcT.r)
    junk = Buf(g.vcT.t, g.vcT.r)
    rr = [sb("rr%d" % i, (128, 512), F32, st) for i in range(2)]
    sq = sb("sqq", (128, 1024), BF16, st)
    srow = Buf(stg.t[0:1, :], stg.r)
    scmp = sb("scmp", (128, 8, 256), F32, st)
    pn = sb("pn", (128, 8, 256), BF16, st)
    pnT = sb("pnT", (128, 16, 128), BF16, st)
    sm = sb("sm", (128, 16), F32, st)
    sm2 = sb("sm2", (128, 16), F32, st)
    imp = sb("imp", (128, 64), F32, st)
    sc1 = sb("sc1", (128, 64), F32, st)
    sc2 = sb("sc2", (128, 64), F32, st)
    m8 = sb("m8", (128, 16), F32, st)
    selb = sb("selb", (128, 64), BF16, st)
    selT4 = [sb("selT4%d" % i, (128, 512), BF16, st) for i in range(2)]
    for s_ in selT4:
        p.memset("pool", s_[:], 0.0, W=[s_.r])
    PT = [sb("PT%d" % i, (128, 512), BF16, st) for i in range(4)]
    ocmp = sb("ocmp", (128, 512), F32, st)
    oa32 = sb("oa32", (128, 512), F32, st)
    oab = sb("oab", (128, 512), BF16, st)
    coef = sb("coef", (128, 32), F32, st)
    oln = sb("oln", (128, 8, 128), BF16, st)
    olT = sb("olT", (128, 8, 128), BF16, st)
    oT = sb("oT", (128, 8, 128), BF16, st)
    bis = sb("bis", (128, 8), F32, st)
    p.memset("pool", pn[:], 0.0, W=[pn.r])
    p.memset("pool", scmp[:], 0.0, W=[scmp.r])

    def bfv(bank):
        return bank.t[:].bitcast(BF16)

    selDs = [selD, junk]
    pw = sb("pw", (128, BIS_ITERS + 1), F32, st)
    wct = sb("wct", (128, BIS_ITERS + 1), F32, st)
    for k in range(BIS_ITERS + 1):
        p.memset("pool", pw[:, k:k + 1], 2.0 ** -(k + 1), W=[pw.r])

    def stream_D(i):
        nk = 128 * (i + 1)
        sD = selDs[i % 2]
        p.dma("sp", qiT[:], S["qi_s"][i, :, :, :].rearrange("d h q -> d (h q)"), R=[S["qi_s"].r], W=[qiT.r])
        for kb in range((nk + 511) // 512):
            k0 = kb * 512
            kn = min(512, nk - k0)
            for hi in range(4):
                bk = B[hi % 2]
                r_ = rr[hi % 2]
                p.mm(bk[:, 0:kn], qiT[:, hi * 128:(hi + 1) * 128], Rz["kiT"][:, k0:k0 + kn], True, True,
                     R=[qiT.r, Rz["kiT"].r], W=[bk.r])
                p.act(r_[:, 0:kn], bk[:, 0:kn], AF.Relu, scale=Rz["wabs"][:, i, hi:hi + 1], R=[bk.r, Rz["wabs"].r], W=[r_.r])
                if hi == 0:
                    p.ts("dve", zidx[:, k0:k0 + kn], r_[:, 0:kn], Rz["wsgn"][:, i, 0:1], None, ALU.mult,
                         R=[r_.r, Rz["wsgn"].r], W=[zidx.r])
                else:
                    p.stt(zidx[:, k0:k0 + kn], r_[:, 0:kn], Rz["wsgn"][:, i, hi:hi + 1], zidx[:, k0:k0 + kn], ALU.mult, ALU.add,
                          R=[r_.r, Rz["wsgn"].r, zidx.r], W=[zidx.r])
            yield
        p.op("dve", lambda e: e.tensor_reduce(bis[:, 0:1], zidx[:, 0:nk], AX.X, ALU.min), R=[zidx.r], W=[bis.r])
        p.op("dve", lambda e: e.reduce_max(bis[:, 1:2], zidx[:, 0:nk], AX.X), R=[zidx.r], W=[bis.r])
        p.stt(bis[:, 2:3], bis[:, 1:2], 1.0, bis[:, 0:1], ALU.add, ALU.subtract, R=[bis.r], W=[bis.r])
        p.ts("dve", wct[:], pw[:], bis[:, 2:3], None, ALU.mult, R=[pw.r, bis.r], W=[wct.r])
        p.tt("dve", bis[:, 3:4], bis[:, 0:1], wct[:, 0:1], ALU.add, R=[bis.r, wct.r], W=[bis.r])
        p.tt("dve", zidx[:, nk - 128:nk], zidx[:, nk - 128:nk], mcq[:], ALU.add, R=[zidx.r, mcq.r], W=[zidx.r])
        yield
        for k in range(BIS_ITERS):
            p.ts("dve", sD[:, 0:nk], zidx[:, 0:nk], bis[:, 3:4], None, ALU.is_ge, ALU.add, R=[zidx.r, bis.r], W=[sD.r, bis.r],
                 accum=bis[:, 4:5])
            p.stt(bis[:, 5:6], bis[:, 4:5], 256.0, wct[:, k:k + 1], ALU.is_ge, ALU.mult, R=[bis.r, wct.r], W=[bis.r])
            p.ts("dve", bis[:, 3:4], bis[:, 3:4], wct[:, k + 1:k + 2], bis[:, 5:6], ALU.subtract, ALU.add, R=[bis.r, wct.r], W=[bis.r])
            yield
        p.tt("dve", bis[:, 6:7], bis[:, 3:4], wct[:, BIS_ITERS:BIS_ITERS + 1], ALU.subtract, R=[bis.r, wct.r], W=[bis.r])
        p.ts("dve", sD[:, 0:nk], zidx[:, 0:nk], bis[:, 6:7], 1.0, ALU.is_ge, ALU.subtract, R=[zidx.r, bis.r], W=[sD.r])
        yield

    def stream_P(i):
        sD = selDs[i % 2]
        for gq in range(2):
            p.dma("sp", qTz[gq][gq * 64:(gq + 1) * 64, :], S["qa_s"][i, gq, :, :, :].rearrange("d h q -> d (h q)"),
                  R=[S["qa_s"].r], W=[qTz[gq].r])
        p.dma("sp", qlT[:], S["ql_s"][i, :, :, :].rearrange("r h q -> r (h q)"), R=[S["ql_s"].r], W=[qlT.r])
        for gq in range(2):
            p.act(sq[:, 0:512], qTz[gq][:], AF.Square, R=[qTz[gq].r], W=[sq.r])
            p.mm(B[7][0:1, :], ones[:, 0:1], sq[:, 0:512], True, True, R=[ones.r, sq.r], W=[B[7].r])
            p.act(srow[:, 0:512], B[7][0:1, :], AF.Sqrt, R=[B[7].r], W=[srow.r])
            p.ts("dve", rowsA[gq][0:1, :], srow[:, 0:512], Rz["kmax"][0:1, 0:1], Rz["kmax"][0:1, 1:2], ALU.mult, ALU.add,
                 R=[srow.r, Rz["kmax"].r], W=[rowsA[gq].r])
            p.dma("sp", selT4[gq][64:65, :], rowsA[gq][0:1, :], R=[rowsA[gq].r], W=[selT4[gq].r])
        p.act(sq[:], qlT[:], AF.Square, R=[qlT.r], W=[sq.r])
        for hf in range(2):
            p.mm(B[7][0:1, :], ones[:, 0:1], sq[:, hf * 512:(hf + 1) * 512], True, True, R=[ones.r, sq.r], W=[B[7].r])
            p.act(srow[:, hf * 512:(hf + 1) * 512], B[7][0:1, :], AF.Sqrt, R=[B[7].r], W=[srow.r])
        p.ts("dve", rowsB[0:1, :], srow[:], Rz["kmax"][0:1, 2:3], Rz["kmax"][0:1, 3:4], ALU.mult, ALU.add,
             R=[srow.r, Rz["kmax"].r], W=[rowsB.r])
        yield
        off = 248 - 8 * i
        for h in range(8):
            gq, hh = h // 4, h % 4
            rows = slice(gq * 64, (gq + 1) * 64)
            bL = B[2 + h // 2]
            p.mm(bL[:, (h % 2) * 256:(h % 2) * 256 + 255], qTz[gq][rows, hh * 128:(hh + 1) * 128], Rz["kcmpT"][rows, 0:255],
                 h % 2 == 0, h % 2 == 1, R=[qTz[gq].r, Rz["kcmpT"].r], W=[bL.r])
        for b4 in range(4):
            bL = B[2 + b4]
            p.tt("dve", scmp[:, 2 * b4:2 * b4 + 2, 0:255], bL[:, :].rearrange("p (h c) -> p h c", c=256)[:, :, 0:255],
                 mcmp[:, 2 * b4:2 * b4 + 2, off:off + 255], ALU.add, R=[bL.r, mcmp.r], W=[scmp.r])
        yield
        p.op("dve", lambda e: e.reduce_max(sm[:, 0:8], scmp[:, :, 0:255], AX.X), R=[scmp.r], W=[sm.r])
        p.ts("dve", sm[:, 8:16], sm[:, 0:8], -1000.0, -1.0, ALU.max, ALU.mult, R=[sm.r], W=[sm.r])
        p.tt("dve", scmp[:, :, 0:255], scmp[:, :, 0:255], sm[:, 8:16].unsqueeze(2).to_broadcast([128, 8, 255]), ALU.add,
             R=[scmp.r, sm.r], W=[scmp.r])
        p.act(scmp[:, :, 0:255], scmp[:, :, 0:255], AF.Exp, R=[scmp.r], W=[scmp.r])
        yield
        p.op("dve", lambda e: e.reduce_sum(sm2[:, 0:8], scmp[:, :, 0:255], AX.X), R=[scmp.r], W=[sm2.r])
        p.ts("dve", sm2[:, 0:8], sm2[:, 0:8], 1e-30, None, ALU.max, R=[sm2.r], W=[sm2.r])
        p.op("dve", lambda e: e.reciprocal(sm2[:, 8:16], sm2[:, 0:8]), R=[sm2.r], W=[sm2.r])
        p.tt("dve", pn[:, :, 0:255], scmp[:, :, 0:255], sm2[:, 8:16].unsqueeze(2).to_broadcast([128, 8, 255]), ALU.mult,
             R=[scmp.r, sm2.r], W=[pn.r])
        yield
        for hf in range(2):
            bk = B[2 + hf]
            tb_ = bfv(bk)
            for j in range(8):
                h, cc = hf * 4 + j // 2, j % 2
                p.tr(tb_[:, j * 128:(j + 1) * 128], pn[:, h, cc * 128:(cc + 1) * 128], ident[:], R=[pn.r, ident.r], W=[bk.r])
            p.cp("act" if hf else "dve", pnT[:, hf * 8:(hf + 1) * 8, :], tb_[:, :].rearrange("p (c q) -> p c q", q=128), R=[bk.r], W=[pnT.r])
        yield
        for gq in range(2):
            accb = B[6 + gq]
            for hh in range(4):
                h = gq * 4 + hh
                for cc in range(2):
                    p.mm(accb[:, hh * 128:(hh + 1) * 128], pnT[:, 2 * h + cc, :], Rz["vcmpM"][:, cc, gq, :], hh == 0 and cc == 0, hh == 3 and cc == 1,
                         R=[pnT.r, Rz["vcmpM"].r], W=[accb.r])
            p.cp("act", ocmp[:, gq * 256:(gq + 1) * 256].rearrange("p (h d) -> p h d", d=64),
                 accb[:, :].rearrange("p (h j) -> p h j", j=128)[:, :, 0:64], R=[accb.r], W=[ocmp.r])
            p.op("dve", lambda e, accb=accb: e.reduce_sum(imp[:], accb[:, :].rearrange("p (h j) -> p j h", j=128)[:, 64:128, :], AX.X),
                 R=[accb.r], W=[imp.r])
            p.tt("dve", sc1[:], imp[:], wext[:, 63 - 2 * i:127 - 2 * i], ALU.add, R=[imp.r, wext.r], W=[sc1.r])
            p.ts("dve", sc1[:, 0:1], imp[:, 0:1], 1e4, None, ALU.add, R=[imp.r], W=[sc1.r])
            p.op("dve", lambda e: e.max(m8[:, 0:8], sc1[:]), R=[sc1.r], W=[m8.r])
            p.op("dve", lambda e: e.match_replace(sc2[:], m8[:, 0:8], sc1[:], -1e30), R=[sc1.r, m8.r], W=[sc2.r])
            p.op("dve", lambda e: e.max(m8[:, 8:16], sc2[:]), R=[sc2.r], W=[m8.r])
            p.ts("dve", m8[:, 15:16], m8[:, 15:16], -1e29, None, ALU.max, R=[m8.r], W=[m8.r])
            p.ts("dve", selb[:], sc1[:], m8[:, 15:16], 1.0, ALU.is_ge, ALU.subtract, R=[sc1.r, m8.r], W=[selb.r])
            tb2 = bfv(accb)
            p.tr(tb2[0:64, 0:128], selb[:], ident[:], R=[selb.r, ident.r], W=[accb.r])
            p.cp("dve", selT4[gq][0:64, :].rearrange("p (h q) -> p h q", q=128),
                 tb2[0:64, 0:128].unsqueeze(1).to_broadcast([64, 4, 128]), R=[accb.r], W=[selT4[gq].r])
            yield
        DEPTH = 4
        sbanks = [B[2], B[3], B[6], B[7]]
        units = []
        for gq in range(2):
            for br_ in range(2):
                kts = list(range(0, i + 1)) if br_ == 0 else list(range(max(0, i - 4), i + 1))
                for n, kt in enumerate(kts):
                    units.append((gq, br_, n, kt, len(kts)))

        def emit_S(ui):
            gq, br_, n, kt, nk_ = units[ui]
            bS = sbanks[ui % DEPTH]
            kT = Rz["ksT"] if br_ == 0 else Rz["kwT"]
            d = i - kt
            p.mm(bS[:, :], kT[:, kt * 128:(kt + 1) * 128], qTz[gq][:], True, False, R=[kT.r, qTz[gq].r], W=[bS.r])
            if br_ == 0:
                p.mm(bS[:, :], eall[:, kt, :], selT4[gq][:], False, d >= 8, R=[eall.r, selT4[gq].r], W=[bS.r])
            if d < 8:
                rel = relTw[:, gq, :] if (br_ == 1 and d == 4) else relTa[:, d, gq, :]
                p.mm(bS[:, :], ident[:], rel, False, br_ == 0, R=[ident.r, relTa.r, relTw.r], W=[bS.r])
            if br_ == 1:
                p.mm(bS[:, :], onesN[:], rowsA[gq][:], False, True, R=[onesN.r, rowsA[gq].r], W=[bS.r])

        def epilogue(gq, br_):
            accb = B[4 + br_]
            accv = accb[:, 0:260].rearrange("p (h e) -> p h e", e=65)
            p.ts("dve", coef[:, 0:4], accv[:, :, 64], 1e-30, None, ALU.max, R=[accb.r], W=[coef.r])
            p.op("dve", lambda e: e.reciprocal(coef[:, 4:8], coef[:, 0:4]), R=[coef.r], W=[coef.r])
            gv = Rz["gn"][:, i, gq * 12:(gq + 1) * 12].rearrange("p (h t) -> p h t", t=3)
            p.tt("dve", coef[:, 8:12], coef[:, 4:8], gv[:, :, 1 + br_], ALU.mult, R=[coef.r, Rz["gn"].r], W=[coef.r])
            for hh in range(4):
                h = gq * 4 + hh
                if br_ == 0:
                    p.ts("dve", oa32[:, h * 64:(h + 1) * 64], ocmp[:, h * 64:(h + 1) * 64], Rz["gn"][:, i, 3 * h:3 * h + 1], None, ALU.mult,
                         R=[ocmp.r, Rz["gn"].r], W=[oa32.r])
                    p.stt(oa32[:, h * 64:(h + 1) * 64], accv[:, hh, 0:64], coef[:, 8 + hh:9 + hh], oa32[:, h * 64:(h + 1) * 64], ALU.mult, ALU.add,
                          R=[accb.r, coef.r, oa32.r], W=[oa32.r])
                else:
                    p.stt(oab[:, h * 64:(h + 1) * 64], accv[:, hh, 0:64], coef[:, 8 + hh:9 + hh], oa32[:, h * 64:(h + 1) * 64], ALU.mult, ALU.add,
                          R=[accb.r, coef.r, oa32.r], W=[oab.r])

        pend = []
        for ui in range(min(DEPTH - 1, len(units))):
            emit_S(ui)
        for ui, (gq, br_, n, kt, nk_) in enumerate(units):
            bS = sbanks[ui % DEPTH]
            pt = PT[ui % DEPTH]
            accb = B[4 + br_]
            vA = Rz["vsA"] if br_ == 0 else Rz["vwA"]
            p.act(pt[:], bS[:, :], AF.Exp, R=[bS.r], W=[pt.r])
            if ui + DEPTH - 1 < len(units):
                emit_S(ui + DEPTH - 1)
            if n == 0:
                for pe_ in list(pend):
                    if pe_[2] == br_:
                        epilogue(pe_[1], pe_[2])
                        pend.remove(pe_)
            for hh in range(4):
                p.mm(accb[:, hh * 65:(hh + 1) * 65], pt[:, hh * 128:(hh + 1) * 128], vA[:, kt, gq, :],
                     n == 0 and hh == 0, n == nk_ - 1, R=[pt.r, vA.r], W=[accb.r])
            if n == nk_ - 1:
                pend.append([2, gq, br_])
            for pe_ in list(pend):
                if pe_[0] == 0:
                    epilogue(pe_[1], pe_[2])
                    pend.remove(pe_)
                else:
                    pe_[0] -= 1
            yield
        for pe_ in pend:
            epilogue(pe_[1], pe_[2])
        tb_ = bfv(B[7])
        for c in range(4):
            p.tr(tb_[:, c * 128:(c + 1) * 128], oab[:, c * 128:(c + 1) * 128], ident[:], R=[oab.r, ident.r], W=[B[7].r])
        p.cp("act", oT[:, 0:4, :], tb_[:, 0:512].rearrange("p (c q) -> p c q", q=128), R=[B[7].r], W=[oT.r])
        yield
        accD = [B[4], B[5], B[6]]
        hb = [(0, 0), (0, 1), (0, 2), (1, 0), (1, 1), (1, 2), (2, 0), (2, 1)]
        dunits = [(kt, hf) for kt in range(i + 1) for hf in range(2)]

        dbanks = [B[2], B[3], B[7]]

        def emit_SD(ui):
            kt, hf = dunits[ui]
            d = i - kt
            bS = dbanks[ui % 3]
            cols = slice(hf * 512, (hf + 1) * 512)
            p.mm(bS[:, :], Rz["ckvT"][:, kt * 128:(kt + 1) * 128], qlT[:, cols], True, False, R=[Rz["ckvT"].r, qlT.r], W=[bS.r])
            p.mm(bS[:, :], sD[:, kt * 128:(kt + 1) * 128], d30[:], False, False, R=[sD.r, d30.r], W=[bS.r])
            if d < 8:
                p.mm(bS[:, :], ident[:], relTb[:, d, cols], False, False, R=[ident.r, relTb.r], W=[bS.r])
            p.mm(bS[:, :], onesN[:], rowsB[:, cols], False, True, R=[onesN.r, rowsB.r], W=[bS.r])

        DD = 3
        for ui in range(min(DD - 1, len(dunits))):
            emit_SD(ui)
        for ui, (kt, hf) in enumerate(dunits):
            bS = dbanks[ui % DD]
            pt = PT[ui % DD]
            p.act(pt[:], bS[:, :], AF.Exp, R=[bS.r], W=[pt.r])
            if ui + DD - 1 < len(dunits):
                emit_SD(ui + DD - 1)
            for hh in range(4):
                h = hf * 4 + hh
                bk, sl = hb[h]
                p.mm(accD[bk][:, sl * 129:(sl + 1) * 129], pt[:, hh * 128:(hh + 1) * 128], Rz["ckvA"][:, kt, :],
                     kt == 0 and sl == 0, kt == i, R=[pt.r, Rz["ckvA"].r], W=[accD[bk].r])
            if hf == 1:
                yield
        for h in range(8):
            bk, sl = hb[h]
            p.ts("dve", coef[:, 16 + h:17 + h], accD[bk][:, sl * 129 + 128:sl * 129 + 129], 1e-30, None, ALU.max, R=[accD[bk].r], W=[coef.r])
        p.op("dve", lambda e: e.reciprocal(coef[:, 24:32], coef[:, 16:24]), R=[coef.r], W=[coef.r])
        for h in range(8):
            bk, sl = hb[h]
            p.ts("dve", oln[:, h, :], accD[bk][:, sl * 129:sl * 129 + 128], coef[:, 24 + h:25 + h], None, ALU.mult,
                 R=[accD[bk].r, coef.r], W=[oln.r])
        yield
        for hf in range(2):
            tb_ = bfv(B[7])
            for hh in range(4):
                p.tr(tb_[:, hh * 128:(hh + 1) * 128], oln[:, hf * 4 + hh, :], ident[:], R=[oln.r, ident.r], W=[B[7].r])
            p.cp("act", olT[:, hf * 4:(hf + 1) * 4, :], tb_[:, 0:512].rearrange("p (c q) -> p c q", q=128), R=[B[7].r], W=[olT.r])
        for c in range(4):
            for hl in range(2):
                p.mm(B[7][:, c * 128:(c + 1) * 128], wuvP[:, 2 * c + hl, :], olT[:, 2 * c + hl, :], c == 0 and hl == 0, c == 3 and hl == 1,
                     R=[wuvP.r, olT.r], W=[B[7].r])
        p.cp("act", oT[:, 4:8, :], B[7][:, :].rearrange("p (c q) -> p c q", q=128), R=[B[7].r], W=[oT.r])
        p.dma("pool", S["oT_s"][i, :, :, :], oT[:], R=[oT.r], W=[S["oT_s"].r])
        yield

    def len_P(i):
        return 1 + 6 + 2 * ((i + 1) + min(5, i + 1)) + 4 + 1 + (i + 1) + 2

    def len_D(i):
        return (128 * (i + 1) + 511) // 512 + 1 + BIS_ITERS + 1

    for _ in stream_D(0):
        pass
    for i in range(ntiles):
        gd = stream_D(i + 1) if i + 1 < ntiles else None
        ratio = (len_D(i + 1) / float(len_P(i))) if gd is not None else 0.0
        credit = 0.0
        for _ in stream_P(i):
            credit += ratio
            while gd is not None and credit >= 1.0:
                credit -= 1.0
                try:
                    next(gd)
                except StopIteration:
                    gd = None
        if gd is not None:
            for _ in gd:
                pass
    return st


def layer_norm_tile(p, r, rs, gbc, bbc, out, tmp, R_extra=()):
    p.tt("dve", rs[:, 2:3], rs[:, 0:1], rs[:, 1:2], ALU.add, R=[rs.r], W=[rs.r])
    p.ts("dve", rs[:, 3:4], rs[:, 2:3], -1.0 / D, None, ALU.mult, R=[rs.r], W=[rs.r])
    p.act(tmp[:], r[:], AF.Square, bias=rs[:, 3:4], accum=rs[:, 4:5], R=[r.r, rs.r], W=[tmp.r, rs.r])
    p.ts("dve", rs[:, 5:6], rs[:, 4:5], 1.0 / D, 1e-5, ALU.mult, ALU.add, R=[rs.r], W=[rs.r])
    p.act(rs[:, 6:7], rs[:, 5:6], AF.Sqrt, R=[rs.r], W=[rs.r])
    p.op("dve", lambda e: e.reciprocal(rs[:, 7:8], rs[:, 6:7]), R=[rs.r], W=[rs.r])
    p.stt(tmp[:], r[:], rs[:, 3:4], gbc[:], ALU.add, ALU.mult, R=[r.r, rs.r, gbc.r], W=[tmp.r])
    p.stt(out[:], tmp[:], rs[:, 7:8], bbc[:], ALU.mult, ALU.add, R=[tmp.r, rs.r, bbc.r], W=[out.r])


def phase3(g):
    nc, p, sb, I, S, Rz = g.nc, g.p, g.sb, g.I, g.S, g.Rz
    st = ExitStack()
    B = g.banks
    stg = sb("w_stg", (128, 1024), F32, st)
    wba = sb("wba", (128, 4, 1024), BF16, st)
    wbb = sb("wbb", (128, 4, 1024), BF16, st)
    wout = sb("wout", (128, 8, 1024), BF16, st)
    for (dst, src, n) in [(wba, "w_ba", 4), (wbb, "w_bb", 4), (wout, "w_out", 8)]:
        for c in range(n):
            p.dma("sp", stg[:], I[src][c * 128:(c + 1) * 128, :], R=[I[src].r], W=[stg.r])
            p.cp("dve", dst[:, c, :], stg[:], R=[stg.r], W=[dst.r])
    bc = {}
    for nm in ("ln1_g", "ln1_b", "ln2_g", "ln2_b"):
        bc[nm] = sb("bc_" + nm, (128, 1024), F32, st)
        p.dma("sp", bc[nm][:], I[nm][0:1, :].partition_broadcast(128), R=[I[nm].r], W=[bc[nm].r])
    oT_l = [sb("oT2_%d" % j, (128, 8, 128), BF16, st) for j in range(2)]
    gT_l = [sb("gT_%d" % j, (128, 16, 128), F32, st) for j in range(2)]
    xt_l = [sb("xt_%d" % j, (128, 1024), F32, st) for j in range(2)]
    t1 = sb("t1", (128, 512), F32, st)
    t2 = sb("t2", (128, 512), F32, st)
    mT = sb("mT", (128, 8, 128), BF16, st)
    r = sb("r", (128, 1024), F32, st)
    h1 = sb("h1", (128, 1024), F32, st)
    tmp = sb("lntmp", (128, 1024), F32, st)
    rs = sb("rs", (128, 8), F32, st)
    h1T = sb("h1T", (128, 8, 128), F32, st)
    wr = sb("wr", (128, 8, 36), F32, st)
    brb = sb("brb", (128, 36), F32, st)
    lg = sb("lg", (128, 36), F32, st)
    rt = sb("rt", (128, 16), F32, st)
    rj = sb("rj", (128, 32), F32, st)
    og = sb("og", (128, 4), F32, st)
    esel = sb("esel", (128, 8), F32, st)
    m8r = sb("m8r", (128, 8), F32, st)
    oh = sb("oh", (128, 2, 8), F32, st)
    Ak = sb("Ak", (128, 2, 32), F32, st)
    Abf = sb("Abf", (128, 32), BF16, st)
    ustr = sb("ustr", (128, 128), BF16, st)
    posf = sb("posf", (128, 32), F32, st)
    base = sb("base", (128, 32), F32, st)
    iot = sb("iot", (128, 32), F32, st)
    tokid = sb("tokid", (128, 32), I32, st)
    tokrow = sb("tokrow", (128, 32, 16), I32, st)
    fill = sb("fill", (128, 2048), I32, st)
    p.dma("sp", wr[:], I["wr"][:, :].rearrange("(c p) n -> p c n", p=128), R=[I["wr"].r], W=[wr.r])
    p.dma("sp", brb[:], I["br"][0:1, :].partition_broadcast(128), R=[I["br"].r], W=[brb.r])
    p.dma("sp", stg[:, 0:128], I["ustrict"][:, :], R=[I["ustrict"].r], W=[stg.r])
    p.cp("dve", ustr[:], stg[:, 0:128], R=[stg.r], W=[ustr.r])
    p.dma("sp", iot[:], I["iota32"][:, :], R=[I["iota32"].r], W=[iot.r])
    p.dma("sp", tokid[:], I["tokid"][:, :], R=[I["tokid"].r], W=[tokid.r])
    p.cp("dve", tokrow[:], tokid[:].unsqueeze(2).to_broadcast([128, 32, 16]), R=[tokid.r], W=[tokrow.r])
    p.memset("dve", base[:], 0.0, W=[base.r])
    p.memset("pool", fill[:], 5000, W=[fill.r])
    p.dma("pool", S["slot"][:, :].rearrange("(p r) c -> p (r c)", p=128), fill[:], R=[fill.r], W=[S["slot"].r])
    nt3 = (g.debug or {}).get("ntiles", NT)

    def loads3(i):
        j = i % 2
        p.dma("sp", oT_l[j][:], S["oT_s"][i, :, :, :], R=[S["oT_s"].r], W=[oT_l[j].r])
        p.dma("sp", gT_l[j][:], S["g_s"][i, :, :, :], R=[S["g_s"].r], W=[gT_l[j].r])
        p.dma("sp", xt_l[j][:], I["x"][i * 128:(i + 1) * 128, :], R=[I["x"].r], W=[xt_l[j].r])

    loads3(0)
    for i in range(nt3):
        oT, gT, xt = oT_l[i % 2], gT_l[i % 2], xt_l[i % 2]
        if i + 1 < nt3:
            loads3(i + 1)
        for hf in range(2):
            for m4 in range(4):
                m = hf * 4 + m4
                for kc in range(4):
                    p.mm(B[0][:, m4 * 128:(m4 + 1) * 128], wba[:, kc, m * 128:(m + 1) * 128], oT[:, kc, :], m4 == 0 and kc == 0, kc == 3,
                         R=[wba.r, oT.r], W=[B[0].r])
                for kc in range(4):
                    p.mm(B[1][:, m4 * 128:(m4 + 1) * 128], wbb[:, kc, m * 128:(m + 1) * 128], oT[:, 4 + kc, :], m4 == 0 and kc == 0, kc == 3,
                         R=[wbb.r, oT.r], W=[B[1].r])
            p.tt("dve", t1[:], B[0][:, :], gT[:, hf * 4:(hf + 1) * 4, :].rearrange("p m q -> p (m q)"), ALU.mult, R=[B[0].r, gT.r], W=[t1.r])
            p.tt("dve", t2[:], B[1][:, :], gT[:, 8 + hf * 4:8 + (hf + 1) * 4, :].rearrange("p m q -> p (m q)"), ALU.mult, R=[B[1].r, gT.r], W=[t2.r])
            p.tt("dve", mT[:, hf * 4:(hf + 1) * 4, :].rearrange("p m q -> p (m q)"), t1[:], t2[:], ALU.add, R=[t1.r, t2.r], W=[mT.r])
        for hf in range(2):
            bk = B[2 + hf]
            for c in range(8):
                p.mm(bk[:, :], mT[:, c, :], wout[:, c, hf * 512:(hf + 1) * 512], c == 0, c == 7, R=[mT.r, wout.r], W=[bk.r])
            p.stt(r[:, hf * 512:(hf + 1) * 512], xt[:, hf * 512:(hf + 1) * 512], ALPHA, bk[:, :], ALU.mult, ALU.add,
                  R=[xt.r, bk.r], W=[r.r, rs.r], accum=rs[:, hf:hf + 1])
        layer_norm_tile(p, r, rs, bc["ln1_g"], bc["ln1_b"], h1, tmp)
        p.dma("pool", S["h1_s"][i * 128:(i + 1) * 128, :], h1[:], R=[h1.r], W=[S["h1_s"].r])
        for c in range(8):
            bk = B[4 + c // 4]
            p.tr(bk[:, (c % 4) * 128:(c % 4 + 1) * 128], h1[:, c * 128:(c + 1) * 128], Rz["identf"][:], R=[h1.r, Rz["identf"].r], W=[bk.r])
        for hf in range(2):
            p.cp("act", h1T[:, hf * 4:(hf + 1) * 4, :], B[4 + hf][:, :].rearrange("p (c q) -> p c q", q=128), R=[B[4 + hf].r], W=[h1T.r])
        for c in range(8):
            p.mm(B[6][:, 0:36], h1T[:, c, :], wr[:, c, :], c == 0, c == 7, R=[h1T.r, wr.r], W=[B[6].r])
        p.tt("dve", lg[:], B[6][:, 0:36], brb[:], ALU.add, R=[B[6].r, brb.r], W=[lg.r])
        p.op("dve", lambda e: e.reduce_max(rt[:, 0:1], lg[:, 0:4], AX.X), R=[lg.r], W=[rt.r])
        p.ts("dve", og[:], lg[:, 0:4], rt[:, 0:1], None, ALU.is_equal, R=[lg.r, rt.r], W=[og.r])
        p.ts("dve", rt[:, 1:2], rt[:, 0:1], -1.0, None, ALU.mult, R=[rt.r], W=[rt.r])
        p.act(rj[:, 0:4], lg[:, 0:4], AF.Exp, bias=rt[:, 1:2], accum=rt[:, 2:3], R=[lg.r, rt.r], W=[rj.r, rt.r])
        p.op("dve", lambda e: e.reciprocal(rt[:, 3:4], rt[:, 2:3]), R=[rt.r], W=[rt.r])
        p.ts("dve", esel[:], lg[:, 4:12], og[:, 0:1], None, ALU.mult, R=[lg.r, og.r], W=[esel.r])
        for gi in range(1, 4):
            p.stt(esel[:], lg[:, 4 + 8 * gi:12 + 8 * gi], og[:, gi:gi + 1], esel[:], ALU.mult, ALU.add, R=[lg.r, og.r, esel.r], W=[esel.r])
        p.op("dve", lambda e: e.max(m8r[:], esel[:]), R=[esel.r], W=[m8r.r])
        p.tt("dve", rt[:, 4:5], m8r[:, 1:2], m8r[:, 0:1], ALU.subtract, R=[m8r.r], W=[rt.r])
        p.act(rt[:, 5:6], rt[:, 4:5], AF.Exp, R=[rt.r], W=[rt.r])
        p.ts("dve", rt[:, 6:7], rt[:, 5:6], 1.0, None, ALU.add, R=[rt.r], W=[rt.r])
        p.op("dve", lambda e: e.reciprocal(rt[:, 7:8], rt[:, 6:7]), R=[rt.r], W=[rt.r])
        p.tt("dve", rt[:, 8:9], rt[:, 7:8], rt[:, 5:6], ALU.mult, R=[rt.r], W=[rt.r])
        p.tt("dve", g.wts[:, i, 0:1], rt[:, 7:8], rt[:, 3:4], ALU.mult, R=[rt.r], W=[g.wts.r])
        p.tt("dve", g.wts[:, i, 1:2], rt[:, 8:9], rt[:, 3:4], ALU.mult, R=[rt.r], W=[g.wts.r])
        for k in range(2):
            p.ts("dve", oh[:, k, :], esel[:], m8r[:, k:k + 1], None, ALU.is_equal, R=[esel.r, m8r.r], W=[oh.r])
            p.tt("dve", Ak[:, k, :].rearrange("p (a b) -> p a b", b=8), og[:].unsqueeze(2).to_broadcast([128, 4, 8]),
                 oh[:, k, :].unsqueeze(1).to_broadcast([128, 4, 8]), ALU.mult, R=[og.r, oh.r], W=[Ak.r])
        p.tt("dve", Abf[:], Ak[:, 0, :], Ak[:, 1, :], ALU.add, R=[Ak.r], W=[Abf.r])
        p.mm(B[7][:, 0:32], ustr[:], Abf[:], True, False, R=[ustr.r, Abf.r], W=[B[7].r])
        p.mm(B[7][:, 64:96], Rz["ones_bf"][:], Abf[:], False, True, R=[Rz["ones_bf"].r, Abf.r], W=[B[7].r])
        p.tt("dve", posf[:], B[7][:, 0:32], base[:], ALU.add, R=[B[7].r, base.r], W=[posf.r])
        p.tt("dve", base[:], B[7][:, 64:96], base[:], ALU.add, R=[B[7].r, base.r], W=[base.r])
        for k in range(2):
            p.stt(rj[:, 0:32], posf[:], 1.0, Ak[:, k, :], ALU.mult, ALU.mult, R=[posf.r, Ak.r], W=[rj.r, rt.r], accum=rt[:, 10 + k:11 + k])
            p.stt(rj[:, 0:32], iot[:], 1.0, Ak[:, k, :], ALU.mult, ALU.mult, R=[iot.r, Ak.r], W=[rj.r, rt.r], accum=rt[:, 12 + k:13 + k])
            p.ts("dve", rt[:, 14:15], rt[:, 10 + k:11 + k], float(CAP), 1e6, ALU.is_ge, ALU.mult, R=[rt.r], W=[rt.r])
            p.ts("dve", rt[:, 9:10], rt[:, 10 + k:11 + k], float(CAP), None, ALU.is_lt, R=[rt.r], W=[rt.r])
            p.tt("dve", g.wts[:, i, k:k + 1], g.wts[:, i, k:k + 1], rt[:, 9:10], ALU.mult, R=[rt.r, g.wts.r], W=[g.wts.r])
            p.stt(rt[:, 15:16], rt[:, 12 + k:13 + k], float(CAP), rt[:, 10 + k:11 + k], ALU.mult, ALU.add, R=[rt.r], W=[rt.r])
            p.tt("dve", rt[:, 15:16], rt[:, 15:16], rt[:, 14:15], ALU.add, R=[rt.r], W=[rt.r])
            p.cp("dve", g.dest[:, i, k:k + 1], rt[:, 15:16], R=[rt.r], W=[g.dest.r])
            p.dma("pool", None, None, R=[g.dest.r, tokrow.r], W=[S["slot"].r],
                  fn=lambda e, i=i, k=k: e.indirect_dma_start(
                      out=S["slot"][:, :], out_offset=bass.IndirectOffsetOnAxis(ap=g.dest[:, i, k:k + 1], axis=0),
                      in_=tokrow[:, i, :], in_offset=None, bounds_check=p.breg(e, 32 * CAP - 1), oob_is_err=False))
    return st


def phase4(g):
    nc, p, sb, I, S, Rz = g.nc, g.p, g.sb, g.I, g.S, g.Rz
    st = ExitStack()
    B = g.banks
    ident = Rz["identb"]
    nexp = (g.debug or {}).get("nexp", 32)
    sid = sb("sid", (128, 4, 16), I32, st)
    xg = [sb("xg%d" % i, (128, 1024), F32, st) for i in range(4)]
    xgb = [sb("xgb%d" % i, (128, 1024), BF16, st) for i in range(2)]
    xgT = sb("xgT", (128, 8, 512), BF16, st)
    hT = sb("hT", (128, 2, 512), BF16, st)
    sg = [sb("sg%d" % i, (128, 512), F32, st) for i in range(2)]
    yb = [sb("yb%d" % i, (128, 1024), F32, st) for i in range(2)]
    wgf = sb("wgf", (128, 8, 256), F32, st)
    wuf = sb("wuf", (128, 8, 256), F32, st)
    wdf = sb("wdf", (128, 2, 1024), F32, st)
    wg = [sb("wg%d" % i, (128, 8, 256), BF16, st) for i in range(2)]
    wu = [sb("wu%d" % i, (128, 8, 256), BF16, st) for i in range(2)]
    wd = [sb("wd%d" % i, (128, 2, 1024), BF16, st) for i in range(2)]
    for x_ in xg:
        p.memset("pool", x_[:], 0.0, W=[x_.r])
    ceng = ["dve", "act", "dve", "act"]
    xgT2 = [xgT, sb("xgTb", (128, 8, 512), BF16, st)]
    sid2 = [sid, sb("sidb", (128, 4, 16), I32, st)]

    def issue_loads(e_):
        k = e_ % 2
        sd = sid2[k]
        p.dma("sp", wgf[:], I["w_gate"][e_, :, :].rearrange("(p c) f -> p c f", c=8), R=[I["w_gate"].r], W=[wgf.r])
        p.dma("sp", wuf[:], I["w_up"][e_, :, :].rearrange("(p c) f -> p c f", c=8), R=[I["w_up"].r], W=[wuf.r])
        p.dma("sp", wdf[:], I["w_down"][e_, :, :].rearrange("(c p) n -> p c n", p=128), R=[I["w_down"].r], W=[wdf.r])
        p.dma("sp", sd[:], S["slot"][e_ * CAP:(e_ + 1) * CAP, :].rearrange("(s p) c -> p s c", p=128),
              R=[S["slot"].r], W=[sd.r])
        for s_ in range(4):
            p.dma("pool", None, None, R=[sd.r, S["h1_s"].r], W=[xg[s_].r],
                  fn=lambda e, s_=s_, sd=sd: e.indirect_dma_start(
                      out=xg[s_][:, :], out_offset=None, in_=S["h1_s"][:, :],
                      in_offset=bass.IndirectOffsetOnAxis(ap=sd[:, s_, 0:1], axis=0), bounds_check=p.breg(e, T - 1), oob_is_err=False))

    def finish_loads(e_):
        k = e_ % 2
        p.cp("act", wg[k][:], wgf[:], R=[wgf.r], W=[wg[k].r])
        p.cp("dve", wu[k][:], wuf[:], R=[wuf.r], W=[wu[k].r])
        p.cp("act", wd[k][:], wdf[:], R=[wdf.r], W=[wd[k].r])
        for s_ in range(4):
            xb = xgb[s_ % 2]
            p.cp(ceng[s_], xb[:], xg[s_][:], R=[xg[s_].r], W=[xb.r])
            bk = B[6 + s_ % 2]
            tv = bk.t[:].bitcast(BF16)
            for c in range(8):
                p.tr(tv[:, c * 128:(c + 1) * 128], xb.t[:, c:1024:8], ident[:], R=[xb.r, ident.r], W=[bk.r])
            p.cp("act" if s_ % 2 else "dve", xgT2[k][:, :, s_ * 128:(s_ + 1) * 128], tv[:, :].rearrange("p (c q) -> p c q", q=128),
                 R=[bk.r], W=[xgT2[k].r])

    def compute(e_):
        k = e_ % 2
        xT_ = xgT2[k]
        for fc in range(2):
            bG, bU = B[2 * fc], B[2 * fc + 1]
            for c in range(8):
                p.mm(bG[:, :], wg[k][:, c, fc * 128:(fc + 1) * 128], xT_[:, c, :], c == 0, c == 7, R=[wg[k].r, xT_.r], W=[bG.r])
            for c in range(8):
                p.mm(bU[:, :], wu[k][:, c, fc * 128:(fc + 1) * 128], xT_[:, c, :], c == 0, c == 7, R=[wu[k].r, xT_.r], W=[bU.r])
            p.act(sg[fc][:], bG[:, :], AF.Silu, R=[bG.r], W=[sg[fc].r])
            p.tt("dve", hT[:, fc, :], sg[fc][:], bU[:, :], ALU.mult, R=[sg[fc].r, bU.r], W=[hT.r])
        for s_ in range(4):
            y_ = yb[s_ % 2]
            for hf in range(2):
                bY = B[4 + (2 * s_ + hf) % 2]
                for fc in range(2):
                    p.mm(bY[:, :], hT[:, fc, s_ * 128:(s_ + 1) * 128], wd[k][:, fc, hf * 512:(hf + 1) * 512], fc == 0, fc == 1,
                         R=[hT.r, wd[k].r], W=[bY.r])
                if hf == 0:
                    p.act(y_[:, 0:512], bY[:, :], AF.Copy, R=[bY.r], W=[y_.r])
                else:
                    p.cp("dve", y_[:, 512:1024], bY[:, :], R=[bY.r], W=[y_.r])
            p.dma("sp", S["y_s"][e_ * CAP + s_ * 128:e_ * CAP + (s_ + 1) * 128, :], y_[:], R=[y_.r], W=[S["y_s"].r])

    issue_loads(0)
    finish_loads(0)
    for e_ in range(nexp):
        if e_ + 1 < nexp:
            issue_loads(e_ + 1)
        compute(e_)
        if e_ + 1 < nexp:
            finish_loads(e_ + 1)
    return st


def phase5(g):
    nc, p, sb, I, S, Rz = g.nc, g.p, g.sb, g.I, g.S, g.Rz
    st = ExitStack()
    bc = {}
    for nm in ("ln2_g", "ln2_b"):
        bc[nm] = sb("bc5_" + nm, (128, 1024), F32, st)
        p.dma("sp", bc[nm][:], I[nm][0:1, :].partition_broadcast(128), R=[I[nm].r], W=[bc[nm].r])
    y = [[sb("y%d_%d" % (k, j), (128, 1024), F32, st) for k in range(2)] for j in range(2)]
    h1 = [sb("h1_5%d" % j, (128, 1024), F32, st) for j in range(2)]
    r = sb("r5", (128, 1024), F32, st)
    tmp = sb("tmp5", (128, 1024), F32, st)
    ot = [sb("ot5%d" % j, (128, 1024), F32, st) for j in range(2)]
    rs = sb("rs5", (128, 8), F32, st)
    for j in range(2):
        for k in range(2):
            p.memset("dve", y[j][k][:], 0.0, W=[y[j][k].r])
    nt5 = (g.debug or {}).get("ntiles", NT)

    def loads5(i):
        j = i % 2
        p.dma("sp", h1[j][:], S["h1_s"][i * 128:(i + 1) * 128, :], R=[S["h1_s"].r], W=[h1[j].r])
        for k in range(2):
            p.dma("pool", None, None, R=[g.dest.r, S["y_s"].r], W=[y[j][k].r],
                  fn=lambda e, i=i, k=k, j=j: e.indirect_dma_start(
                      out=y[j][k][:, :], out_offset=None, in_=S["y_s"][:, :],
                      in_offset=bass.IndirectOffsetOnAxis(ap=g.dest[:, i, k:k + 1], axis=0), bounds_check=p.breg(e, 32 * CAP - 1), oob_is_err=False))

    loads5(0)
    for i in range(nt5):
        j = i % 2
        if i + 1 < nt5:
            loads5(i + 1)
        p.ts("dve", r[:], h1[j][:], ALPHA, None, ALU.mult, R=[h1[j].r], W=[r.r])
        p.stt(r[:], y[j][0][:], g.wts[:, i, 0:1], r[:], ALU.mult, ALU.add, R=[y[j][0].r, g.wts.r, r.r], W=[r.r])
        p.stt(r[:], y[j][1][:], g.wts[:, i, 1:2], r[:], ALU.mult, ALU.add, R=[y[j][1].r, g.wts.r, r.r], W=[r.r, rs.r],
              accum=rs[:, 0:1])
        p.memset("dve", rs[:, 1:2], 0.0, W=[rs.r])
        layer_norm_tile(p, r, rs, bc["ln2_g"], bc["ln2_b"], ot[j], tmp)
        p.dma("sp", g.out[i * 128:(i + 1) * 128, :], ot[j][:], R=[ot[j].r], W=[g.out.r])
    return st


def host_inputs(inputs):
    f = lambda a: np.ascontiguousarray(np.asarray(a, dtype=np.float32))
    rel_bias = f(inputs["rel_bias"])
    shared = {
        "w_in": f(inputs["w_in"][0]),
        "pe_kT": f(inputs["cmp_pe_k"][0].T), "pe_vT": f(inputs["cmp_pe_v"][0].T),
        "cw1_k": f(inputs["cmp_w1_k"][0]), "cw2_k": f(inputs["cmp_w2_k"][0]),
        "cw1_v": f(inputs["cmp_w1_v"][0]), "cw2_v": f(inputs["cmp_w2_v"][0]),
        "ckv_g": f(inputs["ckv_norm_g"][0]).reshape(1, 128),
        "w_uk": f(inputs["w_uk"][0]), "w_uv": f(inputs["w_uv"][0]),
        "relflat": rel_bias.reshape(1, 512),
        "w_ba": f(inputs["w_branch_a"][0]), "w_bb": f(inputs["w_branch_b"][0]), "w_out": f(inputs["w_out"][0]),
        "ln1_g": f(inputs["ln1_g"][0]).reshape(1, D), "ln1_b": f(inputs["ln1_b"][0]).reshape(1, D),
        "wr": f(np.concatenate([np.asarray(inputs["w_grp"][0]), np.asarray(inputs["w_rtr"][0])], axis=1)),
        "br": f(np.concatenate([np.asarray(inputs["b_grp"][0]), np.asarray(inputs["b_rtr"][0])], axis=0)).reshape(1, 36),
        "w_gate": f(inputs["w_gate"][0]), "w_up": f(inputs["w_up"][0]), "w_down": f(inputs["w_down"][0]),
        "ln2_g": f(inputs["ln2_g"][0]).reshape(1, D), "ln2_b": f(inputs["ln2_b"][0]).reshape(1, D),
    }
    shared.update(_host_consts(rel_bias))
    x = np.asarray(inputs["x"], dtype=np.float32)
    maps = []
    for b in range(x.shape[0]):
        m = dict(shared)
        m["x"] = np.ascontiguousarray(x[b])
        m["xT"] = np.ascontiguousarray(x[b].T)
        maps.append(m)
    return maps


def kernel(**inputs):
    maps = host_inputs(inputs)
    nc, g = build()
    res = run_bass_kernel_spmd(nc, maps, core_ids=list(range(8)))
    return np.stack([np.asarray(r["out"], dtype=np.float32) for r in res.results], axis=0)
```
